# Optimizing a Trainium2 kernel written in Bass

```python
import jax, jax.numpy as jnp
from jax import lax
import numpy as np

D_MODEL = 1024
BATCH = 2
SEQ = 16384
DEPTH = 2

GRID_W = 64
CTX_LEN = 256
N_MIXERS = 2
N_HGRN_LAYERS = (DEPTH + N_MIXERS - 1) // N_MIXERS
N_CONV_LAYERS = DEPTH // N_MIXERS
HGRN_DK = 128
HGRN_HEADS = D_MODEL // HGRN_DK
HGRN_DV = D_MODEL // HGRN_HEADS
HGRN_CHUNK = 64
CONV_WIDTH = 31
N_EXPERTS = 32
TOP_K = 4
D_EXPERT = D_MODEL
SWIGLU_ALPHA = 1.702
SWIGLU_LIMIT = 7.0
MOE_BLOCK = 256
EPS = 1e-6

kernel_name = "hybrid_hgrn2_conformer_moe_dit"


def rmsnorm(x, g):
    x32 = x.astype(jnp.float32)
    y = x32 * lax.rsqrt(jnp.mean(x32 * x32, axis=-1, keepdims=True) + EPS)
    return (y * g.astype(jnp.float32)).astype(x.dtype)


def layernorm(x, g, b):
    x32 = x.astype(jnp.float32)
    mu = jnp.mean(x32, axis=-1, keepdims=True)
    xc = x32 - mu
    y = xc * lax.rsqrt(jnp.mean(xc * xc, axis=-1, keepdims=True) + EPS)
    return (y * g.astype(jnp.float32) + b.astype(jnp.float32)).astype(x.dtype)


def ada_mods(cond, w, b):
    return jnp.split(jax.nn.silu(cond) @ w + b, 6, axis=-1)


def gla_chunk_scan(q, k, v, logf, s0):
    bsz, nh, length, dk = q.shape
    dv = v.shape[-1]
    nc = length // HGRN_CHUNK

    def to_chunks(t):
        return jnp.moveaxis(t.reshape(bsz, nh, nc, HGRN_CHUNK, t.shape[-1]), 2, 0)

    mask = jnp.tril(jnp.ones((HGRN_CHUNK, HGRN_CHUNK), dtype=bool))[:, :, None]

    def step(s, inp):
        qc, kc, vc, gc = inp
        bcum = jnp.cumsum(gc, axis=-2)
        diff = bcum[..., :, None, :] - bcum[..., None, :, :]
        decay = jnp.exp(jnp.where(mask, diff, -jnp.inf))
        a = jnp.einsum('bhtk,bhsk,bhtsk->bhts', qc, kc, decay)
        o = (jnp.einsum('bhts,bhsv->bhtv', a, vc)
             + jnp.einsum('bhtk,bhkv->bhtv', qc * jnp.exp(bcum), s))
        blast = bcum[..., -1:, :]
        s_new = (jnp.exp(blast[..., 0, :])[..., None] * s
                 + jnp.einsum('bhsk,bhsv->bhkv', kc * jnp.exp(blast - bcum), vc))
        return s_new, o

    s_fin, o = lax.scan(step, s0, (to_chunks(q), to_chunks(k), to_chunks(v), to_chunks(logf)))
    o = jnp.moveaxis(o, 0, 2).reshape(bsz, nh, length, dv)
    return o, s_fin


def hgrn2_mixer(h_lat, h_ctx, w_in, lb_fwd, lb_bwd, g_out, w_out, return_ctx):
    hk = HGRN_HEADS * HGRN_DK
    hv = HGRN_HEADS * HGRN_DV
    splits = [hk, hk + hv, 2 * hk + hv, 3 * hk + hv]

    def heads(t):
        return t.reshape(t.shape[0], t.shape[1], HGRN_HEADS, -1).transpose(0, 2, 1, 3)

    def project(h):
        p = (h @ w_in).astype(jnp.float32)
        q, i, zf, zb, g = jnp.split(p, splits, axis=-1)
        f_f = lb_fwd + (1.0 - lb_fwd) * jax.nn.sigmoid(zf)
        f_b = lb_bwd + (1.0 - lb_bwd) * jax.nn.sigmoid(zb)
        return heads(q), heads(i), heads(f_f), heads(f_b), g

    def flip(t):
        return jnp.flip(t, axis=2)

    def scan_both(q, i, f_f, f_b, s_f, s_b):
        o_f, s_f_new = gla_chunk_scan(q, 1.0 - f_f, i, jnp.log(f_f), s_f)
        o_b, s_b_new = gla_chunk_scan(flip(q), flip(1.0 - f_b), flip(i), flip(jnp.log(f_b)), s_b)
        return o_f + flip(o_b), s_f_new, s_b_new

    def readout(o, g, dtype):
        o = o * lax.rsqrt(jnp.mean(o * o, axis=-1, keepdims=True) + EPS)
        o = o * g_out.astype(jnp.float32).reshape(HGRN_HEADS, HGRN_DV)[None, :, None, :]
        o = o.transpose(0, 2, 1, 3).reshape(o.shape[0], o.shape[2], hv)
        return (o * jax.nn.silu(g)).astype(dtype) @ w_out

    bsz = h_lat.shape[0]
    s0 = jnp.zeros((bsz, HGRN_HEADS, HGRN_DK, HGRN_DV), jnp.float32)
    qc, ic, ffc, fbc, gc = project(h_ctx)
    o_ctx, s_ctx_f, s_ctx_b = scan_both(qc, ic, ffc, fbc, s0, s0)
    ql, il, ffl, fbl, gl = project(h_lat)
    o_lat, _, _ = scan_both(ql, il, ffl, fbl, s_ctx_f, s_ctx_b)
    y_lat = readout(o_lat, gl, h_lat.dtype)
    y_ctx = readout(o_ctx, gc, h_ctx.dtype) if return_ctx else None
    return y_lat, y_ctx


def dwconv_latent(u, w_dw):
    bsz, length, d = u.shape
    rows = length // GRID_W
    grid = u.reshape(bsz, rows, GRID_W, d)
    half = d // 2
    dn = ('NHWC', 'HWIO', 'NHWC')
    horiz = lax.conv_general_dilated(grid[..., :half], w_dw[:, :half].reshape(1, CONV_WIDTH, 1, half),
                                     (1, 1), 'SAME', dimension_numbers=dn, feature_group_count=half)
    vert = lax.conv_general_dilated(grid[..., half:], w_dw[:, half:].reshape(CONV_WIDTH, 1, 1, d - half),
                                    (1, 1), 'SAME', dimension_numbers=dn, feature_group_count=d - half)
    return jnp.concatenate([horiz, vert], axis=-1).reshape(bsz, length, d)


def dwconv_seq(u, w_dw):
    d = u.shape[-1]
    return lax.conv_general_dilated(u, w_dw.reshape(CONV_WIDTH, 1, d), (1,), 'SAME',
                                    dimension_numbers=('NWC', 'WIO', 'NWC'), feature_group_count=d)


def conformer_conv(h, dwconv, w_in, b_in, w_dw, b_dw, g_ln, b_ln, w_out, b_out):
    a, gt = jnp.split(h @ w_in + b_in, 2, axis=-1)
    u = a * jax.nn.sigmoid(gt)
    z = dwconv(u, w_dw) + b_dw
    z = jax.nn.silu(layernorm(z, g_ln, b_ln))
    return z @ w_out + b_out


def moe_ffn(x, w_r, b_r, w1, b1, w2, b2):
    shp = x.shape
    xf = x.reshape(-1, shp[-1])
    n = xf.shape[0]
    nk = n * TOP_K
    logits = (xf @ w_r + b_r).astype(jnp.float32)
    top_val, top_idx = lax.top_k(logits, TOP_K)
    gates = jax.nn.softmax(top_val, axis=-1)
    e_flat = top_idx.reshape(-1)
    w_flat = gates.reshape(-1)
    tok_flat = jnp.arange(nk, dtype=jnp.int32) // TOP_K
    order = jnp.argsort(e_flat, stable=True)
    e_sorted = e_flat[order]
    sizes = jnp.bincount(e_flat, length=N_EXPERTS)
    group_start = jnp.cumsum(sizes) - sizes
    padded = (sizes + MOE_BLOCK - 1) // MOE_BLOCK * MOE_BLOCK
    padded_end = jnp.cumsum(padded)
    padded_start = padded_end - padded
    dest = padded_start[e_sorted] + jnp.arange(nk, dtype=jnp.int32) - group_start[e_sorted]
    n_blocks = -(-(nk + N_EXPERTS * (MOE_BLOCK - 1)) // MOE_BLOCK)
    cap = n_blocks * MOE_BLOCK
    buf_tok = jnp.zeros((cap,), jnp.int32).at[dest].set(tok_flat[order])
    buf_w = jnp.zeros((cap,), jnp.float32).at[dest].set(w_flat[order])
    block_e = jnp.minimum(jnp.searchsorted(padded_end, jnp.arange(n_blocks) * MOE_BLOCK, side='right'),
                          N_EXPERTS - 1)

    def expert_block(args):
        tok_b, e = args
        xb = xf[tok_b]
        gate, up = jnp.split(xb @ w1[e] + b1[e], 2, axis=-1)
        gate = jnp.minimum(gate, SWIGLU_LIMIT)
        up = jnp.clip(up, -SWIGLU_LIMIT, SWIGLU_LIMIT)
        act = (up + 1.0) * gate * jax.nn.sigmoid(SWIGLU_ALPHA * gate)
        return act @ w2[e] + b2[e]

    out = lax.map(expert_block, (buf_tok.reshape(n_blocks, MOE_BLOCK), block_e))
    out = out.reshape(cap, shp[-1]) * buf_w[:, None].astype(out.dtype)
    y = jax.ops.segment_sum(out, buf_tok, num_segments=n)
    return y.reshape(shp)


def setup_inputs(seed: int = 0) -> dict:
    key = jax.random.key(seed)
    ks = iter(jax.random.split(key, 32))
    d = D_MODEL
    hk = HGRN_HEADS * HGRN_DK
    hv = HGRN_HEADS * HGRN_DV
    nh, ncv = N_HGRN_LAYERS, N_CONV_LAYERS

    def nrm(shape, scale):
        return jax.random.normal(next(ks), shape, jnp.float32) * scale

    def gain(shape):
        return 1.0 + nrm(shape, 0.02)

    return {
        "x": nrm((BATCH, SEQ, d), 1.0),
        "c": nrm((BATCH, d), 1.0),
        "ctx": nrm((BATCH, CTX_LEN, d), 1.0),
        "c_ctx": nrm((d,), 1.0),
        "w_ada": nrm((DEPTH, d, 6 * d), 0.5 * d ** -0.5),
        "b_ada": nrm((DEPTH, 6 * d), 0.02),
        "g_mix": gain((DEPTH, d)),
        "g_ffn": gain((DEPTH, d)),
        "w_hgrn_in": nrm((nh, d, 3 * hk + 2 * hv), d ** -0.5),
        "hgrn_gamma": nrm((2, nh + 1, hk), 0.5),
        "g_hgrn_out": gain((nh, hv)),
        "w_hgrn_out": nrm((nh, hv, d), hv ** -0.5),
        "w_cv_in": nrm((ncv, d, 2 * d), d ** -0.5),
        "b_cv_in": nrm((ncv, 2 * d), 0.02),
        "w_cv_dw": nrm((ncv, CONV_WIDTH, d), CONV_WIDTH ** -0.5),
        "b_cv_dw": nrm((ncv, d), 0.02),
        "g_cv_ln": gain((ncv, d)),
        "b_cv_ln": nrm((ncv, d), 0.02),
        "w_cv_out": nrm((ncv, d, d), d ** -0.5),
        "b_cv_out": nrm((ncv, d), 0.02),
        "w_router": nrm((DEPTH, d, N_EXPERTS), d ** -0.5),
        "b_router": nrm((DEPTH, N_EXPERTS), 0.01),
        "w_exp_in": nrm((DEPTH, N_EXPERTS, d, 2 * D_EXPERT), d ** -0.5),
        "b_exp_in": nrm((DEPTH, N_EXPERTS, 2 * D_EXPERT), 0.01),
        "w_exp_out": nrm((DEPTH, N_EXPERTS, D_EXPERT, d), D_EXPERT ** -0.5),
        "b_exp_out": nrm((DEPTH, N_EXPERTS, d), 0.01),
        "g_final": gain((d,)),
    }


def reference(x, c, ctx, c_ctx, w_ada, b_ada, g_mix, g_ffn, w_hgrn_in, hgrn_gamma, g_hgrn_out,
              w_hgrn_out, w_cv_in, b_cv_in, w_cv_dw, b_cv_dw, g_cv_ln, b_cv_ln, w_cv_out, b_cv_out,
              w_router, b_router, w_exp_in, b_exp_in, w_exp_out, b_exp_out, g_final):
    lb_all = jnp.cumsum(jax.nn.softmax(hgrn_gamma.astype(jnp.float32), axis=1), axis=1)
    ctx_s = ctx
    for l in range(DEPTH):
        mixer = l % N_MIXERS
        j = l // N_MIXERS
        ctx_later = any(m % N_MIXERS == 0 for m in range(l + 1, DEPTH))
        sh_m, sc_m, gt_m, sh_f, sc_f, gt_f = [m[:, None, :] for m in ada_mods(c, w_ada[l], b_ada[l])]
        h_lat = rmsnorm(x, g_mix[l]) * (1.0 + sc_m) + sh_m
        if mixer == 0 or ctx_later:
            csh_m, csc_m, cgt_m, csh_f, csc_f, cgt_f = ada_mods(c_ctx, w_ada[l], b_ada[l])
            h_ctx = rmsnorm(ctx_s, g_mix[l]) * (1.0 + csc_m) + csh_m
        if mixer == 0:
            y_lat, y_ctx = hgrn2_mixer(h_lat, h_ctx, w_hgrn_in[j], lb_all[0, j], lb_all[1, j],
                                       g_hgrn_out[j], w_hgrn_out[j], ctx_later)
        else:
            conv_p = (w_cv_in[j], b_cv_in[j], w_cv_dw[j], b_cv_dw[j], g_cv_ln[j], b_cv_ln[j],
                      w_cv_out[j], b_cv_out[j])
            y_lat = conformer_conv(h_lat, dwconv_latent, *conv_p)
            y_ctx = conformer_conv(h_ctx, dwconv_seq, *conv_p) if ctx_later else None
        x = x + gt_m * y_lat
        hf_lat = rmsnorm(x, g_ffn[l]) * (1.0 + sc_f) + sh_f
        moe_p = (w_router[l], b_router[l], w_exp_in[l], b_exp_in[l], w_exp_out[l], b_exp_out[l])
        if ctx_later:
            ctx_s = ctx_s + cgt_m * y_ctx
            hf_ctx = rmsnorm(ctx_s, g_ffn[l]) * (1.0 + csc_f) + csh_f
            n_lat = hf_lat.shape[0] * hf_lat.shape[1]
            y_all = moe_ffn(jnp.concatenate([hf_lat.reshape(-1, D_MODEL), hf_ctx.reshape(-1, D_MODEL)], axis=0),
                            *moe_p)
            x = x + gt_f * y_all[:n_lat].reshape(x.shape)
            ctx_s = ctx_s + cgt_f * y_all[n_lat:].reshape(ctx_s.shape)
        else:
            x = x + gt_f * moe_ffn(hf_lat, *moe_p)
    return rmsnorm(x, g_final)
```

```python
import numpy as np
from contextlib import ExitStack
import concourse.bass as bass
import concourse.mybir as mybir
from concourse.bass_utils import run_bass_kernel_spmd

F32 = mybir.dt.float32
BF16 = mybir.dt.bfloat16
AF = mybir.ActivationFunctionType
ALU = mybir.AluOpType
AX = mybir.AxisListType

SEM_LIMIT = 12000
D = 1024
T = 4096
TCTX = 256
NH = 8
EPS = 1e-6
NE = 32
NQ_FFN = 8


class Prog:
    def __init__(self, nc):
        self.nc = nc
        self.es = ExitStack()
        self.eng = {"pe": nc.tensor, "dve": nc.vector, "act": nc.scalar,
                    "pool": nc.gpsimd, "sp": nc.sync}
        self.sem = {}
        self.cnt = {}
        self.nsem = 0
        for e in ("pe", "dve", "act", "pool"):
            self._new_sem(e)
        self.waited = {e: {} for e in self.eng}
        self.res = {}
        self.dsem = {}
        self.dcnt = {}
        self.ninst = {e: 0 for e in self.eng}
        self.psum_ids = set()

    def _alloc_sem(self, name):
        self.nsem += 1
        return self.es.enter_context(self.nc.semaphore(name))

    def _new_sem(self, e):
        self.sem[e] = self._alloc_sem("e_%s_%d" % (e, self.nsem))
        self.cnt[e] = 0

    def sb(self, name, shape, dt):
        return self.es.enter_context(self.nc.sbuf_tensor("s_" + name, list(shape), dt))

    def ps(self, name, shape, dt=F32):
        t = self.es.enter_context(self.nc.psum_tensor("p_" + name, list(shape), dt))
        self.psum_ids.add(id(t))
        return t

    def _r(self, key):
        k = key if isinstance(key, (str, tuple)) else id(key)
        r = self.res.get(k)
        if r is None:
            r = {"w": None, "r": {}}
            self.res[k] = r
        return r

    def _wait(self, e, sem, val):
        w = self.waited[e]
        k = id(sem)
        if w.get(k, 0) >= val:
            return
        w[k] = val
        self.eng[e].wait_ge(sem, val)

    def _deps(self, e, reads, writes):
        deps = []
        for r in reads:
            rr = self._r(r)
            if rr["w"] is not None:
                deps.append(rr["w"])
            if id(r) in self.psum_ids:
                deps.extend(rr["r"].values())
        for wk in writes:
            rr = self._r(wk)
            if rr["w"] is not None:
                deps.append(rr["w"])
            deps.extend(rr["r"].values())
        own = self.sem.get(e)
        for (sem, val) in deps:
            if e == "pe" and sem is own:
                continue
            self._wait(e, sem, val)

    def _mark(self, sem, val, reads, writes):
        for r in reads:
            self._r(r)["r"][id(sem)] = (sem, val)
        for wk in writes:
            rr = self._r(wk)
            rr["w"] = (sem, val)
            rr["r"] = {}

    def op(self, e, fn, reads=(), writes=()):
        if self.cnt[e] >= SEM_LIMIT:
            self._new_sem(e)
        self._deps(e, reads, writes)
        inst = fn(self.eng[e])
        self.cnt[e] += 1
        self.ninst[e] += 1
        inst.then_inc(self.sem[e], 1)
        self._mark(self.sem[e], self.cnt[e], reads, writes)
        return inst

    def dma(self, q, out, in_, reads=(), writes=(), semkey=None, **kw):
        self._deps(q, reads, writes)
        key = semkey if semkey is not None else writes[0]
        key = key if isinstance(key, (str, tuple)) else id(key)
        if key not in self.dsem or self.dcnt[key] >= 2 * SEM_LIMIT:
            self.dsem[key] = self._alloc_sem("d%d" % self.nsem)
            self.dcnt[key] = 0
        s = self.dsem[key]
        inst = self.eng[q].dma_start(out=out, in_=in_, **kw)
        self.dcnt[key] += 16
        self.ninst[q] += 1
        inst.then_inc(s, 16)
        self._mark(s, self.dcnt[key], reads, writes)
        return inst

    def finish(self, out_keys, e="sp"):
        for k in out_keys:
            rr = self._r(k)
            if rr["w"] is not None:
                self._wait(e, *rr["w"])
        for x in ("pe", "dve", "act", "pool"):
            if self.cnt[x] > 0:
                self._wait(e, self.sem[x], self.cnt[x])

    def act(self, out, in_, func, reads, writes, **kw):
        return self.op("act", lambda e: e.activation(out, in_, func, **kw), reads, writes)

    def tt(self, eng, out, a, b, op, reads, writes):
        return self.op(eng, lambda e: e.tensor_tensor(out, a, b, op), reads, writes)

    def ts(self, eng, out, a, s1, s2, op0, op1, reads, writes):
        if s2 is None:
            return self.op(eng, lambda e: e.tensor_scalar(out, a, s1, None, op0), reads, writes)
        return self.op(eng, lambda e: e.tensor_scalar(out, a, s1, s2, op0, op1), reads, writes)

    def copy(self, eng, out, in_, reads, writes):
        if eng == "act":
            return self.op("act", lambda e: e.copy(out, in_), reads, writes)
        return self.op(eng, lambda e: e.tensor_copy(out, in_), reads, writes)

    def mm(self, out, lhsT, rhs, start, stop, reads, writes):
        return self.op("pe", lambda e: e.matmul(out, lhsT, rhs, start=start, stop=stop), reads, writes)


def dram_in(nc, name, shape, dt=F32):
    return nc.dram_tensor(name, list(shape), dt, kind="ExternalInput").ap()


def dram_out(nc, name, shape, dt=F32):
    return nc.dram_tensor(name, list(shape), dt, kind="ExternalOutput").ap()


class Common:
    def __init__(self, p, nc):
        self.p = p
        self.ident_d = dram_in(nc, "ident", [128, 128])
        self.identf = p.sb("identf", [128, 128], F32)
        self.identb = p.sb("identb", [128, 128], BF16)
        self.ones = p.sb("ones", [128, 128], F32)
        p.dma("sp", self.identf[:], self.ident_d, writes=[self.identf])
        p.copy("dve", self.identb[:], self.identf[:], [self.identf], [self.identb])
        p.op("dve", lambda e: e.memset(self.ones[:], 1.0), [], [self.ones])
        self.ones5 = p.sb("ones5", [128, 512], F32)
        p.op("dve", lambda e: e.memset(self.ones5[:], 1.0), [], [self.ones5])
        self.psb = [p.ps("psb%d" % i, [128, 512]) for i in range(7)]
        self.psT = p.ps("psT", [128, 1024], BF16)


def emit_mods(p, cm, w_ada, b_ada, lo, hi, pairs, tag):
    lts = []
    for i, (ccol, out) in enumerate(pairs):
        p.dma("sp", out[:], b_ada[:, lo:hi].partition_broadcast(128), writes=[out])
        cc = p.sb("mod_cc%s%d" % (tag, i), [128, 8], F32)
        sg = p.sb("mod_sg%s%d" % (tag, i), [128, 8], F32)
        lt = p.sb("mod_lt%s%d" % (tag, i), [128, 8, 128], F32)
        p.dma("sp", cc[:], ccol, writes=[cc])
        p.act(sg[:], cc[:], AF.Sigmoid, [cc], [sg])
        p.tt("dve", sg[:], sg[:], cc[:], ALU.mult, [sg, cc], [sg])
        for kc in range(8):
            p.ts("dve", lt[:, kc, :], cm.ones[:], sg[:, kc:kc + 1], None, ALU.mult, None, [cm.ones, sg], [lt])
        lts.append(lt)
    wc = p.sb("mod_w%s" % tag, [128, 8, 256], F32)
    for bi, n0 in enumerate(range(lo, hi, 256)):
        p.dma("sp", wc[:], w_ada[:, n0:n0 + 256].rearrange("(kc q) n -> q kc n", q=128), writes=[wc])
        for i, (ccol, out) in enumerate(pairs):
            ps = cm.psb[i % 2]
            for kc in range(8):
                p.mm(ps[:, 0:256], lts[i][:, kc, :], wc[:, kc, :], kc == 0, kc == 7, [lts[i], wc], [ps])
            p.tt("dve", out[:, n0 - lo:n0 - lo + 256], ps[:, 0:256], out[:, n0 - lo:n0 - lo + 256], ALU.add, [ps, out], [out])


def emit_norm_T(p, cm, src, ntiles, Arow, Brow, modt, hT, tagres, xt, junk, h, st, hTf=None):
    for t in range(ntiles):
        xx = xt[t % 2]
        p.dma("sp", xx[:], src[t * 128:(t + 1) * 128, :], reads=[tagres] if tagres else [], writes=[xx])
        ss, rstd = st
        p.act(junk[:], xx[:], AF.Square, [xx], [junk, ss], accum_out=ss[:])
        p.act(rstd[:], ss[:], AF.Sqrt, [ss], [rstd], scale=1.0 / D, bias=EPS)
        p.op("dve", lambda e: e.reciprocal(rstd[:], rstd[:]), [rstd], [rstd])
        p.op("dve", lambda e: e.scalar_tensor_tensor(h[:], xx[:], rstd[:], Arow, ALU.mult, ALU.mult),
             [xx, rstd, modt], [h])
        p.tt("dve", h[:], h[:], Brow, ALU.add, [h, modt], [h])
        for half in range(2):
            pt = cm.psb[half]
            for j in range(4):
                kc = half * 4 + j
                p.op("pe", lambda e: e.transpose(pt[:, j * 128:(j + 1) * 128], h[:, kc * 128:(kc + 1) * 128], cm.identf[:]),
                     [h, cm.identf], [pt])
            p.copy("act", hT[:, half * 4:(half + 1) * 4, t * 128:(t + 1) * 128],
                   pt[:].rearrange("q (a b) -> q a b", a=4), [pt], [hT])
            if hTf is not None:
                p.copy("dve", hTf[:, half * 4:(half + 1) * 4, t * 128:(t + 1) * 128],
                       pt[:].rearrange("q (a b) -> q a b", a=4), [pt], [hTf])


DBG_STOP = None
DBG_LVL = 0
DBG_SKIP = ''


def build_l1():
    nc = bass.Bass("TRN2", target_bir_lowering=False)
    p = Prog(nc)
    xs = dram_in(nc, "xs", [T, D])
    ctxb = dram_in(nc, "ctxb", [TCTX, D])
    ccol = dram_in(nc, "ccol", [128, 8])
    cctx = dram_in(nc, "cctx", [128, 8])
    w_ada = dram_in(nc, "w_ada0", [D, 6 * D])
    b_ada = dram_in(nc, "b_ada0", [1, 6 * D])
    g_mix = dram_in(nc, "g_mix0", [1, D])
    w_hin = dram_in(nc, "w_hin", [D, 5 * D])
    gam = dram_in(nc, "gam", [128, 2, 2, 8])
    maskf_d = dram_in(nc, "maskf", [64, 64])
    maskb_d = dram_in(nc, "maskb", [64, 64])
    rm_d = dram_in(nc, "rm", [128, 512])
    o_d = [dram_out(nc, "o_f", [T, D]), dram_out(nc, "o_b", [T, D])]
    sg_d = dram_out(nc, "sg", [T, D], BF16)
    Q_d = [dram_out(nc, "QF", [NH, 128, T], BF16), dram_out(nc, "QB", [NH, 128, T], BF16)]
    Sloc_d = dram_out(nc, "Sloc", [2, NH, 128, 128])
    Sctx_d = dram_out(nc, "Sctx", [2, NH, 128, 128])
    Dloc_d = dram_out(nc, "Dloc", [128, 16])
    cm = Common(p, nc)

    maskt = [p.sb("maskf", [64, 64], F32), p.sb("maskb", [64, 64], F32)]
    rm = p.sb("rm", [128, 512], F32)
    p.dma("sp", maskt[0][:], maskf_d, writes=[maskt[0]])
    p.dma("sp", maskt[1][:], maskb_d, writes=[maskt[1]])
    p.dma("sp", rm[:], rm_d, writes=[rm])

    gm = p.sb("gm", [128, 2, 2, 8], F32)
    lb = p.sb("lb", [128, 16], F32)
    oml = p.sb("oml", [128, 16], F32)
    p.dma("sp", gm[:], gam, writes=[gm])
    for d in range(2):
        p.tt("dve", lb[:, d * 8:(d + 1) * 8], gm[:, d, 0, :], gm[:, d, 1, :], ALU.subtract, [gm], [lb])
    p.act(lb[:], lb[:], AF.Sigmoid, [lb], [lb])
    p.ts("dve", oml[:], lb[:], -1.0, 1.0, ALU.mult, ALU.add, [lb], [oml])

    if DBG_LVL == 1:
        p.finish([])
        return nc, p
    modb = p.sb("modb", [128, 2048], F32)
    modc = p.sb("modc", [128, 2048], F32)
    emit_mods(p, cm, w_ada, b_ada, 0, 2048, [(ccol, modb), (cctx, modc)], "a")
    gmix = p.sb("gmix", [128, D], F32)
    p.dma("sp", gmix[:], g_mix.partition_broadcast(128), writes=[gmix])
    for m in (modb, modc):
        p.op("dve", lambda e: e.scalar_tensor_tensor(m[:, 1024:2048], m[:, 1024:2048], 1.0, gmix[:], ALU.add, ALU.mult),
             [m, gmix], [m])

    if DBG_LVL == 2:
        p.finish([])
        return nc, p
    hT = p.sb("hT", [128, 8, T], BF16)
    hTc = p.sb("hTc", [128, 8, TCTX], BF16)
    xt = [p.sb("xt%d" % i, [128, D], F32) for i in range(2)]
    h = p.sb("h", [128, D], F32)
    junk = h
    st = (p.sb("ss", [128, 1], F32), p.sb("rstd", [128, 1], F32))
    emit_norm_T(p, cm, ctxb, TCTX // 128, modc[:, 1024:2048], modc[:, 0:1024], modc, hTc, None, xt, junk, h, st)
    if DBG_LVL == 3:
        p.finish([])
        return nc, p
    emit_norm_T(p, cm, xs, (T // 128) if DBG_LVL != 4 else 4, modb[:, 1024:2048], modb[:, 0:1024], modb, hT, None, xt, junk, h, st)
    if DBG_LVL in (4, 5):
        p.finish([])
        return nc, p

    col0 = [0, 2048, 3072, 1024, 4096]
    whf = [p.sb("whf%d" % i, [128, 8, 128], F32) for i in range(2)]
    whb = [p.sb("whb%d" % i, [128, 8, 5, 128], BF16) for i in range(2)]

    def blk_tiles(i):
        s = "_%d" % i
        return dict(
            q=p.sb("q" + s, [128, 512], F32), F=p.sb("F" + s, [128, 512], F32),
            LF=p.sb("LF" + s, [128, 512], F32), U=p.sb("U" + s, [128, 512], F32),
            E=p.sb("E" + s, [128, 512], F32), Bc=p.sb("Bc" + s, [128, 512], F32),
            BL=p.sb("BL" + s, [128, 8], F32), DEC=p.sb("DEC" + s, [128, 8], F32),
            QG=p.sb("QG" + s, [128, 512], BF16), KD=p.sb("KD" + s, [128, 512], BF16),
            KGT=p.sb("KGT" + s, [128, 512], BF16), QX=p.sb("QX" + s, [128, 512], BF16),
            kg=p.sb("kg" + s, [64, 8, 128], BF16), v=p.sb("v" + s, [64, 8, 128], BF16),
            sgt=p.sb("sgt" + s, [64, 8, 128], BF16), ob=p.sb("ob" + s, [64, 8, 128], F32),
            tc=p.sb("tc" + s, [128, 1], F32), sgs=p.sb("sgs" + s, [64, 2, 128], F32), sgg=p.sb("sgg" + s, [64, 2, 128], F32))

    bt = [blk_tiles(0), blk_tiles(1)]
    S32 = p.sb("S32", [128, 128], F32)
    Sb = p.sb("Sb", [128, 128], BF16)
    ATs = p.sb("ATs", [64, 64], BF16)
    Dcol = p.sb("Dcol", [128, 16], F32)
    psq, psz, psv, psA, pso, psS = cm.psb[0], cm.psb[1], cm.psb[2], cm.psb[3], cm.psb[4], cm.psb[5]
    psT = cm.psT
    bcount = 0

    for hh in range(NH):
        if DBG_STOP is not None and hh >= DBG_STOP[0]:
            break
        wb = whb[hh % 2]
        for s in range(5):
            c0 = col0[s] + hh * 128
            wf_ = whf[s % 2]
            p.dma("sp", wf_[:], w_hin[:, c0:c0 + 128].rearrange("(kc q) n -> q kc n", q=128), writes=[wf_])
            p.copy("pool", wb[:, :, s, :], wf_[:], [wf_], [wb])
        for (is_ctx, hTs, TT) in ((True, hTc, TCTX), (False, hT, T)):
            nb_tok = 256 if is_ctx else 512
            nblk = TT // nb_tok
            nch = nb_tok // 64
            for d in range(2):
                if DBG_STOP is not None and (is_ctx, d) == DBG_STOP[1]:
                    break
                col = d * 8 + hh
                blocks = list(range(nblk)) if d == 0 else list(range(nblk - 1, -1, -1))
                carry = None
                first = True
                for blk in blocks:
                    b = bt[bcount % 2]
                    bcount += 1
                    t0 = blk * nb_tok
                    n = nb_tok
                    q, Ft, LF, U, E, Bc = b["q"], b["F"], b["LF"], b["U"], b["E"], b["Bc"]
                    for kc in range(8):
                        p.mm(psq[:, 0:n], wb[:, kc, 0, :], hTs[:, kc, t0:t0 + n], kc == 0, kc == 7, [wb, hTs], [psq])
                    p.copy("act", q[:, 0:n], psq[:, 0:n], [psq], [q])
                    for kc in range(8):
                        p.mm(psz[:, 0:n], wb[:, kc, 1 + d, :], hTs[:, kc, t0:t0 + n], kc == 0, kc == 7, [wb, hTs], [psz])
                    p.act(Ft[:, 0:n], psz[:, 0:n], AF.Sigmoid, [psz], [Ft])
                    for c2 in range(nch // 2):
                        for j in range(2):
                            c = c2 * 2 + j
                            for kc in range(8):
                                p.mm(psv[0:64, j * 256:(j + 1) * 256], hTs[:, kc, t0 + c * 64:t0 + (c + 1) * 64],
                                     wb[:, kc, 3:5, :].rearrange("q a b -> q (a b)"), kc == 0, kc == 7, [wb, hTs], [psv])
                        pv = psv[0:64, :].rearrange("q (j x) -> q j x", j=2)
                        p.copy("dve", b["v"][:, c2 * 2:c2 * 2 + 2, :], pv[:, :, 0:128], [psv], [b["v"]])
                        if d == 0 and not is_ctx:
                            p.copy("dve", b["sgg"][:, 0:2, :], pv[:, :, 128:256], [psv], [b["sgg"]])
                            p.act(b["sgs"][:, 0:2, :], b["sgg"][:, 0:2, :], AF.Sigmoid, [b["sgg"]], [b["sgs"]])
                            p.tt("pool", b["sgt"][:, c2 * 2:c2 * 2 + 2, :], b["sgg"][:, 0:2, :], b["sgs"][:, 0:2, :], ALU.mult,
                                 [b["sgg"], b["sgs"]], [b["sgt"]])
                    if d == 0 and not is_ctx and "C" not in DBG_SKIP:
                        p.dma("sp", sg_d[t0:t0 + n, hh * 128:(hh + 1) * 128].rearrange("(c q) v -> q c v", q=64),
                              b["sgt"][:, 0:nch, :], reads=[b["sgt"]], writes=["sg_d"])
                    if DBG_LVL == 6:
                        p.finish([]); return nc, p
                    p.ts("dve", Ft[:, 0:n], Ft[:, 0:n], oml[:, col:col + 1], lb[:, col:col + 1], ALU.mult, ALU.add,
                         [Ft, oml, lb], [Ft])
                    p.act(LF[:, 0:n], Ft[:, 0:n], AF.Ln, [Ft], [LF])
                    p.ts("pool", Ft[:, 0:n], Ft[:, 0:n], -1.0, 1.0, ALU.mult, ALU.add, [Ft], [Ft])
                    p.op("dve", lambda e: e.tensor_tensor_scan(U[:, 0:n], rm[:, 0:n], LF[:, 0:n], 0.0, ALU.mult, ALU.add),
                         [rm, LF], [U])
                    U3 = U[:, 0:n].rearrange("q (c k) -> q c k", k=64)
                    p.copy("pool", b["BL"][:, 0:nch].rearrange("q (c o) -> q c o", o=1), U3[:, :, 63:64], [U], [b["BL"]])
                    p.act(b["DEC"][:, 0:nch], b["BL"][:, 0:nch], AF.Exp, [b["BL"]], [b["DEC"]])
                    if not is_ctx and "B" not in DBG_SKIP:
                        init = 0.0 if (carry is None or d == 1) else carry[0]
                        rdc = [] if (carry is None or d == 1) else [carry[1]]
                        p.op("dve", lambda e: e.tensor_tensor_scan(Bc[:, 0:n], cm.ones5[:, 0:n], LF[:, 0:n], init, ALU.mult, ALU.add),
                             [cm.ones5, LF] + rdc, [Bc])
                        if d == 0:
                            carry = (Bc[:, n - 1:n], Bc)
                        else:
                            if carry is None:
                                p.copy("pool", b["tc"][:], Bc[:, n - 1:n], [Bc], [b["tc"]])
                            else:
                                p.tt("pool", b["tc"][:], Bc[:, n - 1:n], carry[0], ALU.add, [Bc, carry[1]], [b["tc"]])
                            carry = (b["tc"][:], b["tc"])
                            p.tt("dve", Bc[:, 0:n], LF[:, 0:n], Bc[:, 0:n], ALU.subtract, [LF, Bc], [Bc])
                            p.ts("dve", Bc[:, 0:n], Bc[:, 0:n], b["tc"][:], None, ALU.add, None, [Bc, b["tc"]], [Bc])
                        p.act(E[:, 0:n], Bc[:, 0:n], AF.Exp, [Bc], [E])
                        p.tt("pool", b["QX"][:, 0:n], q[:, 0:n], E[:, 0:n], ALU.mult, [q, E], [b["QX"]])
                        p.dma("sp", Q_d[d][hh, :, t0:t0 + n], b["QX"][:, 0:n], reads=[b["QX"]], writes=["Q_d%d" % d])
                        if d == 1 and carry is not None:
                            pass
                    if d == 1:
                        p.tt("dve", U[:, 0:n], LF[:, 0:n], U[:, 0:n], ALU.subtract, [LF, U], [U])
                        p.tt("dve", U3, U3, b["BL"][:, 0:nch].rearrange("q (c o) -> q c o", o=1).to_broadcast([128, nch, 64]),
                             ALU.add, [U, b["BL"]], [U])
                    p.act(E[:, 0:n], U[:, 0:n], AF.Exp, [U], [E])
                    p.tt("dve", b["QG"][:, 0:n], q[:, 0:n], E[:, 0:n], ALU.mult, [q, E], [b["QG"]])
                    p.act(LF[:, 0:n], U[:, 0:n], AF.Exp, [U], [LF], scale=-1.0)
                    p.tt("pool", b["KD"][:, 0:n], Ft[:, 0:n], LF[:, 0:n], ALU.mult, [Ft, LF], [b["KD"]])
                    p.tt("dve", b["KGT"][:, 0:n].rearrange("q (c k) -> q c k", k=64),
                         b["KD"][:, 0:n].rearrange("q (c k) -> q c k", k=64),
                         b["DEC"][:, 0:nch].rearrange("q (c o) -> q c o", o=1).to_broadcast([128, nch, 64]),
                         ALU.mult, [b["KD"], b["DEC"]], [b["KGT"]])
                    if DBG_LVL == 7:
                        p.finish([]); return nc, p
                    for c in range(nch):
                        p.op("pe", lambda e: e.transpose(psT[0:64, c * 128:(c + 1) * 128], b["KGT"][:, c * 64:(c + 1) * 64], cm.identb[:]),
                             [b["KGT"], cm.identb], [psT])
                    p.copy("act", b["kg"][:, 0:nch, :], psT[0:64, 0:nch * 128].rearrange("q (c k) -> q c k", k=128), [psT], [b["kg"]])
                    if DBG_LVL == 8:
                        p.finish([]); return nc, p
                    order = list(range(nch)) if d == 0 else list(range(nch - 1, -1, -1))
                    if "A" in DBG_SKIP:
                        order = []
                    for c in order:
                        cs = slice(c * 64, (c + 1) * 64)
                        p.mm(psA[0:64, 0:64], b["KD"][:, cs], b["QG"][:, cs], True, True, [b["KD"], b["QG"]], [psA])
                        p.tt("dve", ATs[:], psA[0:64, 0:64], maskt[d][:], ALU.mult, [psA, maskt[d]], [ATs])
                        if DBG_LVL == 9:
                            continue
                        p.mm(pso[0:64, 0:128], ATs[:], b["v"][:, c, :], True, first, [ATs, b["v"]], [pso])
                        if not first:
                            p.mm(pso[0:64, 0:128], b["QG"][:, cs], Sb[:], False, True, [b["QG"], Sb], [pso])
                        if not is_ctx:
                            p.copy("act", b["ob"][:, c, :], pso[0:64, 0:128], [pso], [b["ob"]])
                        if DBG_LVL == 10:
                            first = False
                            continue
                        p.mm(psS[:, 0:128], b["kg"][:, c, :], b["v"][:, c, :], True, True, [b["kg"], b["v"]], [psS])
                        if first:
                            p.copy("dve", S32[:], psS[:, 0:128], [psS], [S32])
                        else:
                            p.op("dve", lambda e: e.scalar_tensor_tensor(S32[:], S32[:], b["DEC"][:, c:c + 1], psS[:, 0:128], ALU.mult, ALU.add),
                                 [S32, b["DEC"], psS], [S32])
                        p.copy("pool", Sb[:], S32[:], [S32], [Sb])
                        first = False
                    if not is_ctx and "D" not in DBG_SKIP:
                        p.dma("sp", o_d[d][t0:t0 + n, hh * 128:(hh + 1) * 128].rearrange("(c q) v -> q c v", q=64),
                              b["ob"][:, 0:nch, :], reads=[b["ob"]], writes=["o_d%d" % d])
                if is_ctx:
                    p.dma("sp", Sctx_d[d, hh], S32[:], reads=[S32], writes=["Sctx_d"])
                else:
                    p.dma("sp", Sloc_d[d, hh], S32[:], reads=[S32], writes=["Sloc_d"])
                    if "B" not in DBG_SKIP:
                        p.act(Dcol[:, col:col + 1], carry[0], AF.Exp, [carry[1]], [Dcol])
    p.dma("sp", Dloc_d, Dcol[:], reads=[Dcol], writes=["Dloc_d"])
    p.finish(["sg_d", "Q_d0", "Q_d1", "o_d0", "o_d1", "Sctx_d", "Sloc_d", "Dloc_d"])
    return nc, p


def consts():
    s = np.arange(64)[:, None]
    t = np.arange(64)[None, :]
    rm = np.ones((128, 512), np.float32)
    rm[:, ::64] = 0.0
    return {"ident": np.eye(128, dtype=np.float32),
            "maskf": (s <= t).astype(np.float32), "maskb": (s >= t).astype(np.float32), "rm": rm}


def col_layout(v):
    return np.ascontiguousarray(np.asarray(v, np.float32).reshape(8, 128).T)


def l1_inputs(inp, core):
    b, j = core // 4, core % 4
    m = dict(consts())
    m["xs"] = np.ascontiguousarray(inp["x"][b, j * T:(j + 1) * T])
    m["ctxb"] = np.ascontiguousarray(inp["ctx"][b])
    m["ccol"] = col_layout(inp["c"][b])
    m["cctx"] = col_layout(inp["c_ctx"])
    m["w_ada0"] = np.ascontiguousarray(inp["w_ada"][0])
    m["b_ada0"] = np.ascontiguousarray(inp["b_ada"][0][None, :])
    m["g_mix0"] = np.ascontiguousarray(inp["g_mix"][0][None, :])
    m["w_hin"] = np.ascontiguousarray(inp["w_hgrn_in"][0])
    g = np.asarray(inp["hgrn_gamma"], np.float32).reshape(2, 2, 8, 128)
    m["gam"] = np.ascontiguousarray(g.transpose(3, 0, 1, 2))
    return m


def emit_ffn(p, cm, x1_d, x1_key, xo_d, xo_key, modt, mo, gffn_d, wr_d, br_d, w1_d, b1c_d, w2_d, b2_d, tl, final_g=None):
    NQ = NQ_FFN
    TQ = T // NQ
    hfT = tl["hfT"]
    xt, h, st = tl["xt"], tl["h"], tl["st"]
    gf = h
    p.dma("sp", gf[:], gffn_d.partition_broadcast(128), writes=[gf])
    p.op("dve", lambda e: e.scalar_tensor_tensor(modt[:, mo + 1024:mo + 2048], modt[:, mo + 1024:mo + 2048], 1.0, gf[:], ALU.add, ALU.mult),
         [modt, gf], [modt])
    if DBG_LVL == 16:
        return
    wr = p.sb("wr", [128, 8, NE], F32)
    brb = p.sb("brb", [128, NE], F32)
    p.dma("sp", wr[:], wr_d.rearrange("(kc q) n -> q kc n", q=128), writes=[wr])
    p.dma("sp", brb[:], br_d.partition_broadcast(128), writes=[brb])
    G = p.sb("G_all", [128, T // 128, NE], F32)
    GT = p.sb("GT_q", [NE, T // NQ_FFN], F32)
    b2s = p.sb("b2s", [NE, D], F32)
    b1c = p.sb("b1c", [128, NE, 16], F32)
    p.dma("sp", b2s[:], b2_d, writes=[b2s])
    p.dma("sp", b1c[:], b1c_d, writes=[b1c])
    if DBG_LVL == 17:
        return
    hTf = p.sb("hTf", [128, 8, 128], F32)
    lg = p.sb("lg", [128, NE], F32)
    m8 = p.sb("m8", [128, 8], F32)
    nmx = p.sb("nmx", [128, 1], F32)
    msk = p.sb("msk", [128, NE], F32)
    ex = p.sb("ex", [128, NE], F32)
    sm = p.sb("sm", [128, 1], F32)
    ps_l = cm.psb[2]
    ss, rstd = st
    for t in range(T // 128):
        xx = xt[t % 2]
        p.dma("sp", xx[:], x1_d[t * 128:(t + 1) * 128, :], reads=[x1_key], writes=[xx])
        p.act(h[:], xx[:], AF.Square, [xx], [h, ss], accum_out=ss[:])
        p.act(rstd[:], ss[:], AF.Sqrt, [ss], [rstd], scale=1.0 / D, bias=EPS)
        p.op("dve", lambda e: e.reciprocal(rstd[:], rstd[:]), [rstd], [rstd])
        p.op("dve", lambda e: e.scalar_tensor_tensor(h[:], xx[:], rstd[:], modt[:, mo + 1024:mo + 2048], ALU.mult, ALU.mult),
             [xx, rstd, modt], [h])
        p.tt("dve", h[:], h[:], modt[:, mo:mo + 1024], ALU.add, [h, modt], [h])
        for half in range(2):
            pt = cm.psb[half]
            for j in range(4):
                kc = half * 4 + j
                p.op("pe", lambda e: e.transpose(pt[:, j * 128:(j + 1) * 128], h[:, kc * 128:(kc + 1) * 128], cm.identf[:]),
                     [h, cm.identf], [pt])
            p.copy("act", hfT[:, half * 4:(half + 1) * 4, t * 128:(t + 1) * 128],
                   pt[:].rearrange("q (a b) -> q a b", a=4), [pt], [hfT])
            p.copy("dve", hTf[:, half * 4:(half + 1) * 4, :], pt[:].rearrange("q (a b) -> q a b", a=4), [pt], [hTf])
        if DBG_LVL == 20:
            return
        for kc in range(8):
            p.mm(ps_l[:, 0:NE], hTf[:, kc, :], wr[:, kc, :], kc == 0, kc == 7, [hTf, wr], [ps_l])
        p.tt("dve", lg[:], ps_l[:, 0:NE], brb[:], ALU.add, [ps_l, brb], [lg])
        if DBG_LVL == 19:
            return
        p.op("dve", lambda e: e.max(m8[:], lg[:]), [lg], [m8])
        p.ts("dve", msk[:], lg[:], m8[:, 3:4], None, ALU.is_ge, None, [lg, m8], [msk])
        p.ts("dve", nmx[:], m8[:, 0:1], -1.0, None, ALU.mult, None, [m8], [nmx])
        if DBG_LVL == 18:
            return
        p.act(ex[:], lg[:], AF.Exp, [lg, nmx], [ex], bias=nmx[:])
        p.tt("dve", ex[:], ex[:], msk[:], ALU.mult, [ex, msk], [ex])
        p.op("dve", lambda e: e.tensor_reduce(sm[:], ex[:], AX.X, ALU.add), [ex], [sm])
        p.op("dve", lambda e: e.reciprocal(sm[:], sm[:]), [sm], [sm])
        p.ts("dve", G[:, t, :], ex[:], sm[:], None, ALU.mult, None, [ex, sm], [G])
        if DBG_LVL == 21:
            return
    if DBG_LVL == 22:
        return
    yacc = tl["yacc"]
    actT = tl["actT"]
    w2b = tl["w2b"]
    wst = tl["wst"]
    w2st = tl["w2st"]
    wgb = tl["wgb"]
    wub = tl["wub"]
    tg, tsg, tu = tl["tg"], tl["tsg"], tl["tu"]
    psg, psu, psy = cm.psb[3], cm.psb[4], cm.psb[5]
    cnt = 0
    for qd in range(NQ):
        tq0 = qd * TQ
        for ti in range(TQ // 128):
            tg_ = qd * (TQ // 128) + ti
            p.op("pe", lambda e: e.transpose(ps_l[0:NE, 128:256], G[:, tg_, :], cm.identf[:]), [G, cm.identf], [ps_l])
            p.copy("dve", GT[:, ti * 128:(ti + 1) * 128], ps_l[0:NE, 128:256], [ps_l], [GT])
        for ti in range(TQ // 128):
            tg_ = qd * (TQ // 128) + ti
            for dh in range(2):
                p.mm(psy[:], GT[:, ti * 128:(ti + 1) * 128], b2s[:, dh * 512:(dh + 1) * 512], True, True, [GT, b2s], [psy])
                p.copy("act", yacc[:, ti, dh * 512:(dh + 1) * 512], psy[:], [psy], [yacc])
        if DBG_LVL == 23:
            return
        for ex_ in range(NE):
            if DBG_LVL == 24 and ex_ == 1:
                return
            for j in range(8):
                k = cnt % 2
                cnt += 1
                p.dma("sp", wst[k][:], w1_d[ex_, :, j * 128:(j + 1) * 128].rearrange("(kc q) n -> q kc n", q=128), writes=[wst[k]])
                p.copy("pool", wgb[k][:], wst[k][:], [wst[k]], [wgb[k]])
                p.dma("sp", wst[k][:], w1_d[ex_, :, 1024 + j * 128:1024 + (j + 1) * 128].rearrange("(kc q) n -> q kc n", q=128), writes=[wst[k]])
                p.copy("pool", wub[k][:], wst[k][:], [wst[k]], [wub[k]])
                p.dma("sp", w2st[k][:], w2_d[ex_, j * 128:(j + 1) * 128, :], writes=[w2st[k]])
                p.copy("act", w2b[:, j, :], w2st[k][:], [w2st[k]], [w2b])
                for tb in range(TQ // 512):
                    tok = slice(tq0 + tb * 512, tq0 + (tb + 1) * 512)
                    for kc in range(8):
                        p.mm(psg[:], wgb[k][:, kc, :], hfT[:, kc, tok], kc == 0, kc == 7, [wgb[k], hfT], [psg])
                    for kc in range(8):
                        p.mm(psu[:], wub[k][:, kc, :], hfT[:, kc, tok], kc == 0, kc == 7, [wub[k], hfT], [psu])
                    p.ts("dve", tg[:], psg[:], b1c[:, ex_, j:j + 1], 7.0, ALU.add, ALU.min, [psg, b1c], [tg])
                    p.act(tsg[:], tg[:], AF.Sigmoid, [tg], [tsg], scale=1.702)
                    p.ts("dve", tu[:], psu[:], b1c[:, ex_, 8 + j:9 + j], 7.0, ALU.add, ALU.min, [psu, b1c], [tu])
                    p.ts("pool", tu[:], tu[:], -7.0, 1.0, ALU.max, ALU.add, [tu], [tu])
                    p.tt("pool", tg[:], tg[:], tsg[:], ALU.mult, [tg, tsg], [tg])
                    p.tt("dve", actT[:, j, tb * 512:(tb + 1) * 512], tg[:], tu[:], ALU.mult, [tg, tu], [actT])
            for ti in range(TQ // 128):
                tg_ = qd * (TQ // 128) + ti
                for dh in range(2):
                    for j in range(8):
                        p.mm(psy[:], actT[:, j, ti * 128:(ti + 1) * 128], w2b[:, j, dh * 512:(dh + 1) * 512], j == 0, j == 7,
                             [actT, w2b], [psy])
                    ya = yacc[:, ti, dh * 512:(dh + 1) * 512]
                    p.op("dve", lambda e: e.scalar_tensor_tensor(ya, psy[:], G[:, tg_, ex_:ex_ + 1], ya, ALU.mult, ALU.add),
                         [psy, G, yacc], [yacc])
        for ti in range(TQ // 128):
            tg_ = qd * (TQ // 128) + ti
            xx = xt[ti % 2]
            p.dma("sp", xx[:], x1_d[tg_ * 128:(tg_ + 1) * 128, :], reads=[x1_key], writes=[xx])
            p.tt("dve", yacc[:, ti, :], yacc[:, ti, :], modt[:, mo + 2048:mo + 3072], ALU.mult, [yacc, modt], [yacc])
            p.tt("dve", xx[:], xx[:], yacc[:, ti, :], ALU.add, [xx, yacc], [xx])
            if final_g is not None:
                p.act(h[:], xx[:], AF.Square, [xx], [h, ss], accum_out=ss[:])
                p.act(rstd[:], ss[:], AF.Sqrt, [ss], [rstd], scale=1.0 / D, bias=EPS)
                p.op("dve", lambda e: e.reciprocal(rstd[:], rstd[:]), [rstd], [rstd])
                p.op("dve", lambda e: e.scalar_tensor_tensor(xx[:], xx[:], rstd[:], final_g[:], ALU.mult, ALU.mult),
                     [xx, rstd, final_g], [xx])
            p.dma("sp", xo_d[tg_ * 128:(tg_ + 1) * 128, :], xx[:], reads=[xx], writes=[xo_key])


def ffn_tiles(p):
    TQ = T // NQ_FFN
    return dict(
        hfT=p.sb("hfT", [128, 8, T], BF16),
        xt=[p.sb("fxt%d" % i, [128, D], F32) for i in range(2)],
        h=p.sb("fh", [128, D], F32),
        st=(p.sb("fss", [128, 1], F32), p.sb("frstd", [128, 1], F32)),
        yacc=p.sb("yacc", [128, TQ // 128, D], F32),
        actT=p.sb("actT", [128, 8, TQ], BF16),
        w2b=p.sb("w2b", [128, 8, D], BF16),
        wst=[p.sb("wst%d" % i, [128, 8, 128], F32) for i in range(2)],
        w2st=[p.sb("w2st%d" % i, [128, D], F32) for i in range(1)] * 2,
        wgb=[p.sb("wgb%d" % i, [128, 8, 128], BF16) for i in range(2)],
        wub=[p.sb("wub%d" % i, [128, 8, 128], BF16) for i in range(2)],
        tg=p.sb("tg", [128, 512], F32), tsg=p.sb("tsg", [128, 512], F32), tu=p.sb("tu", [128, 512], F32))


def build_l2(do_ffn=False):
    nc = bass.Bass("TRN2", target_bir_lowering=False)
    p = Prog(nc)
    xs = dram_in(nc, "xs", [T, D])
    o_f = dram_in(nc, "o_f", [T, D])
    o_b = dram_in(nc, "o_b", [T, D])
    sg = dram_in(nc, "sg", [T, D], BF16)
    QF = dram_in(nc, "QF", [NH, 128, T], BF16)
    QB = dram_in(nc, "QB", [NH, 128, T], BF16)
    SlocA = dram_in(nc, "SlocA", [4, 2, NH, 128, 128])
    DlocA = dram_in(nc, "DlocA", [4, 128, 16])
    Sctx = dram_in(nc, "Sctx", [2, NH, 128, 128])
    fmask = dram_in(nc, "fmask", [128, 8])
    ccol = dram_in(nc, "ccol", [128, 8])
    w_ada = dram_in(nc, "w_ada0", [D, 6 * D])
    b_ada = dram_in(nc, "b_ada0", [1, 6 * D])
    gout = dram_in(nc, "gout", [1, D])
    w_out = dram_in(nc, "w_hout", [D, D])
    x1_d = nc.dram_tensor("x1_scr", [T, D], F32, kind="Internal").ap() if do_ffn else dram_out(nc, "x1", [T, D])
    x2_d = dram_out(nc, "x2", [T, D]) if do_ffn else None
    cm = Common(p, nc)

    modm = p.sb("modm", [128, 4096], F32)
    emit_mods(p, cm, w_ada, b_ada, 2048, 6144, [(ccol, modm)], "b")

    fm = p.sb("fm", [128, 8], F32)
    DA = p.sb("DA", [128, 4, 16], F32)
    p.dma("sp", fm[:], fmask, writes=[fm])
    for i in range(4):
        p.dma("sp", DA[:, i, :], DlocA[i], writes=[DA])
    Sin = p.sb("Sin", [128, 2, NH, 128], BF16)
    Sf = p.sb("Sfold", [128, 128], F32)
    Sl = [p.sb("Sl%d" % i, [128, 128], F32) for i in range(2)]
    tmp = p.sb("ftmp", [128, 128], F32)
    k = 0
    for d in range(2):
        for hh in range(NH):
            p.dma("sp", Sf[:], Sctx[d, hh], writes=[Sf])
            order = range(4) if d == 0 else range(3, -1, -1)
            for i in order:
                sl = Sl[k % 2]
                k += 1
                p.dma("sp", sl[:], SlocA[i, d, hh], writes=[sl])
                col = d * 8 + hh
                p.op("dve", lambda e: e.scalar_tensor_tensor(tmp[:], Sf[:], DA[:, i, col:col + 1], sl[:], ALU.mult, ALU.add),
                     [Sf, DA, sl], [tmp])
                p.tt("dve", tmp[:], tmp[:], Sf[:], ALU.subtract, [tmp, Sf], [tmp])
                p.op("dve", lambda e: e.scalar_tensor_tensor(Sf[:], tmp[:], fm[:, d * 4 + i:d * 4 + i + 1], Sf[:], ALU.mult, ALU.add),
                     [tmp, fm, Sf], [Sf])
            p.copy("pool", Sin[:, d, hh, :], Sf[:], [Sf], [Sin])

    goutb = p.sb("goutb", [128, D], F32)
    p.dma("sp", goutb[:], gout.partition_broadcast(128), writes=[goutb])
    wo = p.sb("wo", [128, 8, D], BF16)
    wos = [p.sb("wos%d" % i, [128, D], F32) for i in range(2)]
    for kc in range(8):
        p.dma("sp", wos[kc % 2][:], w_out[kc * 128:(kc + 1) * 128, :], writes=[wos[kc % 2]])
        p.copy("pool", wo[:, kc, :], wos[kc % 2][:], [wos[kc % 2]], [wo])

    xt = [p.sb("xt%d" % i, [128, D], F32) for i in range(2)]
    of_t = [p.sb("oft%d" % i, [128, D], F32) for i in range(2)]
    ob_t = [p.sb("obt%d" % i, [128, D], F32) for i in range(2)]
    sg_t = [p.sb("sgt%d" % i, [128, D], BF16) for i in range(2)]
    qf_t = [p.sb("qft%d" % i, [128, NH, 128], BF16) for i in range(2)]
    qb_t = [p.sb("qbt%d" % i, [128, NH, 128], BF16) for i in range(2)]
    sq = p.sb("sq", [128, D], F32)
    ssq = p.sb("ssq", [128, NH], F32)
    ogT = p.sb("ogT", [128, 8, 128], BF16)
    for t in range(T // 128):
        k = t % 2
        tok = slice(t * 128, (t + 1) * 128)
        p.dma("sp", xt[k][:], xs[tok, :], writes=[xt[k]])
        p.dma("sp", of_t[k][:], o_f[tok, :], writes=[of_t[k]])
        p.dma("sp", ob_t[k][:], o_b[tok, :], writes=[ob_t[k]])
        p.dma("sp", sg_t[k][:], sg[tok, :], writes=[sg_t[k]])
        p.dma("sp", qf_t[k][:], QF[:, :, tok].rearrange("h q t -> q h t"), writes=[qf_t[k]])
        p.dma("sp", qb_t[k][:], QB[:, :, tok].rearrange("h q t -> q h t"), writes=[qb_t[k]])
        o = of_t[k]
        p.tt("pool", o[:], o[:], ob_t[k][:], ALU.add, [o, ob_t[k]], [o])
        for half in range(2):
            ps = cm.psb[half]
            for j in range(4):
                hh = half * 4 + j
                p.mm(ps[:, j * 128:(j + 1) * 128], qf_t[k][:, hh, :], Sin[:, 0, hh, :], True, False, [qf_t[k], Sin], [ps])
                p.mm(ps[:, j * 128:(j + 1) * 128], qb_t[k][:, hh, :], Sin[:, 1, hh, :], False, True, [qb_t[k], Sin], [ps])
            p.tt("dve", o[:, half * 512:(half + 1) * 512], o[:, half * 512:(half + 1) * 512], ps[:], ALU.add, [o, ps], [o])
        p.tt("pool", sq[:], o[:], o[:], ALU.mult, [o], [sq])
        p.op("dve", lambda e: e.tensor_reduce(ssq[:], sq[:].rearrange("q (h v) -> q h v", h=NH), AX.X, ALU.add), [sq], [ssq])
        p.act(ssq[:], ssq[:], AF.Sqrt, [ssq], [ssq], scale=1.0 / 128, bias=EPS)
        p.op("dve", lambda e: e.reciprocal(ssq[:], ssq[:]), [ssq], [ssq])
        p.tt("dve", o[:].rearrange("q (h v) -> q h v", h=NH), o[:].rearrange("q (h v) -> q h v", h=NH),
             ssq[:].rearrange("q (h u) -> q h u", u=1).to_broadcast([128, NH, 128]), ALU.mult, [o, ssq], [o])
        p.tt("pool", o[:], o[:], goutb[:], ALU.mult, [o, goutb], [o])
        p.tt("dve", o[:], o[:], sg_t[k][:], ALU.mult, [o, sg_t[k]], [o])
        for half in range(2):
            pt = cm.psb[2 + half]
            for j in range(4):
                kc = half * 4 + j
                p.op("pe", lambda e: e.transpose(pt[:, j * 128:(j + 1) * 128], o[:, kc * 128:(kc + 1) * 128], cm.identf[:]),
                     [o, cm.identf], [pt])
            p.copy("act", ogT[:, half * 4:(half + 1) * 4, :], pt[:].rearrange("q (a b) -> q a b", a=4), [pt], [ogT])
        for dh in range(2):
            ps = cm.psb[4 + dh]
            for kc in range(8):
                p.mm(ps[:], ogT[:, kc, :], wo[:, kc, dh * 512:(dh + 1) * 512], kc == 0, kc == 7, [ogT, wo], [ps])
            p.tt("dve", sq[:, dh * 512:(dh + 1) * 512], ps[:], modm[:, dh * 512:(dh + 1) * 512], ALU.mult, [ps, modm], [sq])
        p.tt("pool", xt[k][:], xt[k][:], sq[:], ALU.add, [xt[k], sq], [xt[k]])
        p.dma("sp", x1_d[tok, :], xt[k][:], reads=[xt[k]], writes=["x1"])
    if do_ffn:
        tl = ffn_tiles(p)
        emit_ffn(p, cm, x1_d, "x1", x2_d, "x2", modm, 1024, gffn, wr_d, br_d, w1_d, b1c_d, w2_d, b2_d, tl)
        p.finish(["x2"])
    else:
        p.finish(["x1"])
    return nc, p


def build_ffn(final):
    nc = bass.Bass("TRN2", target_bir_lowering=False)
    p = Prog(nc)
    x1_d = dram_in(nc, "x1", [T, D])
    ccol = dram_in(nc, "ccol", [128, 8])
    w_ada = dram_in(nc, "w_ada", [D, 6 * D])
    b_ada = dram_in(nc, "b_ada", [1, 6 * D])
    gffn = dram_in(nc, "g_ffn", [1, D])
    wr_d = dram_in(nc, "w_router", [D, NE])
    br_d = dram_in(nc, "b_router", [1, NE])
    w1_d = dram_in(nc, "w_exp_in", [NE, D, 2 * D])
    b1c_d = dram_in(nc, "b1c", [128, NE, 16])
    w2_d = dram_in(nc, "w_exp_out", [NE, D, D])
    b2_d = dram_in(nc, "b_exp_out", [NE, D])
    gfin_d = dram_in(nc, "g_final", [1, D])
    x2_d = dram_out(nc, "x2", [T, D])
    cm = Common(p, nc)
    modf = p.sb("modf", [128, 3072], F32)
    emit_mods(p, cm, w_ada, b_ada, 3072, 6144, [(ccol, modf)], "f")
    gfin = None
    if final:
        gfin = p.sb("gfin", [128, D], F32)
        p.dma("sp", gfin[:], gfin_d.partition_broadcast(128), writes=[gfin])
    tl = ffn_tiles(p)
    emit_ffn(p, cm, x1_d, "x1in", x2_d, "x2", modf, 0, gffn, wr_d, br_d, w1_d, b1c_d, w2_d, b2_d, tl, final_g=gfin)
    p.finish(["x2"])
    return nc, p


def ffn_inputs(inp, core, layer, x1):
    b = core // 4
    m = {"ident": np.eye(128, dtype=np.float32)}
    m["x1"] = np.ascontiguousarray(x1)
    m["ccol"] = col_layout(inp["c"][b])
    m["w_ada"] = np.ascontiguousarray(inp["w_ada"][layer])
    m["b_ada"] = np.ascontiguousarray(inp["b_ada"][layer][None, :])
    m["g_ffn"] = np.ascontiguousarray(inp["g_ffn"][layer][None, :])
    m["w_router"] = np.ascontiguousarray(inp["w_router"][layer])
    m["b_router"] = np.ascontiguousarray(inp["b_router"][layer][None, :])
    m["w_exp_in"] = np.ascontiguousarray(inp["w_exp_in"][layer])
    b1 = np.asarray(inp["b_exp_in"][layer], np.float32).reshape(NE, 16, 128)
    m["b1c"] = np.ascontiguousarray(b1.transpose(2, 0, 1))
    m["w_exp_out"] = np.ascontiguousarray(inp["w_exp_out"][layer])
    m["b_exp_out"] = np.ascontiguousarray(inp["b_exp_out"][layer])
    m["g_final"] = np.ascontiguousarray(inp["g_final"][None, :])
    return m


def l2_inputs(inp, core, r1):
    b, j = core // 4, core % 4
    m = {"ident": np.eye(128, dtype=np.float32)}
    m["xs"] = np.ascontiguousarray(inp["x"][b, j * T:(j + 1) * T])
    for k in ("o_f", "o_b", "sg", "QF", "QB", "Sctx"):
        m[k] = r1[core][k]
    m["SlocA"] = np.ascontiguousarray(np.stack([r1[b * 4 + i]["Sloc"] for i in range(4)]))
    m["DlocA"] = np.ascontiguousarray(np.stack([r1[b * 4 + i]["Dloc"] for i in range(4)]))
    fm = np.zeros((128, 8), np.float32)
    for i in range(4):
        fm[:, i] = 1.0 if i < j else 0.0
        fm[:, 4 + i] = 1.0 if i > j else 0.0
    m["fmask"] = fm
    m["ccol"] = col_layout(inp["c"][b])
    m["w_ada0"] = np.ascontiguousarray(inp["w_ada"][0])
    m["b_ada0"] = np.ascontiguousarray(inp["b_ada"][0][None, :])
    m["gout"] = np.ascontiguousarray(inp["g_hgrn_out"][0][None, :])
    m["w_hout"] = np.ascontiguousarray(inp["w_hgrn_out"][0])
    return m


HALO = 15 * 64


def build_conv_a():
    nc = bass.Bass("TRN2", target_bir_lowering=False)
    p = Prog(nc)
    x2 = dram_in(nc, "x2", [T, D])
    ccol = dram_in(nc, "ccol", [128, 8])
    w_ada = dram_in(nc, "w_ada", [D, 6 * D])
    b_ada = dram_in(nc, "b_ada", [1, 6 * D])
    g_mix = dram_in(nc, "g_mix", [1, D])
    w_in = dram_in(nc, "w_cv_in", [D, 2 * D])
    bin_c = dram_in(nc, "bin_c", [128, 16])
    u_d = dram_out(nc, "u", [8, 128, T])
    gtm_d = dram_out(nc, "gtm", [1, D])
    cm = Common(p, nc)
    modb = p.sb("modb", [128, 3072], F32)
    emit_mods(p, cm, w_ada, b_ada, 0, 3072, [(ccol, modb)], "c")
    p.dma("sp", gtm_d, modb[0:1, 2048:3072], reads=[modb], writes=["gtm"])
    gmix = p.sb("gmix", [128, D], F32)
    p.dma("sp", gmix[:], g_mix.partition_broadcast(128), writes=[gmix])
    p.op("dve", lambda e: e.scalar_tensor_tensor(modb[:, 1024:2048], modb[:, 1024:2048], 1.0, gmix[:], ALU.add, ALU.mult),
         [modb, gmix], [modb])
    hT = p.sb("hT", [128, 8, T], BF16)
    xt = [p.sb("xt%d" % i, [128, D], F32) for i in range(2)]
    h = p.sb("h", [128, D], F32)
    st = (p.sb("ss", [128, 1], F32), p.sb("rstd", [128, 1], F32))
    emit_norm_T(p, cm, x2, T // 128, modb[:, 1024:2048], modb[:, 0:1024], modb, hT, None, xt, h, h, st)
    bc = p.sb("bc", [128, 16], F32)
    p.dma("sp", bc[:], bin_c, writes=[bc])
    wst = [p.sb("wst%d" % i, [128, 8, 128], F32) for i in range(2)]
    wab = [p.sb("wab%d" % i, [128, 8, 128], BF16) for i in range(2)]
    wgb = [p.sb("wgb%d" % i, [128, 8, 128], BF16) for i in range(2)]
    sgt = [p.sb("sgt%d" % i, [128, 512], F32) for i in range(2)]
    ut = [p.sb("ut%d" % i, [128, 512], F32) for i in range(2)]
    psa, psg = cm.psb[2], cm.psb[3]
    n = 0
    for j in range(8):
        k = j % 2
        p.dma("sp", wst[0][:], w_in[:, j * 128:(j + 1) * 128].rearrange("(kc q) n -> q kc n", q=128), writes=[wst[0]])
        p.copy("pool", wab[k][:], wst[0][:], [wst[0]], [wab[k]])
        p.dma("sp", wst[1][:], w_in[:, 1024 + j * 128:1024 + (j + 1) * 128].rearrange("(kc q) n -> q kc n", q=128), writes=[wst[1]])
        p.copy("pool", wgb[k][:], wst[1][:], [wst[1]], [wgb[k]])
        for tb in range(T // 512):
            tok = slice(tb * 512, (tb + 1) * 512)
            for kc in range(8):
                p.mm(psa[:], wab[k][:, kc, :], hT[:, kc, tok], kc == 0, kc == 7, [wab[k], hT], [psa])
            for kc in range(8):
                p.mm(psg[:], wgb[k][:, kc, :], hT[:, kc, tok], kc == 0, kc == 7, [wgb[k], hT], [psg])
            sg_, u_ = sgt[n % 2], ut[n % 2]
            n += 1
            p.act(sg_[:], psg[:], AF.Sigmoid, [psg, bc], [sg_], bias=bc[:, 8 + j:9 + j])
            p.op("dve", lambda e: e.scalar_tensor_tensor(u_[:], psa[:], bc[:, j:j + 1], sg_[:], ALU.add, ALU.mult),
                 [psa, bc, sg_], [u_])
            p.dma("sp", u_d[j, :, tok], u_[:], reads=[u_], writes=["u_d"])
    p.finish(["u_d", "gtm"])
    return nc, p


def conv_a_inputs(inp, core, x2):
    b = core // 4
    m = {"ident": np.eye(128, dtype=np.float32)}
    m["x2"] = np.ascontiguousarray(x2)
    m["ccol"] = col_layout(inp["c"][b])
    m["w_ada"] = np.ascontiguousarray(inp["w_ada"][1])
    m["b_ada"] = np.ascontiguousarray(inp["b_ada"][1][None, :])
    m["g_mix"] = np.ascontiguousarray(inp["g_mix"][1][None, :])
    m["w_cv_in"] = np.ascontiguousarray(inp["w_cv_in"][0])
    m["bin_c"] = np.ascontiguousarray(np.asarray(inp["b_cv_in"][0], np.float32).reshape(16, 128).T)
    return m


def build_conv_b():
    nc = bass.Bass("TRN2", target_bir_lowering=False)
    p = Prog(nc)
    x2 = dram_in(nc, "x2", [T, D])
    u_d = dram_in(nc, "u", [8, 128, T])
    up_d = dram_in(nc, "u_prev", [4, 128, HALO])
    un_d = dram_in(nc, "u_next", [4, 128, HALO])
    gtm_d = dram_in(nc, "gtm", [1, D])
    wdw_d = dram_in(nc, "wdw", [128, 8, 31])
    cvec_d = dram_in(nc, "cvec", [128, 3, 8])
    wout_d = dram_in(nc, "w_cv_out", [D, D])
    bout_d = dram_in(nc, "b_cv_out", [1, D])
    x3_d = dram_out(nc, "x3", [T, D])
    cm = Common(p, nc)
    wdw = p.sb("wdw", [128, 8, 31], F32)
    cvec = p.sb("cvec", [128, 3, 8], F32)
    gtm = p.sb("gtm", [128, D], F32)
    bout = p.sb("bout", [128, D], F32)
    p.dma("sp", wdw[:], wdw_d, writes=[wdw])
    p.dma("sp", cvec[:], cvec_d, writes=[cvec])
    p.dma("sp", gtm[:], gtm_d.partition_broadcast(128), writes=[gtm])
    p.dma("sp", bout[:], bout_d.partition_broadcast(128), writes=[bout])
    wo = p.sb("wo", [128, 8, D], BF16)
    wos = p.sb("wos", [128, D], F32)
    for kc in range(8):
        p.dma("sp", wos[:], wout_d[kc * 128:(kc + 1) * 128, :], writes=[wos])
        p.copy("pool", wo[:, kc, :], wos[:], [wos], [wo])
    z = p.sb("z_all", [128, 8, T], F32)
    ue = p.sb("ue", [128, T + 2 * HALO], F32)
    for j in range(8):
        zj = z[:, j, :]
        zkey = ("z", j)
        if j < 4:
            p.dma("sp", ue[:, 0:T], u_d[j], writes=[ue])
            p.ts("dve", zj, ue[:, 0:T], wdw[:, j, 15:16], cvec[:, 0, j:j + 1], ALU.mult, ALU.add, [ue, wdw, cvec], [zkey])
            z3 = zj.rearrange("q (r w) -> q r w", w=64)
            u3 = ue[:, 0:T].rearrange("q (r w) -> q r w", w=64)
            for o in range(-15, 16):
                if o == 0:
                    continue
                lo, hi = max(0, -o), min(64, 64 - o)
                p.op("dve", lambda e: e.scalar_tensor_tensor(z3[:, :, lo:hi], u3[:, :, lo + o:hi + o], wdw[:, j, o + 15:o + 16],
                                                             z3[:, :, lo:hi], ALU.mult, ALU.add), [ue, wdw, zkey], [zkey])
        else:
            p.dma("sp", ue[:, 0:HALO], up_d[j - 4], writes=[ue])
            p.dma("sp", ue[:, HALO:HALO + T], u_d[j], writes=[ue])
            p.dma("sp", ue[:, HALO + T:HALO + T + HALO], un_d[j - 4], writes=[ue])
            p.ts("dve", zj, ue[:, HALO:HALO + T], wdw[:, j, 15:16], cvec[:, 0, j:j + 1], ALU.mult, ALU.add, [ue, wdw, cvec], [zkey])
            for o in range(-15, 16):
                if o == 0:
                    continue
                a0 = HALO + o * 64
                p.op("dve", lambda e: e.scalar_tensor_tensor(zj, ue[:, a0:a0 + T], wdw[:, j, o + 15:o + 16], zj, ALU.mult, ALU.add),
                     [ue, wdw, zkey], [zkey])
    zkeys = [("z", j) for j in range(8)]
    sqt = [p.sb("sqt%d" % i, [128, 512], F32) for i in range(2)]
    mean = p.sb("mean", [128, 512], F32)
    msq = p.sb("msq", [128, 512], F32)
    rstd = p.sb("rstdv", [128, 512], F32)
    zn = [p.sb("zn%d" % i, [128, 512], F32) for i in range(2)]
    sgm = [p.sb("sgm0", [128, 512], F32)] * 2
    zs = p.sb("zs", [128, 8, 512], BF16)
    yt = wos
    ps1, ps2 = cm.psb[0], cm.psb[1]
    psy = [cm.psb[2], cm.psb[3]]
    n = 0
    for tb in range(T // 512):
        tok = slice(tb * 512, (tb + 1) * 512)
        for j in range(8):
            sq = sqt[n % 2]
            n += 1
            p.act(sq[:], z[:, j, tok], AF.Square, [zkeys[j]], [sq])
            p.mm(ps1[:], cm.ones[:], z[:, j, tok], j == 0, j == 7, [cm.ones, zkeys[j]], [ps1])
            p.mm(ps2[:], cm.ones[:], sq[:], j == 0, j == 7, [cm.ones, sq], [ps2])
        p.act(mean[:], ps1[:], AF.Copy, [ps1], [mean], scale=1.0 / D)
        p.tt("pool", msq[:], mean[:], mean[:], ALU.mult, [mean], [msq])
        p.op("dve", lambda e: e.scalar_tensor_tensor(rstd[:], ps2[:], 1.0 / D, msq[:], ALU.mult, ALU.subtract), [ps2, msq], [rstd])
        p.act(rstd[:], rstd[:], AF.Sqrt, [rstd], [rstd], bias=EPS)
        p.op("dve", lambda e: e.reciprocal(rstd[:], rstd[:]), [rstd], [rstd])
        for j in range(8):
            a, sg_ = zn[j % 2], sgm[j % 2]
            p.tt("dve", a[:], z[:, j, tok], mean[:], ALU.subtract, [zkeys[j], mean], [a])
            p.tt("pool", a[:], a[:], rstd[:], ALU.mult, [a, rstd], [a])
            p.ts("dve", a[:], a[:], cvec[:, 1, j:j + 1], cvec[:, 2, j:j + 1], ALU.mult, ALU.add, [a, cvec], [a])
            p.act(sg_[:], a[:], AF.Sigmoid, [a], [sg_])
            p.tt("pool", zs[:, j, :], a[:], sg_[:], ALU.mult, [a, sg_], [zs])
        for ti in range(4):
            t = tb * 4 + ti
            xa = ue[:, 0:D]
            p.dma("sp", xa, x2[t * 128:(t + 1) * 128, :], writes=[ue])
            for dh in range(2):
                ps = psy[dh]
                for j in range(8):
                    p.mm(ps[:], zs[:, j, ti * 128:(ti + 1) * 128], wo[:, j, dh * 512:(dh + 1) * 512], j == 0, j == 7, [zs, wo], [ps])
                p.tt("dve", yt[:, dh * 512:(dh + 1) * 512], ps[:], bout[:, dh * 512:(dh + 1) * 512], ALU.add, [ps, bout], [yt])
            p.tt("pool", yt[:], yt[:], gtm[:], ALU.mult, [yt, gtm], [yt])
            p.tt("dve", xa, xa, yt[:], ALU.add, [ue, yt], [ue])
            p.dma("sp", x3_d[t * 128:(t + 1) * 128, :], xa, reads=[ue], writes=["x3"])
    p.finish(["x3"])
    return nc, p


def conv_b_inputs(inp, core, x2, ra):
    b, j = core // 4, core % 4
    m = {"ident": np.eye(128, dtype=np.float32)}
    m["x2"] = np.ascontiguousarray(x2)
    m["u"] = ra[core]["u"]
    zero = np.zeros((4, 128, HALO), np.float32)
    m["u_prev"] = np.ascontiguousarray(ra[core - 1]["u"][4:8, :, T - HALO:T]) if j > 0 else zero
    m["u_next"] = np.ascontiguousarray(ra[core + 1]["u"][4:8, :, 0:HALO]) if j < 3 else zero
    m["gtm"] = ra[core]["gtm"]
    w = np.asarray(inp["w_cv_dw"][0], np.float32)
    m["wdw"] = np.ascontiguousarray(w.reshape(31, 8, 128).transpose(2, 1, 0))
    cv = np.stack([np.asarray(inp[k][0], np.float32).reshape(8, 128).T for k in ("b_cv_dw", "g_cv_ln", "b_cv_ln")], axis=1)
    m["cvec"] = np.ascontiguousarray(cv)
    m["w_cv_out"] = np.ascontiguousarray(inp["w_cv_out"][0])
    m["b_cv_out"] = np.ascontiguousarray(inp["b_cv_out"][0][None, :])
    return m


_DBG = {}


def _run(nc, maps):
    res = run_bass_kernel_spmd(nc, maps, core_ids=list(range(8)))
    return res.results


def kernel(**inp):
    inp = {k: np.asarray(v) for k, v in inp.items()}
    nc1, _ = build_l1()
    r1 = _run(nc1, [l1_inputs(inp, c) for c in range(8)])
    nc2, _ = build_l2()
    r2 = _run(nc2, [l2_inputs(inp, c, r1) for c in range(8)])
    _DBG["x1"] = [r["x1"] for r in r2]
    if _DBG.get("skip_ffn"):
        return None
    nc3, _ = build_ffn(False)
    r3 = _run(nc3, [ffn_inputs(inp, c, 0, r2[c]["x1"]) for c in range(8)])
    _DBG["x2"] = [r["x2"] for r in r3]
    nca, _ = build_conv_a()
    ra = _run(nca, [conv_a_inputs(inp, c, r3[c]["x2"]) for c in range(8)])
    ncb, _ = build_conv_b()
    rb = _run(ncb, [conv_b_inputs(inp, c, r3[c]["x2"], ra) for c in range(8)])
    _DBG["x3"] = [r["x3"] for r in rb]
    nc4, _ = build_ffn(True)
    r4 = _run(nc4, [ffn_inputs(inp, c, 1, rb[c]["x3"]) for c in range(8)])
    out = np.zeros((2, 4 * T, D), np.float32)
    for c in range(8):
        out[c // 4, (c % 4) * T:(c % 4 + 1) * T] = r4[c]["x2"]
    return out
```

```python
import numpy as np
from contextlib import ExitStack
import concourse.bass as bass
import concourse.mybir as mybir
from concourse.bass_utils import run_bass_kernel_spmd

F32 = mybir.dt.float32
BF16 = mybir.dt.bfloat16
AF = mybir.ActivationFunctionType
ALU = mybir.AluOpType
AX = mybir.AxisListType

SEM_LIMIT = 12000
D = 1024
T = 4096
TCTX = 256
NH = 8
EPS = 1e-6
NE = 32
NQ_FFN = 8


class Prog:
    def __init__(self, nc):
        self.nc = nc
        self.es = ExitStack()
        self.eng = {"pe": nc.tensor, "dve": nc.vector, "act": nc.scalar,
                    "pool": nc.gpsimd, "sp": nc.sync}
        self.sem = {}
        self.cnt = {}
        self.nsem = 0
        for e in ("pe", "dve", "act", "pool"):
            self._new_sem(e)
        self.waited = {e: {} for e in self.eng}
        self.res = {}
        self.dsem = {}
        self.dcnt = {}
        self.ninst = {e: 0 for e in self.eng}
        self.psum_ids = set()
        self.prev_sem = {}
        self.dtot = {}
        self.free_dsems = []
        self.phase_es = None
        self.cc_sems = []

    def _alloc_sem(self, name):
        self.nsem += 1
        return self.es.enter_context(self.nc.semaphore(name))

    def _new_sem(self, e):
        if e in self.sem:
            self.prev_sem.setdefault(e, []).append((self.sem[e], self.cnt[e]))
        self.sem[e] = self._alloc_sem("e_%s_%d" % (e, self.nsem))
        self.cnt[e] = 0

    def begin_phase(self):
        self.phase_es = ExitStack()

    def end_phase(self):
        self.barrier()
        self.phase_es.close()
        self.phase_es = None
        self.res = {}
        self.free_dsems = [sem for (sem, _) in self.dtot.values()]
        self.dsem = {}

    def barrier(self):
        targets = []
        for x in ("pe", "dve", "act", "pool"):
            for (sm, c) in self.prev_sem.get(x, []):
                targets.append((sm, c))
            if self.cnt[x] > 0:
                targets.append((self.sem[x], self.cnt[x]))
        targets.extend(self.dtot.values())
        targets.extend(self.cc_sems)
        for e in ("pe", "dve", "act", "pool", "sp"):
            for (sm, c) in targets:
                if c > 0:
                    self._wait(e, sm, c)

    def allgather(self, in_ap, out_ap, in_key, out_key):
        self._deps("pool", [in_key], [out_key])
        inst = self.nc.gpsimd.collective_compute("AllGather", ALU.bypass, replica_groups=[list(range(8))],
                                                 ins=[in_ap.opt()], outs=[out_ap.opt()])
        sm = self._alloc_sem("cc%d" % self.nsem)
        inst.then_inc(sm, 1)
        self.cc_sems.append((sm, 1))
        self._mark(sm, 1, [in_key], [out_key])

    def sb(self, name, shape, dt):
        es = self.phase_es if self.phase_es is not None else self.es
        self.ntile = getattr(self, "ntile", 0) + 1
        return es.enter_context(self.nc.sbuf_tensor("s%d_%s" % (self.ntile, name), list(shape), dt))

    def ps(self, name, shape, dt=F32):
        t = self.es.enter_context(self.nc.psum_tensor("p_" + name, list(shape), dt))
        self.psum_ids.add(id(t))
        return t

    def _r(self, key):
        k = key if isinstance(key, (str, tuple)) else id(key)
        r = self.res.get(k)
        if r is None:
            r = {"w": None, "r": {}}
            self.res[k] = r
        return r

    def _wait(self, e, sem, val):
        w = self.waited[e]
        k = id(sem)
        if w.get(k, 0) >= val:
            return
        w[k] = val
        self.eng[e].wait_ge(sem, val)

    def _deps(self, e, reads, writes):
        deps = []
        for r in reads:
            rr = self._r(r)
            if rr["w"] is not None:
                deps.append(rr["w"])
            if id(r) in self.psum_ids:
                deps.extend(rr["r"].values())
        for wk in writes:
            rr = self._r(wk)
            if rr["w"] is not None:
                deps.append(rr["w"])
            deps.extend(rr["r"].values())
        own = self.sem.get(e)
        for (sem, val) in deps:
            if e == "pe" and sem is own:
                continue
            self._wait(e, sem, val)

    def _mark(self, sem, val, reads, writes):
        for r in reads:
            self._r(r)["r"][id(sem)] = (sem, val)
        for wk in writes:
            rr = self._r(wk)
            rr["w"] = (sem, val)
            rr["r"] = {}

    def op(self, e, fn, reads=(), writes=()):
        if self.cnt[e] >= SEM_LIMIT:
            self._new_sem(e)
        self._deps(e, reads, writes)
        inst = fn(self.eng[e])
        self.cnt[e] += 1
        self.ninst[e] += 1
        inst.then_inc(self.sem[e], 1)
        self._mark(self.sem[e], self.cnt[e], reads, writes)
        return inst

    def dma(self, q, out, in_, reads=(), writes=(), semkey=None, **kw):
        self._deps(q, reads, writes)
        key = semkey if semkey is not None else writes[0]
        key = key if isinstance(key, (str, tuple)) else id(key)
        if key not in self.dsem or self.dtot[id(self.dsem[key])][1] >= 2 * SEM_LIMIT:
            if key not in self.dsem and self.free_dsems:
                self.dsem[key] = self.free_dsems.pop()
            else:
                sm = self._alloc_sem("d%d" % self.nsem)
                self.dsem[key] = sm
                self.dtot[id(sm)] = (sm, 0)
        s = self.dsem[key]
        inst = self.eng[q].dma_start(out=out, in_=in_, **kw)
        tot = self.dtot[id(s)][1] + 16
        self.dtot[id(s)] = (s, tot)
        self.ninst[q] += 1
        inst.then_inc(s, 16)
        self._mark(s, tot, reads, writes)
        return inst

    def finish(self, out_keys, e="sp"):
        for k in out_keys:
            rr = self._r(k)
            if rr["w"] is not None:
                self._wait(e, *rr["w"])
        for x in ("pe", "dve", "act", "pool"):
            if self.cnt[x] > 0:
                self._wait(e, self.sem[x], self.cnt[x])

    def act(self, out, in_, func, reads, writes, **kw):
        return self.op("act", lambda e: e.activation(out, in_, func, **kw), reads, writes)

    def tt(self, eng, out, a, b, op, reads, writes):
        return self.op(eng, lambda e: e.tensor_tensor(out, a, b, op), reads, writes)

    def ts(self, eng, out, a, s1, s2, op0, op1, reads, writes):
        if s2 is None:
            return self.op(eng, lambda e: e.tensor_scalar(out, a, s1, None, op0), reads, writes)
        return self.op(eng, lambda e: e.tensor_scalar(out, a, s1, s2, op0, op1), reads, writes)

    def copy(self, eng, out, in_, reads, writes):
        if eng == "act":
            return self.op("act", lambda e: e.copy(out, in_), reads, writes)
        return self.op(eng, lambda e: e.tensor_copy(out, in_), reads, writes)

    def mm(self, out, lhsT, rhs, start, stop, reads, writes):
        return self.op("pe", lambda e: e.matmul(out, lhsT, rhs, start=start, stop=stop), reads, writes)


REG = {}
FUSED = [False]
FINAL_OUT = "out"


def dram_in(nc, name, shape, dt=F32):
    if FUSED[0] and name in REG:
        return REG[name]
    ap = nc.dram_tensor(name, list(shape), dt, kind="ExternalInput").ap()
    if FUSED[0]:
        REG[name] = ap
    return ap


def dram_out(nc, name, shape, dt=F32):
    if FUSED[0]:
        kind = "ExternalOutput" if name == FINAL_OUT else "Internal"
        ap = nc.dram_tensor(name, list(shape), dt, kind=kind).ap()
        REG[name] = ap
        return ap
    return nc.dram_tensor(name, list(shape), dt, kind="ExternalOutput").ap()


class Ctx:
    def __init__(self):
        self.nc = bass.Bass("TRN2", target_bir_lowering=False)
        self.p = Prog(self.nc)
        self.cm = None


def stage_begin(ctx):
    if ctx is None:
        c = Ctx()
        c.cm = Common(c.p, c.nc)
        c.standalone = True
        return c
    ctx.standalone = False
    ctx.p.begin_phase()
    return ctx


def stage_end(ctx, out_keys):
    if ctx.standalone:
        ctx.p.finish(out_keys)
    else:
        ctx.p.end_phase()
    return ctx.nc, ctx.p


class Common:
    def __init__(self, p, nc):
        self.p = p
        self.ident_d = dram_in(nc, "ident", [128, 128])
        self.identf = p.sb("identf", [128, 128], F32)
        self.identb = p.sb("identb", [128, 128], BF16)
        self.ones = p.sb("ones", [128, 128], F32)
        p.dma("sp", self.identf[:], self.ident_d, writes=[self.identf])
        p.copy("dve", self.identb[:], self.identf[:], [self.identf], [self.identb])
        p.op("dve", lambda e: e.memset(self.ones[:], 1.0), [], [self.ones])
        self.ones5 = p.sb("ones5", [128, 512], F32)
        p.op("dve", lambda e: e.memset(self.ones5[:], 1.0), [], [self.ones5])
        self.psb = [p.ps("psb%d" % i, [128, 512]) for i in range(7)]
        self.psT = p.ps("psT", [128, 1024], BF16)


def emit_mods(p, cm, w_ada, b_ada, lo, hi, pairs, tag):
    lts = []
    for i, (ccol, out) in enumerate(pairs):
        p.dma("sp", out[:], b_ada[:, lo:hi].partition_broadcast(128), writes=[out])
        cc = p.sb("mod_cc%s%d" % (tag, i), [128, 8], F32)
        sg = p.sb("mod_sg%s%d" % (tag, i), [128, 8], F32)
        lt = p.sb("mod_lt%s%d" % (tag, i), [128, 8, 128], F32)
        p.dma("sp", cc[:], ccol, writes=[cc])
        p.act(sg[:], cc[:], AF.Sigmoid, [cc], [sg])
        p.tt("dve", sg[:], sg[:], cc[:], ALU.mult, [sg, cc], [sg])
        for kc in range(8):
            p.ts("dve", lt[:, kc, :], cm.ones[:], sg[:, kc:kc + 1], None, ALU.mult, None, [cm.ones, sg], [lt])
        lts.append(lt)
    wc = p.sb("mod_w%s" % tag, [128, 8, 256], F32)
    for bi, n0 in enumerate(range(lo, hi, 256)):
        p.dma("sp", wc[:], w_ada[:, n0:n0 + 256].rearrange("(kc q) n -> q kc n", q=128), writes=[wc])
        for i, (ccol, out) in enumerate(pairs):
            ps = cm.psb[i % 2]
            for kc in range(8):
                p.mm(ps[:, 0:256], lts[i][:, kc, :], wc[:, kc, :], kc == 0, kc == 7, [lts[i], wc], [ps])
            p.tt("dve", out[:, n0 - lo:n0 - lo + 256], ps[:, 0:256], out[:, n0 - lo:n0 - lo + 256], ALU.add, [ps, out], [out])


def emit_norm_T(p, cm, src, ntiles, Arow, Brow, modt, hT, tagres, xt, junk, h, st, hTf=None):
    for t in range(ntiles):
        xx = xt[t % 2]
        p.dma("sp", xx[:], src[t * 128:(t + 1) * 128, :], reads=[tagres] if tagres else [], writes=[xx])
        ss, rstd = st
        p.act(junk[:], xx[:], AF.Square, [xx], [junk, ss], accum_out=ss[:])
        p.act(rstd[:], ss[:], AF.Sqrt, [ss], [rstd], scale=1.0 / D, bias=EPS)
        p.op("dve", lambda e: e.reciprocal(rstd[:], rstd[:]), [rstd], [rstd])
        p.op("dve", lambda e: e.scalar_tensor_tensor(h[:], xx[:], rstd[:], Arow, ALU.mult, ALU.mult),
             [xx, rstd, modt], [h])
        p.tt("dve", h[:], h[:], Brow, ALU.add, [h, modt], [h])
        for half in range(2):
            pt = cm.psb[half]
            for j in range(4):
                kc = half * 4 + j
                p.op("pe", lambda e: e.transpose(pt[:, j * 128:(j + 1) * 128], h[:, kc * 128:(kc + 1) * 128], cm.identf[:]),
                     [h, cm.identf], [pt])
            p.copy("act", hT[:, half * 4:(half + 1) * 4, t * 128:(t + 1) * 128],
                   pt[:].rearrange("q (a b) -> q a b", a=4), [pt], [hT])
            if hTf is not None:
                p.copy("dve", hTf[:, half * 4:(half + 1) * 4, t * 128:(t + 1) * 128],
                       pt[:].rearrange("q (a b) -> q a b", a=4), [pt], [hTf])


DBG_STOP = None
DBG_LVL = 0
DBG_SKIP = ''


def build_l1(ctx=None):
    ctx = stage_begin(ctx)
    nc, p, cm = ctx.nc, ctx.p, ctx.cm
    xs = dram_in(nc, "xs", [T, D])
    ctxb = dram_in(nc, "ctxb", [TCTX, D])
    ccol = dram_in(nc, "ccol", [128, 8])
    cctx = dram_in(nc, "cctx", [128, 8])
    w_ada = dram_in(nc, "w_ada0", [D, 6 * D])
    b_ada = dram_in(nc, "b_ada0", [1, 6 * D])
    g_mix = dram_in(nc, "g_mix0", [1, D])
    w_hin = dram_in(nc, "w_hin", [D, 5 * D])
    gam = dram_in(nc, "gam", [128, 2, 2, 8])
    maskf_d = dram_in(nc, "maskf", [64, 64])
    maskb_d = dram_in(nc, "maskb", [64, 64])
    rm_d = dram_in(nc, "rm", [128, 512])
    o_d = [dram_out(nc, "o_f", [T, D]), dram_out(nc, "o_b", [T, D])]
    sg_d = dram_out(nc, "sg", [T, D], BF16)
    Q_d = [dram_out(nc, "QF", [NH, 128, T], BF16), dram_out(nc, "QB", [NH, 128, T], BF16)]
    Sloc_d = dram_out(nc, "Sloc", [2, NH, 128, 128])
    Sctx_d = dram_out(nc, "Sctx", [2, NH, 128, 128])
    Dloc_d = dram_out(nc, "Dloc", [128, 16])

    maskt = [p.sb("maskf", [64, 64], F32), p.sb("maskb", [64, 64], F32)]
    rm = p.sb("rm", [128, 512], F32)
    p.dma("sp", maskt[0][:], maskf_d, writes=[maskt[0]])
    p.dma("sp", maskt[1][:], maskb_d, writes=[maskt[1]])
    p.dma("sp", rm[:], rm_d, writes=[rm])

    gm = p.sb("gm", [128, 2, 2, 8], F32)
    lb = p.sb("lb", [128, 16], F32)
    oml = p.sb("oml", [128, 16], F32)
    p.dma("sp", gm[:], gam, writes=[gm])
    for d in range(2):
        p.tt("dve", lb[:, d * 8:(d + 1) * 8], gm[:, d, 0, :], gm[:, d, 1, :], ALU.subtract, [gm], [lb])
    p.act(lb[:], lb[:], AF.Sigmoid, [lb], [lb])
    p.ts("dve", oml[:], lb[:], -1.0, 1.0, ALU.mult, ALU.add, [lb], [oml])

    if DBG_LVL == 1:
        p.finish([])
        return nc, p
    modb = p.sb("modb", [128, 2048], F32)
    modc = p.sb("modc", [128, 2048], F32)
    emit_mods(p, cm, w_ada, b_ada, 0, 2048, [(ccol, modb), (cctx, modc)], "a")
    gmix = p.sb("gmix", [128, D], F32)
    p.dma("sp", gmix[:], g_mix.partition_broadcast(128), writes=[gmix])
    for m in (modb, modc):
        p.op("dve", lambda e: e.scalar_tensor_tensor(m[:, 1024:2048], m[:, 1024:2048], 1.0, gmix[:], ALU.add, ALU.mult),
             [m, gmix], [m])

    if DBG_LVL == 2:
        p.finish([])
        return nc, p
    hT = p.sb("hT", [128, 8, T], BF16)
    hTc = p.sb("hTc", [128, 8, TCTX], BF16)
    xt = [p.sb("xt%d" % i, [128, D], F32) for i in range(2)]
    h = p.sb("h", [128, D], F32)
    junk = h
    st = (p.sb("ss", [128, 1], F32), p.sb("rstd", [128, 1], F32))
    emit_norm_T(p, cm, ctxb, TCTX // 128, modc[:, 1024:2048], modc[:, 0:1024], modc, hTc, None, xt, junk, h, st)
    if DBG_LVL == 3:
        p.finish([])
        return nc, p
    emit_norm_T(p, cm, xs, (T // 128) if DBG_LVL != 4 else 4, modb[:, 1024:2048], modb[:, 0:1024], modb, hT, None, xt, junk, h, st)
    if DBG_LVL in (4, 5):
        p.finish([])
        return nc, p

    col0 = [0, 2048, 3072, 1024, 4096]
    whf = [p.sb("whf%d" % i, [128, 8, 128], F32) for i in range(2)]
    whb = [p.sb("whb%d" % i, [128, 8, 5, 128], BF16) for i in range(2)]

    def blk_tiles(i):
        s = "_%d" % i
        return dict(
            q=p.sb("q" + s, [128, 512], F32), F=p.sb("F" + s, [128, 512], F32),
            LF=p.sb("LF" + s, [128, 512], F32), U=p.sb("U" + s, [128, 512], F32),
            E=p.sb("E" + s, [128, 512], F32), Bc=p.sb("Bc" + s, [128, 512], F32),
            BL=p.sb("BL" + s, [128, 8], F32), DEC=p.sb("DEC" + s, [128, 8], F32),
            QG=p.sb("QG" + s, [128, 512], BF16), KD=p.sb("KD" + s, [128, 512], BF16),
            KGT=p.sb("KGT" + s, [128, 512], BF16), QX=p.sb("QX" + s, [128, 512], BF16),
            kg=p.sb("kg" + s, [64, 8, 128], BF16), v=p.sb("v" + s, [64, 8, 128], BF16),
            sgt=p.sb("sgt" + s, [64, 8, 128], BF16), ob=p.sb("ob" + s, [64, 8, 128], F32),
            tc=p.sb("tc" + s, [128, 1], F32), sgs=p.sb("sgs" + s, [64, 2, 128], F32), sgg=p.sb("sgg" + s, [64, 2, 128], F32))

    bt = [blk_tiles(0), blk_tiles(1)]
    S32 = p.sb("S32", [128, 128], F32)
    Sb = p.sb("Sb", [128, 128], BF16)
    ATs = p.sb("ATs", [64, 64], BF16)
    Dcol = p.sb("Dcol", [128, 16], F32)
    psq, psz, psv, psA, pso, psS = cm.psb[0], cm.psb[1], cm.psb[2], cm.psb[3], cm.psb[4], cm.psb[5]
    psT = cm.psT
    bcount = 0

    for hh in range(NH):
        if DBG_STOP is not None and hh >= DBG_STOP[0]:
            break
        wb = whb[hh % 2]
        for s in range(5):
            c0 = col0[s] + hh * 128
            wf_ = whf[s % 2]
            p.dma("sp", wf_[:], w_hin[:, c0:c0 + 128].rearrange("(kc q) n -> q kc n", q=128), writes=[wf_])
            p.copy("pool", wb[:, :, s, :], wf_[:], [wf_], [wb])
        for (is_ctx, hTs, TT) in ((True, hTc, TCTX), (False, hT, T)):
            nb_tok = 256 if is_ctx else 512
            nblk = TT // nb_tok
            nch = nb_tok // 64
            for d in range(2):
                if DBG_STOP is not None and (is_ctx, d) == DBG_STOP[1]:
                    break
                col = d * 8 + hh
                blocks = list(range(nblk)) if d == 0 else list(range(nblk - 1, -1, -1))
                carry = None
                first = True
                for blk in blocks:
                    b = bt[bcount % 2]
                    bcount += 1
                    t0 = blk * nb_tok
                    n = nb_tok
                    q, Ft, LF, U, E, Bc = b["q"], b["F"], b["LF"], b["U"], b["E"], b["Bc"]
                    for kc in range(8):
                        p.mm(psq[:, 0:n], wb[:, kc, 0, :], hTs[:, kc, t0:t0 + n], kc == 0, kc == 7, [wb, hTs], [psq])
                    p.copy("act", q[:, 0:n], psq[:, 0:n], [psq], [q])
                    for kc in range(8):
                        p.mm(psz[:, 0:n], wb[:, kc, 1 + d, :], hTs[:, kc, t0:t0 + n], kc == 0, kc == 7, [wb, hTs], [psz])
                    p.act(Ft[:, 0:n], psz[:, 0:n], AF.Sigmoid, [psz], [Ft])
                    for c2 in range(nch // 2):
                        for j in range(2):
                            c = c2 * 2 + j
                            for kc in range(8):
                                p.mm(psv[0:64, j * 256:(j + 1) * 256], hTs[:, kc, t0 + c * 64:t0 + (c + 1) * 64],
                                     wb[:, kc, 3:5, :].rearrange("q a b -> q (a b)"), kc == 0, kc == 7, [wb, hTs], [psv])
                        pv = psv[0:64, :].rearrange("q (j x) -> q j x", j=2)
                        p.copy("dve", b["v"][:, c2 * 2:c2 * 2 + 2, :], pv[:, :, 0:128], [psv], [b["v"]])
                        if d == 0 and not is_ctx:
                            p.copy("dve", b["sgg"][:, 0:2, :], pv[:, :, 128:256], [psv], [b["sgg"]])
                            p.act(b["sgs"][:, 0:2, :], b["sgg"][:, 0:2, :], AF.Sigmoid, [b["sgg"]], [b["sgs"]])
                            p.tt("pool", b["sgt"][:, c2 * 2:c2 * 2 + 2, :], b["sgg"][:, 0:2, :], b["sgs"][:, 0:2, :], ALU.mult,
                                 [b["sgg"], b["sgs"]], [b["sgt"]])
                    if d == 0 and not is_ctx and "C" not in DBG_SKIP:
                        p.dma("sp", sg_d[t0:t0 + n, hh * 128:(hh + 1) * 128].rearrange("(c q) v -> q c v", q=64),
                              b["sgt"][:, 0:nch, :], reads=[b["sgt"]], writes=["sg_d"])
                    if DBG_LVL == 6:
                        p.finish([]); return nc, p
                    p.ts("dve", Ft[:, 0:n], Ft[:, 0:n], oml[:, col:col + 1], lb[:, col:col + 1], ALU.mult, ALU.add,
                         [Ft, oml, lb], [Ft])
                    p.act(LF[:, 0:n], Ft[:, 0:n], AF.Ln, [Ft], [LF])
                    p.ts("pool", Ft[:, 0:n], Ft[:, 0:n], -1.0, 1.0, ALU.mult, ALU.add, [Ft], [Ft])
                    p.op("dve", lambda e: e.tensor_tensor_scan(U[:, 0:n], rm[:, 0:n], LF[:, 0:n], 0.0, ALU.mult, ALU.add),
                         [rm, LF], [U])
                    U3 = U[:, 0:n].rearrange("q (c k) -> q c k", k=64)
                    p.copy("pool", b["BL"][:, 0:nch].rearrange("q (c o) -> q c o", o=1), U3[:, :, 63:64], [U], [b["BL"]])
                    p.act(b["DEC"][:, 0:nch], b["BL"][:, 0:nch], AF.Exp, [b["BL"]], [b["DEC"]])
                    if not is_ctx and "B" not in DBG_SKIP:
                        init = 0.0 if (carry is None or d == 1) else carry[0]
                        rdc = [] if (carry is None or d == 1) else [carry[1]]
                        p.op("dve", lambda e: e.tensor_tensor_scan(Bc[:, 0:n], cm.ones5[:, 0:n], LF[:, 0:n], init, ALU.mult, ALU.add),
                             [cm.ones5, LF] + rdc, [Bc])
                        if d == 0:
                            carry = (Bc[:, n - 1:n], Bc)
                        else:
                            if carry is None:
                                p.copy("pool", b["tc"][:], Bc[:, n - 1:n], [Bc], [b["tc"]])
                            else:
                                p.tt("pool", b["tc"][:], Bc[:, n - 1:n], carry[0], ALU.add, [Bc, carry[1]], [b["tc"]])
                            carry = (b["tc"][:], b["tc"])
                            p.tt("dve", Bc[:, 0:n], LF[:, 0:n], Bc[:, 0:n], ALU.subtract, [LF, Bc], [Bc])
                            p.ts("dve", Bc[:, 0:n], Bc[:, 0:n], b["tc"][:], None, ALU.add, None, [Bc, b["tc"]], [Bc])
                        p.act(E[:, 0:n], Bc[:, 0:n], AF.Exp, [Bc], [E])
                        p.tt("pool", b["QX"][:, 0:n], q[:, 0:n], E[:, 0:n], ALU.mult, [q, E], [b["QX"]])
                        p.dma("sp", Q_d[d][hh, :, t0:t0 + n], b["QX"][:, 0:n], reads=[b["QX"]], writes=["Q_d%d" % d])
                        if d == 1 and carry is not None:
                            pass
                    if d == 1:
                        p.tt("dve", U[:, 0:n], LF[:, 0:n], U[:, 0:n], ALU.subtract, [LF, U], [U])
                        p.tt("dve", U3, U3, b["BL"][:, 0:nch].rearrange("q (c o) -> q c o", o=1).to_broadcast([128, nch, 64]),
                             ALU.add, [U, b["BL"]], [U])
                    p.act(E[:, 0:n], U[:, 0:n], AF.Exp, [U], [E])
                    p.tt("dve", b["QG"][:, 0:n], q[:, 0:n], E[:, 0:n], ALU.mult, [q, E], [b["QG"]])
                    p.act(LF[:, 0:n], U[:, 0:n], AF.Exp, [U], [LF], scale=-1.0)
                    p.tt("pool", b["KD"][:, 0:n], Ft[:, 0:n], LF[:, 0:n], ALU.mult, [Ft, LF], [b["KD"]])
                    p.tt("dve", b["KGT"][:, 0:n].rearrange("q (c k) -> q c k", k=64),
                         b["KD"][:, 0:n].rearrange("q (c k) -> q c k", k=64),
                         b["DEC"][:, 0:nch].rearrange("q (c o) -> q c o", o=1).to_broadcast([128, nch, 64]),
                         ALU.mult, [b["KD"], b["DEC"]], [b["KGT"]])
                    if DBG_LVL == 7:
                        p.finish([]); return nc, p
                    for c in range(nch):
                        p.op("pe", lambda e: e.transpose(psT[0:64, c * 128:(c + 1) * 128], b["KGT"][:, c * 64:(c + 1) * 64], cm.identb[:]),
                             [b["KGT"], cm.identb], [psT])
                    p.copy("act", b["kg"][:, 0:nch, :], psT[0:64, 0:nch * 128].rearrange("q (c k) -> q c k", k=128), [psT], [b["kg"]])
                    if DBG_LVL == 8:
                        p.finish([]); return nc, p
                    order = list(range(nch)) if d == 0 else list(range(nch - 1, -1, -1))
                    if "A" in DBG_SKIP:
                        order = []
                    for c in order:
                        cs = slice(c * 64, (c + 1) * 64)
                        p.mm(psA[0:64, 0:64], b["KD"][:, cs], b["QG"][:, cs], True, True, [b["KD"], b["QG"]], [psA])
                        p.tt("dve", ATs[:], psA[0:64, 0:64], maskt[d][:], ALU.mult, [psA, maskt[d]], [ATs])
                        if DBG_LVL == 9:
                            continue
                        p.mm(pso[0:64, 0:128], ATs[:], b["v"][:, c, :], True, first, [ATs, b["v"]], [pso])
                        if not first:
                            p.mm(pso[0:64, 0:128], b["QG"][:, cs], Sb[:], False, True, [b["QG"], Sb], [pso])
                        if not is_ctx:
                            p.copy("act", b["ob"][:, c, :], pso[0:64, 0:128], [pso], [b["ob"]])
                        if DBG_LVL == 10:
                            first = False
                            continue
                        p.mm(psS[:, 0:128], b["kg"][:, c, :], b["v"][:, c, :], True, True, [b["kg"], b["v"]], [psS])
                        if first:
                            p.copy("dve", S32[:], psS[:, 0:128], [psS], [S32])
                        else:
                            p.op("dve", lambda e: e.scalar_tensor_tensor(S32[:], S32[:], b["DEC"][:, c:c + 1], psS[:, 0:128], ALU.mult, ALU.add),
                                 [S32, b["DEC"], psS], [S32])
                        p.copy("pool", Sb[:], S32[:], [S32], [Sb])
                        first = False
                    if not is_ctx and "D" not in DBG_SKIP:
                        p.dma("sp", o_d[d][t0:t0 + n, hh * 128:(hh + 1) * 128].rearrange("(c q) v -> q c v", q=64),
                              b["ob"][:, 0:nch, :], reads=[b["ob"]], writes=["o_d%d" % d])
                if is_ctx:
                    p.dma("sp", Sctx_d[d, hh], S32[:], reads=[S32], writes=["Sctx_d"])
                else:
                    p.dma("sp", Sloc_d[d, hh], S32[:], reads=[S32], writes=["Sloc_d"])
                    if "B" not in DBG_SKIP:
                        p.act(Dcol[:, col:col + 1], carry[0], AF.Exp, [carry[1]], [Dcol])
    p.dma("sp", Dloc_d, Dcol[:], reads=[Dcol], writes=["Dloc_d"])
    return stage_end(ctx, ["sg_d", "Q_d0", "Q_d1", "o_d0", "o_d1", "Sctx_d", "Sloc_d", "Dloc_d"])


def consts():
    s = np.arange(64)[:, None]
    t = np.arange(64)[None, :]
    rm = np.ones((128, 512), np.float32)
    rm[:, ::64] = 0.0
    return {"ident": np.eye(128, dtype=np.float32),
            "maskf": (s <= t).astype(np.float32), "maskb": (s >= t).astype(np.float32), "rm": rm}


def col_layout(v):
    return np.ascontiguousarray(np.asarray(v, np.float32).reshape(8, 128).T)


def l1_inputs(inp, core):
    b, j = core // 4, core % 4
    m = dict(consts())
    m["xs"] = np.ascontiguousarray(inp["x"][b, j * T:(j + 1) * T])
    m["ctxb"] = np.ascontiguousarray(inp["ctx"][b])
    m["ccol"] = col_layout(inp["c"][b])
    m["cctx"] = col_layout(inp["c_ctx"])
    m["w_ada0"] = np.ascontiguousarray(inp["w_ada"][0])
    m["b_ada0"] = np.ascontiguousarray(inp["b_ada"][0][None, :])
    m["g_mix0"] = np.ascontiguousarray(inp["g_mix"][0][None, :])
    m["w_hin"] = np.ascontiguousarray(inp["w_hgrn_in"][0])
    g = np.asarray(inp["hgrn_gamma"], np.float32).reshape(2, 2, 8, 128)
    m["gam"] = np.ascontiguousarray(g.transpose(3, 0, 1, 2))
    return m


def emit_ffn(p, cm, x1_d, x1_key, xo_d, xo_key, modt, mo, gffn_d, wr_d, br_d, w1_d, b1c_d, w2_d, b2_d, tl, final_g=None):
    NQ = NQ_FFN
    TQ = T // NQ
    hfT = tl["hfT"]
    xt, h, st = tl["xt"], tl["h"], tl["st"]
    gf = h
    p.dma("sp", gf[:], gffn_d.partition_broadcast(128), writes=[gf])
    p.op("dve", lambda e: e.scalar_tensor_tensor(modt[:, mo + 1024:mo + 2048], modt[:, mo + 1024:mo + 2048], 1.0, gf[:], ALU.add, ALU.mult),
         [modt, gf], [modt])
    if DBG_LVL == 16:
        return
    wr = p.sb("wr", [128, 8, NE], F32)
    brb = p.sb("brb", [128, NE], F32)
    p.dma("sp", wr[:], wr_d.rearrange("(kc q) n -> q kc n", q=128), writes=[wr])
    p.dma("sp", brb[:], br_d.partition_broadcast(128), writes=[brb])
    G = p.sb("G_all", [128, T // 128, NE], F32)
    GT = p.sb("GT_q", [NE, T // NQ_FFN], F32)
    b2s = p.sb("b2s", [NE, D], F32)
    b1c = p.sb("b1c", [128, NE, 16], F32)
    p.dma("sp", b2s[:], b2_d, writes=[b2s])
    p.dma("sp", b1c[:], b1c_d, writes=[b1c])
    if DBG_LVL == 17:
        return
    hTf = p.sb("hTf", [128, 8, 128], F32)
    lg = p.sb("lg", [128, NE], F32)
    m8 = p.sb("m8", [128, 8], F32)
    nmx = p.sb("nmx", [128, 1], F32)
    msk = p.sb("msk", [128, NE], F32)
    ex = p.sb("ex", [128, NE], F32)
    sm = p.sb("sm", [128, 1], F32)
    ps_l = cm.psb[2]
    ss, rstd = st
    for t in range(T // 128):
        xx = xt[t % 2]
        p.dma("sp", xx[:], x1_d[t * 128:(t + 1) * 128, :], reads=[x1_key], writes=[xx])
        p.act(h[:], xx[:], AF.Square, [xx], [h, ss], accum_out=ss[:])
        p.act(rstd[:], ss[:], AF.Sqrt, [ss], [rstd], scale=1.0 / D, bias=EPS)
        p.op("dve", lambda e: e.reciprocal(rstd[:], rstd[:]), [rstd], [rstd])
        p.op("dve", lambda e: e.scalar_tensor_tensor(h[:], xx[:], rstd[:], modt[:, mo + 1024:mo + 2048], ALU.mult, ALU.mult),
             [xx, rstd, modt], [h])
        p.tt("dve", h[:], h[:], modt[:, mo:mo + 1024], ALU.add, [h, modt], [h])
        for half in range(2):
            pt = cm.psb[half]
            for j in range(4):
                kc = half * 4 + j
                p.op("pe", lambda e: e.transpose(pt[:, j * 128:(j + 1) * 128], h[:, kc * 128:(kc + 1) * 128], cm.identf[:]),
                     [h, cm.identf], [pt])
            p.copy("act", hfT[:, half * 4:(half + 1) * 4, t * 128:(t + 1) * 128],
                   pt[:].rearrange("q (a b) -> q a b", a=4), [pt], [hfT])
            p.copy("dve", hTf[:, half * 4:(half + 1) * 4, :], pt[:].rearrange("q (a b) -> q a b", a=4), [pt], [hTf])
        if DBG_LVL == 20:
            return
        for kc in range(8):
            p.mm(ps_l[:, 0:NE], hTf[:, kc, :], wr[:, kc, :], kc == 0, kc == 7, [hTf, wr], [ps_l])
        p.tt("dve", lg[:], ps_l[:, 0:NE], brb[:], ALU.add, [ps_l, brb], [lg])
        if DBG_LVL == 19:
            return
        p.op("dve", lambda e: e.max(m8[:], lg[:]), [lg], [m8])
        p.ts("dve", msk[:], lg[:], m8[:, 3:4], None, ALU.is_ge, None, [lg, m8], [msk])
        p.ts("dve", nmx[:], m8[:, 0:1], -1.0, None, ALU.mult, None, [m8], [nmx])
        if DBG_LVL == 18:
            return
        p.act(ex[:], lg[:], AF.Exp, [lg, nmx], [ex], bias=nmx[:])
        p.tt("dve", ex[:], ex[:], msk[:], ALU.mult, [ex, msk], [ex])
        p.op("dve", lambda e: e.tensor_reduce(sm[:], ex[:], AX.X, ALU.add), [ex], [sm])
        p.op("dve", lambda e: e.reciprocal(sm[:], sm[:]), [sm], [sm])
        p.ts("dve", G[:, t, :], ex[:], sm[:], None, ALU.mult, None, [ex, sm], [G])
        if DBG_LVL == 21:
            return
    if DBG_LVL == 22:
        return
    yacc = tl["yacc"]
    actT = tl["actT"]
    w2b = tl["w2b"]
    wst = tl["wst"]
    w2st = tl["w2st"]
    wgb = tl["wgb"]
    wub = tl["wub"]
    tg, tsg, tu = tl["tg"], tl["tsg"], tl["tu"]
    psg, psu, psy = cm.psb[3], cm.psb[4], cm.psb[5]
    cnt = 0
    for qd in range(NQ):
        tq0 = qd * TQ
        for ti in range(TQ // 128):
            tg_ = qd * (TQ // 128) + ti
            p.op("pe", lambda e: e.transpose(ps_l[0:NE, 128:256], G[:, tg_, :], cm.identf[:]), [G, cm.identf], [ps_l])
            p.copy("dve", GT[:, ti * 128:(ti + 1) * 128], ps_l[0:NE, 128:256], [ps_l], [GT])
        for ti in range(TQ // 128):
            tg_ = qd * (TQ // 128) + ti
            for dh in range(2):
                p.mm(psy[:], GT[:, ti * 128:(ti + 1) * 128], b2s[:, dh * 512:(dh + 1) * 512], True, True, [GT, b2s], [psy])
                p.copy("act", yacc[:, ti, dh * 512:(dh + 1) * 512], psy[:], [psy], [yacc])
        if DBG_LVL == 23:
            return
        for ex_ in range(NE):
            if DBG_LVL == 24 and ex_ == 1:
                return
            for j in range(8):
                k = cnt % 2
                cnt += 1
                p.dma("sp", wst[k][:], w1_d[ex_, :, j * 128:(j + 1) * 128].rearrange("(kc q) n -> q kc n", q=128), writes=[wst[k]])
                p.copy("pool", wgb[k][:], wst[k][:], [wst[k]], [wgb[k]])
                p.dma("sp", wst[k][:], w1_d[ex_, :, 1024 + j * 128:1024 + (j + 1) * 128].rearrange("(kc q) n -> q kc n", q=128), writes=[wst[k]])
                p.copy("pool", wub[k][:], wst[k][:], [wst[k]], [wub[k]])
                p.dma("sp", w2st[k][:], w2_d[ex_, j * 128:(j + 1) * 128, :], writes=[w2st[k]])
                p.copy("act", w2b[:, j, :], w2st[k][:], [w2st[k]], [w2b])
                for tb in range(TQ // 512):
                    tok = slice(tq0 + tb * 512, tq0 + (tb + 1) * 512)
                    for kc in range(8):
                        p.mm(psg[:], wgb[k][:, kc, :], hfT[:, kc, tok], kc == 0, kc == 7, [wgb[k], hfT], [psg])
                    for kc in range(8):
                        p.mm(psu[:], wub[k][:, kc, :], hfT[:, kc, tok], kc == 0, kc == 7, [wub[k], hfT], [psu])
                    p.ts("dve", tg[:], psg[:], b1c[:, ex_, j:j + 1], 7.0, ALU.add, ALU.min, [psg, b1c], [tg])
                    p.act(tsg[:], tg[:], AF.Sigmoid, [tg], [tsg], scale=1.702)
                    p.ts("dve", tu[:], psu[:], b1c[:, ex_, 8 + j:9 + j], 7.0, ALU.add, ALU.min, [psu, b1c], [tu])
                    p.ts("pool", tu[:], tu[:], -7.0, 1.0, ALU.max, ALU.add, [tu], [tu])
                    p.tt("pool", tg[:], tg[:], tsg[:], ALU.mult, [tg, tsg], [tg])
                    p.tt("dve", actT[:, j, tb * 512:(tb + 1) * 512], tg[:], tu[:], ALU.mult, [tg, tu], [actT])
            for ti in range(TQ // 128):
                tg_ = qd * (TQ // 128) + ti
                for dh in range(2):
                    for j in range(8):
                        p.mm(psy[:], actT[:, j, ti * 128:(ti + 1) * 128], w2b[:, j, dh * 512:(dh + 1) * 512], j == 0, j == 7,
                             [actT, w2b], [psy])
                    ya = yacc[:, ti, dh * 512:(dh + 1) * 512]
                    p.op("dve", lambda e: e.scalar_tensor_tensor(ya, psy[:], G[:, tg_, ex_:ex_ + 1], ya, ALU.mult, ALU.add),
                         [psy, G, yacc], [yacc])
        for ti in range(TQ // 128):
            tg_ = qd * (TQ // 128) + ti
            xx = xt[ti % 2]
            p.dma("sp", xx[:], x1_d[tg_ * 128:(tg_ + 1) * 128, :], reads=[x1_key], writes=[xx])
            p.tt("dve", yacc[:, ti, :], yacc[:, ti, :], modt[:, mo + 2048:mo + 3072], ALU.mult, [yacc, modt], [yacc])
            p.tt("dve", xx[:], xx[:], yacc[:, ti, :], ALU.add, [xx, yacc], [xx])
            if final_g is not None:
                p.act(h[:], xx[:], AF.Square, [xx], [h, ss], accum_out=ss[:])
                p.act(rstd[:], ss[:], AF.Sqrt, [ss], [rstd], scale=1.0 / D, bias=EPS)
                p.op("dve", lambda e: e.reciprocal(rstd[:], rstd[:]), [rstd], [rstd])
                p.op("dve", lambda e: e.scalar_tensor_tensor(xx[:], xx[:], rstd[:], final_g[:], ALU.mult, ALU.mult),
                     [xx, rstd, final_g], [xx])
            p.dma("sp", xo_d[tg_ * 128:(tg_ + 1) * 128, :], xx[:], reads=[xx], writes=[xo_key])


def ffn_tiles(p):
    TQ = T // NQ_FFN
    return dict(
        hfT=p.sb("hfT", [128, 8, T], BF16),
        xt=[p.sb("fxt%d" % i, [128, D], F32) for i in range(2)],
        h=p.sb("fh", [128, D], F32),
        st=(p.sb("fss", [128, 1], F32), p.sb("frstd", [128, 1], F32)),
        yacc=p.sb("yacc", [128, TQ // 128, D], F32),
        actT=p.sb("actT", [128, 8, TQ], BF16),
        w2b=p.sb("w2b", [128, 8, D], BF16),
        wst=[p.sb("wst%d" % i, [128, 8, 128], F32) for i in range(2)],
        w2st=[p.sb("w2st%d" % i, [128, D], F32) for i in range(1)] * 2,
        wgb=[p.sb("wgb%d" % i, [128, 8, 128], BF16) for i in range(2)],
        wub=[p.sb("wub%d" % i, [128, 8, 128], BF16) for i in range(2)],
        tg=p.sb("tg", [128, 512], F32), tsg=p.sb("tsg", [128, 512], F32), tu=p.sb("tu", [128, 512], F32))


def build_l2(ctx=None):
    do_ffn = False
    ctx = stage_begin(ctx)
    nc, p, cm = ctx.nc, ctx.p, ctx.cm
    xs = dram_in(nc, "xs", [T, D])
    o_f = dram_in(nc, "o_f", [T, D])
    o_b = dram_in(nc, "o_b", [T, D])
    sg = dram_in(nc, "sg", [T, D], BF16)
    QF = dram_in(nc, "QF", [NH, 128, T], BF16)
    QB = dram_in(nc, "QB", [NH, 128, T], BF16)
    SlocG = dram_in(nc, "SlocG", [8 * 2 * NH * 128, 128])
    DlocG = dram_in(nc, "DlocG", [8 * 128, 16])
    Sctx = dram_in(nc, "Sctx", [2, NH, 128, 128])
    fmask = dram_in(nc, "fmask", [128, 16])
    ccol = dram_in(nc, "ccol", [128, 8])
    w_ada = dram_in(nc, "w_ada0", [D, 6 * D])
    b_ada = dram_in(nc, "b_ada0", [1, 6 * D])
    gout = dram_in(nc, "gout", [1, D])
    w_out = dram_in(nc, "w_hout", [D, D])
    x1_d = nc.dram_tensor("x1_scr", [T, D], F32, kind="Internal").ap() if do_ffn else dram_out(nc, "x1", [T, D])
    x2_d = dram_out(nc, "x2", [T, D]) if do_ffn else None

    modm = p.sb("modm", [128, 4096], F32)
    emit_mods(p, cm, w_ada, b_ada, 2048, 6144, [(ccol, modm)], "b")

    fm = p.sb("fm", [128, 16], F32)
    DA = p.sb("DA", [128, 8, 16], F32)
    p.dma("sp", fm[:], fmask, writes=[fm])
    for i in range(8):
        p.dma("sp", DA[:, i, :], DlocG[i * 128:(i + 1) * 128, :], reads=["DlocG"], writes=[DA])
    Sin = p.sb("Sin", [128, 2, NH, 128], BF16)
    Sf = p.sb("Sfold", [128, 128], F32)
    Sl = [p.sb("Sl%d" % i, [128, 128], F32) for i in range(2)]
    tmp = p.sb("ftmp", [128, 128], F32)
    k = 0
    for d in range(2):
        for hh in range(NH):
            p.dma("sp", Sf[:], Sctx[d, hh], writes=[Sf])
            order = range(8) if d == 0 else range(7, -1, -1)
            for i in order:
                sl = Sl[k % 2]
                k += 1
                r0 = (i * 2 * NH + d * NH + hh) * 128
                p.dma("sp", sl[:], SlocG[r0:r0 + 128, :], reads=["SlocG"], writes=[sl])
                col = d * 8 + hh
                p.op("dve", lambda e: e.scalar_tensor_tensor(tmp[:], Sf[:], DA[:, i, col:col + 1], sl[:], ALU.mult, ALU.add),
                     [Sf, DA, sl], [tmp])
                p.tt("dve", tmp[:], tmp[:], Sf[:], ALU.subtract, [tmp, Sf], [tmp])
                p.op("dve", lambda e: e.scalar_tensor_tensor(Sf[:], tmp[:], fm[:, d * 8 + i:d * 8 + i + 1], Sf[:], ALU.mult, ALU.add),
                     [tmp, fm, Sf], [Sf])
            p.copy("pool", Sin[:, d, hh, :], Sf[:], [Sf], [Sin])

    goutb = p.sb("goutb", [128, D], F32)
    p.dma("sp", goutb[:], gout.partition_broadcast(128), writes=[goutb])
    wo = p.sb("wo", [128, 8, D], BF16)
    wos = [p.sb("wos%d" % i, [128, D], F32) for i in range(2)]
    for kc in range(8):
        p.dma("sp", wos[kc % 2][:], w_out[kc * 128:(kc + 1) * 128, :], writes=[wos[kc % 2]])
        p.copy("pool", wo[:, kc, :], wos[kc % 2][:], [wos[kc % 2]], [wo])

    xt = [p.sb("xt%d" % i, [128, D], F32) for i in range(2)]
    of_t = [p.sb("oft%d" % i, [128, D], F32) for i in range(2)]
    ob_t = [p.sb("obt%d" % i, [128, D], F32) for i in range(2)]
    sg_t = [p.sb("sgt%d" % i, [128, D], BF16) for i in range(2)]
    qf_t = [p.sb("qft%d" % i, [128, NH, 128], BF16) for i in range(2)]
    qb_t = [p.sb("qbt%d" % i, [128, NH, 128], BF16) for i in range(2)]
    sq = p.sb("sq", [128, D], F32)
    ssq = p.sb("ssq", [128, NH], F32)
    ogT = p.sb("ogT", [128, 8, 128], BF16)
    for t in range(T // 128):
        k = t % 2
        tok = slice(t * 128, (t + 1) * 128)
        p.dma("sp", xt[k][:], xs[tok, :], writes=[xt[k]])
        p.dma("sp", of_t[k][:], o_f[tok, :], writes=[of_t[k]])
        p.dma("sp", ob_t[k][:], o_b[tok, :], writes=[ob_t[k]])
        p.dma("sp", sg_t[k][:], sg[tok, :], writes=[sg_t[k]])
        p.dma("sp", qf_t[k][:], QF[:, :, tok].rearrange("h q t -> q h t"), writes=[qf_t[k]])
        p.dma("sp", qb_t[k][:], QB[:, :, tok].rearrange("h q t -> q h t"), writes=[qb_t[k]])
        o = of_t[k]
        p.tt("pool", o[:], o[:], ob_t[k][:], ALU.add, [o, ob_t[k]], [o])
        for half in range(2):
            ps = cm.psb[half]
            for j in range(4):
                hh = half * 4 + j
                p.mm(ps[:, j * 128:(j + 1) * 128], qf_t[k][:, hh, :], Sin[:, 0, hh, :], True, False, [qf_t[k], Sin], [ps])
                p.mm(ps[:, j * 128:(j + 1) * 128], qb_t[k][:, hh, :], Sin[:, 1, hh, :], False, True, [qb_t[k], Sin], [ps])
            p.tt("dve", o[:, half * 512:(half + 1) * 512], o[:, half * 512:(half + 1) * 512], ps[:], ALU.add, [o, ps], [o])
        p.tt("pool", sq[:], o[:], o[:], ALU.mult, [o], [sq])
        p.op("dve", lambda e: e.tensor_reduce(ssq[:], sq[:].rearrange("q (h v) -> q h v", h=NH), AX.X, ALU.add), [sq], [ssq])
        p.act(ssq[:], ssq[:], AF.Sqrt, [ssq], [ssq], scale=1.0 / 128, bias=EPS)
        p.op("dve", lambda e: e.reciprocal(ssq[:], ssq[:]), [ssq], [ssq])
        p.tt("dve", o[:].rearrange("q (h v) -> q h v", h=NH), o[:].rearrange("q (h v) -> q h v", h=NH),
             ssq[:].rearrange("q (h u) -> q h u", u=1).to_broadcast([128, NH, 128]), ALU.mult, [o, ssq], [o])
        p.tt("pool", o[:], o[:], goutb[:], ALU.mult, [o, goutb], [o])
        p.tt("dve", o[:], o[:], sg_t[k][:], ALU.mult, [o, sg_t[k]], [o])
        for half in range(2):
            pt = cm.psb[2 + half]
            for j in range(4):
                kc = half * 4 + j
                p.op("pe", lambda e: e.transpose(pt[:, j * 128:(j + 1) * 128], o[:, kc * 128:(kc + 1) * 128], cm.identf[:]),
                     [o, cm.identf], [pt])
            p.copy("act", ogT[:, half * 4:(half + 1) * 4, :], pt[:].rearrange("q (a b) -> q a b", a=4), [pt], [ogT])
        for dh in range(2):
            ps = cm.psb[4 + dh]
            for kc in range(8):
                p.mm(ps[:], ogT[:, kc, :], wo[:, kc, dh * 512:(dh + 1) * 512], kc == 0, kc == 7, [ogT, wo], [ps])
            p.tt("dve", sq[:, dh * 512:(dh + 1) * 512], ps[:], modm[:, dh * 512:(dh + 1) * 512], ALU.mult, [ps, modm], [sq])
        p.tt("pool", xt[k][:], xt[k][:], sq[:], ALU.add, [xt[k], sq], [xt[k]])
        p.dma("sp", x1_d[tok, :], xt[k][:], reads=[xt[k]], writes=["x1"])
    if do_ffn:
        tl = ffn_tiles(p)
        emit_ffn(p, cm, x1_d, "x1", x2_d, "x2", modm, 1024, gffn, wr_d, br_d, w1_d, b1c_d, w2_d, b2_d, tl)
    return stage_end(ctx, ["x1"])


def build_ffn(final, ctx=None, layer=0, in_name="x1", out_name="x2"):
    ctx = stage_begin(ctx)
    nc, p, cm = ctx.nc, ctx.p, ctx.cm
    L = str(layer)
    x1_d = dram_in(nc, in_name, [T, D])
    ccol = dram_in(nc, "ccol", [128, 8])
    w_ada = dram_in(nc, "w_ada" + L, [D, 6 * D])
    b_ada = dram_in(nc, "b_ada" + L, [1, 6 * D])
    gffn = dram_in(nc, "g_ffn" + L, [1, D])
    wr_d = dram_in(nc, "w_router" + L, [D, NE])
    br_d = dram_in(nc, "b_router" + L, [1, NE])
    w1_d = dram_in(nc, "w_exp_in" + L, [NE, D, 2 * D])
    b1c_d = dram_in(nc, "b1c" + L, [128, NE, 16])
    w2_d = dram_in(nc, "w_exp_out" + L, [NE, D, D])
    b2_d = dram_in(nc, "b_exp_out" + L, [NE, D])
    gfin_d = dram_in(nc, "g_final", [1, D])
    x2_d = dram_out(nc, out_name, [T, D])
    modf = p.sb("modf", [128, 3072], F32)
    emit_mods(p, cm, w_ada, b_ada, 3072, 6144, [(ccol, modf)], "f")
    gfin = None
    if final:
        gfin = p.sb("gfin", [128, D], F32)
        p.dma("sp", gfin[:], gfin_d.partition_broadcast(128), writes=[gfin])
    tl = ffn_tiles(p)
    emit_ffn(p, cm, x1_d, "x1in", x2_d, "x2", modf, 0, gffn, wr_d, br_d, w1_d, b1c_d, w2_d, b2_d, tl, final_g=gfin)
    return stage_end(ctx, ["x2"])


def ffn_inputs(inp, core, layer, x1):
    b = core // 4
    m = {"ident": np.eye(128, dtype=np.float32)}
    m["x1"] = np.ascontiguousarray(x1)
    m["ccol"] = col_layout(inp["c"][b])
    m["w_ada"] = np.ascontiguousarray(inp["w_ada"][layer])
    m["b_ada"] = np.ascontiguousarray(inp["b_ada"][layer][None, :])
    m["g_ffn"] = np.ascontiguousarray(inp["g_ffn"][layer][None, :])
    m["w_router"] = np.ascontiguousarray(inp["w_router"][layer])
    m["b_router"] = np.ascontiguousarray(inp["b_router"][layer][None, :])
    m["w_exp_in"] = np.ascontiguousarray(inp["w_exp_in"][layer])
    b1 = np.asarray(inp["b_exp_in"][layer], np.float32).reshape(NE, 16, 128)
    m["b1c"] = np.ascontiguousarray(b1.transpose(2, 0, 1))
    m["w_exp_out"] = np.ascontiguousarray(inp["w_exp_out"][layer])
    m["b_exp_out"] = np.ascontiguousarray(inp["b_exp_out"][layer])
    m["g_final"] = np.ascontiguousarray(inp["g_final"][None, :])
    return m


def l2_inputs(inp, core, r1):
    b, j = core // 4, core % 4
    m = {"ident": np.eye(128, dtype=np.float32)}
    m["xs"] = np.ascontiguousarray(inp["x"][b, j * T:(j + 1) * T])
    for k in ("o_f", "o_b", "sg", "QF", "QB", "Sctx"):
        m[k] = r1[core][k]
    m["SlocA"] = np.ascontiguousarray(np.stack([r1[b * 4 + i]["Sloc"] for i in range(4)]))
    m["DlocA"] = np.ascontiguousarray(np.stack([r1[b * 4 + i]["Dloc"] for i in range(4)]))
    fm = np.zeros((128, 8), np.float32)
    for i in range(4):
        fm[:, i] = 1.0 if i < j else 0.0
        fm[:, 4 + i] = 1.0 if i > j else 0.0
    m["fmask"] = fm
    m["ccol"] = col_layout(inp["c"][b])
    m["w_ada0"] = np.ascontiguousarray(inp["w_ada"][0])
    m["b_ada0"] = np.ascontiguousarray(inp["b_ada"][0][None, :])
    m["gout"] = np.ascontiguousarray(inp["g_hgrn_out"][0][None, :])
    m["w_hout"] = np.ascontiguousarray(inp["w_hgrn_out"][0])
    return m


HALO = 15 * 64


def build_conv_a(ctx=None):
    ctx = stage_begin(ctx)
    nc, p, cm = ctx.nc, ctx.p, ctx.cm
    x2 = dram_in(nc, "x2", [T, D])
    ccol = dram_in(nc, "ccol", [128, 8])
    w_ada = dram_in(nc, "w_ada1", [D, 6 * D])
    b_ada = dram_in(nc, "b_ada1", [1, 6 * D])
    g_mix = dram_in(nc, "g_mix1", [1, D])
    w_in = dram_in(nc, "w_cv_in", [D, 2 * D])
    bin_c = dram_in(nc, "bin_c", [128, 16])
    u_d = dram_out(nc, "u", [8, 128, T])
    gtm_d = dram_out(nc, "gtm", [1, D])
    modb = p.sb("modb", [128, 3072], F32)
    emit_mods(p, cm, w_ada, b_ada, 0, 3072, [(ccol, modb)], "c")
    p.dma("sp", gtm_d, modb[0:1, 2048:3072], reads=[modb], writes=["gtm"])
    gmix = p.sb("gmix", [128, D], F32)
    p.dma("sp", gmix[:], g_mix.partition_broadcast(128), writes=[gmix])
    p.op("dve", lambda e: e.scalar_tensor_tensor(modb[:, 1024:2048], modb[:, 1024:2048], 1.0, gmix[:], ALU.add, ALU.mult),
         [modb, gmix], [modb])
    hT = p.sb("hT", [128, 8, T], BF16)
    xt = [p.sb("xt%d" % i, [128, D], F32) for i in range(2)]
    h = p.sb("h", [128, D], F32)
    st = (p.sb("ss", [128, 1], F32), p.sb("rstd", [128, 1], F32))
    emit_norm_T(p, cm, x2, T // 128, modb[:, 1024:2048], modb[:, 0:1024], modb, hT, None, xt, h, h, st)
    bc = p.sb("bc", [128, 16], F32)
    p.dma("sp", bc[:], bin_c, writes=[bc])
    wst = [p.sb("wst%d" % i, [128, 8, 128], F32) for i in range(2)]
    wab = [p.sb("wab%d" % i, [128, 8, 128], BF16) for i in range(2)]
    wgb = [p.sb("wgb%d" % i, [128, 8, 128], BF16) for i in range(2)]
    sgt = [p.sb("sgt%d" % i, [128, 512], F32) for i in range(2)]
    ut = [p.sb("ut%d" % i, [128, 512], F32) for i in range(2)]
    psa, psg = cm.psb[2], cm.psb[3]
    n = 0
    for j in range(8):
        k = j % 2
        p.dma("sp", wst[0][:], w_in[:, j * 128:(j + 1) * 128].rearrange("(kc q) n -> q kc n", q=128), writes=[wst[0]])
        p.copy("pool", wab[k][:], wst[0][:], [wst[0]], [wab[k]])
        p.dma("sp", wst[1][:], w_in[:, 1024 + j * 128:1024 + (j + 1) * 128].rearrange("(kc q) n -> q kc n", q=128), writes=[wst[1]])
        p.copy("pool", wgb[k][:], wst[1][:], [wst[1]], [wgb[k]])
        for tb in range(T // 512):
            tok = slice(tb * 512, (tb + 1) * 512)
            for kc in range(8):
                p.mm(psa[:], wab[k][:, kc, :], hT[:, kc, tok], kc == 0, kc == 7, [wab[k], hT], [psa])
            for kc in range(8):
                p.mm(psg[:], wgb[k][:, kc, :], hT[:, kc, tok], kc == 0, kc == 7, [wgb[k], hT], [psg])
            sg_, u_ = sgt[n % 2], ut[n % 2]
            n += 1
            p.act(sg_[:], psg[:], AF.Sigmoid, [psg, bc], [sg_], bias=bc[:, 8 + j:9 + j])
            p.op("dve", lambda e: e.scalar_tensor_tensor(u_[:], psa[:], bc[:, j:j + 1], sg_[:], ALU.add, ALU.mult),
                 [psa, bc, sg_], [u_])
            p.dma("sp", u_d[j, :, tok], u_[:], reads=[u_], writes=["u_d"])
    if not ctx.standalone:
        hb = nc.dram_tensor("halo_b", [8 * 128, HALO], F32, kind="Internal").ap()
        REG["halo_b"] = hb
        for jj in range(4):
            p.dma("sp", hb[jj * 128:(jj + 1) * 128, :], u_d[4 + jj, :, T - HALO:T], reads=["u_d"], writes=["halo_b"])
            p.dma("sp", hb[(4 + jj) * 128:(5 + jj) * 128, :], u_d[4 + jj, :, 0:HALO], reads=["u_d"], writes=["halo_b"])
    return stage_end(ctx, ["u_d", "gtm"])


def conv_a_inputs(inp, core, x2):
    b = core // 4
    m = {"ident": np.eye(128, dtype=np.float32)}
    m["x2"] = np.ascontiguousarray(x2)
    m["ccol"] = col_layout(inp["c"][b])
    m["w_ada"] = np.ascontiguousarray(inp["w_ada"][1])
    m["b_ada"] = np.ascontiguousarray(inp["b_ada"][1][None, :])
    m["g_mix"] = np.ascontiguousarray(inp["g_mix"][1][None, :])
    m["w_cv_in"] = np.ascontiguousarray(inp["w_cv_in"][0])
    m["bin_c"] = np.ascontiguousarray(np.asarray(inp["b_cv_in"][0], np.float32).reshape(16, 128).T)
    return m


def build_conv_b(ctx=None):
    ctx = stage_begin(ctx)
    nc, p, cm = ctx.nc, ctx.p, ctx.cm
    x2 = dram_in(nc, "x2", [T, D])
    u_d = dram_in(nc, "u", [8, 128, T])
    G2 = dram_in(nc, "halo_g", [8 * 8 * 128, HALO])
    sel_d = dram_in(nc, "sel", [128, 16])
    gtm_d = dram_in(nc, "gtm", [1, D])
    wdw_d = dram_in(nc, "wdw", [128, 8, 31])
    cvec_d = dram_in(nc, "cvec", [128, 3, 8])
    wout_d = dram_in(nc, "w_cv_out", [D, D])
    bout_d = dram_in(nc, "b_cv_out", [1, D])
    x3_d = dram_out(nc, "x3", [T, D])
    wdw = p.sb("wdw", [128, 8, 31], F32)
    cvec = p.sb("cvec", [128, 3, 8], F32)
    gtm = p.sb("gtm", [128, D], F32)
    bout = p.sb("bout", [128, D], F32)
    sel = p.sb("sel", [128, 16], F32)
    p.dma("sp", sel[:], sel_d, writes=[sel])
    p.dma("sp", wdw[:], wdw_d, writes=[wdw])
    p.dma("sp", cvec[:], cvec_d, writes=[cvec])
    p.dma("sp", gtm[:], gtm_d.partition_broadcast(128), writes=[gtm])
    p.dma("sp", bout[:], bout_d.partition_broadcast(128), writes=[bout])
    wo = p.sb("wo", [128, 8, D], BF16)
    wos = p.sb("wos", [128, D], F32)
    for kc in range(8):
        p.dma("sp", wos[:], wout_d[kc * 128:(kc + 1) * 128, :], writes=[wos])
        p.copy("pool", wo[:, kc, :], wos[:], [wos], [wo])
    z = p.sb("z_all", [128, 8, T], F32)
    ue = p.sb("ue", [128, T + 2 * HALO], F32)
    for j in range(8):
        zj = z[:, j, :]
        zkey = ("z", j)
        if j < 4:
            p.dma("sp", ue[:, 0:T], u_d[j], writes=[ue])
            p.ts("dve", zj, ue[:, 0:T], wdw[:, j, 15:16], cvec[:, 0, j:j + 1], ALU.mult, ALU.add, [ue, wdw, cvec], [zkey])
            z3 = zj.rearrange("q (r w) -> q r w", w=64)
            u3 = ue[:, 0:T].rearrange("q (r w) -> q r w", w=64)
            for o in range(-15, 16):
                if o == 0:
                    continue
                lo, hi = max(0, -o), min(64, 64 - o)
                p.op("dve", lambda e: e.scalar_tensor_tensor(z3[:, :, lo:hi], u3[:, :, lo + o:hi + o], wdw[:, j, o + 15:o + 16],
                                                             z3[:, :, lo:hi], ALU.mult, ALU.add), [ue, wdw, zkey], [zkey])
        else:
            p.dma("sp", ue[:, HALO:HALO + T], u_d[j], writes=[ue])
            for side, (c0, dst) in enumerate(((0, ue[:, 0:HALO]), (8, ue[:, HALO + T:HALO + T + HALO]))):
                for r in range(8):
                    r0 = r * 1024 + ((0 if side == 0 else 4) + (j - 4)) * 128
                    stg = wos[:, 0:HALO]
                    p.dma("sp", stg, G2[r0:r0 + 128, :], reads=["halo_g"], writes=[wos])
                    if r == 0:
                        p.ts("dve", dst, stg, sel[:, c0:c0 + 1], None, ALU.mult, None, [wos, sel], [ue])
                    else:
                        p.op("dve", lambda e: e.scalar_tensor_tensor(dst, stg, sel[:, c0 + r:c0 + r + 1], dst, ALU.mult, ALU.add),
                             [wos, sel, ue], [ue])
            p.ts("dve", zj, ue[:, HALO:HALO + T], wdw[:, j, 15:16], cvec[:, 0, j:j + 1], ALU.mult, ALU.add, [ue, wdw, cvec], [zkey])
            for o in range(-15, 16):
                if o == 0:
                    continue
                a0 = HALO + o * 64
                p.op("dve", lambda e: e.scalar_tensor_tensor(zj, ue[:, a0:a0 + T], wdw[:, j, o + 15:o + 16], zj, ALU.mult, ALU.add),
                     [ue, wdw, zkey], [zkey])
    zkeys = [("z", j) for j in range(8)]
    sqt = [p.sb("sqt%d" % i, [128, 512], F32) for i in range(2)]
    mean = p.sb("mean", [128, 512], F32)
    msq = p.sb("msq", [128, 512], F32)
    rstd = p.sb("rstdv", [128, 512], F32)
    zn = [p.sb("zn0", [128, 512], F32)] * 2
    sgm = [p.sb("sgm0", [128, 512], F32)] * 2
    zs = p.sb("zs", [128, 8, 512], BF16)
    yt = wos
    ps1, ps2 = cm.psb[0], cm.psb[1]
    psy = [cm.psb[2], cm.psb[3]]
    n = 0
    for tb in range(T // 512):
        tok = slice(tb * 512, (tb + 1) * 512)
        for j in range(8):
            sq = sqt[n % 2]
            n += 1
            p.act(sq[:], z[:, j, tok], AF.Square, [zkeys[j]], [sq])
            p.mm(ps1[:], cm.ones[:], z[:, j, tok], j == 0, j == 7, [cm.ones, zkeys[j]], [ps1])
            p.mm(ps2[:], cm.ones[:], sq[:], j == 0, j == 7, [cm.ones, sq], [ps2])
        p.act(mean[:], ps1[:], AF.Copy, [ps1], [mean], scale=1.0 / D)
        p.tt("pool", msq[:], mean[:], mean[:], ALU.mult, [mean], [msq])
        p.op("dve", lambda e: e.scalar_tensor_tensor(rstd[:], ps2[:], 1.0 / D, msq[:], ALU.mult, ALU.subtract), [ps2, msq], [rstd])
        p.act(rstd[:], rstd[:], AF.Sqrt, [rstd], [rstd], bias=EPS)
        p.op("dve", lambda e: e.reciprocal(rstd[:], rstd[:]), [rstd], [rstd])
        for j in range(8):
            a, sg_ = zn[j % 2], sgm[j % 2]
            p.tt("dve", a[:], z[:, j, tok], mean[:], ALU.subtract, [zkeys[j], mean], [a])
            p.tt("pool", a[:], a[:], rstd[:], ALU.mult, [a, rstd], [a])
            p.ts("dve", a[:], a[:], cvec[:, 1, j:j + 1], cvec[:, 2, j:j + 1], ALU.mult, ALU.add, [a, cvec], [a])
            p.act(sg_[:], a[:], AF.Sigmoid, [a], [sg_])
            p.tt("pool", zs[:, j, :], a[:], sg_[:], ALU.mult, [a, sg_], [zs])
        for ti in range(4):
            t = tb * 4 + ti
            xa = ue[:, 0:D]
            p.dma("sp", xa, x2[t * 128:(t + 1) * 128, :], writes=[ue])
            for dh in range(2):
                ps = psy[dh]
                for j in range(8):
                    p.mm(ps[:], zs[:, j, ti * 128:(ti + 1) * 128], wo[:, j, dh * 512:(dh + 1) * 512], j == 0, j == 7, [zs, wo], [ps])
                p.tt("dve", yt[:, dh * 512:(dh + 1) * 512], ps[:], bout[:, dh * 512:(dh + 1) * 512], ALU.add, [ps, bout], [yt])
            p.tt("pool", yt[:], yt[:], gtm[:], ALU.mult, [yt, gtm], [yt])
            p.tt("dve", xa, xa, yt[:], ALU.add, [ue, yt], [ue])
            p.dma("sp", x3_d[t * 128:(t + 1) * 128, :], xa, reads=[ue], writes=["x3"])
    return stage_end(ctx, ["x3"])


def conv_b_inputs(inp, core, x2, ra):
    b, j = core // 4, core % 4
    m = {"ident": np.eye(128, dtype=np.float32)}
    m["x2"] = np.ascontiguousarray(x2)
    m["u"] = ra[core]["u"]
    zero = np.zeros((4, 128, HALO), np.float32)
    m["u_prev"] = np.ascontiguousarray(ra[core - 1]["u"][4:8, :, T - HALO:T]) if j > 0 else zero
    m["u_next"] = np.ascontiguousarray(ra[core + 1]["u"][4:8, :, 0:HALO]) if j < 3 else zero
    m["gtm"] = ra[core]["gtm"]
    w = np.asarray(inp["w_cv_dw"][0], np.float32)
    m["wdw"] = np.ascontiguousarray(w.reshape(31, 8, 128).transpose(2, 1, 0))
    cv = np.stack([np.asarray(inp[k][0], np.float32).reshape(8, 128).T for k in ("b_cv_dw", "g_cv_ln", "b_cv_ln")], axis=1)
    m["cvec"] = np.ascontiguousarray(cv)
    m["w_cv_out"] = np.ascontiguousarray(inp["w_cv_out"][0])
    m["b_cv_out"] = np.ascontiguousarray(inp["b_cv_out"][0][None, :])
    return m


_DBG = {}


def build_fused():
    REG.clear()
    FUSED[0] = True
    try:
        ctx = Ctx()
        nc, p = ctx.nc, ctx.p
        ctx.cm = Common(p, nc)
        build_l1(ctx)
        SlocG = nc.dram_tensor("SlocG", [8 * 2 * NH * 128, 128], F32, kind="Internal").ap()
        DlocG = nc.dram_tensor("DlocG", [8 * 128, 16], F32, kind="Internal").ap()
        REG["SlocG"], REG["DlocG"] = SlocG, DlocG
        p.allgather(REG["Sloc"].rearrange("d h k v -> (d h k) v"), SlocG, "Sloc_d", "SlocG")
        p.allgather(REG["Dloc"], DlocG, "Dloc_d", "DlocG")
        build_l2(ctx)
        build_ffn(False, ctx, 0, "x1", "x2")
        build_conv_a(ctx)
        halo_g = nc.dram_tensor("halo_g", [8 * 8 * 128, HALO], F32, kind="Internal").ap()
        REG["halo_g"] = halo_g
        p.allgather(REG["halo_b"], halo_g, "halo_b", "halo_g")
        build_conv_b(ctx)
        build_ffn(True, ctx, 1, "x3", FINAL_OUT)
        p.barrier()
    finally:
        FUSED[0] = False
    return nc, p


def fused_inputs(inp, core):
    b, j = core // 4, core % 4
    m = l1_inputs(inp, core)
    fm = np.zeros((128, 16), np.float32)
    sel = np.zeros((128, 16), np.float32)
    for r in range(8):
        if r // 4 == b:
            fm[:, r] = 1.0 if (r % 4) < j else 0.0
            fm[:, 8 + r] = 1.0 if (r % 4) > j else 0.0
    if j > 0:
        sel[:, core - 1] = 1.0
    if j < 3:
        sel[:, 8 + core + 1] = 1.0
    m["fmask"], m["sel"] = fm, sel
    m["gout"] = np.ascontiguousarray(inp["g_hgrn_out"][0][None, :])
    m["w_hout"] = np.ascontiguousarray(inp["w_hgrn_out"][0])
    for layer in range(2):
        L = str(layer)
        m["w_ada" + L] = np.ascontiguousarray(inp["w_ada"][layer])
        m["b_ada" + L] = np.ascontiguousarray(inp["b_ada"][layer][None, :])
        m["g_ffn" + L] = np.ascontiguousarray(inp["g_ffn"][layer][None, :])
        m["w_router" + L] = np.ascontiguousarray(inp["w_router"][layer])
        m["b_router" + L] = np.ascontiguousarray(inp["b_router"][layer][None, :])
        m["w_exp_in" + L] = np.ascontiguousarray(inp["w_exp_in"][layer])
        b1 = np.asarray(inp["b_exp_in"][layer], np.float32).reshape(NE, 16, 128)
        m["b1c" + L] = np.ascontiguousarray(b1.transpose(2, 0, 1))
        m["w_exp_out" + L] = np.ascontiguousarray(inp["w_exp_out"][layer])
        m["b_exp_out" + L] = np.ascontiguousarray(inp["b_exp_out"][layer])
    m["g_final"] = np.ascontiguousarray(inp["g_final"][None, :])
    m["g_mix1"] = np.ascontiguousarray(inp["g_mix"][1][None, :])
    m["w_cv_in"] = np.ascontiguousarray(inp["w_cv_in"][0])
    m["bin_c"] = np.ascontiguousarray(np.asarray(inp["b_cv_in"][0], np.float32).reshape(16, 128).T)
    w = np.asarray(inp["w_cv_dw"][0], np.float32)
    m["wdw"] = np.ascontiguousarray(w.reshape(31, 8, 128).transpose(2, 1, 0))
    cv = np.stack([np.asarray(inp[k][0], np.float32).reshape(8, 128).T for k in ("b_cv_dw", "g_cv_ln", "b_cv_ln")], axis=1)
    m["cvec"] = np.ascontiguousarray(cv)
    m["w_cv_out"] = np.ascontiguousarray(inp["w_cv_out"][0])
    m["b_cv_out"] = np.ascontiguousarray(inp["b_cv_out"][0][None, :])
    return m


def kernel(**inp):
    inp = {k: np.asarray(v) for k, v in inp.items()}
    nc, _ = build_fused()
    res = run_bass_kernel_spmd(nc, [fused_inputs(inp, c) for c in range(8)], core_ids=list(range(8))).results
    out = np.zeros((2, 4 * T, D), np.float32)
    for c in range(8):
        out[c // 4, (c % 4) * T:(c % 4 + 1) * T] = res[c][FINAL_OUT]
    return out
```

```python
import numpy as np
from contextlib import ExitStack
import concourse.bass as bass
import concourse.mybir as mybir
from concourse.bass_utils import run_bass_kernel_spmd

F32 = mybir.dt.float32
BF16 = mybir.dt.bfloat16
AF = mybir.ActivationFunctionType
ALU = mybir.AluOpType
AX = mybir.AxisListType

SEM_LIMIT = 12000
D = 1024
T = 4096
TCTX = 256
NH = 8
EPS = 1e-6
NE = 32
NQ_FFN = 8


class Prog:
    def __init__(self, nc):
        self.nc = nc
        self.es = ExitStack()
        self.eng = {"pe": nc.tensor, "dve": nc.vector, "act": nc.scalar,
                    "pool": nc.gpsimd, "sp": nc.sync}
        self.sem = {}
        self.cnt = {}
        self.nsem = 0
        for e in ("pe", "dve", "act", "pool"):
            self._new_sem(e)
        self.waited = {e: {} for e in self.eng}
        self.res = {}
        self.dsem = {}
        self.dcnt = {}
        self.ninst = {e: 0 for e in self.eng}
        self.psum_ids = set()
        self.prev_sem = {}
        self.dtot = {}
        self.free_dsems = []
        self.phase_es = None
        self.cc_sems = []

    def _alloc_sem(self, name):
        self.nsem += 1
        return self.es.enter_context(self.nc.semaphore(name))

    def _new_sem(self, e):
        if e in self.sem:
            self.prev_sem.setdefault(e, []).append((self.sem[e], self.cnt[e]))
        self.sem[e] = self._alloc_sem("e_%s_%d" % (e, self.nsem))
        self.cnt[e] = 0

    def begin_phase(self):
        self.phase_es = ExitStack()

    def end_phase(self):
        self.barrier()
        self.phase_es.close()
        self.phase_es = None
        self.res = {}
        self.free_dsems = [sem for (sem, _) in self.dtot.values()]
        self.dsem = {}

    def barrier(self):
        targets = []
        for x in ("pe", "dve", "act", "pool"):
            for (sm, c) in self.prev_sem.get(x, []):
                targets.append((sm, c))
            if self.cnt[x] > 0:
                targets.append((self.sem[x], self.cnt[x]))
        targets.extend(self.dtot.values())
        targets.extend(self.cc_sems)
        for e in ("pe", "dve", "act", "pool", "sp"):
            for (sm, c) in targets:
                if c > 0:
                    self._wait(e, sm, c)

    def allgather(self, in_ap, out_ap, in_key, out_key):
        self._deps("pool", [in_key], [out_key])
        inst = self.nc.gpsimd.collective_compute("AllGather", ALU.bypass, replica_groups=[list(range(8))],
                                                 ins=[in_ap.opt()], outs=[out_ap.opt()])
        sm = self._alloc_sem("cc%d" % self.nsem)
        inst.then_inc(sm, 1)
        self.cc_sems.append((sm, 1))
        self._mark(sm, 1, [in_key], [out_key])

    def sb(self, name, shape, dt):
        es = self.phase_es if self.phase_es is not None else self.es
        self.ntile = getattr(self, "ntile", 0) + 1
        return es.enter_context(self.nc.sbuf_tensor("s%d_%s" % (self.ntile, name), list(shape), dt))

    def ps(self, name, shape, dt=F32):
        t = self.es.enter_context(self.nc.psum_tensor("p_" + name, list(shape), dt))
        self.psum_ids.add(id(t))
        return t

    def _r(self, key):
        k = key if isinstance(key, (str, tuple)) else id(key)
        r = self.res.get(k)
        if r is None:
            r = {"w": None, "r": {}}
            self.res[k] = r
        return r

    def _wait(self, e, sem, val):
        w = self.waited[e]
        k = id(sem)
        if w.get(k, 0) >= val:
            return
        w[k] = val
        self.eng[e].wait_ge(sem, val)

    def _deps(self, e, reads, writes):
        deps = []
        for r in reads:
            rr = self._r(r)
            if rr["w"] is not None:
                deps.append(rr["w"])
            if id(r) in self.psum_ids:
                deps.extend(rr["r"].values())
        for wk in writes:
            rr = self._r(wk)
            if rr["w"] is not None:
                deps.append(rr["w"])
            deps.extend(rr["r"].values())
        own = self.sem.get(e)
        for (sem, val) in deps:
            if e == "pe" and sem is own:
                continue
            self._wait(e, sem, val)

    def _mark(self, sem, val, reads, writes):
        for r in reads:
            self._r(r)["r"][id(sem)] = (sem, val)
        for wk in writes:
            rr = self._r(wk)
            rr["w"] = (sem, val)
            rr["r"] = {}

    def op(self, e, fn, reads=(), writes=()):
        if self.cnt[e] >= SEM_LIMIT:
            self._new_sem(e)
        self._deps(e, reads, writes)
        inst = fn(self.eng[e])
        self.cnt[e] += 1
        self.ninst[e] += 1
        inst.then_inc(self.sem[e], 1)
        self._mark(self.sem[e], self.cnt[e], reads, writes)
        return inst

    def dma(self, q, out, in_, reads=(), writes=(), semkey=None, **kw):
        self._deps(q, reads, writes)
        key = semkey if semkey is not None else writes[0]
        key = key if isinstance(key, (str, tuple)) else id(key)
        if key not in self.dsem or self.dtot[id(self.dsem[key])][1] >= 2 * SEM_LIMIT:
            if key not in self.dsem and self.free_dsems:
                self.dsem[key] = self.free_dsems.pop()
            else:
                sm = self._alloc_sem("d%d" % self.nsem)
                self.dsem[key] = sm
                self.dtot[id(sm)] = (sm, 0)
        s = self.dsem[key]
        inst = self.eng[q].dma_start(out=out, in_=in_, **kw)
        tot = self.dtot[id(s)][1] + 16
        self.dtot[id(s)] = (s, tot)
        self.ninst[q] += 1
        inst.then_inc(s, 16)
        self._mark(s, tot, reads, writes)
        return inst

    def finish(self, out_keys, e="sp"):
        for k in out_keys:
            rr = self._r(k)
            if rr["w"] is not None:
                self._wait(e, *rr["w"])
        for x in ("pe", "dve", "act", "pool"):
            if self.cnt[x] > 0:
                self._wait(e, self.sem[x], self.cnt[x])

    def act(self, out, in_, func, reads, writes, **kw):
        return self.op("act", lambda e: e.activation(out, in_, func, **kw), reads, writes)

    def tt(self, eng, out, a, b, op, reads, writes):
        return self.op(eng, lambda e: e.tensor_tensor(out, a, b, op), reads, writes)

    def ts(self, eng, out, a, s1, s2, op0, op1, reads, writes):
        if s2 is None:
            return self.op(eng, lambda e: e.tensor_scalar(out, a, s1, None, op0), reads, writes)
        return self.op(eng, lambda e: e.tensor_scalar(out, a, s1, s2, op0, op1), reads, writes)

    def copy(self, eng, out, in_, reads, writes):
        if eng == "act":
            return self.op("act", lambda e: e.copy(out, in_), reads, writes)
        return self.op(eng, lambda e: e.tensor_copy(out, in_), reads, writes)

    def mm(self, out, lhsT, rhs, start, stop, reads, writes):
        return self.op("pe", lambda e: e.matmul(out, lhsT, rhs, start=start, stop=stop), reads, writes)


REG = {}
FUSED = [False]
FINAL_OUT = "out"


def dram_in(nc, name, shape, dt=F32):
    if FUSED[0] and name in REG:
        return REG[name]
    ap = nc.dram_tensor(name, list(shape), dt, kind="ExternalInput").ap()
    if FUSED[0]:
        REG[name] = ap
    return ap


def dram_out(nc, name, shape, dt=F32):
    if FUSED[0]:
        kind = "ExternalOutput" if name == FINAL_OUT else "Internal"
        ap = nc.dram_tensor(name, list(shape), dt, kind=kind).ap()
        REG[name] = ap
        return ap
    return nc.dram_tensor(name, list(shape), dt, kind="ExternalOutput").ap()


class Ctx:
    def __init__(self):
        self.nc = bass.Bass("TRN2", target_bir_lowering=False)
        self.p = Prog(self.nc)
        self.cm = None


def stage_begin(ctx):
    if ctx is None:
        c = Ctx()
        c.cm = Common(c.p, c.nc)
        c.standalone = True
        return c
    ctx.standalone = False
    ctx.p.begin_phase()
    return ctx


def stage_end(ctx, out_keys):
    if ctx.standalone:
        ctx.p.finish(out_keys)
    else:
        ctx.p.end_phase()
    return ctx.nc, ctx.p


class Common:
    def __init__(self, p, nc):
        self.p = p
        self.ident_d = dram_in(nc, "ident", [128, 128])
        self.identf = p.sb("identf", [128, 128], F32)
        self.identb = p.sb("identb", [128, 128], BF16)
        self.ones = p.sb("ones", [128, 128], F32)
        p.dma("sp", self.identf[:], self.ident_d, writes=[self.identf])
        p.copy("dve", self.identb[:], self.identf[:], [self.identf], [self.identb])
        p.op("dve", lambda e: e.memset(self.ones[:], 1.0), [], [self.ones])
        self.ones5 = p.sb("ones5", [128, 512], F32)
        p.op("dve", lambda e: e.memset(self.ones5[:], 1.0), [], [self.ones5])
        self.psb = [p.ps("psb%d" % i, [128, 512]) for i in range(7)]
        self.psT = p.ps("psT", [128, 1024], BF16)


def emit_mods(p, cm, w_ada, b_ada, lo, hi, pairs, tag):
    lts = []
    for i, (ccol, out) in enumerate(pairs):
        p.dma("sp", out[:], b_ada[:, lo:hi].partition_broadcast(128), writes=[out])
        cc = p.sb("mod_cc%s%d" % (tag, i), [128, 8], F32)
        sg = p.sb("mod_sg%s%d" % (tag, i), [128, 8], F32)
        lt = p.sb("mod_lt%s%d" % (tag, i), [128, 8, 128], F32)
        p.dma("sp", cc[:], ccol, writes=[cc])
        p.act(sg[:], cc[:], AF.Sigmoid, [cc], [sg])
        p.tt("dve", sg[:], sg[:], cc[:], ALU.mult, [sg, cc], [sg])
        for kc in range(8):
            p.ts("dve", lt[:, kc, :], cm.ones[:], sg[:, kc:kc + 1], None, ALU.mult, None, [cm.ones, sg], [lt])
        lts.append(lt)
    wc = p.sb("mod_w%s" % tag, [128, 8, 256], F32)
    for bi, n0 in enumerate(range(lo, hi, 256)):
        p.dma("sp", wc[:], w_ada[:, n0:n0 + 256].rearrange("(kc q) n -> q kc n", q=128), writes=[wc])
        for i, (ccol, out) in enumerate(pairs):
            ps = cm.psb[i % 2]
            for kc in range(8):
                p.mm(ps[:, 0:256], lts[i][:, kc, :], wc[:, kc, :], kc == 0, kc == 7, [lts[i], wc], [ps])
            p.tt("dve", out[:, n0 - lo:n0 - lo + 256], ps[:, 0:256], out[:, n0 - lo:n0 - lo + 256], ALU.add, [ps, out], [out])


def emit_norm_T(p, cm, src, ntiles, Arow, Brow, modt, hT, tagres, xt, junk, h, st, hTf=None):
    for t in range(ntiles):
        xx = xt[t % 2]
        p.dma("sp", xx[:], src[t * 128:(t + 1) * 128, :], reads=[tagres] if tagres else [], writes=[xx])
        ss, rstd = st
        p.act(junk[:], xx[:], AF.Square, [xx], [junk, ss], accum_out=ss[:])
        p.act(rstd[:], ss[:], AF.Sqrt, [ss], [rstd], scale=1.0 / D, bias=EPS)
        p.op("dve", lambda e: e.reciprocal(rstd[:], rstd[:]), [rstd], [rstd])
        p.op("dve", lambda e: e.scalar_tensor_tensor(h[:], xx[:], rstd[:], Arow, ALU.mult, ALU.mult),
             [xx, rstd, modt], [h])
        p.tt("dve", h[:], h[:], Brow, ALU.add, [h, modt], [h])
        for half in range(2):
            pt = cm.psb[half]
            for j in range(4):
                kc = half * 4 + j
                p.op("pe", lambda e: e.transpose(pt[:, j * 128:(j + 1) * 128], h[:, kc * 128:(kc + 1) * 128], cm.identf[:]),
                     [h, cm.identf], [pt])
            p.copy("act", hT[:, half * 4:(half + 1) * 4, t * 128:(t + 1) * 128],
                   pt[:].rearrange("q (a b) -> q a b", a=4), [pt], [hT])
            if hTf is not None:
                p.copy("dve", hTf[:, half * 4:(half + 1) * 4, t * 128:(t + 1) * 128],
                       pt[:].rearrange("q (a b) -> q a b", a=4), [pt], [hTf])


DBG_STOP = None
DBG_LVL = 0
DBG_SKIP = ''


def build_l1(ctx=None):
    ctx = stage_begin(ctx)
    nc, p, cm = ctx.nc, ctx.p, ctx.cm
    xs = dram_in(nc, "xs", [T, D])
    ctxb = dram_in(nc, "ctxb", [TCTX, D])
    ccol = dram_in(nc, "ccol", [128, 8])
    cctx = dram_in(nc, "cctx", [128, 8])
    w_ada = dram_in(nc, "w_ada0", [D, 6 * D])
    b_ada = dram_in(nc, "b_ada0", [1, 6 * D])
    g_mix = dram_in(nc, "g_mix0", [1, D])
    w_hin = dram_in(nc, "w_hin", [D, 5 * D])
    gam = dram_in(nc, "gam", [128, 2, 2, 8])
    maskf_d = dram_in(nc, "maskf", [64, 64])
    maskb_d = dram_in(nc, "maskb", [64, 64])
    rm_d = dram_in(nc, "rm", [128, 512])
    o_d = [dram_out(nc, "o_f", [T, D]), dram_out(nc, "o_b", [T, D])]
    sg_d = dram_out(nc, "sg", [T, D], BF16)
    Q_d = [dram_out(nc, "QF", [NH, 128, T], BF16), dram_out(nc, "QB", [NH, 128, T], BF16)]
    Sloc_d = dram_out(nc, "Sloc", [2, NH, 128, 128])
    Sctx_d = dram_out(nc, "Sctx", [2, NH, 128, 128])
    Dloc_d = dram_out(nc, "Dloc", [128, 16])

    maskt = [p.sb("maskf", [64, 64], F32), p.sb("maskb", [64, 64], F32)]
    rm = p.sb("rm", [128, 512], F32)
    p.dma("sp", maskt[0][:], maskf_d, writes=[maskt[0]])
    p.dma("sp", maskt[1][:], maskb_d, writes=[maskt[1]])
    p.dma("sp", rm[:], rm_d, writes=[rm])

    gm = p.sb("gm", [128, 2, 2, 8], F32)
    lb = p.sb("lb", [128, 16], F32)
    oml = p.sb("oml", [128, 16], F32)
    p.dma("sp", gm[:], gam, writes=[gm])
    for d in range(2):
        p.tt("dve", lb[:, d * 8:(d + 1) * 8], gm[:, d, 0, :], gm[:, d, 1, :], ALU.subtract, [gm], [lb])
    p.act(lb[:], lb[:], AF.Sigmoid, [lb], [lb])
    p.ts("dve", oml[:], lb[:], -1.0, 1.0, ALU.mult, ALU.add, [lb], [oml])

    if DBG_LVL == 1:
        p.finish([])
        return nc, p
    modb = p.sb("modb", [128, 2048], F32)
    modc = p.sb("modc", [128, 2048], F32)
    emit_mods(p, cm, w_ada, b_ada, 0, 2048, [(ccol, modb), (cctx, modc)], "a")
    gmix = p.sb("gmix", [128, D], F32)
    p.dma("sp", gmix[:], g_mix.partition_broadcast(128), writes=[gmix])
    for m in (modb, modc):
        p.op("dve", lambda e: e.scalar_tensor_tensor(m[:, 1024:2048], m[:, 1024:2048], 1.0, gmix[:], ALU.add, ALU.mult),
             [m, gmix], [m])

    if DBG_LVL == 2:
        p.finish([])
        return nc, p
    hT = p.sb("hT", [128, 8, T], BF16)
    hTc = p.sb("hTc", [128, 8, TCTX], BF16)
    xt = [p.sb("xt%d" % i, [128, D], F32) for i in range(2)]
    h = p.sb("h", [128, D], F32)
    junk = h
    st = (p.sb("ss", [128, 1], F32), p.sb("rstd", [128, 1], F32))
    emit_norm_T(p, cm, ctxb, TCTX // 128, modc[:, 1024:2048], modc[:, 0:1024], modc, hTc, None, xt, junk, h, st)
    if DBG_LVL == 3:
        p.finish([])
        return nc, p
    emit_norm_T(p, cm, xs, (T // 128) if DBG_LVL != 4 else 4, modb[:, 1024:2048], modb[:, 0:1024], modb, hT, None, xt, junk, h, st)
    if DBG_LVL in (4, 5):
        p.finish([])
        return nc, p

    col0 = [0, 2048, 3072, 1024, 4096]
    whf = [p.sb("whf%d" % i, [128, 8, 128], F32) for i in range(2)]
    whb = [p.sb("whb%d" % i, [128, 8, 5, 128], BF16) for i in range(2)]

    def blk_tiles(i):
        s = "_%d" % i
        return dict(
            q=p.sb("q" + s, [128, 512], F32), F=p.sb("F" + s, [128, 512], F32),
            LF=p.sb("LF" + s, [128, 512], F32), U=p.sb("U" + s, [128, 512], F32),
            E=p.sb("E" + s, [128, 512], F32), Bc=p.sb("Bc" + s, [128, 512], F32),
            BL=p.sb("BL" + s, [128, 8], F32), DEC=p.sb("DEC" + s, [128, 8], F32),
            QG=p.sb("QG" + s, [128, 512], BF16), KD=p.sb("KD" + s, [128, 512], BF16),
            KGT=p.sb("KGT" + s, [128, 512], BF16), QX=p.sb("QX" + s, [128, 512], BF16),
            kg=p.sb("kg" + s, [64, 8, 128], BF16), v=p.sb("v" + s, [64, 8, 128], BF16),
            sgt=p.sb("sgt" + s, [64, 8, 128], BF16), ob=p.sb("ob" + s, [64, 8, 128], F32),
            tc=p.sb("tc" + s, [128, 1], F32), sgs=p.sb("sgs" + s, [64, 2, 128], F32), sgg=p.sb("sgg" + s, [64, 2, 128], F32))

    bt = [blk_tiles(0), blk_tiles(1)]
    S32 = p.sb("S32", [128, 128], F32)
    Sb = p.sb("Sb", [128, 128], BF16)
    ATs = p.sb("ATs", [64, 64], BF16)
    Dcol = p.sb("Dcol", [128, 16], F32)
    psq, psz, psv, psA, pso, psS = cm.psb[0], cm.psb[1], cm.psb[2], cm.psb[3], cm.psb[4], cm.psb[5]
    psT = cm.psT
    bcount = 0

    for hh in range(NH):
        if DBG_STOP is not None and hh >= DBG_STOP[0]:
            break
        wb = whb[hh % 2]
        for s in range(5):
            c0 = col0[s] + hh * 128
            wf_ = whf[s % 2]
            p.dma("sp", wf_[:], w_hin[:, c0:c0 + 128].rearrange("(kc q) n -> q kc n", q=128), writes=[wf_])
            p.copy("pool", wb[:, :, s, :], wf_[:], [wf_], [wb])
        for (is_ctx, hTs, TT) in ((True, hTc, TCTX), (False, hT, T)):
            nb_tok = 256 if is_ctx else 512
            nblk = TT // nb_tok
            nch = nb_tok // 64
            for d in range(2):
                if DBG_STOP is not None and (is_ctx, d) == DBG_STOP[1]:
                    break
                col = d * 8 + hh
                blocks = list(range(nblk)) if d == 0 else list(range(nblk - 1, -1, -1))
                carry = None
                first = True
                for blk in blocks:
                    b = bt[bcount % 2]
                    bcount += 1
                    t0 = blk * nb_tok
                    n = nb_tok
                    q, Ft, LF, U, E, Bc = b["q"], b["F"], b["LF"], b["U"], b["E"], b["Bc"]
                    for kc in range(8):
                        p.mm(psq[:, 0:n], wb[:, kc, 0, :], hTs[:, kc, t0:t0 + n], kc == 0, kc == 7, [wb, hTs], [psq])
                    p.copy("act", q[:, 0:n], psq[:, 0:n], [psq], [q])
                    for kc in range(8):
                        p.mm(psz[:, 0:n], wb[:, kc, 1 + d, :], hTs[:, kc, t0:t0 + n], kc == 0, kc == 7, [wb, hTs], [psz])
                    p.act(Ft[:, 0:n], psz[:, 0:n], AF.Sigmoid, [psz], [Ft])
                    for c2 in range(nch // 2):
                        for j in range(2):
                            c = c2 * 2 + j
                            for kc in range(8):
                                p.mm(psv[0:64, j * 256:(j + 1) * 256], hTs[:, kc, t0 + c * 64:t0 + (c + 1) * 64],
                                     wb[:, kc, 3:5, :].rearrange("q a b -> q (a b)"), kc == 0, kc == 7, [wb, hTs], [psv])
                        pv = psv[0:64, :].rearrange("q (j x) -> q j x", j=2)
                        p.copy("dve", b["v"][:, c2 * 2:c2 * 2 + 2, :], pv[:, :, 0:128], [psv], [b["v"]])
                        if d == 0 and not is_ctx:
                            p.copy("dve", b["sgg"][:, 0:2, :], pv[:, :, 128:256], [psv], [b["sgg"]])
                            p.act(b["sgs"][:, 0:2, :], b["sgg"][:, 0:2, :], AF.Sigmoid, [b["sgg"]], [b["sgs"]])
                            p.tt("pool", b["sgt"][:, c2 * 2:c2 * 2 + 2, :], b["sgg"][:, 0:2, :], b["sgs"][:, 0:2, :], ALU.mult,
                                 [b["sgg"], b["sgs"]], [b["sgt"]])
                    if d == 0 and not is_ctx and "C" not in DBG_SKIP:
                        p.dma("sp", sg_d[t0:t0 + n, hh * 128:(hh + 1) * 128].rearrange("(c q) v -> q c v", q=64),
                              b["sgt"][:, 0:nch, :], reads=[b["sgt"]], writes=["sg_d"])
                    if DBG_LVL == 6:
                        p.finish([]); return nc, p
                    p.ts("dve", Ft[:, 0:n], Ft[:, 0:n], oml[:, col:col + 1], lb[:, col:col + 1], ALU.mult, ALU.add,
                         [Ft, oml, lb], [Ft])
                    p.act(LF[:, 0:n], Ft[:, 0:n], AF.Ln, [Ft], [LF])
                    p.ts("pool", Ft[:, 0:n], Ft[:, 0:n], -1.0, 1.0, ALU.mult, ALU.add, [Ft], [Ft])
                    p.op("dve", lambda e: e.tensor_tensor_scan(U[:, 0:n], rm[:, 0:n], LF[:, 0:n], 0.0, ALU.mult, ALU.add),
                         [rm, LF], [U])
                    U3 = U[:, 0:n].rearrange("q (c k) -> q c k", k=64)
                    p.copy("pool", b["BL"][:, 0:nch].rearrange("q (c o) -> q c o", o=1), U3[:, :, 63:64], [U], [b["BL"]])
                    p.act(b["DEC"][:, 0:nch], b["BL"][:, 0:nch], AF.Exp, [b["BL"]], [b["DEC"]])
                    if not is_ctx and "B" not in DBG_SKIP:
                        init = 0.0 if (carry is None or d == 1) else carry[0]
                        rdc = [] if (carry is None or d == 1) else [carry[1]]
                        p.op("dve", lambda e: e.tensor_tensor_scan(Bc[:, 0:n], cm.ones5[:, 0:n], LF[:, 0:n], init, ALU.mult, ALU.add),
                             [cm.ones5, LF] + rdc, [Bc])
                        if d == 0:
                            carry = (Bc[:, n - 1:n], Bc)
                        else:
                            if carry is None:
                                p.copy("pool", b["tc"][:], Bc[:, n - 1:n], [Bc], [b["tc"]])
                            else:
                                p.tt("pool", b["tc"][:], Bc[:, n - 1:n], carry[0], ALU.add, [Bc, carry[1]], [b["tc"]])
                            carry = (b["tc"][:], b["tc"])
                            p.tt("dve", Bc[:, 0:n], LF[:, 0:n], Bc[:, 0:n], ALU.subtract, [LF, Bc], [Bc])
                            p.ts("dve", Bc[:, 0:n], Bc[:, 0:n], b["tc"][:], None, ALU.add, None, [Bc, b["tc"]], [Bc])
                        p.act(E[:, 0:n], Bc[:, 0:n], AF.Exp, [Bc], [E])
                        p.tt("pool", b["QX"][:, 0:n], q[:, 0:n], E[:, 0:n], ALU.mult, [q, E], [b["QX"]])
                        p.dma("sp", Q_d[d][hh, :, t0:t0 + n], b["QX"][:, 0:n], reads=[b["QX"]], writes=["Q_d%d" % d])
                        if d == 1 and carry is not None:
                            pass
                    if d == 1:
                        p.tt("dve", U[:, 0:n], LF[:, 0:n], U[:, 0:n], ALU.subtract, [LF, U], [U])
                        p.tt("dve", U3, U3, b["BL"][:, 0:nch].rearrange("q (c o) -> q c o", o=1).to_broadcast([128, nch, 64]),
                             ALU.add, [U, b["BL"]], [U])
                    p.act(E[:, 0:n], U[:, 0:n], AF.Exp, [U], [E])
                    p.tt("dve", b["QG"][:, 0:n], q[:, 0:n], E[:, 0:n], ALU.mult, [q, E], [b["QG"]])
                    p.act(LF[:, 0:n], U[:, 0:n], AF.Exp, [U], [LF], scale=-1.0)
                    p.tt("pool", b["KD"][:, 0:n], Ft[:, 0:n], LF[:, 0:n], ALU.mult, [Ft, LF], [b["KD"]])
                    p.tt("dve", b["KGT"][:, 0:n].rearrange("q (c k) -> q c k", k=64),
                         b["KD"][:, 0:n].rearrange("q (c k) -> q c k", k=64),
                         b["DEC"][:, 0:nch].rearrange("q (c o) -> q c o", o=1).to_broadcast([128, nch, 64]),
                         ALU.mult, [b["KD"], b["DEC"]], [b["KGT"]])
                    if DBG_LVL == 7:
                        p.finish([]); return nc, p
                    for c in range(nch):
                        p.op("pe", lambda e: e.transpose(psT[0:64, c * 128:(c + 1) * 128], b["KGT"][:, c * 64:(c + 1) * 64], cm.identb[:]),
                             [b["KGT"], cm.identb], [psT])
                    p.copy("act", b["kg"][:, 0:nch, :], psT[0:64, 0:nch * 128].rearrange("q (c k) -> q c k", k=128), [psT], [b["kg"]])
                    if DBG_LVL == 8:
                        p.finish([]); return nc, p
                    order = list(range(nch)) if d == 0 else list(range(nch - 1, -1, -1))
                    if "A" in DBG_SKIP:
                        order = []
                    for c in order:
                        cs = slice(c * 64, (c + 1) * 64)
                        p.mm(psA[0:64, 0:64], b["KD"][:, cs], b["QG"][:, cs], True, True, [b["KD"], b["QG"]], [psA])
                        p.tt("dve", ATs[:], psA[0:64, 0:64], maskt[d][:], ALU.mult, [psA, maskt[d]], [ATs])
                        if DBG_LVL == 9:
                            continue
                        p.mm(pso[0:64, 0:128], ATs[:], b["v"][:, c, :], True, first, [ATs, b["v"]], [pso])
                        if not first:
                            p.mm(pso[0:64, 0:128], b["QG"][:, cs], Sb[:], False, True, [b["QG"], Sb], [pso])
                        if not is_ctx:
                            p.copy("act", b["ob"][:, c, :], pso[0:64, 0:128], [pso], [b["ob"]])
                        if DBG_LVL == 10:
                            first = False
                            continue
                        p.mm(psS[:, 0:128], b["kg"][:, c, :], b["v"][:, c, :], True, True, [b["kg"], b["v"]], [psS])
                        if first:
                            p.copy("dve", S32[:], psS[:, 0:128], [psS], [S32])
                        else:
                            p.op("dve", lambda e: e.scalar_tensor_tensor(S32[:], S32[:], b["DEC"][:, c:c + 1], psS[:, 0:128], ALU.mult, ALU.add),
                                 [S32, b["DEC"], psS], [S32])
                        p.copy("pool", Sb[:], S32[:], [S32], [Sb])
                        first = False
                    if not is_ctx and "D" not in DBG_SKIP:
                        p.dma("sp", o_d[d][t0:t0 + n, hh * 128:(hh + 1) * 128].rearrange("(c q) v -> q c v", q=64),
                              b["ob"][:, 0:nch, :], reads=[b["ob"]], writes=["o_d%d" % d])
                if is_ctx:
                    p.dma("sp", Sctx_d[d, hh], S32[:], reads=[S32], writes=["Sctx_d"])
                else:
                    p.dma("sp", Sloc_d[d, hh], S32[:], reads=[S32], writes=["Sloc_d"])
                    if "B" not in DBG_SKIP:
                        p.act(Dcol[:, col:col + 1], carry[0], AF.Exp, [carry[1]], [Dcol])
    p.dma("sp", Dloc_d, Dcol[:], reads=[Dcol], writes=["Dloc_d"])
    return stage_end(ctx, ["sg_d", "Q_d0", "Q_d1", "o_d0", "o_d1", "Sctx_d", "Sloc_d", "Dloc_d"])


def consts():
    s = np.arange(64)[:, None]
    t = np.arange(64)[None, :]
    rm = np.ones((128, 512), np.float32)
    rm[:, ::64] = 0.0
    return {"ident": np.eye(128, dtype=np.float32),
            "maskf": (s <= t).astype(np.float32), "maskb": (s >= t).astype(np.float32), "rm": rm}


def col_layout(v):
    return np.ascontiguousarray(np.asarray(v, np.float32).reshape(8, 128).T)


def l1_inputs(inp, core):
    b, j = core // 4, core % 4
    m = dict(consts())
    m["xs"] = np.ascontiguousarray(inp["x"][b, j * T:(j + 1) * T])
    m["ctxb"] = np.ascontiguousarray(inp["ctx"][b])
    m["ccol"] = col_layout(inp["c"][b])
    m["cctx"] = col_layout(inp["c_ctx"])
    m["w_ada0"] = np.ascontiguousarray(inp["w_ada"][0])
    m["b_ada0"] = np.ascontiguousarray(inp["b_ada"][0][None, :])
    m["g_mix0"] = np.ascontiguousarray(inp["g_mix"][0][None, :])
    m["w_hin"] = np.ascontiguousarray(inp["w_hgrn_in"][0])
    g = np.asarray(inp["hgrn_gamma"], np.float32).reshape(2, 2, 8, 128)
    m["gam"] = np.ascontiguousarray(g.transpose(3, 0, 1, 2))
    return m


def emit_ffn(p, cm, x1_d, x1_key, xo_d, xo_key, modt, mo, gffn_d, wr_d, br_d, w1_d, b1c_d, w2_d, b2_d, tl, final_g=None):
    NQ = NQ_FFN
    TQ = T // NQ
    hfT = tl["hfT"]
    xt, h, st = tl["xt"], tl["h"], tl["st"]
    gf = h
    p.dma("sp", gf[:], gffn_d.partition_broadcast(128), writes=[gf])
    p.op("dve", lambda e: e.scalar_tensor_tensor(modt[:, mo + 1024:mo + 2048], modt[:, mo + 1024:mo + 2048], 1.0, gf[:], ALU.add, ALU.mult),
         [modt, gf], [modt])
    if DBG_LVL == 16:
        return
    wr = p.sb("wr", [128, 8, NE], F32)
    brb = p.sb("brb", [128, NE], F32)
    p.dma("sp", wr[:], wr_d.rearrange("(kc q) n -> q kc n", q=128), writes=[wr])
    p.dma("sp", brb[:], br_d.partition_broadcast(128), writes=[brb])
    G = p.sb("G_all", [128, T // 128, NE], F32)
    GT = p.sb("GT_q", [NE, T // NQ_FFN], F32)
    b2s = p.sb("b2s", [NE, D], F32)
    b1c = p.sb("b1c", [128, NE, 16], F32)
    p.dma("sp", b2s[:], b2_d, writes=[b2s])
    p.dma("sp", b1c[:], b1c_d, writes=[b1c])
    if DBG_LVL == 17:
        return
    hTf = p.sb("hTf", [128, 8, 128], F32)
    lg = p.sb("lg", [128, NE], F32)
    m8 = p.sb("m8", [128, 8], F32)
    nmx = p.sb("nmx", [128, 1], F32)
    msk = p.sb("msk", [128, NE], F32)
    ex = p.sb("ex", [128, NE], F32)
    sm = p.sb("sm", [128, 1], F32)
    ps_l = cm.psb[2]
    ss, rstd = st
    for t in range(T // 128):
        xx = xt[t % 2]
        p.dma("sp", xx[:], x1_d[t * 128:(t + 1) * 128, :], reads=[x1_key], writes=[xx])
        p.act(h[:], xx[:], AF.Square, [xx], [h, ss], accum_out=ss[:])
        p.act(rstd[:], ss[:], AF.Sqrt, [ss], [rstd], scale=1.0 / D, bias=EPS)
        p.op("dve", lambda e: e.reciprocal(rstd[:], rstd[:]), [rstd], [rstd])
        p.op("dve", lambda e: e.scalar_tensor_tensor(h[:], xx[:], rstd[:], modt[:, mo + 1024:mo + 2048], ALU.mult, ALU.mult),
             [xx, rstd, modt], [h])
        p.tt("dve", h[:], h[:], modt[:, mo:mo + 1024], ALU.add, [h, modt], [h])
        for half in range(2):
            pt = cm.psb[half]
            for j in range(4):
                kc = half * 4 + j
                p.op("pe", lambda e: e.transpose(pt[:, j * 128:(j + 1) * 128], h[:, kc * 128:(kc + 1) * 128], cm.identf[:]),
                     [h, cm.identf], [pt])
            p.copy("act", hfT[:, half * 4:(half + 1) * 4, t * 128:(t + 1) * 128],
                   pt[:].rearrange("q (a b) -> q a b", a=4), [pt], [hfT])
            p.copy("dve", hTf[:, half * 4:(half + 1) * 4, :], pt[:].rearrange("q (a b) -> q a b", a=4), [pt], [hTf])
        if DBG_LVL == 20:
            return
        for kc in range(8):
            p.mm(ps_l[:, 0:NE], hTf[:, kc, :], wr[:, kc, :], kc == 0, kc == 7, [hTf, wr], [ps_l])
        p.tt("dve", lg[:], ps_l[:, 0:NE], brb[:], ALU.add, [ps_l, brb], [lg])
        if DBG_LVL == 19:
            return
        p.op("dve", lambda e: e.max(m8[:], lg[:]), [lg], [m8])
        p.ts("dve", msk[:], lg[:], m8[:, 3:4], None, ALU.is_ge, None, [lg, m8], [msk])
        p.ts("dve", nmx[:], m8[:, 0:1], -1.0, None, ALU.mult, None, [m8], [nmx])
        if DBG_LVL == 18:
            return
        p.act(ex[:], lg[:], AF.Exp, [lg, nmx], [ex], bias=nmx[:])
        p.tt("dve", ex[:], ex[:], msk[:], ALU.mult, [ex, msk], [ex])
        p.op("dve", lambda e: e.tensor_reduce(sm[:], ex[:], AX.X, ALU.add), [ex], [sm])
        p.op("dve", lambda e: e.reciprocal(sm[:], sm[:]), [sm], [sm])
        p.ts("dve", G[:, t, :], ex[:], sm[:], None, ALU.mult, None, [ex, sm], [G])
        if DBG_LVL == 21:
            return
    if DBG_LVL == 22:
        return
    yacc = tl["yacc"]
    actT = tl["actT"]
    w2b = tl["w2b"]
    wst = tl["wst"]
    w2st = tl["w2st"]
    wgb = tl["wgb"]
    wub = tl["wub"]
    tg, tsg, tu = tl["tg"], tl["tsg"], tl["tu"]
    psg, psu, psy = cm.psb[3], cm.psb[4], cm.psb[5]
    cnt = 0
    for qd in range(NQ):
        tq0 = qd * TQ
        for ti in range(TQ // 128):
            tg_ = qd * (TQ // 128) + ti
            p.op("pe", lambda e: e.transpose(ps_l[0:NE, 128:256], G[:, tg_, :], cm.identf[:]), [G, cm.identf], [ps_l])
            p.copy("dve", GT[:, ti * 128:(ti + 1) * 128], ps_l[0:NE, 128:256], [ps_l], [GT])
        for ti in range(TQ // 128):
            tg_ = qd * (TQ // 128) + ti
            for dh in range(2):
                p.mm(psy[:], GT[:, ti * 128:(ti + 1) * 128], b2s[:, dh * 512:(dh + 1) * 512], True, True, [GT, b2s], [psy])
                p.copy("act", yacc[:, ti, dh * 512:(dh + 1) * 512], psy[:], [psy], [yacc])
        if DBG_LVL == 23:
            return
        for ex_ in range(NE):
            if DBG_LVL == 24 and ex_ == 1:
                return
            for j in range(8):
                k = cnt % 2
                cnt += 1
                p.dma("sp", wst[k][:], w1_d[ex_, :, j * 128:(j + 1) * 128].rearrange("(kc q) n -> q kc n", q=128), writes=[wst[k]])
                p.copy("pool", wgb[k][:], wst[k][:], [wst[k]], [wgb[k]])
                p.dma("sp", wst[k][:], w1_d[ex_, :, 1024 + j * 128:1024 + (j + 1) * 128].rearrange("(kc q) n -> q kc n", q=128), writes=[wst[k]])
                p.copy("pool", wub[k][:], wst[k][:], [wst[k]], [wub[k]])
                p.dma("sp", w2st[k][:], w2_d[ex_, j * 128:(j + 1) * 128, :], writes=[w2st[k]])
                p.copy("act", w2b[:, j, :], w2st[k][:], [w2st[k]], [w2b])
                for tb in range(TQ // 512):
                    tok = slice(tq0 + tb * 512, tq0 + (tb + 1) * 512)
                    for kc in range(8):
                        p.mm(psg[:], wgb[k][:, kc, :], hfT[:, kc, tok], kc == 0, kc == 7, [wgb[k], hfT], [psg])
                    for kc in range(8):
                        p.mm(psu[:], wub[k][:, kc, :], hfT[:, kc, tok], kc == 0, kc == 7, [wub[k], hfT], [psu])
                    p.ts("dve", tg[:], psg[:], b1c[:, ex_, j:j + 1], 7.0, ALU.add, ALU.min, [psg, b1c], [tg])
                    p.act(tsg[:], tg[:], AF.Sigmoid, [tg], [tsg], scale=1.702)
                    p.ts("dve", tu[:], psu[:], b1c[:, ex_, 8 + j:9 + j], 7.0, ALU.add, ALU.min, [psu, b1c], [tu])
                    p.ts("pool", tu[:], tu[:], -7.0, 1.0, ALU.max, ALU.add, [tu], [tu])
                    p.tt("pool", tg[:], tg[:], tsg[:], ALU.mult, [tg, tsg], [tg])
                    p.tt("dve", actT[:, j, tb * 512:(tb + 1) * 512], tg[:], tu[:], ALU.mult, [tg, tu], [actT])
            for ti in range(TQ // 128):
                tg_ = qd * (TQ // 128) + ti
                for dh in range(2):
                    for j in range(8):
                        p.mm(psy[:], actT[:, j, ti * 128:(ti + 1) * 128], w2b[:, j, dh * 512:(dh + 1) * 512], j == 0, j == 7,
                             [actT, w2b], [psy])
                    ya = yacc[:, ti, dh * 512:(dh + 1) * 512]
                    p.op("dve", lambda e: e.scalar_tensor_tensor(ya, psy[:], G[:, tg_, ex_:ex_ + 1], ya, ALU.mult, ALU.add),
                         [psy, G, yacc], [yacc])
        for ti in range(TQ // 128):
            tg_ = qd * (TQ // 128) + ti
            xx = xt[ti % 2]
            p.dma("sp", xx[:], x1_d[tg_ * 128:(tg_ + 1) * 128, :], reads=[x1_key], writes=[xx])
            p.tt("dve", yacc[:, ti, :], yacc[:, ti, :], modt[:, mo + 2048:mo + 3072], ALU.mult, [yacc, modt], [yacc])
            p.tt("dve", xx[:], xx[:], yacc[:, ti, :], ALU.add, [xx, yacc], [xx])
            if final_g is not None:
                p.act(h[:], xx[:], AF.Square, [xx], [h, ss], accum_out=ss[:])
                p.act(rstd[:], ss[:], AF.Sqrt, [ss], [rstd], scale=1.0 / D, bias=EPS)
                p.op("dve", lambda e: e.reciprocal(rstd[:], rstd[:]), [rstd], [rstd])
                p.op("dve", lambda e: e.scalar_tensor_tensor(xx[:], xx[:], rstd[:], final_g[:], ALU.mult, ALU.mult),
                     [xx, rstd, final_g], [xx])
            p.dma("sp", xo_d[tg_ * 128:(tg_ + 1) * 128, :], xx[:], reads=[xx], writes=[xo_key])


def ffn_tiles(p):
    TQ = T // NQ_FFN
    return dict(
        hfT=p.sb("hfT", [128, 8, T], BF16),
        xt=[p.sb("fxt%d" % i, [128, D], F32) for i in range(2)],
        h=p.sb("fh", [128, D], F32),
        st=(p.sb("fss", [128, 1], F32), p.sb("frstd", [128, 1], F32)),
        yacc=p.sb("yacc", [128, TQ // 128, D], F32),
        actT=p.sb("actT", [128, 8, TQ], BF16),
        w2b=p.sb("w2b", [128, 8, D], BF16),
        wst=[p.sb("wst%d" % i, [128, 8, 128], F32) for i in range(2)],
        w2st=[p.sb("w2st%d" % i, [128, D], F32) for i in range(1)] * 2,
        wgb=[p.sb("wgb%d" % i, [128, 8, 128], BF16) for i in range(2)],
        wub=[p.sb("wub%d" % i, [128, 8, 128], BF16) for i in range(2)],
        tg=p.sb("tg", [128, 512], F32), tsg=p.sb("tsg", [128, 512], F32), tu=p.sb("tu", [128, 512], F32))


def build_l2(ctx=None):
    do_ffn = False
    ctx = stage_begin(ctx)
    nc, p, cm = ctx.nc, ctx.p, ctx.cm
    xs = dram_in(nc, "xs", [T, D])
    o_f = dram_in(nc, "o_f", [T, D])
    o_b = dram_in(nc, "o_b", [T, D])
    sg = dram_in(nc, "sg", [T, D], BF16)
    QF = dram_in(nc, "QF", [NH, 128, T], BF16)
    QB = dram_in(nc, "QB", [NH, 128, T], BF16)
    SlocG = dram_in(nc, "SlocG", [8 * 2 * NH * 128, 128])
    DlocG = dram_in(nc, "DlocG", [8 * 128, 16])
    Sctx = dram_in(nc, "Sctx", [2, NH, 128, 128])
    fmask = dram_in(nc, "fmask", [128, 16])
    ccol = dram_in(nc, "ccol", [128, 8])
    w_ada = dram_in(nc, "w_ada0", [D, 6 * D])
    b_ada = dram_in(nc, "b_ada0", [1, 6 * D])
    gout = dram_in(nc, "gout", [1, D])
    w_out = dram_in(nc, "w_hout", [D, D])
    x1_d = nc.dram_tensor("x1_scr", [T, D], F32, kind="Internal").ap() if do_ffn else dram_out(nc, "x1", [T, D])
    x2_d = dram_out(nc, "x2", [T, D]) if do_ffn else None

    modm = p.sb("modm", [128, 4096], F32)
    emit_mods(p, cm, w_ada, b_ada, 2048, 6144, [(ccol, modm)], "b")

    fm = p.sb("fm", [128, 16], F32)
    DA = p.sb("DA", [128, 8, 16], F32)
    p.dma("sp", fm[:], fmask, writes=[fm])
    for i in range(8):
        p.dma("sp", DA[:, i, :], DlocG[i * 128:(i + 1) * 128, :], reads=["DlocG"], writes=[DA])
    Sin = p.sb("Sin", [128, 2, NH, 128], BF16)
    Sf = p.sb("Sfold", [128, 128], F32)
    Sl = [p.sb("Sl%d" % i, [128, 128], F32) for i in range(2)]
    tmp = p.sb("ftmp", [128, 128], F32)
    k = 0
    for d in range(2):
        for hh in range(NH):
            p.dma("sp", Sf[:], Sctx[d, hh], writes=[Sf])
            order = range(8) if d == 0 else range(7, -1, -1)
            for i in order:
                sl = Sl[k % 2]
                k += 1
                r0 = (i * 2 * NH + d * NH + hh) * 128
                p.dma("sp", sl[:], SlocG[r0:r0 + 128, :], reads=["SlocG"], writes=[sl])
                col = d * 8 + hh
                p.op("dve", lambda e: e.scalar_tensor_tensor(tmp[:], Sf[:], DA[:, i, col:col + 1], sl[:], ALU.mult, ALU.add),
                     [Sf, DA, sl], [tmp])
                p.tt("dve", tmp[:], tmp[:], Sf[:], ALU.subtract, [tmp, Sf], [tmp])
                p.op("dve", lambda e: e.scalar_tensor_tensor(Sf[:], tmp[:], fm[:, d * 8 + i:d * 8 + i + 1], Sf[:], ALU.mult, ALU.add),
                     [tmp, fm, Sf], [Sf])
            p.copy("pool", Sin[:, d, hh, :], Sf[:], [Sf], [Sin])

    goutb = p.sb("goutb", [128, D], F32)
    p.dma("sp", goutb[:], gout.partition_broadcast(128), writes=[goutb])
    wo = p.sb("wo", [128, 8, D], BF16)
    wos = [p.sb("wos%d" % i, [128, D], F32) for i in range(2)]
    for kc in range(8):
        p.dma("sp", wos[kc % 2][:], w_out[kc * 128:(kc + 1) * 128, :], writes=[wos[kc % 2]])
        p.copy("pool", wo[:, kc, :], wos[kc % 2][:], [wos[kc % 2]], [wo])

    xt = [p.sb("xt%d" % i, [128, D], F32) for i in range(2)]
    of_t = [p.sb("oft%d" % i, [128, D], F32) for i in range(2)]
    ob_t = [p.sb("obt%d" % i, [128, D], F32) for i in range(2)]
    sg_t = [p.sb("sgt%d" % i, [128, D], BF16) for i in range(2)]
    qf_t = [p.sb("qft%d" % i, [128, NH, 128], BF16) for i in range(2)]
    qb_t = [p.sb("qbt%d" % i, [128, NH, 128], BF16) for i in range(2)]
    sq = p.sb("sq", [128, D], F32)
    ssq = p.sb("ssq", [128, NH], F32)
    ogT = p.sb("ogT", [128, 8, 128], BF16)
    for t in range(T // 128):
        k = t % 2
        tok = slice(t * 128, (t + 1) * 128)
        p.dma("sp", xt[k][:], xs[tok, :], writes=[xt[k]])
        p.dma("sp", of_t[k][:], o_f[tok, :], writes=[of_t[k]])
        p.dma("sp", ob_t[k][:], o_b[tok, :], writes=[ob_t[k]])
        p.dma("sp", sg_t[k][:], sg[tok, :], writes=[sg_t[k]])
        p.dma("sp", qf_t[k][:], QF[:, :, tok].rearrange("h q t -> q h t"), writes=[qf_t[k]])
        p.dma("sp", qb_t[k][:], QB[:, :, tok].rearrange("h q t -> q h t"), writes=[qb_t[k]])
        o = of_t[k]
        p.tt("pool", o[:], o[:], ob_t[k][:], ALU.add, [o, ob_t[k]], [o])
        for half in range(2):
            ps = cm.psb[half]
            for j in range(4):
                hh = half * 4 + j
                p.mm(ps[:, j * 128:(j + 1) * 128], qf_t[k][:, hh, :], Sin[:, 0, hh, :], True, False, [qf_t[k], Sin], [ps])
                p.mm(ps[:, j * 128:(j + 1) * 128], qb_t[k][:, hh, :], Sin[:, 1, hh, :], False, True, [qb_t[k], Sin], [ps])
            p.tt("dve", o[:, half * 512:(half + 1) * 512], o[:, half * 512:(half + 1) * 512], ps[:], ALU.add, [o, ps], [o])
        p.tt("pool", sq[:], o[:], o[:], ALU.mult, [o], [sq])
        p.op("dve", lambda e: e.tensor_reduce(ssq[:], sq[:].rearrange("q (h v) -> q h v", h=NH), AX.X, ALU.add), [sq], [ssq])
        p.act(ssq[:], ssq[:], AF.Sqrt, [ssq], [ssq], scale=1.0 / 128, bias=EPS)
        p.op("dve", lambda e: e.reciprocal(ssq[:], ssq[:]), [ssq], [ssq])
        p.tt("dve", o[:].rearrange("q (h v) -> q h v", h=NH), o[:].rearrange("q (h v) -> q h v", h=NH),
             ssq[:].rearrange("q (h u) -> q h u", u=1).to_broadcast([128, NH, 128]), ALU.mult, [o, ssq], [o])
        p.tt("pool", o[:], o[:], goutb[:], ALU.mult, [o, goutb], [o])
        p.tt("dve", o[:], o[:], sg_t[k][:], ALU.mult, [o, sg_t[k]], [o])
        for half in range(2):
            pt = cm.psb[2 + half]
            for j in range(4):
                kc = half * 4 + j
                p.op("pe", lambda e: e.transpose(pt[:, j * 128:(j + 1) * 128], o[:, kc * 128:(kc + 1) * 128], cm.identf[:]),
                     [o, cm.identf], [pt])
            p.copy("act", ogT[:, half * 4:(half + 1) * 4, :], pt[:].rearrange("q (a b) -> q a b", a=4), [pt], [ogT])
        for dh in range(2):
            ps = cm.psb[4 + dh]
            for kc in range(8):
                p.mm(ps[:], ogT[:, kc, :], wo[:, kc, dh * 512:(dh + 1) * 512], kc == 0, kc == 7, [ogT, wo], [ps])
            p.tt("dve", sq[:, dh * 512:(dh + 1) * 512], ps[:], modm[:, dh * 512:(dh + 1) * 512], ALU.mult, [ps, modm], [sq])
        p.tt("pool", xt[k][:], xt[k][:], sq[:], ALU.add, [xt[k], sq], [xt[k]])
        p.dma("sp", x1_d[tok, :], xt[k][:], reads=[xt[k]], writes=["x1"])
    if do_ffn:
        tl = ffn_tiles(p)
        emit_ffn(p, cm, x1_d, "x1", x2_d, "x2", modm, 1024, gffn, wr_d, br_d, w1_d, b1c_d, w2_d, b2_d, tl)
    return stage_end(ctx, ["x1"])


def build_ffn(final, ctx=None, layer=0, in_name="x1", out_name="x2"):
    assert ctx is not None, "fused build only"
    nc, p, cm = ctx.nc, ctx.p, ctx.cm
    L = str(layer)
    x1_d = dram_in(nc, in_name, [T, D])
    ccol = dram_in(nc, "ccol", [128, 8])
    w_ada = dram_in(nc, "w_ada" + L, [D, 6 * D])
    b_ada = dram_in(nc, "b_ada" + L, [1, 6 * D])
    gffn_d = dram_in(nc, "g_ffn" + L, [1, D])
    wr_d = dram_in(nc, "w_router" + L, [D, NE])
    br_d = dram_in(nc, "b_router" + L, [1, NE])
    w1_d = dram_in(nc, "w_exp_in" + L, [NE, D, 2 * D])
    b1c_d = dram_in(nc, "b1c" + L, [128, NE, 16])
    w2_d = dram_in(nc, "w_exp_out" + L, [NE, D, D])
    b2_d = dram_in(nc, "b_exp_out" + L, [NE, D])
    gfin_d = dram_in(nc, "g_final", [1, D])
    xo_d = dram_out(nc, out_name, [T, D])
    hfT_scr = nc.dram_tensor("hfT_scr" + L, [8, 128, T], BF16, kind="Internal").ap()
    G_scr = nc.dram_tensor("G_scr" + L, [128, T // 128, NE], F32, kind="Internal").ap()
    gtf_scr = nc.dram_tensor("gtf_scr" + L, [1, D], F32, kind="Internal").ap()

    ctx.standalone = False
    p.begin_phase()
    modf = p.sb("modf", [128, 3072], F32)
    emit_mods(p, cm, w_ada, b_ada, 3072, 6144, [(ccol, modf)], "f")
    p.dma("sp", gtf_scr, modf[0:1, 2048:3072], reads=[modf], writes=["gtf_scr"])
    xt = [p.sb("fxt%d" % i, [128, D], F32) for i in range(2)]
    h = p.sb("fh", [128, D], F32)
    ss, rstd = p.sb("fss", [128, 1], F32), p.sb("frstd", [128, 1], F32)
    p.dma("sp", h[:], gffn_d.partition_broadcast(128), writes=[h])
    p.op("dve", lambda e: e.scalar_tensor_tensor(modf[:, 1024:2048], modf[:, 1024:2048], 1.0, h[:], ALU.add, ALU.mult),
         [modf, h], [modf])
    wr = p.sb("wr", [128, 8, NE], F32)
    brb = p.sb("brb", [128, NE], F32)
    p.dma("sp", wr[:], wr_d.rearrange("(kc q) n -> q kc n", q=128), writes=[wr])
    p.dma("sp", brb[:], br_d.partition_broadcast(128), writes=[brb])
    G = p.sb("G_all", [128, T // 128, NE], F32)
    hTf = p.sb("hTf", [128, 8, 128], F32)
    hTb = [p.sb("hTb%d" % i, [128, 8, 128], BF16) for i in range(2)]
    lg = p.sb("lg", [128, NE], F32)
    m8 = p.sb("m8", [128, 8], F32)
    nmx = p.sb("nmx", [128, 1], F32)
    msk = p.sb("msk", [128, NE], F32)
    ex = p.sb("ex", [128, NE], F32)
    sm = p.sb("sm", [128, 1], F32)
    ps_l = cm.psb[2]
    for t in range(T // 128):
        xx = xt[t % 2]
        hb = hTb[t % 2]
        p.dma("sp", xx[:], x1_d[t * 128:(t + 1) * 128, :], writes=[xx])
        p.act(h[:], xx[:], AF.Square, [xx], [h, ss], accum_out=ss[:])
        p.act(rstd[:], ss[:], AF.Sqrt, [ss], [rstd], scale=1.0 / D, bias=EPS)
        p.op("dve", lambda e: e.reciprocal(rstd[:], rstd[:]), [rstd], [rstd])
        p.op("dve", lambda e: e.scalar_tensor_tensor(h[:], xx[:], rstd[:], modf[:, 1024:2048], ALU.mult, ALU.mult),
             [xx, rstd, modf], [h])
        p.tt("dve", h[:], h[:], modf[:, 0:1024], ALU.add, [h, modf], [h])
        for half in range(2):
            pt = cm.psb[half]
            for j in range(4):
                kc = half * 4 + j
                p.op("pe", lambda e: e.transpose(pt[:, j * 128:(j + 1) * 128], h[:, kc * 128:(kc + 1) * 128], cm.identf[:]),
                     [h, cm.identf], [pt])
            p.copy("dve", hTf[:, half * 4:(half + 1) * 4, :], pt[:].rearrange("q (a b) -> q a b", a=4), [pt], [hTf])
        p.copy("pool", hb[:], hTf[:], [hTf], [hb])
        p.dma("sp", hfT_scr[:, :, t * 128:(t + 1) * 128].rearrange("k q t -> q k t"), hb[:], reads=[hb], writes=["hfT_scr"])
        for kc in range(8):
            p.mm(ps_l[:, 0:NE], hTf[:, kc, :], wr[:, kc, :], kc == 0, kc == 7, [hTf, wr], [ps_l])
        p.tt("dve", lg[:], ps_l[:, 0:NE], brb[:], ALU.add, [ps_l, brb], [lg])
        p.op("dve", lambda e: e.max(m8[:], lg[:]), [lg], [m8])
        p.ts("dve", msk[:], lg[:], m8[:, 3:4], None, ALU.is_ge, None, [lg, m8], [msk])
        p.ts("dve", nmx[:], m8[:, 0:1], -1.0, None, ALU.mult, None, [m8], [nmx])
        p.act(ex[:], lg[:], AF.Exp, [lg, nmx], [ex], bias=nmx[:])
        p.tt("dve", ex[:], ex[:], msk[:], ALU.mult, [ex, msk], [ex])
        p.op("dve", lambda e: e.tensor_reduce(sm[:], ex[:], AX.X, ALU.add), [ex], [sm])
        p.op("dve", lambda e: e.reciprocal(sm[:], sm[:]), [sm], [sm])
        p.ts("dve", G[:, t, :], ex[:], sm[:], None, ALU.mult, None, [ex, sm], [G])
    p.dma("sp", G_scr, G[:], reads=[G], writes=["G_scr"])
    p.end_phase()

    p.begin_phase()
    NPASS = 4
    TH = T // NPASS
    NB = TH // 512
    G = p.sb("G_all", [128, T // 128, NE], F32)
    gtf = p.sb("gtf", [128, D], F32)
    b2s = p.sb("b2s", [NE, D], F32)
    b1c = p.sb("b1c", [128, NE, 16], F32)
    p.dma("sp", G[:], G_scr, writes=[G])
    p.dma("sp", gtf[:], gtf_scr.partition_broadcast(128), writes=[gtf])
    p.dma("sp", b2s[:], b2_d, writes=[b2s])
    p.dma("sp", b1c[:], b1c_d, writes=[b1c])
    gfin = None
    if final:
        gfin = p.sb("gfin", [128, D], F32)
        p.dma("sp", gfin[:], gfin_d.partition_broadcast(128), writes=[gfin])
    yacc = p.sb("yacc", [128, TH // 128, D], F32)
    w1b = [p.sb("w1b%d" % i, [128, 8, 2 * D], BF16) for i in range(2)]
    w2b = [p.sb("w2b%d" % i, [128, 8, D], BF16) for i in range(2)]
    stg = [p.sb("stg%d" % i, [128, D], F32) for i in range(3)]
    hbk = [p.sb("hbk%d" % i, [128, 8, 512], BF16) for i in range(2)]
    actT = [p.sb("actT%d" % i, [128, 8, 512], BF16) for i in range(2)]
    tg = [p.sb("tg0", [128, 512], F32)]
    tsg = p.sb("tsg", [128, 512], F32)
    tu = [p.sb("tu0", [128, 512], F32)]
    xxk, hhk = stg[1], stg[0]
    xx, hh_ = stg[1][:, 0:D], stg[0][:, 0:D]
    ss, rstd = p.sb("fss", [128, 1], F32), p.sb("frstd", [128, 1], F32)
    GTt = p.sb("GTt", [NE, 128], F32)
    psg, psu, psy, ps_l = [cm.psb[0], cm.psb[1]], [cm.psb[2], cm.psb[3]], [cm.psb[4], cm.psb[5]], cm.psb[6]

    def pieces(e, k):
        out = []
        for kc in range(8):
            for hf in range(2):
                def d(sb_, kc=kc, hf=hf):
                    p.dma("sp", sb_[:], w1_d[e, kc * 128:(kc + 1) * 128, hf * D:(hf + 1) * D], writes=[sb_])
                def c(sb_, eng, kc=kc, hf=hf):
                    p.copy(eng, w1b[k][:, kc, hf * D:(hf + 1) * D], sb_[:], [sb_], [w1b[k]])
                out.append((d, c))
        for jj in range(8):
            def d(sb_, jj=jj):
                p.dma("sp", sb_[:], w2_d[e, jj * 128:(jj + 1) * 128, :], writes=[sb_])
            def c(sb_, eng, jj=jj):
                p.copy(eng, w2b[k][:, jj, :], sb_[:], [sb_], [w2b[k]])
            out.append((d, c))
        return out

    NPC = 24
    ROUNDS = NPC // (NB * 3)
    JPR = 8 // ROUNDS
    assert ROUNDS * NB * 3 == NPC and JPR * ROUNDS == 8
    nblk = 0
    ncast = 0
    njj = 0
    for half in range(NPASS):
        for ti in range(TH // 128):
            tg_ = half * (TH // 128) + ti
            p.op("pe", lambda e: e.transpose(ps_l[0:NE, 0:128], G[:, tg_, :], cm.identf[:]), [G, cm.identf], [ps_l])
            p.copy("dve", GTt[:], ps_l[0:NE, 0:128], [ps_l], [GTt])
            for dh in range(2):
                py = psy[dh]
                p.mm(py[:], GTt[:], b2s[:, dh * 512:(dh + 1) * 512], True, True, [GTt, b2s], [py])
                p.copy("act", yacc[:, ti, dh * 512:(dh + 1) * 512], py[:], [py], [yacc])
        for i, (d_, c_) in enumerate(pieces(0, 0)):
            sb_ = stg[i % 3]
            d_(sb_)
            c_(sb_, "pool" if i % 2 else "act")
        hb = hbk[nblk % 2]
        p.dma("sp", hb[:], hfT_scr[:, :, half * TH:half * TH + 512].rearrange("k q t -> q k t"), reads=["hfT_scr"], writes=[hb])
        for ex_ in range(NE):
            k = ex_ % 2
            nxt = pieces(ex_ + 1, 1 - k) if ex_ + 1 < NE else []
            pend = None
            for tb in range(NB):
                hb = hbk[nblk % 2]
                nblk += 1
                if tb + 1 < NB or ex_ + 1 < NE:
                    ntb = (tb + 1) % NB
                    hn = hbk[nblk % 2]
                    p.dma("sp", hn[:], hfT_scr[:, :, half * TH + ntb * 512:half * TH + (ntb + 1) * 512].rearrange("k q t -> q k t"),
                          reads=["hfT_scr"], writes=[hn])
                at = actT[tb % 2]
                for rd in range(ROUNDS):
                    mine = nxt[(tb * ROUNDS + rd) * 3:(tb * ROUNDS + rd + 1) * 3]
                    for i, (d_, c_) in enumerate(mine):
                        d_(stg[i])
                    for j in range(rd * JPR, (rd + 1) * JPR):
                        pg, pu = psg[njj % 2], psu[njj % 2]
                        g1, u1 = tg[0], tu[0]
                        njj += 1
                        for kc in range(8):
                            p.mm(pg[:], w1b[k][:, kc, j * 128:(j + 1) * 128], hb[:, kc, :], kc == 0, kc == 7, [w1b[k], hb], [pg])
                        for kc in range(8):
                            p.mm(pu[:], w1b[k][:, kc, D + j * 128:D + (j + 1) * 128], hb[:, kc, :], kc == 0, kc == 7, [w1b[k], hb], [pu])
                        p.ts("dve", g1[:], pg[:], b1c[:, ex_, j:j + 1], 7.0, ALU.add, ALU.min, [pg, b1c], [g1])
                        p.act(tsg[:], g1[:], AF.Sigmoid, [g1], [tsg], scale=1.702)
                        p.ts("dve", u1[:], pu[:], b1c[:, ex_, 8 + j:9 + j], 7.0, ALU.add, ALU.min, [pu, b1c], [u1])
                        p.ts("pool", u1[:], u1[:], -7.0, 1.0, ALU.max, ALU.add, [u1], [u1])
                        p.tt("pool", g1[:], g1[:], tsg[:], ALU.mult, [g1, tsg], [g1])
                        p.tt("dve", at[:, j, :], g1[:], u1[:], ALU.mult, [g1, u1], [at])
                    for i, (d_, c_) in enumerate(mine):
                        c_(stg[i], "pool" if ncast % 2 else "act")
                        ncast += 1
                todo = [pend] if pend is not None else []
                pend = (tb, at)
                if tb == NB - 1:
                    todo.append(pend)
                    pend = None
                for (tb2, at2) in todo:
                    for ti in range(4):
                        tl_ = tb2 * 4 + ti
                        tg_ = half * (TH // 128) + tl_
                        for dh in range(2):
                            py = psy[dh]
                            for j in range(8):
                                p.mm(py[:], at2[:, j, ti * 128:(ti + 1) * 128], w2b[k][:, j, dh * 512:(dh + 1) * 512], j == 0, j == 7,
                                     [at2, w2b[k]], [py])
                            ya = yacc[:, tl_, dh * 512:(dh + 1) * 512]
                            p.op("dve", lambda e: e.scalar_tensor_tensor(ya, py[:], G[:, tg_, ex_:ex_ + 1], ya, ALU.mult, ALU.add),
                                 [py, G, yacc], [yacc])
        for ti in range(TH // 128):
            tg_ = half * (TH // 128) + ti
            p.dma("sp", xx, x1_d[tg_ * 128:(tg_ + 1) * 128, :], writes=[xxk])
            p.tt("pool", yacc[:, ti, :], yacc[:, ti, :], gtf[:], ALU.mult, [yacc, gtf], [yacc])
            p.tt("dve", xx, xx, yacc[:, ti, :], ALU.add, [xxk, yacc], [xxk])
            if final:
                p.act(hh_, xx, AF.Square, [xxk], [hhk, ss], accum_out=ss[:])
                p.act(rstd[:], ss[:], AF.Sqrt, [ss], [rstd], scale=1.0 / D, bias=EPS)
                p.op("dve", lambda e: e.reciprocal(rstd[:], rstd[:]), [rstd], [rstd])
                p.op("dve", lambda e: e.scalar_tensor_tensor(xx, xx, rstd[:], gfin[:], ALU.mult, ALU.mult),
                     [xxk, rstd, gfin], [xxk])
            p.dma("sp", xo_d[tg_ * 128:(tg_ + 1) * 128, :], xx, reads=[xxk], writes=["xo"])
    p.end_phase()
    return nc, p


def ffn_inputs(inp, core, layer, x1):
    b = core // 4
    m = {"ident": np.eye(128, dtype=np.float32)}
    m["x1"] = np.ascontiguousarray(x1)
    m["ccol"] = col_layout(inp["c"][b])
    m["w_ada"] = np.ascontiguousarray(inp["w_ada"][layer])
    m["b_ada"] = np.ascontiguousarray(inp["b_ada"][layer][None, :])
    m["g_ffn"] = np.ascontiguousarray(inp["g_ffn"][layer][None, :])
    m["w_router"] = np.ascontiguousarray(inp["w_router"][layer])
    m["b_router"] = np.ascontiguousarray(inp["b_router"][layer][None, :])
    m["w_exp_in"] = np.ascontiguousarray(inp["w_exp_in"][layer])
    b1 = np.asarray(inp["b_exp_in"][layer], np.float32).reshape(NE, 16, 128)
    m["b1c"] = np.ascontiguousarray(b1.transpose(2, 0, 1))
    m["w_exp_out"] = np.ascontiguousarray(inp["w_exp_out"][layer])
    m["b_exp_out"] = np.ascontiguousarray(inp["b_exp_out"][layer])
    m["g_final"] = np.ascontiguousarray(inp["g_final"][None, :])
    return m


def l2_inputs(inp, core, r1):
    b, j = core // 4, core % 4
    m = {"ident": np.eye(128, dtype=np.float32)}
    m["xs"] = np.ascontiguousarray(inp["x"][b, j * T:(j + 1) * T])
    for k in ("o_f", "o_b", "sg", "QF", "QB", "Sctx"):
        m[k] = r1[core][k]
    m["SlocA"] = np.ascontiguousarray(np.stack([r1[b * 4 + i]["Sloc"] for i in range(4)]))
    m["DlocA"] = np.ascontiguousarray(np.stack([r1[b * 4 + i]["Dloc"] for i in range(4)]))
    fm = np.zeros((128, 8), np.float32)
    for i in range(4):
        fm[:, i] = 1.0 if i < j else 0.0
        fm[:, 4 + i] = 1.0 if i > j else 0.0
    m["fmask"] = fm
    m["ccol"] = col_layout(inp["c"][b])
    m["w_ada0"] = np.ascontiguousarray(inp["w_ada"][0])
    m["b_ada0"] = np.ascontiguousarray(inp["b_ada"][0][None, :])
    m["gout"] = np.ascontiguousarray(inp["g_hgrn_out"][0][None, :])
    m["w_hout"] = np.ascontiguousarray(inp["w_hgrn_out"][0])
    return m


HALO = 15 * 64


def build_conv_a(ctx=None):
    ctx = stage_begin(ctx)
    nc, p, cm = ctx.nc, ctx.p, ctx.cm
    x2 = dram_in(nc, "x2", [T, D])
    ccol = dram_in(nc, "ccol", [128, 8])
    w_ada = dram_in(nc, "w_ada1", [D, 6 * D])
    b_ada = dram_in(nc, "b_ada1", [1, 6 * D])
    g_mix = dram_in(nc, "g_mix1", [1, D])
    w_in = dram_in(nc, "w_cv_in", [D, 2 * D])
    bin_c = dram_in(nc, "bin_c", [128, 16])
    u_d = dram_out(nc, "u", [8, 128, T])
    gtm_d = dram_out(nc, "gtm", [1, D])
    modb = p.sb("modb", [128, 3072], F32)
    emit_mods(p, cm, w_ada, b_ada, 0, 3072, [(ccol, modb)], "c")
    p.dma("sp", gtm_d, modb[0:1, 2048:3072], reads=[modb], writes=["gtm"])
    gmix = p.sb("gmix", [128, D], F32)
    p.dma("sp", gmix[:], g_mix.partition_broadcast(128), writes=[gmix])
    p.op("dve", lambda e: e.scalar_tensor_tensor(modb[:, 1024:2048], modb[:, 1024:2048], 1.0, gmix[:], ALU.add, ALU.mult),
         [modb, gmix], [modb])
    hT = p.sb("hT", [128, 8, T], BF16)
    xt = [p.sb("xt%d" % i, [128, D], F32) for i in range(2)]
    h = p.sb("h", [128, D], F32)
    st = (p.sb("ss", [128, 1], F32), p.sb("rstd", [128, 1], F32))
    emit_norm_T(p, cm, x2, T // 128, modb[:, 1024:2048], modb[:, 0:1024], modb, hT, None, xt, h, h, st)
    bc = p.sb("bc", [128, 16], F32)
    p.dma("sp", bc[:], bin_c, writes=[bc])
    wst = [p.sb("wst%d" % i, [128, 8, 128], F32) for i in range(2)]
    wab = [p.sb("wab%d" % i, [128, 8, 128], BF16) for i in range(2)]
    wgb = [p.sb("wgb%d" % i, [128, 8, 128], BF16) for i in range(2)]
    sgt = [p.sb("sgt%d" % i, [128, 512], F32) for i in range(2)]
    ut = [p.sb("ut%d" % i, [128, 512], F32) for i in range(2)]
    psa, psg = cm.psb[2], cm.psb[3]
    n = 0
    for j in range(8):
        k = j % 2
        p.dma("sp", wst[0][:], w_in[:, j * 128:(j + 1) * 128].rearrange("(kc q) n -> q kc n", q=128), writes=[wst[0]])
        p.copy("pool", wab[k][:], wst[0][:], [wst[0]], [wab[k]])
        p.dma("sp", wst[1][:], w_in[:, 1024 + j * 128:1024 + (j + 1) * 128].rearrange("(kc q) n -> q kc n", q=128), writes=[wst[1]])
        p.copy("pool", wgb[k][:], wst[1][:], [wst[1]], [wgb[k]])
        for tb in range(T // 512):
            tok = slice(tb * 512, (tb + 1) * 512)
            for kc in range(8):
                p.mm(psa[:], wab[k][:, kc, :], hT[:, kc, tok], kc == 0, kc == 7, [wab[k], hT], [psa])
            for kc in range(8):
                p.mm(psg[:], wgb[k][:, kc, :], hT[:, kc, tok], kc == 0, kc == 7, [wgb[k], hT], [psg])
            sg_, u_ = sgt[n % 2], ut[n % 2]
            n += 1
            p.act(sg_[:], psg[:], AF.Sigmoid, [psg, bc], [sg_], bias=bc[:, 8 + j:9 + j])
            p.op("dve", lambda e: e.scalar_tensor_tensor(u_[:], psa[:], bc[:, j:j + 1], sg_[:], ALU.add, ALU.mult),
                 [psa, bc, sg_], [u_])
            p.dma("sp", u_d[j, :, tok], u_[:], reads=[u_], writes=["u_d"])
    if not ctx.standalone:
        hb = nc.dram_tensor("halo_b", [8 * 128, HALO], F32, kind="Internal").ap()
        REG["halo_b"] = hb
        for jj in range(4):
            p.dma("sp", hb[jj * 128:(jj + 1) * 128, :], u_d[4 + jj, :, T - HALO:T], reads=["u_d"], writes=["halo_b"])
            p.dma("sp", hb[(4 + jj) * 128:(5 + jj) * 128, :], u_d[4 + jj, :, 0:HALO], reads=["u_d"], writes=["halo_b"])
    return stage_end(ctx, ["u_d", "gtm"])


def conv_a_inputs(inp, core, x2):
    b = core // 4
    m = {"ident": np.eye(128, dtype=np.float32)}
    m["x2"] = np.ascontiguousarray(x2)
    m["ccol"] = col_layout(inp["c"][b])
    m["w_ada"] = np.ascontiguousarray(inp["w_ada"][1])
    m["b_ada"] = np.ascontiguousarray(inp["b_ada"][1][None, :])
    m["g_mix"] = np.ascontiguousarray(inp["g_mix"][1][None, :])
    m["w_cv_in"] = np.ascontiguousarray(inp["w_cv_in"][0])
    m["bin_c"] = np.ascontiguousarray(np.asarray(inp["b_cv_in"][0], np.float32).reshape(16, 128).T)
    return m


def build_conv_b(ctx=None):
    ctx = stage_begin(ctx)
    nc, p, cm = ctx.nc, ctx.p, ctx.cm
    x2 = dram_in(nc, "x2", [T, D])
    u_d = dram_in(nc, "u", [8, 128, T])
    G2 = dram_in(nc, "halo_g", [8 * 8 * 128, HALO])
    sel_d = dram_in(nc, "sel", [128, 16])
    gtm_d = dram_in(nc, "gtm", [1, D])
    wdw_d = dram_in(nc, "wdw", [128, 8, 31])
    cvec_d = dram_in(nc, "cvec", [128, 3, 8])
    wout_d = dram_in(nc, "w_cv_out", [D, D])
    bout_d = dram_in(nc, "b_cv_out", [1, D])
    x3_d = dram_out(nc, "x3", [T, D])
    wdw = p.sb("wdw", [128, 8, 31], F32)
    cvec = p.sb("cvec", [128, 3, 8], F32)
    gtm = p.sb("gtm", [128, D], F32)
    bout = p.sb("bout", [128, D], F32)
    sel = p.sb("sel", [128, 16], F32)
    p.dma("sp", sel[:], sel_d, writes=[sel])
    p.dma("sp", wdw[:], wdw_d, writes=[wdw])
    p.dma("sp", cvec[:], cvec_d, writes=[cvec])
    p.dma("sp", gtm[:], gtm_d.partition_broadcast(128), writes=[gtm])
    p.dma("sp", bout[:], bout_d.partition_broadcast(128), writes=[bout])
    wo = p.sb("wo", [128, 8, D], BF16)
    wos = p.sb("wos", [128, D], F32)
    for kc in range(8):
        p.dma("sp", wos[:], wout_d[kc * 128:(kc + 1) * 128, :], writes=[wos])
        p.copy("pool", wo[:, kc, :], wos[:], [wos], [wo])
    z = p.sb("z_all", [128, 8, T], F32)
    ue = p.sb("ue", [128, T + 2 * HALO], F32)
    for j in range(8):
        zj = z[:, j, :]
        zkey = ("z", j)
        if j < 4:
            p.dma("sp", ue[:, 0:T], u_d[j], writes=[ue])
            p.ts("dve", zj, ue[:, 0:T], wdw[:, j, 15:16], cvec[:, 0, j:j + 1], ALU.mult, ALU.add, [ue, wdw, cvec], [zkey])
            z3 = zj.rearrange("q (r w) -> q r w", w=64)
            u3 = ue[:, 0:T].rearrange("q (r w) -> q r w", w=64)
            for o in range(-15, 16):
                if o == 0:
                    continue
                lo, hi = max(0, -o), min(64, 64 - o)
                p.op("dve", lambda e: e.scalar_tensor_tensor(z3[:, :, lo:hi], u3[:, :, lo + o:hi + o], wdw[:, j, o + 15:o + 16],
                                                             z3[:, :, lo:hi], ALU.mult, ALU.add), [ue, wdw, zkey], [zkey])
        else:
            p.dma("sp", ue[:, HALO:HALO + T], u_d[j], writes=[ue])
            for side, (c0, dst) in enumerate(((0, ue[:, 0:HALO]), (8, ue[:, HALO + T:HALO + T + HALO]))):
                for r in range(8):
                    r0 = r * 1024 + ((0 if side == 0 else 4) + (j - 4)) * 128
                    stg = wos[:, 0:HALO]
                    p.dma("sp", stg, G2[r0:r0 + 128, :], reads=["halo_g"], writes=[wos])
                    if r == 0:
                        p.ts("dve", dst, stg, sel[:, c0:c0 + 1], None, ALU.mult, None, [wos, sel], [ue])
                    else:
                        p.op("dve", lambda e: e.scalar_tensor_tensor(dst, stg, sel[:, c0 + r:c0 + r + 1], dst, ALU.mult, ALU.add),
                             [wos, sel, ue], [ue])
            p.ts("dve", zj, ue[:, HALO:HALO + T], wdw[:, j, 15:16], cvec[:, 0, j:j + 1], ALU.mult, ALU.add, [ue, wdw, cvec], [zkey])
            for o in range(-15, 16):
                if o == 0:
                    continue
                a0 = HALO + o * 64
                p.op("dve", lambda e: e.scalar_tensor_tensor(zj, ue[:, a0:a0 + T], wdw[:, j, o + 15:o + 16], zj, ALU.mult, ALU.add),
                     [ue, wdw, zkey], [zkey])
    zkeys = [("z", j) for j in range(8)]
    sqt = [p.sb("sqt%d" % i, [128, 512], F32) for i in range(2)]
    mean = p.sb("mean", [128, 512], F32)
    msq = p.sb("msq", [128, 512], F32)
    rstd = p.sb("rstdv", [128, 512], F32)
    zn = [p.sb("zn0", [128, 512], F32)] * 2
    sgm = [p.sb("sgm0", [128, 512], F32)] * 2
    zs = p.sb("zs", [128, 8, 512], BF16)
    yt = wos
    ps1, ps2 = cm.psb[0], cm.psb[1]
    psy = [cm.psb[2], cm.psb[3]]
    n = 0
    for tb in range(T // 512):
        tok = slice(tb * 512, (tb + 1) * 512)
        for j in range(8):
            sq = sqt[n % 2]
            n += 1
            p.act(sq[:], z[:, j, tok], AF.Square, [zkeys[j]], [sq])
            p.mm(ps1[:], cm.ones[:], z[:, j, tok], j == 0, j == 7, [cm.ones, zkeys[j]], [ps1])
            p.mm(ps2[:], cm.ones[:], sq[:], j == 0, j == 7, [cm.ones, sq], [ps2])
        p.act(mean[:], ps1[:], AF.Copy, [ps1], [mean], scale=1.0 / D)
        p.tt("pool", msq[:], mean[:], mean[:], ALU.mult, [mean], [msq])
        p.op("dve", lambda e: e.scalar_tensor_tensor(rstd[:], ps2[:], 1.0 / D, msq[:], ALU.mult, ALU.subtract), [ps2, msq], [rstd])
        p.act(rstd[:], rstd[:], AF.Sqrt, [rstd], [rstd], bias=EPS)
        p.op("dve", lambda e: e.reciprocal(rstd[:], rstd[:]), [rstd], [rstd])
        for j in range(8):
            a, sg_ = zn[j % 2], sgm[j % 2]
            p.tt("dve", a[:], z[:, j, tok], mean[:], ALU.subtract, [zkeys[j], mean], [a])
            p.tt("pool", a[:], a[:], rstd[:], ALU.mult, [a, rstd], [a])
            p.ts("dve", a[:], a[:], cvec[:, 1, j:j + 1], cvec[:, 2, j:j + 1], ALU.mult, ALU.add, [a, cvec], [a])
            p.act(sg_[:], a[:], AF.Sigmoid, [a], [sg_])
            p.tt("pool", zs[:, j, :], a[:], sg_[:], ALU.mult, [a, sg_], [zs])
        for ti in range(4):
            t = tb * 4 + ti
            xa = ue[:, 0:D]
            p.dma("sp", xa, x2[t * 128:(t + 1) * 128, :], writes=[ue])
            for dh in range(2):
                ps = psy[dh]
                for j in range(8):
                    p.mm(ps[:], zs[:, j, ti * 128:(ti + 1) * 128], wo[:, j, dh * 512:(dh + 1) * 512], j == 0, j == 7, [zs, wo], [ps])
                p.tt("dve", yt[:, dh * 512:(dh + 1) * 512], ps[:], bout[:, dh * 512:(dh + 1) * 512], ALU.add, [ps, bout], [yt])
            p.tt("pool", yt[:], yt[:], gtm[:], ALU.mult, [yt, gtm], [yt])
            p.tt("dve", xa, xa, yt[:], ALU.add, [ue, yt], [ue])
            p.dma("sp", x3_d[t * 128:(t + 1) * 128, :], xa, reads=[ue], writes=["x3"])
    return stage_end(ctx, ["x3"])


def conv_b_inputs(inp, core, x2, ra):
    b, j = core // 4, core % 4
    m = {"ident": np.eye(128, dtype=np.float32)}
    m["x2"] = np.ascontiguousarray(x2)
    m["u"] = ra[core]["u"]
    zero = np.zeros((4, 128, HALO), np.float32)
    m["u_prev"] = np.ascontiguousarray(ra[core - 1]["u"][4:8, :, T - HALO:T]) if j > 0 else zero
    m["u_next"] = np.ascontiguousarray(ra[core + 1]["u"][4:8, :, 0:HALO]) if j < 3 else zero
    m["gtm"] = ra[core]["gtm"]
    w = np.asarray(inp["w_cv_dw"][0], np.float32)
    m["wdw"] = np.ascontiguousarray(w.reshape(31, 8, 128).transpose(2, 1, 0))
    cv = np.stack([np.asarray(inp[k][0], np.float32).reshape(8, 128).T for k in ("b_cv_dw", "g_cv_ln", "b_cv_ln")], axis=1)
    m["cvec"] = np.ascontiguousarray(cv)
    m["w_cv_out"] = np.ascontiguousarray(inp["w_cv_out"][0])
    m["b_cv_out"] = np.ascontiguousarray(inp["b_cv_out"][0][None, :])
    return m


_DBG = {}


def build_fused():
    REG.clear()
    FUSED[0] = True
    try:
        ctx = Ctx()
        nc, p = ctx.nc, ctx.p
        ctx.cm = Common(p, nc)
        build_l1(ctx)
        SlocG = nc.dram_tensor("SlocG", [8 * 2 * NH * 128, 128], F32, kind="Internal").ap()
        DlocG = nc.dram_tensor("DlocG", [8 * 128, 16], F32, kind="Internal").ap()
        REG["SlocG"], REG["DlocG"] = SlocG, DlocG
        p.allgather(REG["Sloc"].rearrange("d h k v -> (d h k) v"), SlocG, "Sloc_d", "SlocG")
        p.allgather(REG["Dloc"], DlocG, "Dloc_d", "DlocG")
        build_l2(ctx)
        build_ffn(False, ctx, 0, "x1", "x2")
        build_conv_a(ctx)
        halo_g = nc.dram_tensor("halo_g", [8 * 8 * 128, HALO], F32, kind="Internal").ap()
        REG["halo_g"] = halo_g
        p.allgather(REG["halo_b"], halo_g, "halo_b", "halo_g")
        build_conv_b(ctx)
        build_ffn(True, ctx, 1, "x3", FINAL_OUT)
        p.barrier()
    finally:
        FUSED[0] = False
    return nc, p


def fused_inputs(inp, core):
    b, j = core // 4, core % 4
    m = l1_inputs(inp, core)
    fm = np.zeros((128, 16), np.float32)
    sel = np.zeros((128, 16), np.float32)
    for r in range(8):
        if r // 4 == b:
            fm[:, r] = 1.0 if (r % 4) < j else 0.0
            fm[:, 8 + r] = 1.0 if (r % 4) > j else 0.0
    if j > 0:
        sel[:, core - 1] = 1.0
    if j < 3:
        sel[:, 8 + core + 1] = 1.0
    m["fmask"], m["sel"] = fm, sel
    m["gout"] = np.ascontiguousarray(inp["g_hgrn_out"][0][None, :])
    m["w_hout"] = np.ascontiguousarray(inp["w_hgrn_out"][0])
    for layer in range(2):
        L = str(layer)
        m["w_ada" + L] = np.ascontiguousarray(inp["w_ada"][layer])
        m["b_ada" + L] = np.ascontiguousarray(inp["b_ada"][layer][None, :])
        m["g_ffn" + L] = np.ascontiguousarray(inp["g_ffn"][layer][None, :])
        m["w_router" + L] = np.ascontiguousarray(inp["w_router"][layer])
        m["b_router" + L] = np.ascontiguousarray(inp["b_router"][layer][None, :])
        m["w_exp_in" + L] = np.ascontiguousarray(inp["w_exp_in"][layer])
        b1 = np.asarray(inp["b_exp_in"][layer], np.float32).reshape(NE, 16, 128)
        m["b1c" + L] = np.ascontiguousarray(b1.transpose(2, 0, 1))
        m["w_exp_out" + L] = np.ascontiguousarray(inp["w_exp_out"][layer])
        m["b_exp_out" + L] = np.ascontiguousarray(inp["b_exp_out"][layer])
    m["g_final"] = np.ascontiguousarray(inp["g_final"][None, :])
    m["g_mix1"] = np.ascontiguousarray(inp["g_mix"][1][None, :])
    m["w_cv_in"] = np.ascontiguousarray(inp["w_cv_in"][0])
    m["bin_c"] = np.ascontiguousarray(np.asarray(inp["b_cv_in"][0], np.float32).reshape(16, 128).T)
    w = np.asarray(inp["w_cv_dw"][0], np.float32)
    m["wdw"] = np.ascontiguousarray(w.reshape(31, 8, 128).transpose(2, 1, 0))
    cv = np.stack([np.asarray(inp[k][0], np.float32).reshape(8, 128).T for k in ("b_cv_dw", "g_cv_ln", "b_cv_ln")], axis=1)
    m["cvec"] = np.ascontiguousarray(cv)
    m["w_cv_out"] = np.ascontiguousarray(inp["w_cv_out"][0])
    m["b_cv_out"] = np.ascontiguousarray(inp["b_cv_out"][0][None, :])
    return m


def kernel(**inp):
    inp = {k: np.asarray(v) for k, v in inp.items()}
    nc, _ = build_fused()
    res = run_bass_kernel_spmd(nc, [fused_inputs(inp, c) for c in range(8)], core_ids=list(range(8))).results
    out = np.zeros((2, 4 * T, D), np.float32)
    for c in range(8):
        out[c // 4, (c % 4) * T:(c % 4 + 1) * T] = res[c][FINAL_OUT]
    return out
```

```python
import numpy as np
from contextlib import ExitStack
import concourse.bass as bass
import concourse.mybir as mybir
from concourse.bass_utils import run_bass_kernel_spmd

F32 = mybir.dt.float32
BF16 = mybir.dt.bfloat16
AF = mybir.ActivationFunctionType
ALU = mybir.AluOpType
AX = mybir.AxisListType

SEM_LIMIT = 12000
D = 1024
T = 4096
TCTX = 256
NH = 8
EPS = 1e-6
NE = 32
NQ_FFN = 8


class Prog:
    def __init__(self, nc):
        self.nc = nc
        self.es = ExitStack()
        self.eng = {"pe": nc.tensor, "dve": nc.vector, "act": nc.scalar,
                    "pool": nc.gpsimd, "sp": nc.sync}
        self.sem = {}
        self.cnt = {}
        self.nsem = 0
        for e in ("pe", "dve", "act", "pool"):
            self._new_sem(e)
        self.waited = {e: {} for e in self.eng}
        self.res = {}
        self.dsem = {}
        self.dcnt = {}
        self.ninst = {e: 0 for e in self.eng}
        self.psum_ids = set()
        self.prev_sem = {}
        self.dtot = {}
        self.free_dsems = []
        self.phase_es = None
        self.cc_sems = []

    def _alloc_sem(self, name):
        self.nsem += 1
        return self.es.enter_context(self.nc.semaphore(name))

    def _new_sem(self, e):
        if e in self.sem:
            self.prev_sem.setdefault(e, []).append((self.sem[e], self.cnt[e]))
        self.sem[e] = self._alloc_sem("e_%s_%d" % (e, self.nsem))
        self.cnt[e] = 0

    def begin_phase(self):
        self.phase_es = ExitStack()

    def end_phase(self):
        self.barrier()
        self.phase_es.close()
        self.phase_es = None
        self.res = {}
        self.free_dsems = [sem for (sem, _) in self.dtot.values()]
        self.dsem = {}

    def barrier(self):
        targets = []
        for x in ("pe", "dve", "act", "pool"):
            for (sm, c) in self.prev_sem.get(x, []):
                targets.append((sm, c))
            if self.cnt[x] > 0:
                targets.append((self.sem[x], self.cnt[x]))
        targets.extend(self.dtot.values())
        targets.extend(self.cc_sems)
        for e in ("pe", "dve", "act", "pool", "sp"):
            for (sm, c) in targets:
                if c > 0:
                    self._wait(e, sm, c)

    def allgather(self, in_ap, out_ap, in_key, out_key):
        self._deps("pool", [in_key], [out_key])
        inst = self.nc.gpsimd.collective_compute("AllGather", ALU.bypass, replica_groups=[list(range(8))],
                                                 ins=[in_ap.opt()], outs=[out_ap.opt()])
        sm = self._alloc_sem("cc%d" % self.nsem)
        inst.then_inc(sm, 1)
        self.cc_sems.append((sm, 1))
        self._mark(sm, 1, [in_key], [out_key])

    def sb(self, name, shape, dt):
        es = self.phase_es if self.phase_es is not None else self.es
        self.ntile = getattr(self, "ntile", 0) + 1
        return es.enter_context(self.nc.sbuf_tensor("s%d_%s" % (self.ntile, name), list(shape), dt))

    def ps(self, name, shape, dt=F32):
        t = self.es.enter_context(self.nc.psum_tensor("p_" + name, list(shape), dt))
        self.psum_ids.add(id(t))
        return t

    def _r(self, key):
        k = key if isinstance(key, (str, tuple)) else id(key)
        r = self.res.get(k)
        if r is None:
            r = {"w": None, "r": {}}
            self.res[k] = r
        return r

    def _wait(self, e, sem, val):
        w = self.waited[e]
        k = id(sem)
        if w.get(k, 0) >= val:
            return
        w[k] = val
        self.eng[e].wait_ge(sem, val)

    def _deps(self, e, reads, writes):
        deps = []
        for r in reads:
            rr = self._r(r)
            if rr["w"] is not None:
                deps.append(rr["w"])
            if id(r) in self.psum_ids:
                deps.extend(rr["r"].values())
        for wk in writes:
            rr = self._r(wk)
            if rr["w"] is not None:
                deps.append(rr["w"])
            deps.extend(rr["r"].values())
        own = self.sem.get(e)
        for (sem, val) in deps:
            if e == "pe" and sem is own:
                continue
            self._wait(e, sem, val)

    def _mark(self, sem, val, reads, writes):
        for r in reads:
            self._r(r)["r"][id(sem)] = (sem, val)
        for wk in writes:
            rr = self._r(wk)
            rr["w"] = (sem, val)
            rr["r"] = {}

    def op(self, e, fn, reads=(), writes=()):
        if self.cnt[e] >= SEM_LIMIT:
            self._new_sem(e)
        self._deps(e, reads, writes)
        inst = fn(self.eng[e])
        self.cnt[e] += 1
        self.ninst[e] += 1
        inst.then_inc(self.sem[e], 1)
        self._mark(self.sem[e], self.cnt[e], reads, writes)
        return inst

    def dma(self, q, out, in_, reads=(), writes=(), semkey=None, **kw):
        self._deps(q, reads, writes)
        key = semkey if semkey is not None else writes[0]
        key = key if isinstance(key, (str, tuple)) else id(key)
        if key not in self.dsem or self.dtot[id(self.dsem[key])][1] >= 2 * SEM_LIMIT:
            if key not in self.dsem and self.free_dsems:
                self.dsem[key] = self.free_dsems.pop()
            else:
                sm = self._alloc_sem("d%d" % self.nsem)
                self.dsem[key] = sm
                self.dtot[id(sm)] = (sm, 0)
        s = self.dsem[key]
        inst = self.eng[q].dma_start(out=out, in_=in_, **kw)
        tot = self.dtot[id(s)][1] + 16
        self.dtot[id(s)] = (s, tot)
        self.ninst[q] += 1
        inst.then_inc(s, 16)
        self._mark(s, tot, reads, writes)
        return inst

    def finish(self, out_keys, e="sp"):
        for k in out_keys:
            rr = self._r(k)
            if rr["w"] is not None:
                self._wait(e, *rr["w"])
        for x in ("pe", "dve", "act", "pool"):
            if self.cnt[x] > 0:
                self._wait(e, self.sem[x], self.cnt[x])

    def act(self, out, in_, func, reads, writes, **kw):
        return self.op("act", lambda e: e.activation(out, in_, func, **kw), reads, writes)

    def tt(self, eng, out, a, b, op, reads, writes):
        return self.op(eng, lambda e: e.tensor_tensor(out, a, b, op), reads, writes)

    def ts(self, eng, out, a, s1, s2, op0, op1, reads, writes):
        if s2 is None:
            return self.op(eng, lambda e: e.tensor_scalar(out, a, s1, None, op0), reads, writes)
        return self.op(eng, lambda e: e.tensor_scalar(out, a, s1, s2, op0, op1), reads, writes)

    def copy(self, eng, out, in_, reads, writes):
        if eng == "act":
            return self.op("act", lambda e: e.copy(out, in_), reads, writes)
        return self.op(eng, lambda e: e.tensor_copy(out, in_), reads, writes)

    def mm(self, out, lhsT, rhs, start, stop, reads, writes):
        return self.op("pe", lambda e: e.matmul(out, lhsT, rhs, start=start, stop=stop), reads, writes)


REG = {}
FUSED = [False]
FINAL_OUT = "out"


def dram_in(nc, name, shape, dt=F32):
    if FUSED[0] and name in REG:
        return REG[name]
    ap = nc.dram_tensor(name, list(shape), dt, kind="ExternalInput").ap()
    if FUSED[0]:
        REG[name] = ap
    return ap


def dram_out(nc, name, shape, dt=F32):
    if FUSED[0]:
        kind = "ExternalOutput" if name == FINAL_OUT else "Internal"
        ap = nc.dram_tensor(name, list(shape), dt, kind=kind).ap()
        REG[name] = ap
        return ap
    return nc.dram_tensor(name, list(shape), dt, kind="ExternalOutput").ap()


class Ctx:
    def __init__(self):
        self.nc = bass.Bass("TRN2", target_bir_lowering=False)
        self.p = Prog(self.nc)
        self.cm = None


def stage_begin(ctx):
    if ctx is None:
        c = Ctx()
        c.cm = Common(c.p, c.nc)
        c.standalone = True
        return c
    ctx.standalone = False
    ctx.p.begin_phase()
    return ctx


def stage_end(ctx, out_keys):
    if ctx.standalone:
        ctx.p.finish(out_keys)
    else:
        ctx.p.end_phase()
    return ctx.nc, ctx.p


class Common:
    def __init__(self, p, nc):
        self.p = p
        self.ident_d = dram_in(nc, "ident", [128, 128])
        self.identf = p.sb("identf", [128, 128], F32)
        self.identb = p.sb("identb", [128, 128], BF16)
        self.ones = p.sb("ones", [128, 128], F32)
        p.dma("sp", self.identf[:], self.ident_d, writes=[self.identf])
        p.copy("dve", self.identb[:], self.identf[:], [self.identf], [self.identb])
        p.op("dve", lambda e: e.memset(self.ones[:], 1.0), [], [self.ones])
        self.ones5 = p.sb("ones5", [128, 512], F32)
        p.op("dve", lambda e: e.memset(self.ones5[:], 1.0), [], [self.ones5])
        self.psb = [p.ps("psb%d" % i, [128, 512]) for i in range(7)]
        self.psT = p.ps("psT", [128, 1024], BF16)


def emit_mods(p, cm, w_ada, b_ada, lo, hi, pairs, tag):
    lts = []
    for i, (ccol, out) in enumerate(pairs):
        p.dma("sp", out[:], b_ada[:, lo:hi].partition_broadcast(128), writes=[out])
        cc = p.sb("mod_cc%s%d" % (tag, i), [128, 8], F32)
        sg = p.sb("mod_sg%s%d" % (tag, i), [128, 8], F32)
        lt = p.sb("mod_lt%s%d" % (tag, i), [128, 8, 128], F32)
        p.dma("sp", cc[:], ccol, writes=[cc])
        p.act(sg[:], cc[:], AF.Sigmoid, [cc], [sg])
        p.tt("dve", sg[:], sg[:], cc[:], ALU.mult, [sg, cc], [sg])
        for kc in range(8):
            p.ts("dve", lt[:, kc, :], cm.ones[:], sg[:, kc:kc + 1], None, ALU.mult, None, [cm.ones, sg], [lt])
        lts.append(lt)
    wc = p.sb("mod_w%s" % tag, [128, 8, 256], F32)
    for bi, n0 in enumerate(range(lo, hi, 256)):
        p.dma("sp", wc[:], w_ada[:, n0:n0 + 256].rearrange("(kc q) n -> q kc n", q=128), writes=[wc])
        for i, (ccol, out) in enumerate(pairs):
            ps = cm.psb[i % 2]
            for kc in range(8):
                p.mm(ps[:, 0:256], lts[i][:, kc, :], wc[:, kc, :], kc == 0, kc == 7, [lts[i], wc], [ps])
            p.tt("dve", out[:, n0 - lo:n0 - lo + 256], ps[:, 0:256], out[:, n0 - lo:n0 - lo + 256], ALU.add, [ps, out], [out])


def emit_norm_T(p, cm, src, ntiles, Arow, Brow, modt, hT, tagres, xt, junk, h, st, hTf=None):
    for t in range(ntiles):
        xx = xt[t % 2]
        p.dma("sp", xx[:], src[t * 128:(t + 1) * 128, :], reads=[tagres] if tagres else [], writes=[xx])
        ss, rstd = st
        p.act(junk[:], xx[:], AF.Square, [xx], [junk, ss], accum_out=ss[:])
        p.act(rstd[:], ss[:], AF.Sqrt, [ss], [rstd], scale=1.0 / D, bias=EPS)
        p.op("dve", lambda e: e.reciprocal(rstd[:], rstd[:]), [rstd], [rstd])
        p.op("dve", lambda e: e.scalar_tensor_tensor(h[:], xx[:], rstd[:], Arow, ALU.mult, ALU.mult),
             [xx, rstd, modt], [h])
        p.tt("dve", h[:], h[:], Brow, ALU.add, [h, modt], [h])
        for half in range(2):
            pt = cm.psb[half]
            for j in range(4):
                kc = half * 4 + j
                p.op("pe", lambda e: e.transpose(pt[:, j * 128:(j + 1) * 128], h[:, kc * 128:(kc + 1) * 128], cm.identf[:]),
                     [h, cm.identf], [pt])
            p.copy("act", hT[:, half * 4:(half + 1) * 4, t * 128:(t + 1) * 128],
                   pt[:].rearrange("q (a b) -> q a b", a=4), [pt], [hT])
            if hTf is not None:
                p.copy("dve", hTf[:, half * 4:(half + 1) * 4, t * 128:(t + 1) * 128],
                       pt[:].rearrange("q (a b) -> q a b", a=4), [pt], [hTf])


DBG_STOP = None
DBG_LVL = 0
DBG_SKIP = ''


def build_l1(ctx=None):
    ctx = stage_begin(ctx)
    nc, p, cm = ctx.nc, ctx.p, ctx.cm
    xs = dram_in(nc, "xs", [T, D])
    ctxb = dram_in(nc, "ctxb", [TCTX, D])
    ccol = dram_in(nc, "ccol", [128, 8])
    cctx = dram_in(nc, "cctx", [128, 8])
    w_ada = dram_in(nc, "w_ada0", [D, 6 * D])
    b_ada = dram_in(nc, "b_ada0", [1, 6 * D])
    g_mix = dram_in(nc, "g_mix0", [1, D])
    w_hin = dram_in(nc, "w_hin", [D, 5 * D])
    gam = dram_in(nc, "gam", [128, 2, 2, 8])
    maskf_d = dram_in(nc, "maskf", [64, 64])
    maskb_d = dram_in(nc, "maskb", [64, 64])
    rm_d = dram_in(nc, "rm", [128, 512])
    o_d = [dram_out(nc, "o_f", [T, D]), dram_out(nc, "o_b", [T, D])]
    sg_d = dram_out(nc, "sg", [T, D], BF16)
    Q_d = [dram_out(nc, "QF", [NH, 128, T], BF16), dram_out(nc, "QB", [NH, 128, T], BF16)]
    Sloc_d = dram_out(nc, "Sloc", [2, NH, 128, 128])
    Sctx_d = dram_out(nc, "Sctx", [2, NH, 128, 128])
    Dloc_d = dram_out(nc, "Dloc", [128, 16])

    maskt = [p.sb("maskf", [64, 64], F32), p.sb("maskb", [64, 64], F32)]
    rm = p.sb("rm", [128, 512], F32)
    p.dma("sp", maskt[0][:], maskf_d, writes=[maskt[0]])
    p.dma("sp", maskt[1][:], maskb_d, writes=[maskt[1]])
    p.dma("sp", rm[:], rm_d, writes=[rm])

    gm = p.sb("gm", [128, 2, 2, 8], F32)
    lb = p.sb("lb", [128, 16], F32)
    oml = p.sb("oml", [128, 16], F32)
    p.dma("sp", gm[:], gam, writes=[gm])
    for d in range(2):
        p.tt("dve", lb[:, d * 8:(d + 1) * 8], gm[:, d, 0, :], gm[:, d, 1, :], ALU.subtract, [gm], [lb])
    p.act(lb[:], lb[:], AF.Sigmoid, [lb], [lb])
    p.ts("dve", oml[:], lb[:], -1.0, 1.0, ALU.mult, ALU.add, [lb], [oml])

    if DBG_LVL == 1:
        p.finish([])
        return nc, p
    modb = p.sb("modb", [128, 2048], F32)
    modc = p.sb("modc", [128, 2048], F32)
    emit_mods(p, cm, w_ada, b_ada, 0, 2048, [(ccol, modb), (cctx, modc)], "a")
    gmix = p.sb("gmix", [128, D], F32)
    p.dma("sp", gmix[:], g_mix.partition_broadcast(128), writes=[gmix])
    for m in (modb, modc):
        p.op("dve", lambda e: e.scalar_tensor_tensor(m[:, 1024:2048], m[:, 1024:2048], 1.0, gmix[:], ALU.add, ALU.mult),
             [m, gmix], [m])

    if DBG_LVL == 2:
        p.finish([])
        return nc, p
    hT = p.sb("hT", [128, 8, T], BF16)
    hTc = p.sb("hTc", [128, 8, TCTX], BF16)
    xt = [p.sb("xt%d" % i, [128, D], F32) for i in range(2)]
    h = p.sb("h", [128, D], F32)
    junk = h
    st = (p.sb("ss", [128, 1], F32), p.sb("rstd", [128, 1], F32))
    emit_norm_T(p, cm, ctxb, TCTX // 128, modc[:, 1024:2048], modc[:, 0:1024], modc, hTc, None, xt, junk, h, st)
    if DBG_LVL == 3:
        p.finish([])
        return nc, p
    emit_norm_T(p, cm, xs, (T // 128) if DBG_LVL != 4 else 4, modb[:, 1024:2048], modb[:, 0:1024], modb, hT, None, xt, junk, h, st)
    if DBG_LVL in (4, 5):
        p.finish([])
        return nc, p

    col0 = [0, 2048, 3072, 1024, 4096]
    whf = [p.sb("whf%d" % i, [128, 8, 128], F32) for i in range(2)]
    whb = [p.sb("whb%d" % i, [128, 8, 5, 128], BF16) for i in range(2)]

    def blk_tiles(i):
        s = "_%d" % i
        return dict(
            q=p.sb("q" + s, [128, 512], F32), F=p.sb("F" + s, [128, 512], F32),
            LF=p.sb("LF" + s, [128, 512], F32), U=p.sb("U" + s, [128, 512], F32),
            E=p.sb("E" + s, [128, 512], F32), Bc=p.sb("Bc" + s, [128, 512], F32),
            BL=p.sb("BL" + s, [128, 8], F32), DEC=p.sb("DEC" + s, [128, 8], F32),
            QG=p.sb("QG" + s, [128, 512], BF16), KD=p.sb("KD" + s, [128, 512], BF16),
            KGT=p.sb("KGT" + s, [128, 512], BF16), QX=p.sb("QX" + s, [128, 512], BF16),
            kg=p.sb("kg" + s, [64, 8, 128], BF16), v=p.sb("v" + s, [64, 8, 128], BF16),
            sgt=p.sb("sgt" + s, [64, 8, 128], BF16), ob=p.sb("ob" + s, [64, 8, 128], F32),
            tc=p.sb("tc" + s, [128, 1], F32), sgs=p.sb("sgs" + s, [64, 2, 128], F32), sgg=p.sb("sgg" + s, [64, 2, 128], F32))

    bt = [blk_tiles(0), blk_tiles(1)]
    S32 = p.sb("S32", [128, 128], F32)
    Sb = p.sb("Sb", [128, 128], BF16)
    ATs = p.sb("ATs", [64, 64], BF16)
    Dcol = p.sb("Dcol", [128, 16], F32)
    psq, psz, psv, psA, pso, psS = cm.psb[0], cm.psb[1], cm.psb[2], cm.psb[3], cm.psb[4], cm.psb[5]
    psT = cm.psT
    bcount = 0

    for hh in range(NH):
        if DBG_STOP is not None and hh >= DBG_STOP[0]:
            break
        wb = whb[hh % 2]
        for s in range(5):
            c0 = col0[s] + hh * 128
            wf_ = whf[s % 2]
            p.dma("sp", wf_[:], w_hin[:, c0:c0 + 128].rearrange("(kc q) n -> q kc n", q=128), writes=[wf_])
            p.copy("act", wb[:, :, s, :], wf_[:], [wf_], [wb])
        for (is_ctx, hTs, TT) in ((True, hTc, TCTX), (False, hT, T)):
            nb_tok = 256 if is_ctx else 512
            nblk = TT // nb_tok
            nch = nb_tok // 64
            for d in range(2):
                if DBG_STOP is not None and (is_ctx, d) == DBG_STOP[1]:
                    break
                col = d * 8 + hh
                blocks = list(range(nblk)) if d == 0 else list(range(nblk - 1, -1, -1))
                carry = None
                first = True
                for blk in blocks:
                    b = bt[bcount % 2]
                    bcount += 1
                    t0 = blk * nb_tok
                    n = nb_tok
                    q, Ft, LF, U, E, Bc = b["q"], b["F"], b["LF"], b["U"], b["E"], b["Bc"]
                    for kc in range(8):
                        p.mm(psq[:, 0:n], wb[:, kc, 0, :], hTs[:, kc, t0:t0 + n], kc == 0, kc == 7, [wb, hTs], [psq])
                    p.copy("act", q[:, 0:n], psq[:, 0:n], [psq], [q])
                    for kc in range(8):
                        p.mm(psz[:, 0:n], wb[:, kc, 1 + d, :], hTs[:, kc, t0:t0 + n], kc == 0, kc == 7, [wb, hTs], [psz])
                    p.act(Ft[:, 0:n], psz[:, 0:n], AF.Sigmoid, [psz], [Ft])
                    for c2 in range(nch // 2):
                        for j in range(2):
                            c = c2 * 2 + j
                            for kc in range(8):
                                p.mm(psv[0:64, j * 256:(j + 1) * 256], hTs[:, kc, t0 + c * 64:t0 + (c + 1) * 64],
                                     wb[:, kc, 3:5, :].rearrange("q a b -> q (a b)"), kc == 0, kc == 7, [wb, hTs], [psv])
                        pv = psv[0:64, :].rearrange("q (j x) -> q j x", j=2)
                        p.copy("dve", b["v"][:, c2 * 2:c2 * 2 + 2, :], pv[:, :, 0:128], [psv], [b["v"]])
                        if d == 0 and not is_ctx:
                            p.copy("dve", b["sgg"][:, 0:2, :], pv[:, :, 128:256], [psv], [b["sgg"]])
                            p.act(b["sgs"][:, 0:2, :], b["sgg"][:, 0:2, :], AF.Sigmoid, [b["sgg"]], [b["sgs"]])
                            p.tt("dve", b["sgt"][:, c2 * 2:c2 * 2 + 2, :], b["sgg"][:, 0:2, :], b["sgs"][:, 0:2, :], ALU.mult,
                                 [b["sgg"], b["sgs"]], [b["sgt"]])
                    if d == 0 and not is_ctx and "C" not in DBG_SKIP:
                        p.dma("sp", sg_d[t0:t0 + n, hh * 128:(hh + 1) * 128].rearrange("(c q) v -> q c v", q=64),
                              b["sgt"][:, 0:nch, :], reads=[b["sgt"]], writes=["sg_d"])
                    if DBG_LVL == 6:
                        p.finish([]); return nc, p
                    p.ts("dve", Ft[:, 0:n], Ft[:, 0:n], oml[:, col:col + 1], lb[:, col:col + 1], ALU.mult, ALU.add,
                         [Ft, oml, lb], [Ft])
                    p.act(LF[:, 0:n], Ft[:, 0:n], AF.Ln, [Ft], [LF])
                    p.ts("dve", Ft[:, 0:n], Ft[:, 0:n], -1.0, 1.0, ALU.mult, ALU.add, [Ft], [Ft])
                    p.op("dve", lambda e: e.tensor_tensor_scan(U[:, 0:n], rm[:, 0:n], LF[:, 0:n], 0.0, ALU.mult, ALU.add),
                         [rm, LF], [U])
                    U3 = U[:, 0:n].rearrange("q (c k) -> q c k", k=64)
                    p.copy("act", b["BL"][:, 0:nch].rearrange("q (c o) -> q c o", o=1), U3[:, :, 63:64], [U], [b["BL"]])
                    p.act(b["DEC"][:, 0:nch], b["BL"][:, 0:nch], AF.Exp, [b["BL"]], [b["DEC"]])
                    if not is_ctx and "B" not in DBG_SKIP:
                        init = 0.0 if (carry is None or d == 1) else carry[0]
                        rdc = [] if (carry is None or d == 1) else [carry[1]]
                        p.op("dve", lambda e: e.tensor_tensor_scan(Bc[:, 0:n], cm.ones5[:, 0:n], LF[:, 0:n], init, ALU.mult, ALU.add),
                             [cm.ones5, LF] + rdc, [Bc])
                        if d == 0:
                            carry = (Bc[:, n - 1:n], Bc)
                        else:
                            if carry is None:
                                p.copy("act", b["tc"][:], Bc[:, n - 1:n], [Bc], [b["tc"]])
                            else:
                                p.tt("dve", b["tc"][:], Bc[:, n - 1:n], carry[0], ALU.add, [Bc, carry[1]], [b["tc"]])
                            carry = (b["tc"][:], b["tc"])
                            p.tt("dve", Bc[:, 0:n], LF[:, 0:n], Bc[:, 0:n], ALU.subtract, [LF, Bc], [Bc])
                            p.ts("dve", Bc[:, 0:n], Bc[:, 0:n], b["tc"][:], None, ALU.add, None, [Bc, b["tc"]], [Bc])
                        p.act(E[:, 0:n], Bc[:, 0:n], AF.Exp, [Bc], [E])
                        p.tt("dve", b["QX"][:, 0:n], q[:, 0:n], E[:, 0:n], ALU.mult, [q, E], [b["QX"]])
                        p.dma("sp", Q_d[d][hh, :, t0:t0 + n], b["QX"][:, 0:n], reads=[b["QX"]], writes=["Q_d%d" % d])
                        if d == 1 and carry is not None:
                            pass
                    if d == 1:
                        p.tt("dve", U[:, 0:n], LF[:, 0:n], U[:, 0:n], ALU.subtract, [LF, U], [U])
                        p.tt("dve", U3, U3, b["BL"][:, 0:nch].rearrange("q (c o) -> q c o", o=1).to_broadcast([128, nch, 64]),
                             ALU.add, [U, b["BL"]], [U])
                    p.act(E[:, 0:n], U[:, 0:n], AF.Exp, [U], [E])
                    p.tt("dve", b["QG"][:, 0:n], q[:, 0:n], E[:, 0:n], ALU.mult, [q, E], [b["QG"]])
                    p.act(LF[:, 0:n], U[:, 0:n], AF.Exp, [U], [LF], scale=-1.0)
                    p.tt("dve", b["KD"][:, 0:n], Ft[:, 0:n], LF[:, 0:n], ALU.mult, [Ft, LF], [b["KD"]])
                    p.tt("dve", b["KGT"][:, 0:n].rearrange("q (c k) -> q c k", k=64),
                         b["KD"][:, 0:n].rearrange("q (c k) -> q c k", k=64),
                         b["DEC"][:, 0:nch].rearrange("q (c o) -> q c o", o=1).to_broadcast([128, nch, 64]),
                         ALU.mult, [b["KD"], b["DEC"]], [b["KGT"]])
                    if DBG_LVL == 7:
                        p.finish([]); return nc, p
                    for c in range(nch):
                        p.op("pe", lambda e: e.transpose(psT[0:64, c * 128:(c + 1) * 128], b["KGT"][:, c * 64:(c + 1) * 64], cm.identb[:]),
                             [b["KGT"], cm.identb], [psT])
                    p.copy("act", b["kg"][:, 0:nch, :], psT[0:64, 0:nch * 128].rearrange("q (c k) -> q c k", k=128), [psT], [b["kg"]])
                    if DBG_LVL == 8:
                        p.finish([]); return nc, p
                    order = list(range(nch)) if d == 0 else list(range(nch - 1, -1, -1))
                    if "A" in DBG_SKIP:
                        order = []
                    for c in order:
                        cs = slice(c * 64, (c + 1) * 64)
                        p.mm(psA[0:64, 0:64], b["KD"][:, cs], b["QG"][:, cs], True, True, [b["KD"], b["QG"]], [psA])
                        p.tt("dve", ATs[:], psA[0:64, 0:64], maskt[d][:], ALU.mult, [psA, maskt[d]], [ATs])
                        if DBG_LVL == 9:
                            continue
                        p.mm(pso[0:64, 0:128], ATs[:], b["v"][:, c, :], True, first, [ATs, b["v"]], [pso])
                        if not first:
                            p.mm(pso[0:64, 0:128], b["QG"][:, cs], Sb[:], False, True, [b["QG"], Sb], [pso])
                        if not is_ctx:
                            p.copy("act", b["ob"][:, c, :], pso[0:64, 0:128], [pso], [b["ob"]])
                        if DBG_LVL == 10:
                            first = False
                            continue
                        p.mm(psS[:, 0:128], b["kg"][:, c, :], b["v"][:, c, :], True, True, [b["kg"], b["v"]], [psS])
                        if first:
                            p.copy("dve", S32[:], psS[:, 0:128], [psS], [S32])
                        else:
                            p.op("dve", lambda e: e.scalar_tensor_tensor(S32[:], S32[:], b["DEC"][:, c:c + 1], psS[:, 0:128], ALU.mult, ALU.add),
                                 [S32, b["DEC"], psS], [S32])
                        p.copy("act", Sb[:], S32[:], [S32], [Sb])
                        first = False
                    if not is_ctx and "D" not in DBG_SKIP:
                        p.dma("sp", o_d[d][t0:t0 + n, hh * 128:(hh + 1) * 128].rearrange("(c q) v -> q c v", q=64),
                              b["ob"][:, 0:nch, :], reads=[b["ob"]], writes=["o_d%d" % d])
                if is_ctx:
                    p.dma("sp", Sctx_d[d, hh], S32[:], reads=[S32], writes=["Sctx_d"])
                else:
                    p.dma("sp", Sloc_d[d, hh], S32[:], reads=[S32], writes=["Sloc_d"])
                    if "B" not in DBG_SKIP:
                        p.act(Dcol[:, col:col + 1], carry[0], AF.Exp, [carry[1]], [Dcol])
    p.dma("sp", Dloc_d, Dcol[:], reads=[Dcol], writes=["Dloc_d"])
    return stage_end(ctx, ["sg_d", "Q_d0", "Q_d1", "o_d0", "o_d1", "Sctx_d", "Sloc_d", "Dloc_d"])


def consts():
    s = np.arange(64)[:, None]
    t = np.arange(64)[None, :]
    rm = np.ones((128, 512), np.float32)
    rm[:, ::64] = 0.0
    return {"ident": np.eye(128, dtype=np.float32),
            "maskf": (s <= t).astype(np.float32), "maskb": (s >= t).astype(np.float32), "rm": rm}


def col_layout(v):
    return np.ascontiguousarray(np.asarray(v, np.float32).reshape(8, 128).T)


def l1_inputs(inp, core):
    b, j = core // 4, core % 4
    m = dict(consts())
    m["xs"] = np.ascontiguousarray(inp["x"][b, j * T:(j + 1) * T])
    m["ctxb"] = np.ascontiguousarray(inp["ctx"][b])
    m["ccol"] = col_layout(inp["c"][b])
    m["cctx"] = col_layout(inp["c_ctx"])
    m["w_ada0"] = np.ascontiguousarray(inp["w_ada"][0])
    m["b_ada0"] = np.ascontiguousarray(inp["b_ada"][0][None, :])
    m["g_mix0"] = np.ascontiguousarray(inp["g_mix"][0][None, :])
    m["w_hin"] = np.ascontiguousarray(inp["w_hgrn_in"][0])
    g = np.asarray(inp["hgrn_gamma"], np.float32).reshape(2, 2, 8, 128)
    m["gam"] = np.ascontiguousarray(g.transpose(3, 0, 1, 2))
    return m


def emit_ffn(p, cm, x1_d, x1_key, xo_d, xo_key, modt, mo, gffn_d, wr_d, br_d, w1_d, b1c_d, w2_d, b2_d, tl, final_g=None):
    NQ = NQ_FFN
    TQ = T // NQ
    hfT = tl["hfT"]
    xt, h, st = tl["xt"], tl["h"], tl["st"]
    gf = h
    p.dma("sp", gf[:], gffn_d.partition_broadcast(128), writes=[gf])
    p.op("dve", lambda e: e.scalar_tensor_tensor(modt[:, mo + 1024:mo + 2048], modt[:, mo + 1024:mo + 2048], 1.0, gf[:], ALU.add, ALU.mult),
         [modt, gf], [modt])
    if DBG_LVL == 16:
        return
    wr = p.sb("wr", [128, 8, NE], F32)
    brb = p.sb("brb", [128, NE], F32)
    p.dma("sp", wr[:], wr_d.rearrange("(kc q) n -> q kc n", q=128), writes=[wr])
    p.dma("sp", brb[:], br_d.partition_broadcast(128), writes=[brb])
    G = p.sb("G_all", [128, T // 128, NE], F32)
    GT = p.sb("GT_q", [NE, T // NQ_FFN], F32)
    b2s = p.sb("b2s", [NE, D], F32)
    b1c = p.sb("b1c", [128, NE, 16], F32)
    p.dma("sp", b2s[:], b2_d, writes=[b2s])
    p.dma("sp", b1c[:], b1c_d, writes=[b1c])
    if DBG_LVL == 17:
        return
    hTf = p.sb("hTf", [128, 8, 128], F32)
    lg = p.sb("lg", [128, NE], F32)
    m8 = p.sb("m8", [128, 8], F32)
    nmx = p.sb("nmx", [128, 1], F32)
    msk = p.sb("msk", [128, NE], F32)
    ex = p.sb("ex", [128, NE], F32)
    sm = p.sb("sm", [128, 1], F32)
    ps_l = cm.psb[2]
    ss, rstd = st
    for t in range(T // 128):
        xx = xt[t % 2]
        p.dma("sp", xx[:], x1_d[t * 128:(t + 1) * 128, :], reads=[x1_key], writes=[xx])
        p.act(h[:], xx[:], AF.Square, [xx], [h, ss], accum_out=ss[:])
        p.act(rstd[:], ss[:], AF.Sqrt, [ss], [rstd], scale=1.0 / D, bias=EPS)
        p.op("dve", lambda e: e.reciprocal(rstd[:], rstd[:]), [rstd], [rstd])
        p.op("dve", lambda e: e.scalar_tensor_tensor(h[:], xx[:], rstd[:], modt[:, mo + 1024:mo + 2048], ALU.mult, ALU.mult),
             [xx, rstd, modt], [h])
        p.tt("dve", h[:], h[:], modt[:, mo:mo + 1024], ALU.add, [h, modt], [h])
        for half in range(2):
            pt = cm.psb[half]
            for j in range(4):
                kc = half * 4 + j
                p.op("pe", lambda e: e.transpose(pt[:, j * 128:(j + 1) * 128], h[:, kc * 128:(kc + 1) * 128], cm.identf[:]),
                     [h, cm.identf], [pt])
            p.copy("act", hfT[:, half * 4:(half + 1) * 4, t * 128:(t + 1) * 128],
                   pt[:].rearrange("q (a b) -> q a b", a=4), [pt], [hfT])
            p.copy("dve", hTf[:, half * 4:(half + 1) * 4, :], pt[:].rearrange("q (a b) -> q a b", a=4), [pt], [hTf])
        if DBG_LVL == 20:
            return
        for kc in range(8):
            p.mm(ps_l[:, 0:NE], hTf[:, kc, :], wr[:, kc, :], kc == 0, kc == 7, [hTf, wr], [ps_l])
        p.tt("dve", lg[:], ps_l[:, 0:NE], brb[:], ALU.add, [ps_l, brb], [lg])
        if DBG_LVL == 19:
            return
        p.op("dve", lambda e: e.max(m8[:], lg[:]), [lg], [m8])
        p.ts("dve", msk[:], lg[:], m8[:, 3:4], None, ALU.is_ge, None, [lg, m8], [msk])
        p.ts("dve", nmx[:], m8[:, 0:1], -1.0, None, ALU.mult, None, [m8], [nmx])
        if DBG_LVL == 18:
            return
        p.act(ex[:], lg[:], AF.Exp, [lg, nmx], [ex], bias=nmx[:])
        p.tt("dve", ex[:], ex[:], msk[:], ALU.mult, [ex, msk], [ex])
        p.op("dve", lambda e: e.tensor_reduce(sm[:], ex[:], AX.X, ALU.add), [ex], [sm])
        p.op("dve", lambda e: e.reciprocal(sm[:], sm[:]), [sm], [sm])
        p.ts("dve", G[:, t, :], ex[:], sm[:], None, ALU.mult, None, [ex, sm], [G])
        if DBG_LVL == 21:
            return
    if DBG_LVL == 22:
        return
    yacc = tl["yacc"]
    actT = tl["actT"]
    w2b = tl["w2b"]
    wst = tl["wst"]
    w2st = tl["w2st"]
    wgb = tl["wgb"]
    wub = tl["wub"]
    tg, tsg, tu = tl["tg"], tl["tsg"], tl["tu"]
    psg, psu, psy = cm.psb[3], cm.psb[4], cm.psb[5]
    cnt = 0
    for qd in range(NQ):
        tq0 = qd * TQ
        for ti in range(TQ // 128):
            tg_ = qd * (TQ // 128) + ti
            p.op("pe", lambda e: e.transpose(ps_l[0:NE, 128:256], G[:, tg_, :], cm.identf[:]), [G, cm.identf], [ps_l])
            p.copy("dve", GT[:, ti * 128:(ti + 1) * 128], ps_l[0:NE, 128:256], [ps_l], [GT])
        for ti in range(TQ // 128):
            tg_ = qd * (TQ // 128) + ti
            for dh in range(2):
                p.mm(psy[:], GT[:, ti * 128:(ti + 1) * 128], b2s[:, dh * 512:(dh + 1) * 512], True, True, [GT, b2s], [psy])
                p.copy("act", yacc[:, ti, dh * 512:(dh + 1) * 512], psy[:], [psy], [yacc])
        if DBG_LVL == 23:
            return
        for ex_ in range(NE):
            if DBG_LVL == 24 and ex_ == 1:
                return
            for j in range(8):
                k = cnt % 2
                cnt += 1
                p.dma("sp", wst[k][:], w1_d[ex_, :, j * 128:(j + 1) * 128].rearrange("(kc q) n -> q kc n", q=128), writes=[wst[k]])
                p.copy("act", wgb[k][:], wst[k][:], [wst[k]], [wgb[k]])
                p.dma("sp", wst[k][:], w1_d[ex_, :, 1024 + j * 128:1024 + (j + 1) * 128].rearrange("(kc q) n -> q kc n", q=128), writes=[wst[k]])
                p.copy("act", wub[k][:], wst[k][:], [wst[k]], [wub[k]])
                p.dma("sp", w2st[k][:], w2_d[ex_, j * 128:(j + 1) * 128, :], writes=[w2st[k]])
                p.copy("act", w2b[:, j, :], w2st[k][:], [w2st[k]], [w2b])
                for tb in range(TQ // 512):
                    tok = slice(tq0 + tb * 512, tq0 + (tb + 1) * 512)
                    for kc in range(8):
                        p.mm(psg[:], wgb[k][:, kc, :], hfT[:, kc, tok], kc == 0, kc == 7, [wgb[k], hfT], [psg])
                    for kc in range(8):
                        p.mm(psu[:], wub[k][:, kc, :], hfT[:, kc, tok], kc == 0, kc == 7, [wub[k], hfT], [psu])
                    p.ts("dve", tg[:], psg[:], b1c[:, ex_, j:j + 1], 7.0, ALU.add, ALU.min, [psg, b1c], [tg])
                    p.act(tsg[:], tg[:], AF.Sigmoid, [tg], [tsg], scale=1.702)
                    p.ts("dve", tu[:], psu[:], b1c[:, ex_, 8 + j:9 + j], 7.0, ALU.add, ALU.min, [psu, b1c], [tu])
                    p.ts("dve", tu[:], tu[:], -7.0, 1.0, ALU.max, ALU.add, [tu], [tu])
                    p.tt("dve", tg[:], tg[:], tsg[:], ALU.mult, [tg, tsg], [tg])
                    p.tt("dve", actT[:, j, tb * 512:(tb + 1) * 512], tg[:], tu[:], ALU.mult, [tg, tu], [actT])
            for ti in range(TQ // 128):
                tg_ = qd * (TQ // 128) + ti
                for dh in range(2):
                    for j in range(8):
                        p.mm(psy[:], actT[:, j, ti * 128:(ti + 1) * 128], w2b[:, j, dh * 512:(dh + 1) * 512], j == 0, j == 7,
                             [actT, w2b], [psy])
                    ya = yacc[:, ti, dh * 512:(dh + 1) * 512]
                    p.op("dve", lambda e: e.scalar_tensor_tensor(ya, psy[:], G[:, tg_, ex_:ex_ + 1], ya, ALU.mult, ALU.add),
                         [psy, G, yacc], [yacc])
        for ti in range(TQ // 128):
            tg_ = qd * (TQ // 128) + ti
            xx = xt[ti % 2]
            p.dma("sp", xx[:], x1_d[tg_ * 128:(tg_ + 1) * 128, :], reads=[x1_key], writes=[xx])
            p.tt("dve", yacc[:, ti, :], yacc[:, ti, :], modt[:, mo + 2048:mo + 3072], ALU.mult, [yacc, modt], [yacc])
            p.tt("dve", xx[:], xx[:], yacc[:, ti, :], ALU.add, [xx, yacc], [xx])
            if final_g is not None:
                p.act(h[:], xx[:], AF.Square, [xx], [h, ss], accum_out=ss[:])
                p.act(rstd[:], ss[:], AF.Sqrt, [ss], [rstd], scale=1.0 / D, bias=EPS)
                p.op("dve", lambda e: e.reciprocal(rstd[:], rstd[:]), [rstd], [rstd])
                p.op("dve", lambda e: e.scalar_tensor_tensor(xx[:], xx[:], rstd[:], final_g[:], ALU.mult, ALU.mult),
                     [xx, rstd, final_g], [xx])
            p.dma("sp", xo_d[tg_ * 128:(tg_ + 1) * 128, :], xx[:], reads=[xx], writes=[xo_key])


def ffn_tiles(p):
    TQ = T // NQ_FFN
    return dict(
        hfT=p.sb("hfT", [128, 8, T], BF16),
        xt=[p.sb("fxt%d" % i, [128, D], F32) for i in range(2)],
        h=p.sb("fh", [128, D], F32),
        st=(p.sb("fss", [128, 1], F32), p.sb("frstd", [128, 1], F32)),
        yacc=p.sb("yacc", [128, TQ // 128, D], F32),
        actT=p.sb("actT", [128, 8, TQ], BF16),
        w2b=p.sb("w2b", [128, 8, D], BF16),
        wst=[p.sb("wst%d" % i, [128, 8, 128], F32) for i in range(2)],
        w2st=[p.sb("w2st%d" % i, [128, D], F32) for i in range(1)] * 2,
        wgb=[p.sb("wgb%d" % i, [128, 8, 128], BF16) for i in range(2)],
        wub=[p.sb("wub%d" % i, [128, 8, 128], BF16) for i in range(2)],
        tg=p.sb("tg", [128, 512], F32), tsg=p.sb("tsg", [128, 512], F32), tu=p.sb("tu", [128, 512], F32))


def build_l2(ctx=None):
    do_ffn = False
    ctx = stage_begin(ctx)
    nc, p, cm = ctx.nc, ctx.p, ctx.cm
    xs = dram_in(nc, "xs", [T, D])
    o_f = dram_in(nc, "o_f", [T, D])
    o_b = dram_in(nc, "o_b", [T, D])
    sg = dram_in(nc, "sg", [T, D], BF16)
    QF = dram_in(nc, "QF", [NH, 128, T], BF16)
    QB = dram_in(nc, "QB", [NH, 128, T], BF16)
    SlocG = dram_in(nc, "SlocG", [8 * 2 * NH * 128, 128])
    DlocG = dram_in(nc, "DlocG", [8 * 128, 16])
    Sctx = dram_in(nc, "Sctx", [2, NH, 128, 128])
    fmask = dram_in(nc, "fmask", [128, 16])
    ccol = dram_in(nc, "ccol", [128, 8])
    w_ada = dram_in(nc, "w_ada0", [D, 6 * D])
    b_ada = dram_in(nc, "b_ada0", [1, 6 * D])
    gout = dram_in(nc, "gout", [1, D])
    w_out = dram_in(nc, "w_hout", [D, D])
    x1_d = nc.dram_tensor("x1_scr", [T, D], F32, kind="Internal").ap() if do_ffn else dram_out(nc, "x1", [T, D])
    x2_d = dram_out(nc, "x2", [T, D]) if do_ffn else None

    modm = p.sb("modm", [128, 4096], F32)
    emit_mods(p, cm, w_ada, b_ada, 2048, 6144, [(ccol, modm)], "b")

    fm = p.sb("fm", [128, 16], F32)
    DA = p.sb("DA", [128, 8, 16], F32)
    p.dma("sp", fm[:], fmask, writes=[fm])
    for i in range(8):
        p.dma("sp", DA[:, i, :], DlocG[i * 128:(i + 1) * 128, :], reads=["DlocG"], writes=[DA])
    Sin = p.sb("Sin", [128, 2, NH, 128], BF16)
    Sf = p.sb("Sfold", [128, 128], F32)
    Sl = [p.sb("Sl%d" % i, [128, 128], F32) for i in range(2)]
    tmp = p.sb("ftmp", [128, 128], F32)
    k = 0
    for d in range(2):
        for hh in range(NH):
            p.dma("sp", Sf[:], Sctx[d, hh], writes=[Sf])
            order = range(8) if d == 0 else range(7, -1, -1)
            for i in order:
                sl = Sl[k % 2]
                k += 1
                r0 = (i * 2 * NH + d * NH + hh) * 128
                p.dma("sp", sl[:], SlocG[r0:r0 + 128, :], reads=["SlocG"], writes=[sl])
                col = d * 8 + hh
                p.op("dve", lambda e: e.scalar_tensor_tensor(tmp[:], Sf[:], DA[:, i, col:col + 1], sl[:], ALU.mult, ALU.add),
                     [Sf, DA, sl], [tmp])
                p.tt("dve", tmp[:], tmp[:], Sf[:], ALU.subtract, [tmp, Sf], [tmp])
                p.op("dve", lambda e: e.scalar_tensor_tensor(Sf[:], tmp[:], fm[:, d * 8 + i:d * 8 + i + 1], Sf[:], ALU.mult, ALU.add),
                     [tmp, fm, Sf], [Sf])
            p.copy("act", Sin[:, d, hh, :], Sf[:], [Sf], [Sin])

    goutb = p.sb("goutb", [128, D], F32)
    p.dma("sp", goutb[:], gout.partition_broadcast(128), writes=[goutb])
    wo = p.sb("wo", [128, 8, D], BF16)
    wos = [p.sb("wos%d" % i, [128, D], F32) for i in range(2)]
    for kc in range(8):
        p.dma("sp", wos[kc % 2][:], w_out[kc * 128:(kc + 1) * 128, :], writes=[wos[kc % 2]])
        p.copy("act", wo[:, kc, :], wos[kc % 2][:], [wos[kc % 2]], [wo])

    xt = [p.sb("xt%d" % i, [128, D], F32) for i in range(2)]
    of_t = [p.sb("oft%d" % i, [128, D], F32) for i in range(2)]
    ob_t = [p.sb("obt%d" % i, [128, D], F32) for i in range(2)]
    sg_t = [p.sb("sgt%d" % i, [128, D], BF16) for i in range(2)]
    qf_t = [p.sb("qft%d" % i, [128, NH, 128], BF16) for i in range(2)]
    qb_t = [p.sb("qbt%d" % i, [128, NH, 128], BF16) for i in range(2)]
    sq = p.sb("sq", [128, D], F32)
    ssq = p.sb("ssq", [128, NH], F32)
    ogT = p.sb("ogT", [128, 8, 128], BF16)
    for t in range(T // 128):
        k = t % 2
        tok = slice(t * 128, (t + 1) * 128)
        p.dma("sp", xt[k][:], xs[tok, :], writes=[xt[k]])
        p.dma("sp", of_t[k][:], o_f[tok, :], writes=[of_t[k]])
        p.dma("sp", ob_t[k][:], o_b[tok, :], writes=[ob_t[k]])
        p.dma("sp", sg_t[k][:], sg[tok, :], writes=[sg_t[k]])
        p.dma("sp", qf_t[k][:], QF[:, :, tok].rearrange("h q t -> q h t"), writes=[qf_t[k]])
        p.dma("sp", qb_t[k][:], QB[:, :, tok].rearrange("h q t -> q h t"), writes=[qb_t[k]])
        o = of_t[k]
        p.tt("dve", o[:], o[:], ob_t[k][:], ALU.add, [o, ob_t[k]], [o])
        for half in range(2):
            ps = cm.psb[half]
            for j in range(4):
                hh = half * 4 + j
                p.mm(ps[:, j * 128:(j + 1) * 128], qf_t[k][:, hh, :], Sin[:, 0, hh, :], True, False, [qf_t[k], Sin], [ps])
                p.mm(ps[:, j * 128:(j + 1) * 128], qb_t[k][:, hh, :], Sin[:, 1, hh, :], False, True, [qb_t[k], Sin], [ps])
            p.tt("dve", o[:, half * 512:(half + 1) * 512], o[:, half * 512:(half + 1) * 512], ps[:], ALU.add, [o, ps], [o])
        p.tt("dve", sq[:], o[:], o[:], ALU.mult, [o], [sq])
        p.op("dve", lambda e: e.tensor_reduce(ssq[:], sq[:].rearrange("q (h v) -> q h v", h=NH), AX.X, ALU.add), [sq], [ssq])
        p.act(ssq[:], ssq[:], AF.Sqrt, [ssq], [ssq], scale=1.0 / 128, bias=EPS)
        p.op("dve", lambda e: e.reciprocal(ssq[:], ssq[:]), [ssq], [ssq])
        p.tt("dve", o[:].rearrange("q (h v) -> q h v", h=NH), o[:].rearrange("q (h v) -> q h v", h=NH),
             ssq[:].rearrange("q (h u) -> q h u", u=1).to_broadcast([128, NH, 128]), ALU.mult, [o, ssq], [o])
        p.tt("dve", o[:], o[:], goutb[:], ALU.mult, [o, goutb], [o])
        p.tt("dve", o[:], o[:], sg_t[k][:], ALU.mult, [o, sg_t[k]], [o])
        for half in range(2):
            pt = cm.psb[2 + half]
            for j in range(4):
                kc = half * 4 + j
                p.op("pe", lambda e: e.transpose(pt[:, j * 128:(j + 1) * 128], o[:, kc * 128:(kc + 1) * 128], cm.identf[:]),
                     [o, cm.identf], [pt])
            p.copy("act", ogT[:, half * 4:(half + 1) * 4, :], pt[:].rearrange("q (a b) -> q a b", a=4), [pt], [ogT])
        for dh in range(2):
            ps = cm.psb[4 + dh]
            for kc in range(8):
                p.mm(ps[:], ogT[:, kc, :], wo[:, kc, dh * 512:(dh + 1) * 512], kc == 0, kc == 7, [ogT, wo], [ps])
            p.tt("dve", sq[:, dh * 512:(dh + 1) * 512], ps[:], modm[:, dh * 512:(dh + 1) * 512], ALU.mult, [ps, modm], [sq])
        p.tt("dve", xt[k][:], xt[k][:], sq[:], ALU.add, [xt[k], sq], [xt[k]])
        p.dma("sp", x1_d[tok, :], xt[k][:], reads=[xt[k]], writes=["x1"])
    if do_ffn:
        tl = ffn_tiles(p)
        emit_ffn(p, cm, x1_d, "x1", x2_d, "x2", modm, 1024, gffn, wr_d, br_d, w1_d, b1c_d, w2_d, b2_d, tl)
    return stage_end(ctx, ["x1"])


def build_ffn(final, ctx=None, layer=0, in_name="x1", out_name="x2"):
    assert ctx is not None, "fused build only"
    nc, p, cm = ctx.nc, ctx.p, ctx.cm
    L = str(layer)
    x1_d = dram_in(nc, in_name, [T, D])
    ccol = dram_in(nc, "ccol", [128, 8])
    w_ada = dram_in(nc, "w_ada" + L, [D, 6 * D])
    b_ada = dram_in(nc, "b_ada" + L, [1, 6 * D])
    gffn_d = dram_in(nc, "g_ffn" + L, [1, D])
    wr_d = dram_in(nc, "w_router" + L, [D, NE])
    br_d = dram_in(nc, "b_router" + L, [1, NE])
    w1_d = dram_in(nc, "w_exp_in" + L, [NE, D, 2 * D])
    b1c_d = dram_in(nc, "b1c" + L, [128, NE, 16])
    w2_d = dram_in(nc, "w_exp_out" + L, [NE, D, D])
    b2_d = dram_in(nc, "b_exp_out" + L, [NE, D])
    gfin_d = dram_in(nc, "g_final", [1, D])
    xo_d = dram_out(nc, out_name, [T, D])
    hfT_scr = nc.dram_tensor("hfT_scr" + L, [8, 128, T], BF16, kind="Internal").ap()
    G_scr = nc.dram_tensor("G_scr" + L, [128, T // 128, NE], F32, kind="Internal").ap()
    gtf_scr = nc.dram_tensor("gtf_scr" + L, [1, D], F32, kind="Internal").ap()

    ctx.standalone = False
    p.begin_phase()
    modf = p.sb("modf", [128, 3072], F32)
    emit_mods(p, cm, w_ada, b_ada, 3072, 6144, [(ccol, modf)], "f")
    p.dma("sp", gtf_scr, modf[0:1, 2048:3072], reads=[modf], writes=["gtf_scr"])
    xt = [p.sb("fxt%d" % i, [128, D], F32) for i in range(2)]
    h = p.sb("fh", [128, D], F32)
    ss, rstd = p.sb("fss", [128, 1], F32), p.sb("frstd", [128, 1], F32)
    p.dma("sp", h[:], gffn_d.partition_broadcast(128), writes=[h])
    p.op("dve", lambda e: e.scalar_tensor_tensor(modf[:, 1024:2048], modf[:, 1024:2048], 1.0, h[:], ALU.add, ALU.mult),
         [modf, h], [modf])
    wr = p.sb("wr", [128, 8, NE], F32)
    brb = p.sb("brb", [128, NE], F32)
    p.dma("sp", wr[:], wr_d.rearrange("(kc q) n -> q kc n", q=128), writes=[wr])
    p.dma("sp", brb[:], br_d.partition_broadcast(128), writes=[brb])
    G = p.sb("G_all", [128, T // 128, NE], F32)
    hTf = p.sb("hTf", [128, 8, 128], F32)
    hTb = [p.sb("hTb%d" % i, [128, 8, 128], BF16) for i in range(2)]
    lg = p.sb("lg", [128, NE], F32)
    m8 = p.sb("m8", [128, 8], F32)
    nmx = p.sb("nmx", [128, 1], F32)
    msk = p.sb("msk", [128, NE], F32)
    ex = p.sb("ex", [128, NE], F32)
    sm = p.sb("sm", [128, 1], F32)
    ps_l = cm.psb[2]
    for t in range(T // 128):
        xx = xt[t % 2]
        hb = hTb[t % 2]
        p.dma("sp", xx[:], x1_d[t * 128:(t + 1) * 128, :], writes=[xx])
        p.act(h[:], xx[:], AF.Square, [xx], [h, ss], accum_out=ss[:])
        p.act(rstd[:], ss[:], AF.Sqrt, [ss], [rstd], scale=1.0 / D, bias=EPS)
        p.op("dve", lambda e: e.reciprocal(rstd[:], rstd[:]), [rstd], [rstd])
        p.op("dve", lambda e: e.scalar_tensor_tensor(h[:], xx[:], rstd[:], modf[:, 1024:2048], ALU.mult, ALU.mult),
             [xx, rstd, modf], [h])
        p.tt("dve", h[:], h[:], modf[:, 0:1024], ALU.add, [h, modf], [h])
        for half in range(2):
            pt = cm.psb[half]
            for j in range(4):
                kc = half * 4 + j
                p.op("pe", lambda e: e.transpose(pt[:, j * 128:(j + 1) * 128], h[:, kc * 128:(kc + 1) * 128], cm.identf[:]),
                     [h, cm.identf], [pt])
            p.copy("dve", hTf[:, half * 4:(half + 1) * 4, :], pt[:].rearrange("q (a b) -> q a b", a=4), [pt], [hTf])
        p.copy("act", hb[:], hTf[:], [hTf], [hb])
        p.dma("sp", hfT_scr[:, :, t * 128:(t + 1) * 128].rearrange("k q t -> q k t"), hb[:], reads=[hb], writes=["hfT_scr"])
        for kc in range(8):
            p.mm(ps_l[:, 0:NE], hTf[:, kc, :], wr[:, kc, :], kc == 0, kc == 7, [hTf, wr], [ps_l])
        p.tt("dve", lg[:], ps_l[:, 0:NE], brb[:], ALU.add, [ps_l, brb], [lg])
        p.op("dve", lambda e: e.max(m8[:], lg[:]), [lg], [m8])
        p.ts("dve", msk[:], lg[:], m8[:, 3:4], None, ALU.is_ge, None, [lg, m8], [msk])
        p.ts("dve", nmx[:], m8[:, 0:1], -1.0, None, ALU.mult, None, [m8], [nmx])
        p.act(ex[:], lg[:], AF.Exp, [lg, nmx], [ex], bias=nmx[:])
        p.tt("dve", ex[:], ex[:], msk[:], ALU.mult, [ex, msk], [ex])
        p.op("dve", lambda e: e.tensor_reduce(sm[:], ex[:], AX.X, ALU.add), [ex], [sm])
        p.op("dve", lambda e: e.reciprocal(sm[:], sm[:]), [sm], [sm])
        p.ts("dve", G[:, t, :], ex[:], sm[:], None, ALU.mult, None, [ex, sm], [G])
    p.dma("sp", G_scr, G[:], reads=[G], writes=["G_scr"])
    p.end_phase()

    p.begin_phase()
    NPASS = 4
    TH = T // NPASS
    NB = TH // 512
    G = p.sb("G_all", [128, T // 128, NE], F32)
    gtf = p.sb("gtf", [128, D], F32)
    b2s = p.sb("b2s", [NE, D], F32)
    b1c = p.sb("b1c", [128, NE, 16], F32)
    p.dma("sp", G[:], G_scr, writes=[G])
    p.dma("sp", gtf[:], gtf_scr.partition_broadcast(128), writes=[gtf])
    p.dma("sp", b2s[:], b2_d, writes=[b2s])
    p.dma("sp", b1c[:], b1c_d, writes=[b1c])
    gfin = None
    if final:
        gfin = p.sb("gfin", [128, D], F32)
        p.dma("sp", gfin[:], gfin_d.partition_broadcast(128), writes=[gfin])
    yacc = p.sb("yacc", [128, TH // 128, D], F32)
    w1b = [p.sb("w1b%d" % i, [128, 8, 2 * D], BF16) for i in range(2)]
    w2b = [p.sb("w2b%d" % i, [128, 8, D], BF16) for i in range(2)]
    stg = [p.sb("stg%d" % i, [128, D], F32) for i in range(3)]
    hbk = [p.sb("hbk%d" % i, [128, 8, 512], BF16) for i in range(2)]
    actT = [p.sb("actT%d" % i, [128, 8, 512], BF16) for i in range(2)]
    tg = [p.sb("tg0", [128, 512], F32)]
    tsg = p.sb("tsg", [128, 512], F32)
    tu = [p.sb("tu0", [128, 512], F32)]
    xxk, hhk = stg[1], stg[0]
    xx, hh_ = stg[1][:, 0:D], stg[0][:, 0:D]
    ss, rstd = p.sb("fss", [128, 1], F32), p.sb("frstd", [128, 1], F32)
    GTt = p.sb("GTt", [NE, 128], F32)
    psg, psu, psy, ps_l = [cm.psb[0], cm.psb[1]], [cm.psb[2], cm.psb[3]], [cm.psb[4], cm.psb[5]], cm.psb[6]

    def pieces(e, k):
        out = []
        for kc in range(8):
            for hf in range(2):
                def d(sb_, kc=kc, hf=hf):
                    p.dma("sp", sb_[:], w1_d[e, kc * 128:(kc + 1) * 128, hf * D:(hf + 1) * D], writes=[sb_])
                def c(sb_, eng, kc=kc, hf=hf):
                    p.copy(eng, w1b[k][:, kc, hf * D:(hf + 1) * D], sb_[:], [sb_], [w1b[k]])
                out.append((d, c))
        for jj in range(8):
            def d(sb_, jj=jj):
                p.dma("sp", sb_[:], w2_d[e, jj * 128:(jj + 1) * 128, :], writes=[sb_])
            def c(sb_, eng, jj=jj):
                p.copy(eng, w2b[k][:, jj, :], sb_[:], [sb_], [w2b[k]])
            out.append((d, c))
        return out

    NPC = 24
    ROUNDS = NPC // (NB * 3)
    JPR = 8 // ROUNDS
    assert ROUNDS * NB * 3 == NPC and JPR * ROUNDS == 8
    nblk = 0
    ncast = 0
    njj = 0
    for half in range(NPASS):
        for ti in range(TH // 128):
            tg_ = half * (TH // 128) + ti
            p.op("pe", lambda e: e.transpose(ps_l[0:NE, 0:128], G[:, tg_, :], cm.identf[:]), [G, cm.identf], [ps_l])
            p.copy("dve", GTt[:], ps_l[0:NE, 0:128], [ps_l], [GTt])
            for dh in range(2):
                py = psy[dh]
                p.mm(py[:], GTt[:], b2s[:, dh * 512:(dh + 1) * 512], True, True, [GTt, b2s], [py])
                p.copy("act", yacc[:, ti, dh * 512:(dh + 1) * 512], py[:], [py], [yacc])
        for i, (d_, c_) in enumerate(pieces(0, 0)):
            sb_ = stg[i % 3]
            d_(sb_)
            c_(sb_, "dve" if i % 2 else "act")
        hb = hbk[nblk % 2]
        p.dma("sp", hb[:], hfT_scr[:, :, half * TH:half * TH + 512].rearrange("k q t -> q k t"), reads=["hfT_scr"], writes=[hb])
        for ex_ in range(NE):
            k = ex_ % 2
            nxt = pieces(ex_ + 1, 1 - k) if ex_ + 1 < NE else []
            pend = None
            for tb in range(NB):
                hb = hbk[nblk % 2]
                nblk += 1
                if tb + 1 < NB or ex_ + 1 < NE:
                    ntb = (tb + 1) % NB
                    hn = hbk[nblk % 2]
                    p.dma("sp", hn[:], hfT_scr[:, :, half * TH + ntb * 512:half * TH + (ntb + 1) * 512].rearrange("k q t -> q k t"),
                          reads=["hfT_scr"], writes=[hn])
                at = actT[tb % 2]
                for rd in range(ROUNDS):
                    mine = nxt[(tb * ROUNDS + rd) * 3:(tb * ROUNDS + rd + 1) * 3]
                    for i, (d_, c_) in enumerate(mine):
                        d_(stg[i])
                    for j in range(rd * JPR, (rd + 1) * JPR):
                        pg, pu = psg[njj % 2], psu[njj % 2]
                        g1, u1 = tg[0], tu[0]
                        njj += 1
                        for kc in range(8):
                            p.mm(pg[:], w1b[k][:, kc, j * 128:(j + 1) * 128], hb[:, kc, :], kc == 0, kc == 7, [w1b[k], hb], [pg])
                        for kc in range(8):
                            p.mm(pu[:], w1b[k][:, kc, D + j * 128:D + (j + 1) * 128], hb[:, kc, :], kc == 0, kc == 7, [w1b[k], hb], [pu])
                        p.ts("dve", g1[:], pg[:], b1c[:, ex_, j:j + 1], 7.0, ALU.add, ALU.min, [pg, b1c], [g1])
                        p.act(tsg[:], g1[:], AF.Sigmoid, [g1], [tsg], scale=1.702)
                        p.act(u1[:], pu[:], AF.Identity, [pu, b1c], [u1], bias=b1c[:, ex_, 8 + j:9 + j])
                        p.ts("dve", u1[:], u1[:], 7.0, -7.0, ALU.min, ALU.max, [u1], [u1])
                        p.tt("dve", g1[:], g1[:], tsg[:], ALU.mult, [g1, tsg], [g1])
                        p.op("dve", lambda e: e.scalar_tensor_tensor(at[:, j, :], u1[:], 1.0, g1[:], ALU.add, ALU.mult),
                             [u1, g1], [at])
                    for i, (d_, c_) in enumerate(mine):
                        c_(stg[i], "act")
                        ncast += 1
                todo = [pend] if pend is not None else []
                pend = (tb, at)
                if tb == NB - 1:
                    todo.append(pend)
                    pend = None
                for (tb2, at2) in todo:
                    for ti in range(4):
                        tl_ = tb2 * 4 + ti
                        tg_ = half * (TH // 128) + tl_
                        for dh in range(2):
                            py = psy[dh]
                            for j in range(8):
                                p.mm(py[:], at2[:, j, ti * 128:(ti + 1) * 128], w2b[k][:, j, dh * 512:(dh + 1) * 512], j == 0, j == 7,
                                     [at2, w2b[k]], [py])
                            ya = yacc[:, tl_, dh * 512:(dh + 1) * 512]
                            p.op("dve", lambda e: e.scalar_tensor_tensor(ya, py[:], G[:, tg_, ex_:ex_ + 1], ya, ALU.mult, ALU.add),
                                 [py, G, yacc], [yacc])
        for ti in range(TH // 128):
            tg_ = half * (TH // 128) + ti
            p.dma("sp", xx, x1_d[tg_ * 128:(tg_ + 1) * 128, :], writes=[xxk])
            p.tt("dve", yacc[:, ti, :], yacc[:, ti, :], gtf[:], ALU.mult, [yacc, gtf], [yacc])
            p.tt("dve", xx, xx, yacc[:, ti, :], ALU.add, [xxk, yacc], [xxk])
            if final:
                p.act(hh_, xx, AF.Square, [xxk], [hhk, ss], accum_out=ss[:])
                p.act(rstd[:], ss[:], AF.Sqrt, [ss], [rstd], scale=1.0 / D, bias=EPS)
                p.op("dve", lambda e: e.reciprocal(rstd[:], rstd[:]), [rstd], [rstd])
                p.op("dve", lambda e: e.scalar_tensor_tensor(xx, xx, rstd[:], gfin[:], ALU.mult, ALU.mult),
                     [xxk, rstd, gfin], [xxk])
            p.dma("sp", xo_d[tg_ * 128:(tg_ + 1) * 128, :], xx, reads=[xxk], writes=["xo"])
    p.end_phase()
    return nc, p


def ffn_inputs(inp, core, layer, x1):
    b = core // 4
    m = {"ident": np.eye(128, dtype=np.float32)}
    m["x1"] = np.ascontiguousarray(x1)
    m["ccol"] = col_layout(inp["c"][b])
    m["w_ada"] = np.ascontiguousarray(inp["w_ada"][layer])
    m["b_ada"] = np.ascontiguousarray(inp["b_ada"][layer][None, :])
    m["g_ffn"] = np.ascontiguousarray(inp["g_ffn"][layer][None, :])
    m["w_router"] = np.ascontiguousarray(inp["w_router"][layer])
    m["b_router"] = np.ascontiguousarray(inp["b_router"][layer][None, :])
    m["w_exp_in"] = np.ascontiguousarray(inp["w_exp_in"][layer])
    b1 = np.asarray(inp["b_exp_in"][layer], np.float32).reshape(NE, 16, 128)
    m["b1c"] = np.ascontiguousarray(b1.transpose(2, 0, 1))
    m["w_exp_out"] = np.ascontiguousarray(inp["w_exp_out"][layer])
    m["b_exp_out"] = np.ascontiguousarray(inp["b_exp_out"][layer])
    m["g_final"] = np.ascontiguousarray(inp["g_final"][None, :])
    return m


def l2_inputs(inp, core, r1):
    b, j = core // 4, core % 4
    m = {"ident": np.eye(128, dtype=np.float32)}
    m["xs"] = np.ascontiguousarray(inp["x"][b, j * T:(j + 1) * T])
    for k in ("o_f", "o_b", "sg", "QF", "QB", "Sctx"):
        m[k] = r1[core][k]
    m["SlocA"] = np.ascontiguousarray(np.stack([r1[b * 4 + i]["Sloc"] for i in range(4)]))
    m["DlocA"] = np.ascontiguousarray(np.stack([r1[b * 4 + i]["Dloc"] for i in range(4)]))
    fm = np.zeros((128, 8), np.float32)
    for i in range(4):
        fm[:, i] = 1.0 if i < j else 0.0
        fm[:, 4 + i] = 1.0 if i > j else 0.0
    m["fmask"] = fm
    m["ccol"] = col_layout(inp["c"][b])
    m["w_ada0"] = np.ascontiguousarray(inp["w_ada"][0])
    m["b_ada0"] = np.ascontiguousarray(inp["b_ada"][0][None, :])
    m["gout"] = np.ascontiguousarray(inp["g_hgrn_out"][0][None, :])
    m["w_hout"] = np.ascontiguousarray(inp["w_hgrn_out"][0])
    return m


HALO = 15 * 64


def build_conv_a(ctx=None):
    ctx = stage_begin(ctx)
    nc, p, cm = ctx.nc, ctx.p, ctx.cm
    x2 = dram_in(nc, "x2", [T, D])
    ccol = dram_in(nc, "ccol", [128, 8])
    w_ada = dram_in(nc, "w_ada1", [D, 6 * D])
    b_ada = dram_in(nc, "b_ada1", [1, 6 * D])
    g_mix = dram_in(nc, "g_mix1", [1, D])
    w_in = dram_in(nc, "w_cv_in", [D, 2 * D])
    bin_c = dram_in(nc, "bin_c", [128, 16])
    u_d = dram_out(nc, "u", [8, 128, T])
    gtm_d = dram_out(nc, "gtm", [1, D])
    modb = p.sb("modb", [128, 3072], F32)
    emit_mods(p, cm, w_ada, b_ada, 0, 3072, [(ccol, modb)], "c")
    p.dma("sp", gtm_d, modb[0:1, 2048:3072], reads=[modb], writes=["gtm"])
    gmix = p.sb("gmix", [128, D], F32)
    p.dma("sp", gmix[:], g_mix.partition_broadcast(128), writes=[gmix])
    p.op("dve", lambda e: e.scalar_tensor_tensor(modb[:, 1024:2048], modb[:, 1024:2048], 1.0, gmix[:], ALU.add, ALU.mult),
         [modb, gmix], [modb])
    hT = p.sb("hT", [128, 8, T], BF16)
    xt = [p.sb("xt%d" % i, [128, D], F32) for i in range(2)]
    h = p.sb("h", [128, D], F32)
    st = (p.sb("ss", [128, 1], F32), p.sb("rstd", [128, 1], F32))
    emit_norm_T(p, cm, x2, T // 128, modb[:, 1024:2048], modb[:, 0:1024], modb, hT, None, xt, h, h, st)
    bc = p.sb("bc", [128, 16], F32)
    p.dma("sp", bc[:], bin_c, writes=[bc])
    wst = [p.sb("wst%d" % i, [128, 8, 128], F32) for i in range(2)]
    wab = [p.sb("wab%d" % i, [128, 8, 128], BF16) for i in range(2)]
    wgb = [p.sb("wgb%d" % i, [128, 8, 128], BF16) for i in range(2)]
    sgt = [p.sb("sgt%d" % i, [128, 512], F32) for i in range(2)]
    ut = [p.sb("ut%d" % i, [128, 512], F32) for i in range(2)]
    psa, psg = cm.psb[2], cm.psb[3]
    n = 0
    for j in range(8):
        k = j % 2
        p.dma("sp", wst[0][:], w_in[:, j * 128:(j + 1) * 128].rearrange("(kc q) n -> q kc n", q=128), writes=[wst[0]])
        p.copy("act", wab[k][:], wst[0][:], [wst[0]], [wab[k]])
        p.dma("sp", wst[1][:], w_in[:, 1024 + j * 128:1024 + (j + 1) * 128].rearrange("(kc q) n -> q kc n", q=128), writes=[wst[1]])
        p.copy("act", wgb[k][:], wst[1][:], [wst[1]], [wgb[k]])
        for tb in range(T // 512):
            tok = slice(tb * 512, (tb + 1) * 512)
            for kc in range(8):
                p.mm(psa[:], wab[k][:, kc, :], hT[:, kc, tok], kc == 0, kc == 7, [wab[k], hT], [psa])
            for kc in range(8):
                p.mm(psg[:], wgb[k][:, kc, :], hT[:, kc, tok], kc == 0, kc == 7, [wgb[k], hT], [psg])
            sg_, u_ = sgt[n % 2], ut[n % 2]
            n += 1
            p.act(sg_[:], psg[:], AF.Sigmoid, [psg, bc], [sg_], bias=bc[:, 8 + j:9 + j])
            p.op("dve", lambda e: e.scalar_tensor_tensor(u_[:], psa[:], bc[:, j:j + 1], sg_[:], ALU.add, ALU.mult),
                 [psa, bc, sg_], [u_])
            p.dma("sp", u_d[j, :, tok], u_[:], reads=[u_], writes=["u_d"])
    if not ctx.standalone:
        hb = nc.dram_tensor("halo_b", [8 * 128, HALO], F32, kind="Internal").ap()
        REG["halo_b"] = hb
        for jj in range(4):
            p.dma("sp", hb[jj * 128:(jj + 1) * 128, :], u_d[4 + jj, :, T - HALO:T], reads=["u_d"], writes=["halo_b"])
            p.dma("sp", hb[(4 + jj) * 128:(5 + jj) * 128, :], u_d[4 + jj, :, 0:HALO], reads=["u_d"], writes=["halo_b"])
    return stage_end(ctx, ["u_d", "gtm"])


def conv_a_inputs(inp, core, x2):
    b = core // 4
    m = {"ident": np.eye(128, dtype=np.float32)}
    m["x2"] = np.ascontiguousarray(x2)
    m["ccol"] = col_layout(inp["c"][b])
    m["w_ada"] = np.ascontiguousarray(inp["w_ada"][1])
    m["b_ada"] = np.ascontiguousarray(inp["b_ada"][1][None, :])
    m["g_mix"] = np.ascontiguousarray(inp["g_mix"][1][None, :])
    m["w_cv_in"] = np.ascontiguousarray(inp["w_cv_in"][0])
    m["bin_c"] = np.ascontiguousarray(np.asarray(inp["b_cv_in"][0], np.float32).reshape(16, 128).T)
    return m


def build_conv_b(ctx=None):
    ctx = stage_begin(ctx)
    nc, p, cm = ctx.nc, ctx.p, ctx.cm
    x2 = dram_in(nc, "x2", [T, D])
    u_d = dram_in(nc, "u", [8, 128, T])
    G2 = dram_in(nc, "halo_g", [8 * 8 * 128, HALO])
    sel_d = dram_in(nc, "sel", [128, 16])
    gtm_d = dram_in(nc, "gtm", [1, D])
    wdw_d = dram_in(nc, "wdw", [128, 8, 31])
    cvec_d = dram_in(nc, "cvec", [128, 3, 8])
    wout_d = dram_in(nc, "w_cv_out", [D, D])
    bout_d = dram_in(nc, "b_cv_out", [1, D])
    x3_d = dram_out(nc, "x3", [T, D])
    wdw = p.sb("wdw", [128, 8, 31], F32)
    cvec = p.sb("cvec", [128, 3, 8], F32)
    gtm = p.sb("gtm", [128, D], F32)
    bout = p.sb("bout", [128, D], F32)
    sel = p.sb("sel", [128, 16], F32)
    p.dma("sp", sel[:], sel_d, writes=[sel])
    p.dma("sp", wdw[:], wdw_d, writes=[wdw])
    p.dma("sp", cvec[:], cvec_d, writes=[cvec])
    p.dma("sp", gtm[:], gtm_d.partition_broadcast(128), writes=[gtm])
    p.dma("sp", bout[:], bout_d.partition_broadcast(128), writes=[bout])
    wo = p.sb("wo", [128, 8, D], BF16)
    wos = p.sb("wos", [128, D], F32)
    for kc in range(8):
        p.dma("sp", wos[:], wout_d[kc * 128:(kc + 1) * 128, :], writes=[wos])
        p.copy("act", wo[:, kc, :], wos[:], [wos], [wo])
    z = p.sb("z_all", [128, 8, T], F32)
    ue = p.sb("ue", [128, T + 2 * HALO], F32)
    for j in range(8):
        zj = z[:, j, :]
        zkey = ("z", j)
        if j < 4:
            p.dma("sp", ue[:, 0:T], u_d[j], writes=[ue])
            p.ts("dve", zj, ue[:, 0:T], wdw[:, j, 15:16], cvec[:, 0, j:j + 1], ALU.mult, ALU.add, [ue, wdw, cvec], [zkey])
            z3 = zj.rearrange("q (r w) -> q r w", w=64)
            u3 = ue[:, 0:T].rearrange("q (r w) -> q r w", w=64)
            for o in range(-15, 16):
                if o == 0:
                    continue
                lo, hi = max(0, -o), min(64, 64 - o)
                p.op("dve", lambda e: e.scalar_tensor_tensor(z3[:, :, lo:hi], u3[:, :, lo + o:hi + o], wdw[:, j, o + 15:o + 16],
                                                             z3[:, :, lo:hi], ALU.mult, ALU.add), [ue, wdw, zkey], [zkey])
        else:
            p.dma("sp", ue[:, HALO:HALO + T], u_d[j], writes=[ue])
            for side, (c0, dst) in enumerate(((0, ue[:, 0:HALO]), (8, ue[:, HALO + T:HALO + T + HALO]))):
                for r in range(8):
                    r0 = r * 1024 + ((0 if side == 0 else 4) + (j - 4)) * 128
                    stg = wos[:, 0:HALO]
                    p.dma("sp", stg, G2[r0:r0 + 128, :], reads=["halo_g"], writes=[wos])
                    if r == 0:
                        p.ts("dve", dst, stg, sel[:, c0:c0 + 1], None, ALU.mult, None, [wos, sel], [ue])
                    else:
                        p.op("dve", lambda e: e.scalar_tensor_tensor(dst, stg, sel[:, c0 + r:c0 + r + 1], dst, ALU.mult, ALU.add),
                             [wos, sel, ue], [ue])
            p.ts("dve", zj, ue[:, HALO:HALO + T], wdw[:, j, 15:16], cvec[:, 0, j:j + 1], ALU.mult, ALU.add, [ue, wdw, cvec], [zkey])
            for o in range(-15, 16):
                if o == 0:
                    continue
                a0 = HALO + o * 64
                p.op("dve", lambda e: e.scalar_tensor_tensor(zj, ue[:, a0:a0 + T], wdw[:, j, o + 15:o + 16], zj, ALU.mult, ALU.add),
                     [ue, wdw, zkey], [zkey])
    zkeys = [("z", j) for j in range(8)]
    sqt = [p.sb("sqt%d" % i, [128, 512], F32) for i in range(2)]
    mean = p.sb("mean", [128, 512], F32)
    msq = p.sb("msq", [128, 512], F32)
    rstd = p.sb("rstdv", [128, 512], F32)
    zn = [p.sb("zn0", [128, 512], F32)] * 2
    sgm = [p.sb("sgm0", [128, 512], F32)] * 2
    zs = p.sb("zs", [128, 8, 512], BF16)
    yt = wos
    ps1, ps2 = cm.psb[0], cm.psb[1]
    psy = [cm.psb[2], cm.psb[3]]
    n = 0
    for tb in range(T // 512):
        tok = slice(tb * 512, (tb + 1) * 512)
        for j in range(8):
            sq = sqt[n % 2]
            n += 1
            p.act(sq[:], z[:, j, tok], AF.Square, [zkeys[j]], [sq])
            p.mm(ps1[:], cm.ones[:], z[:, j, tok], j == 0, j == 7, [cm.ones, zkeys[j]], [ps1])
            p.mm(ps2[:], cm.ones[:], sq[:], j == 0, j == 7, [cm.ones, sq], [ps2])
        p.act(mean[:], ps1[:], AF.Copy, [ps1], [mean], scale=1.0 / D)
        p.tt("dve", msq[:], mean[:], mean[:], ALU.mult, [mean], [msq])
        p.op("dve", lambda e: e.scalar_tensor_tensor(rstd[:], ps2[:], 1.0 / D, msq[:], ALU.mult, ALU.subtract), [ps2, msq], [rstd])
        p.act(rstd[:], rstd[:], AF.Sqrt, [rstd], [rstd], bias=EPS)
        p.op("dve", lambda e: e.reciprocal(rstd[:], rstd[:]), [rstd], [rstd])
        for j in range(8):
            a, sg_ = zn[j % 2], sgm[j % 2]
            p.tt("dve", a[:], z[:, j, tok], mean[:], ALU.subtract, [zkeys[j], mean], [a])
            p.tt("dve", a[:], a[:], rstd[:], ALU.mult, [a, rstd], [a])
            p.ts("dve", a[:], a[:], cvec[:, 1, j:j + 1], cvec[:, 2, j:j + 1], ALU.mult, ALU.add, [a, cvec], [a])
            p.act(sg_[:], a[:], AF.Sigmoid, [a], [sg_])
            p.tt("dve", zs[:, j, :], a[:], sg_[:], ALU.mult, [a, sg_], [zs])
        for ti in range(4):
            t = tb * 4 + ti
            xa = ue[:, 0:D]
            p.dma("sp", xa, x2[t * 128:(t + 1) * 128, :], writes=[ue])
            for dh in range(2):
                ps = psy[dh]
                for j in range(8):
                    p.mm(ps[:], zs[:, j, ti * 128:(ti + 1) * 128], wo[:, j, dh * 512:(dh + 1) * 512], j == 0, j == 7, [zs, wo], [ps])
                p.tt("dve", yt[:, dh * 512:(dh + 1) * 512], ps[:], bout[:, dh * 512:(dh + 1) * 512], ALU.add, [ps, bout], [yt])
            p.tt("dve", yt[:], yt[:], gtm[:], ALU.mult, [yt, gtm], [yt])
            p.tt("dve", xa, xa, yt[:], ALU.add, [ue, yt], [ue])
            p.dma("sp", x3_d[t * 128:(t + 1) * 128, :], xa, reads=[ue], writes=["x3"])
    return stage_end(ctx, ["x3"])


def conv_b_inputs(inp, core, x2, ra):
    b, j = core // 4, core % 4
    m = {"ident": np.eye(128, dtype=np.float32)}
    m["x2"] = np.ascontiguousarray(x2)
    m["u"] = ra[core]["u"]
    zero = np.zeros((4, 128, HALO), np.float32)
    m["u_prev"] = np.ascontiguousarray(ra[core - 1]["u"][4:8, :, T - HALO:T]) if j > 0 else zero
    m["u_next"] = np.ascontiguousarray(ra[core + 1]["u"][4:8, :, 0:HALO]) if j < 3 else zero
    m["gtm"] = ra[core]["gtm"]
    w = np.asarray(inp["w_cv_dw"][0], np.float32)
    m["wdw"] = np.ascontiguousarray(w.reshape(31, 8, 128).transpose(2, 1, 0))
    cv = np.stack([np.asarray(inp[k][0], np.float32).reshape(8, 128).T for k in ("b_cv_dw", "g_cv_ln", "b_cv_ln")], axis=1)
    m["cvec"] = np.ascontiguousarray(cv)
    m["w_cv_out"] = np.ascontiguousarray(inp["w_cv_out"][0])
    m["b_cv_out"] = np.ascontiguousarray(inp["b_cv_out"][0][None, :])
    return m


_DBG = {}


def build_fused():
    REG.clear()
    FUSED[0] = True
    try:
        ctx = Ctx()
        nc, p = ctx.nc, ctx.p
        ctx.cm = Common(p, nc)
        build_l1(ctx)
        SlocG = nc.dram_tensor("SlocG", [8 * 2 * NH * 128, 128], F32, kind="Internal").ap()
        DlocG = nc.dram_tensor("DlocG", [8 * 128, 16], F32, kind="Internal").ap()
        REG["SlocG"], REG["DlocG"] = SlocG, DlocG
        p.allgather(REG["Sloc"].rearrange("d h k v -> (d h k) v"), SlocG, "Sloc_d", "SlocG")
        p.allgather(REG["Dloc"], DlocG, "Dloc_d", "DlocG")
        build_l2(ctx)
        build_ffn(False, ctx, 0, "x1", "x2")
        build_conv_a(ctx)
        halo_g = nc.dram_tensor("halo_g", [8 * 8 * 128, HALO], F32, kind="Internal").ap()
        REG["halo_g"] = halo_g
        p.allgather(REG["halo_b"], halo_g, "halo_b", "halo_g")
        build_conv_b(ctx)
        build_ffn(True, ctx, 1, "x3", FINAL_OUT)
        p.barrier()
    finally:
        FUSED[0] = False
    return nc, p


def fused_inputs(inp, core):
    b, j = core // 4, core % 4
    m = l1_inputs(inp, core)
    fm = np.zeros((128, 16), np.float32)
    sel = np.zeros((128, 16), np.float32)
    for r in range(8):
        if r // 4 == b:
            fm[:, r] = 1.0 if (r % 4) < j else 0.0
            fm[:, 8 + r] = 1.0 if (r % 4) > j else 0.0
    if j > 0:
        sel[:, core - 1] = 1.0
    if j < 3:
        sel[:, 8 + core + 1] = 1.0
    m["fmask"], m["sel"] = fm, sel
    m["gout"] = np.ascontiguousarray(inp["g_hgrn_out"][0][None, :])
    m["w_hout"] = np.ascontiguousarray(inp["w_hgrn_out"][0])
    for layer in range(2):
        L = str(layer)
        m["w_ada" + L] = np.ascontiguousarray(inp["w_ada"][layer])
        m["b_ada" + L] = np.ascontiguousarray(inp["b_ada"][layer][None, :])
        m["g_ffn" + L] = np.ascontiguousarray(inp["g_ffn"][layer][None, :])
        m["w_router" + L] = np.ascontiguousarray(inp["w_router"][layer])
        m["b_router" + L] = np.ascontiguousarray(inp["b_router"][layer][None, :])
        m["w_exp_in" + L] = np.ascontiguousarray(inp["w_exp_in"][layer])
        b1 = np.asarray(inp["b_exp_in"][layer], np.float32).reshape(NE, 16, 128)
        m["b1c" + L] = np.ascontiguousarray(b1.transpose(2, 0, 1))
        m["w_exp_out" + L] = np.ascontiguousarray(inp["w_exp_out"][layer])
        m["b_exp_out" + L] = np.ascontiguousarray(inp["b_exp_out"][layer])
    m["g_final"] = np.ascontiguousarray(inp["g_final"][None, :])
    m["g_mix1"] = np.ascontiguousarray(inp["g_mix"][1][None, :])
    m["w_cv_in"] = np.ascontiguousarray(inp["w_cv_in"][0])
    m["bin_c"] = np.ascontiguousarray(np.asarray(inp["b_cv_in"][0], np.float32).reshape(16, 128).T)
    w = np.asarray(inp["w_cv_dw"][0], np.float32)
    m["wdw"] = np.ascontiguousarray(w.reshape(31, 8, 128).transpose(2, 1, 0))
    cv = np.stack([np.asarray(inp[k][0], np.float32).reshape(8, 128).T for k in ("b_cv_dw", "g_cv_ln", "b_cv_ln")], axis=1)
    m["cvec"] = np.ascontiguousarray(cv)
    m["w_cv_out"] = np.ascontiguousarray(inp["w_cv_out"][0])
    m["b_cv_out"] = np.ascontiguousarray(inp["b_cv_out"][0][None, :])
    return m


def kernel(**inp):
    inp = {k: np.asarray(v) for k, v in inp.items()}
    nc, _ = build_fused()
    res = run_bass_kernel_spmd(nc, [fused_inputs(inp, c) for c in range(8)], core_ids=list(range(8))).results
    out = np.zeros((2, 4 * T, D), np.float32)
    for c in range(8):
        out[c // 4, (c % 4) * T:(c % 4 + 1) * T] = res[c][FINAL_OUT]
    return out
```

```python
import numpy as np
from contextlib import ExitStack
import concourse.bass as bass
import concourse.mybir as mybir
from concourse.bass_utils import run_bass_kernel_spmd

F32 = mybir.dt.float32
BF16 = mybir.dt.bfloat16
AF = mybir.ActivationFunctionType
ALU = mybir.AluOpType
AX = mybir.AxisListType

SEM_LIMIT = 12000
D = 1024
T = 4096
TCTX = 256
NH = 8
EPS = 1e-6
NE = 32
NQ_FFN = 8


class Prog:
    def __init__(self, nc):
        self.nc = nc
        self.es = ExitStack()
        self.eng = {"pe": nc.tensor, "dve": nc.vector, "act": nc.scalar,
                    "pool": nc.gpsimd, "sp": nc.sync}
        self.sem = {}
        self.cnt = {}
        self.nsem = 0
        for e in ("pe", "dve", "act", "pool"):
            self._new_sem(e)
        self.waited = {e: {} for e in self.eng}
        self.res = {}
        self.dsem = {}
        self.dcnt = {}
        self.ninst = {e: 0 for e in self.eng}
        self.psum_ids = set()
        self.prev_sem = {}
        self.dtot = {}
        self.free_dsems = []
        self.phase_es = None
        self.cc_sems = []

    def _alloc_sem(self, name):
        self.nsem += 1
        return self.es.enter_context(self.nc.semaphore(name))

    def _new_sem(self, e):
        if e in self.sem:
            self.prev_sem.setdefault(e, []).append((self.sem[e], self.cnt[e]))
        self.sem[e] = self._alloc_sem("e_%s_%d" % (e, self.nsem))
        self.cnt[e] = 0

    def begin_phase(self):
        self.phase_es = ExitStack()

    def end_phase(self):
        self.barrier()
        self.phase_es.close()
        self.phase_es = None
        self.res = {}
        self.free_dsems = [sem for (sem, _) in self.dtot.values()]
        self.dsem = {}

    def barrier(self):
        targets = []
        for x in ("pe", "dve", "act", "pool"):
            for (sm, c) in self.prev_sem.get(x, []):
                targets.append((sm, c))
            if self.cnt[x] > 0:
                targets.append((self.sem[x], self.cnt[x]))
        targets.extend(self.dtot.values())
        targets.extend(self.cc_sems)
        for e in ("pe", "dve", "act", "pool", "sp"):
            for (sm, c) in targets:
                if c > 0:
                    self._wait(e, sm, c)

    def allgather(self, in_ap, out_ap, in_key, out_key):
        self._deps("pool", [in_key], [out_key])
        inst = self.nc.gpsimd.collective_compute("AllGather", ALU.bypass, replica_groups=[list(range(8))],
                                                 ins=[in_ap.opt()], outs=[out_ap.opt()])
        sm = self._alloc_sem("cc%d" % self.nsem)
        inst.then_inc(sm, 1)
        self.cc_sems.append((sm, 1))
        self._mark(sm, 1, [in_key], [out_key])

    def sb(self, name, shape, dt):
        es = self.phase_es if self.phase_es is not None else self.es
        self.ntile = getattr(self, "ntile", 0) + 1
        return es.enter_context(self.nc.sbuf_tensor("s%d_%s" % (self.ntile, name), list(shape), dt))

    def ps(self, name, shape, dt=F32):
        t = self.es.enter_context(self.nc.psum_tensor("p_" + name, list(shape), dt))
        self.psum_ids.add(id(t))
        return t

    def _r(self, key):
        k = key if isinstance(key, (str, tuple)) else id(key)
        r = self.res.get(k)
        if r is None:
            r = {"w": None, "r": {}}
            self.res[k] = r
        return r

    def _wait(self, e, sem, val):
        w = self.waited[e]
        k = id(sem)
        if w.get(k, 0) >= val:
            return
        w[k] = val
        self.eng[e].wait_ge(sem, val)

    def _deps(self, e, reads, writes):
        deps = []
        for r in reads:
            rr = self._r(r)
            if rr["w"] is not None:
                deps.append(rr["w"])
            if id(r) in self.psum_ids:
                deps.extend(rr["r"].values())
        for wk in writes:
            rr = self._r(wk)
            if rr["w"] is not None:
                deps.append(rr["w"])
            deps.extend(rr["r"].values())
        own = self.sem.get(e)
        for (sem, val) in deps:
            if e == "pe" and sem is own:
                continue
            self._wait(e, sem, val)

    def _mark(self, sem, val, reads, writes):
        for r in reads:
            self._r(r)["r"][id(sem)] = (sem, val)
        for wk in writes:
            rr = self._r(wk)
            rr["w"] = (sem, val)
            rr["r"] = {}

    def op(self, e, fn, reads=(), writes=()):
        if self.cnt[e] >= SEM_LIMIT:
            self._new_sem(e)
        self._deps(e, reads, writes)
        inst = fn(self.eng[e])
        self.cnt[e] += 1
        self.ninst[e] += 1
        inst.then_inc(self.sem[e], 1)
        self._mark(self.sem[e], self.cnt[e], reads, writes)
        return inst

    def dma(self, q, out, in_, reads=(), writes=(), semkey=None, **kw):
        self._deps(q, reads, writes)
        key = semkey if semkey is not None else writes[0]
        key = key if isinstance(key, (str, tuple)) else id(key)
        if key not in self.dsem or self.dtot[id(self.dsem[key])][1] >= 2 * SEM_LIMIT:
            if key not in self.dsem and self.free_dsems:
                self.dsem[key] = self.free_dsems.pop()
            else:
                sm = self._alloc_sem("d%d" % self.nsem)
                self.dsem[key] = sm
                self.dtot[id(sm)] = (sm, 0)
        s = self.dsem[key]
        inst = self.eng[q].dma_start(out=out, in_=in_, **kw)
        tot = self.dtot[id(s)][1] + 16
        self.dtot[id(s)] = (s, tot)
        self.ninst[q] += 1
        inst.then_inc(s, 16)
        self._mark(s, tot, reads, writes)
        return inst

    def finish(self, out_keys, e="sp"):
        for k in out_keys:
            rr = self._r(k)
            if rr["w"] is not None:
                self._wait(e, *rr["w"])
        for x in ("pe", "dve", "act", "pool"):
            if self.cnt[x] > 0:
                self._wait(e, self.sem[x], self.cnt[x])

    def act(self, out, in_, func, reads, writes, **kw):
        return self.op("act", lambda e: e.activation(out, in_, func, **kw), reads, writes)

    def tt(self, eng, out, a, b, op, reads, writes):
        return self.op(eng, lambda e: e.tensor_tensor(out, a, b, op), reads, writes)

    def ts(self, eng, out, a, s1, s2, op0, op1, reads, writes):
        if s2 is None:
            return self.op(eng, lambda e: e.tensor_scalar(out, a, s1, None, op0), reads, writes)
        return self.op(eng, lambda e: e.tensor_scalar(out, a, s1, s2, op0, op1), reads, writes)

    def copy(self, eng, out, in_, reads, writes):
        if eng == "act":
            return self.op("act", lambda e: e.copy(out, in_), reads, writes)
        return self.op(eng, lambda e: e.tensor_copy(out, in_), reads, writes)

    def mm(self, out, lhsT, rhs, start, stop, reads, writes):
        return self.op("pe", lambda e: e.matmul(out, lhsT, rhs, start=start, stop=stop), reads, writes)


REG = {}
FUSED = [False]
FINAL_OUT = "out"


def dram_in(nc, name, shape, dt=F32):
    if FUSED[0] and name in REG:
        return REG[name]
    ap = nc.dram_tensor(name, list(shape), dt, kind="ExternalInput").ap()
    if FUSED[0]:
        REG[name] = ap
    return ap


def dram_out(nc, name, shape, dt=F32):
    if FUSED[0]:
        kind = "ExternalOutput" if name == FINAL_OUT else "Internal"
        ap = nc.dram_tensor(name, list(shape), dt, kind=kind).ap()
        REG[name] = ap
        return ap
    return nc.dram_tensor(name, list(shape), dt, kind="ExternalOutput").ap()


class Ctx:
    def __init__(self):
        self.nc = bass.Bass("TRN2", target_bir_lowering=False)
        self.p = Prog(self.nc)
        self.cm = None


def stage_begin(ctx):
    if ctx is None:
        c = Ctx()
        c.cm = Common(c.p, c.nc)
        c.standalone = True
        return c
    ctx.standalone = False
    ctx.p.begin_phase()
    return ctx


def stage_end(ctx, out_keys):
    if ctx.standalone:
        ctx.p.finish(out_keys)
    else:
        ctx.p.end_phase()
    return ctx.nc, ctx.p


class Common:
    def __init__(self, p, nc):
        self.p = p
        self.ident_d = dram_in(nc, "ident", [128, 128])
        self.identf = p.sb("identf", [128, 128], F32)
        self.identb = p.sb("identb", [128, 128], BF16)
        self.ones = p.sb("ones", [128, 128], F32)
        p.dma("sp", self.identf[:], self.ident_d, writes=[self.identf])
        p.copy("dve", self.identb[:], self.identf[:], [self.identf], [self.identb])
        p.op("dve", lambda e: e.memset(self.ones[:], 1.0), [], [self.ones])
        self.ones5 = p.sb("ones5", [128, 512], F32)
        p.op("dve", lambda e: e.memset(self.ones5[:], 1.0), [], [self.ones5])
        self.psb = [p.ps("psb%d" % i, [128, 512]) for i in range(7)]
        self.psT = p.ps("psT", [128, 1024], BF16)


def emit_mods(p, cm, w_ada, b_ada, lo, hi, pairs, tag):
    lts = []
    for i, (ccol, out) in enumerate(pairs):
        p.dma("sp", out[:], b_ada[:, lo:hi].partition_broadcast(128), writes=[out])
        cc = p.sb("mod_cc%s%d" % (tag, i), [128, 8], F32)
        sg = p.sb("mod_sg%s%d" % (tag, i), [128, 8], F32)
        lt = p.sb("mod_lt%s%d" % (tag, i), [128, 8, 128], F32)
        p.dma("sp", cc[:], ccol, writes=[cc])
        p.act(sg[:], cc[:], AF.Sigmoid, [cc], [sg])
        p.tt("dve", sg[:], sg[:], cc[:], ALU.mult, [sg, cc], [sg])
        for kc in range(8):
            p.ts("dve", lt[:, kc, :], cm.ones[:], sg[:, kc:kc + 1], None, ALU.mult, None, [cm.ones, sg], [lt])
        lts.append(lt)
    wc = p.sb("mod_w%s" % tag, [128, 8, 256], F32)
    for bi, n0 in enumerate(range(lo, hi, 256)):
        p.dma("sp", wc[:], w_ada[:, n0:n0 + 256].rearrange("(kc q) n -> q kc n", q=128), writes=[wc])
        for i, (ccol, out) in enumerate(pairs):
            ps = cm.psb[i % 2]
            for kc in range(8):
                p.mm(ps[:, 0:256], lts[i][:, kc, :], wc[:, kc, :], kc == 0, kc == 7, [lts[i], wc], [ps])
            p.tt("dve", out[:, n0 - lo:n0 - lo + 256], ps[:, 0:256], out[:, n0 - lo:n0 - lo + 256], ALU.add, [ps, out], [out])


def emit_norm_T(p, cm, src, ntiles, Arow, Brow, modt, hT, tagres, xt, junk, h, st, hTf=None):
    for t in range(ntiles):
        xx = xt[t % 2]
        p.dma("sp", xx[:], src[t * 128:(t + 1) * 128, :], reads=[tagres] if tagres else [], writes=[xx])
        ss, rstd = st
        p.act(junk[:], xx[:], AF.Square, [xx], [junk, ss], accum_out=ss[:])
        p.act(rstd[:], ss[:], AF.Sqrt, [ss], [rstd], scale=1.0 / D, bias=EPS)
        p.op("dve", lambda e: e.reciprocal(rstd[:], rstd[:]), [rstd], [rstd])
        p.op("dve", lambda e: e.scalar_tensor_tensor(h[:], xx[:], rstd[:], Arow, ALU.mult, ALU.mult),
             [xx, rstd, modt], [h])
        p.tt("dve", h[:], h[:], Brow, ALU.add, [h, modt], [h])
        for half in range(2):
            pt = cm.psb[half]
            for j in range(4):
                kc = half * 4 + j
                p.op("pe", lambda e: e.transpose(pt[:, j * 128:(j + 1) * 128], h[:, kc * 128:(kc + 1) * 128], cm.identf[:]),
                     [h, cm.identf], [pt])
            p.copy("act", hT[:, half * 4:(half + 1) * 4, t * 128:(t + 1) * 128],
                   pt[:].rearrange("q (a b) -> q a b", a=4), [pt], [hT])
            if hTf is not None:
                p.copy("dve", hTf[:, half * 4:(half + 1) * 4, t * 128:(t + 1) * 128],
                       pt[:].rearrange("q (a b) -> q a b", a=4), [pt], [hTf])


DBG_STOP = None
DBG_LVL = 0
DBG_SKIP = ''


def build_l1(ctx=None):
    ctx = stage_begin(ctx)
    nc, p, cm = ctx.nc, ctx.p, ctx.cm
    xs = dram_in(nc, "xs", [T, D])
    ctxb = dram_in(nc, "ctxb", [TCTX, D])
    ccol = dram_in(nc, "ccol", [128, 8])
    cctx = dram_in(nc, "cctx", [128, 8])
    w_ada = dram_in(nc, "w_ada0", [D, 6 * D])
    b_ada = dram_in(nc, "b_ada0", [1, 6 * D])
    g_mix = dram_in(nc, "g_mix0", [1, D])
    w_hin = dram_in(nc, "w_hin", [D, 5 * D])
    gam = dram_in(nc, "gam", [128, 2, 2, 8])
    maskf_d = dram_in(nc, "maskf", [64, 64])
    maskb_d = dram_in(nc, "maskb", [64, 64])
    rm_d = dram_in(nc, "rm", [128, 512])
    o_d = [dram_out(nc, "o_f", [T, D]), dram_out(nc, "o_b", [T, D])]
    sg_d = dram_out(nc, "sg", [T, D], BF16)
    Q_d = [dram_out(nc, "QF", [NH, 128, T], BF16), dram_out(nc, "QB", [NH, 128, T], BF16)]
    Sloc_d = dram_out(nc, "Sloc", [2, NH, 128, 128])
    Sctx_d = dram_out(nc, "Sctx", [2, NH, 128, 128])
    Dloc_d = dram_out(nc, "Dloc", [128, 16])

    maskt = [p.sb("maskf", [64, 64], F32), p.sb("maskb", [64, 64], F32)]
    rm = p.sb("rm", [128, 512], F32)
    p.dma("sp", maskt[0][:], maskf_d, writes=[maskt[0]])
    p.dma("sp", maskt[1][:], maskb_d, writes=[maskt[1]])
    p.dma("sp", rm[:], rm_d, writes=[rm])

    gm = p.sb("gm", [128, 2, 2, 8], F32)
    lb = p.sb("lb", [128, 16], F32)
    oml = p.sb("oml", [128, 16], F32)
    p.dma("sp", gm[:], gam, writes=[gm])
    for d in range(2):
        p.tt("dve", lb[:, d * 8:(d + 1) * 8], gm[:, d, 0, :], gm[:, d, 1, :], ALU.subtract, [gm], [lb])
    p.act(lb[:], lb[:], AF.Sigmoid, [lb], [lb])
    p.ts("dve", oml[:], lb[:], -1.0, 1.0, ALU.mult, ALU.add, [lb], [oml])

    if DBG_LVL == 1:
        p.finish([])
        return nc, p
    modb = p.sb("modb", [128, 2048], F32)
    modc = p.sb("modc", [128, 2048], F32)
    emit_mods(p, cm, w_ada, b_ada, 0, 2048, [(ccol, modb), (cctx, modc)], "a")
    gmix = p.sb("gmix", [128, D], F32)
    p.dma("sp", gmix[:], g_mix.partition_broadcast(128), writes=[gmix])
    for m in (modb, modc):
        p.op("dve", lambda e: e.scalar_tensor_tensor(m[:, 1024:2048], m[:, 1024:2048], 1.0, gmix[:], ALU.add, ALU.mult),
             [m, gmix], [m])

    if DBG_LVL == 2:
        p.finish([])
        return nc, p
    hT = p.sb("hT", [128, 8, T], BF16)
    hTc = p.sb("hTc", [128, 8, TCTX], BF16)
    xt = [p.sb("xt%d" % i, [128, D], F32) for i in range(2)]
    h = p.sb("h", [128, D], F32)
    junk = h
    st = (p.sb("ss", [128, 1], F32), p.sb("rstd", [128, 1], F32))
    emit_norm_T(p, cm, ctxb, TCTX // 128, modc[:, 1024:2048], modc[:, 0:1024], modc, hTc, None, xt, junk, h, st)
    if DBG_LVL == 3:
        p.finish([])
        return nc, p
    emit_norm_T(p, cm, xs, (T // 128) if DBG_LVL != 4 else 4, modb[:, 1024:2048], modb[:, 0:1024], modb, hT, None, xt, junk, h, st)
    if DBG_LVL in (4, 5):
        p.finish([])
        return nc, p

    col0 = [0, 2048, 3072, 1024, 4096]
    whf = [p.sb("whf%d" % i, [128, 8, 128], F32) for i in range(2)]
    whb = [p.sb("whb%d" % i, [128, 8, 5, 128], BF16) for i in range(2)]

    def blk_tiles(i):
        s = "_%d" % i
        return dict(
            q=p.sb("q" + s, [128, 512], F32), F=p.sb("F" + s, [128, 512], F32),
            LF=p.sb("LF" + s, [128, 512], F32), U=p.sb("U" + s, [128, 512], F32),
            E=p.sb("E" + s, [128, 512], F32), Bc=p.sb("Bc" + s, [128, 512], F32),
            BL=p.sb("BL" + s, [128, 8], F32), DEC=p.sb("DEC" + s, [128, 8], F32),
            QG=p.sb("QG" + s, [128, 512], BF16), KD=p.sb("KD" + s, [128, 512], BF16),
            KGT=p.sb("KGT" + s, [128, 512], BF16), QX=p.sb("QX" + s, [128, 512], BF16),
            kg=p.sb("kg" + s, [64, 8, 128], BF16), v=p.sb("v" + s, [64, 8, 128], BF16),
            ob=p.sb("ob" + s, [64, 8, 128], F32), tc=p.sb("tc" + s, [128, 1], F32),
            sgt=p.sb("sgt" + s, [64, 8, 128], BF16) if i == 0 else None,
            sgs=p.sb("sgs" + s, [64, 2, 128], F32) if i == 0 else None,
            sgg=p.sb("sgg" + s, [64, 2, 128], F32) if i == 0 else None)

    bt = [blk_tiles(0), blk_tiles(1)]
    SD = [(p.sb("S32_%d" % d, [128, 128], F32), p.sb("Sb_%d" % d, [128, 128], BF16), p.sb("ATs_%d" % d, [64, 64], BF16))
          for d in range(2)]
    Dcol = p.sb("Dcol", [128, 16], F32)
    psq, psz, psv = cm.psb[0], cm.psb[1], cm.psb[2]
    PSD = [(cm.psb[3], cm.psb[4], cm.psb[5]), (cm.psb[6], cm.psb[0], cm.psb[1])]
    psT = cm.psT
    bcount = 0

    for hh in range(NH):
        if DBG_STOP is not None and hh >= DBG_STOP[0]:
            break
        wb = whb[hh % 2]
        for s in range(5):
            c0 = col0[s] + hh * 128
            wf_ = whf[s % 2]
            p.dma("sp", wf_[:], w_hin[:, c0:c0 + 128].rearrange("(kc q) n -> q kc n", q=128), writes=[wf_])
            p.copy("act", wb[:, :, s, :], wf_[:], [wf_], [wb])
        for (is_ctx, hTs, TT) in ((True, hTc, TCTX), (False, hT, T)):
            nb_tok = 256 if is_ctx else 512
            nblk = TT // nb_tok
            nch = nb_tok // 64
            n = nb_tok
            dst = [dict(carry=False, first=True), dict(carry=False, first=True)]

            def stage_a(d, blk, is_ctx=is_ctx, hTs=hTs, nch=nch, n=n, dst=dst, wb=wb, hh=hh):
                b = bt[d]
                st_ = dst[d]
                col = d * 8 + hh
                t0 = blk * n
                q, Ft, LF, U, E, Bc = b["q"], b["F"], b["LF"], b["U"], b["E"], b["Bc"]
                for kc in range(8):
                    p.mm(psq[:, 0:n], wb[:, kc, 0, :], hTs[:, kc, t0:t0 + n], kc == 0, kc == 7, [wb, hTs], [psq])
                p.copy("act", q[:, 0:n], psq[:, 0:n], [psq], [q])
                for kc in range(8):
                    p.mm(psz[:, 0:n], wb[:, kc, 1 + d, :], hTs[:, kc, t0:t0 + n], kc == 0, kc == 7, [wb, hTs], [psz])
                p.act(Ft[:, 0:n], psz[:, 0:n], AF.Sigmoid, [psz], [Ft])
                for c2 in range(nch // 2):
                    for j in range(2):
                        c = c2 * 2 + j
                        for kc in range(8):
                            p.mm(psv[0:64, j * 256:(j + 1) * 256], hTs[:, kc, t0 + c * 64:t0 + (c + 1) * 64],
                                 wb[:, kc, 3:5, :].rearrange("q a b -> q (a b)"), kc == 0, kc == 7, [wb, hTs], [psv])
                    pv = psv[0:64, :].rearrange("q (j x) -> q j x", j=2)
                    p.copy("dve", b["v"][:, c2 * 2:c2 * 2 + 2, :], pv[:, :, 0:128], [psv], [b["v"]])
                    if d == 0 and not is_ctx:
                        p.copy("dve", b["sgg"][:, 0:2, :], pv[:, :, 128:256], [psv], [b["sgg"]])
                        p.act(b["sgs"][:, 0:2, :], b["sgg"][:, 0:2, :], AF.Sigmoid, [b["sgg"]], [b["sgs"]])
                        p.tt("dve", b["sgt"][:, c2 * 2:c2 * 2 + 2, :], b["sgg"][:, 0:2, :], b["sgs"][:, 0:2, :], ALU.mult,
                             [b["sgg"], b["sgs"]], [b["sgt"]])
                if d == 0 and not is_ctx:
                    p.dma("sp", sg_d[t0:t0 + n, hh * 128:(hh + 1) * 128].rearrange("(c q) v -> q c v", q=64),
                          b["sgt"][:, 0:nch, :], reads=[b["sgt"]], writes=["sg_d"])
                p.ts("dve", Ft[:, 0:n], Ft[:, 0:n], oml[:, col:col + 1], lb[:, col:col + 1], ALU.mult, ALU.add,
                     [Ft, oml, lb], [Ft])
                p.act(LF[:, 0:n], Ft[:, 0:n], AF.Ln, [Ft], [LF])
                p.ts("dve", Ft[:, 0:n], Ft[:, 0:n], -1.0, 1.0, ALU.mult, ALU.add, [Ft], [Ft])
                p.op("dve", lambda e: e.tensor_tensor_scan(U[:, 0:n], rm[:, 0:n], LF[:, 0:n], 0.0, ALU.mult, ALU.add),
                     [rm, LF], [U])
                U3 = U[:, 0:n].rearrange("q (c k) -> q c k", k=64)
                BL3 = b["BL"][:, 0:nch].rearrange("q (c o) -> q c o", o=1)
                p.copy("act", BL3, U3[:, :, 63:64], [U], [b["BL"]])
                p.act(b["DEC"][:, 0:nch], b["BL"][:, 0:nch], AF.Exp, [b["BL"]], [b["DEC"]])
                if not is_ctx:
                    tc = b["tc"]
                    if d == 0:
                        if st_["carry"]:
                            p.op("dve", lambda e: e.tensor_tensor_scan(Bc[:, 0:n], cm.ones5[:, 0:n], LF[:, 0:n], tc[:], ALU.mult, ALU.add),
                                 [cm.ones5, LF, tc], [Bc])
                        else:
                            p.op("dve", lambda e: e.tensor_tensor_scan(Bc[:, 0:n], cm.ones5[:, 0:n], LF[:, 0:n], 0.0, ALU.mult, ALU.add),
                                 [cm.ones5, LF], [Bc])
                        p.copy("dve", tc[:], Bc[:, n - 1:n], [Bc], [tc])
                    else:
                        p.op("dve", lambda e: e.tensor_tensor_scan(Bc[:, 0:n], cm.ones5[:, 0:n], LF[:, 0:n], 0.0, ALU.mult, ALU.add),
                             [cm.ones5, LF], [Bc])
                        if st_["carry"]:
                            p.tt("dve", tc[:], Bc[:, n - 1:n], tc[:], ALU.add, [Bc, tc], [tc])
                        else:
                            p.copy("dve", tc[:], Bc[:, n - 1:n], [Bc], [tc])
                        p.tt("dve", Bc[:, 0:n], LF[:, 0:n], Bc[:, 0:n], ALU.subtract, [LF, Bc], [Bc])
                        p.ts("dve", Bc[:, 0:n], Bc[:, 0:n], tc[:], None, ALU.add, None, [Bc, tc], [Bc])
                    st_["carry"] = True
                    p.act(E[:, 0:n], Bc[:, 0:n], AF.Exp, [Bc], [E])
                    p.tt("dve", b["QX"][:, 0:n], q[:, 0:n], E[:, 0:n], ALU.mult, [q, E], [b["QX"]])
                    p.dma("sp", Q_d[d][hh, :, t0:t0 + n], b["QX"][:, 0:n], reads=[b["QX"]], writes=["Q_d%d" % d])
                if d == 1:
                    p.tt("dve", U[:, 0:n], LF[:, 0:n], U[:, 0:n], ALU.subtract, [LF, U], [U])
                    p.tt("dve", U3, U3, BL3.to_broadcast([128, nch, 64]), ALU.add, [U, b["BL"]], [U])
                p.act(E[:, 0:n], U[:, 0:n], AF.Exp, [U], [E])
                p.tt("dve", b["QG"][:, 0:n], q[:, 0:n], E[:, 0:n], ALU.mult, [q, E], [b["QG"]])
                p.act(LF[:, 0:n], U[:, 0:n], AF.Exp, [U], [LF], scale=-1.0)
                p.tt("dve", b["KD"][:, 0:n], Ft[:, 0:n], LF[:, 0:n], ALU.mult, [Ft, LF], [b["KD"]])
                p.tt("dve", b["KGT"][:, 0:n].rearrange("q (c k) -> q c k", k=64),
                     b["KD"][:, 0:n].rearrange("q (c k) -> q c k", k=64),
                     b["DEC"][:, 0:nch].rearrange("q (c o) -> q c o", o=1).to_broadcast([128, nch, 64]),
                     ALU.mult, [b["KD"], b["DEC"]], [b["KGT"]])
                for c in range(nch):
                    p.op("pe", lambda e: e.transpose(psT[0:64, c * 128:(c + 1) * 128], b["KGT"][:, c * 64:(c + 1) * 64], cm.identb[:]),
                         [b["KGT"], cm.identb], [psT])
                p.copy("act", b["kg"][:, 0:nch, :], psT[0:64, 0:nch * 128].rearrange("q (c k) -> q c k", k=128), [psT], [b["kg"]])

            def chunk_step(d, c, is_ctx=is_ctx, dst=dst):
                b = bt[d]
                st_ = dst[d]
                first = st_["first"]
                pA, pO, pS = PSD[d]
                S32_, Sb_, ATs_ = SD[d]
                cs = slice(c * 64, (c + 1) * 64)
                p.mm(pA[0:64, 0:64], b["KD"][:, cs], b["QG"][:, cs], True, True, [b["KD"], b["QG"]], [pA])
                p.tt("dve", ATs_[:], pA[0:64, 0:64], maskt[d][:], ALU.mult, [pA, maskt[d]], [ATs_])
                p.mm(pO[0:64, 0:128], ATs_[:], b["v"][:, c, :], True, first, [ATs_, b["v"]], [pO])
                if not first:
                    p.mm(pO[0:64, 0:128], b["QG"][:, cs], Sb_[:], False, True, [b["QG"], Sb_], [pO])
                if not is_ctx:
                    p.copy("act", b["ob"][:, c, :], pO[0:64, 0:128], [pO], [b["ob"]])
                p.mm(pS[:, 0:128], b["kg"][:, c, :], b["v"][:, c, :], True, True, [b["kg"], b["v"]], [pS])
                if first:
                    p.copy("dve", S32_[:], pS[:, 0:128], [pS], [S32_])
                else:
                    p.op("dve", lambda e: e.scalar_tensor_tensor(S32_[:], S32_[:], b["DEC"][:, c:c + 1], pS[:, 0:128], ALU.mult, ALU.add),
                         [S32_, b["DEC"], pS], [S32_])
                p.copy("act", Sb_[:], S32_[:], [S32_], [Sb_])
                st_["first"] = False

            for i in range(nblk):
                blks = (i, nblk - 1 - i)
                for d in range(2):
                    stage_a(d, blks[d])
                for ci in range(nch):
                    for d in range(2):
                        chunk_step(d, ci if d == 0 else nch - 1 - ci)
                if not is_ctx:
                    for d in range(2):
                        t0 = blks[d] * n
                        p.dma("sp", o_d[d][t0:t0 + n, hh * 128:(hh + 1) * 128].rearrange("(c q) v -> q c v", q=64),
                              bt[d]["ob"][:, 0:nch, :], reads=[bt[d]["ob"]], writes=["o_d%d" % d])
            for d in range(2):
                col = d * 8 + hh
                if is_ctx:
                    p.dma("sp", Sctx_d[d, hh], SD[d][0][:], reads=[SD[d][0]], writes=["Sctx_d"])
                else:
                    p.dma("sp", Sloc_d[d, hh], SD[d][0][:], reads=[SD[d][0]], writes=["Sloc_d"])
                    p.act(Dcol[:, col:col + 1], bt[d]["tc"][:], AF.Exp, [bt[d]["tc"]], [Dcol])
    p.dma("sp", Dloc_d, Dcol[:], reads=[Dcol], writes=["Dloc_d"])
    return stage_end(ctx, ["sg_d", "Q_d0", "Q_d1", "o_d0", "o_d1", "Sctx_d", "Sloc_d", "Dloc_d"])


def consts():
    s = np.arange(64)[:, None]
    t = np.arange(64)[None, :]
    rm = np.ones((128, 512), np.float32)
    rm[:, ::64] = 0.0
    return {"ident": np.eye(128, dtype=np.float32),
            "maskf": (s <= t).astype(np.float32), "maskb": (s >= t).astype(np.float32), "rm": rm}


def col_layout(v):
    return np.ascontiguousarray(np.asarray(v, np.float32).reshape(8, 128).T)


def l1_inputs(inp, core):
    b, j = core // 4, core % 4
    m = dict(consts())
    m["xs"] = np.ascontiguousarray(inp["x"][b, j * T:(j + 1) * T])
    m["ctxb"] = np.ascontiguousarray(inp["ctx"][b])
    m["ccol"] = col_layout(inp["c"][b])
    m["cctx"] = col_layout(inp["c_ctx"])
    m["w_ada0"] = np.ascontiguousarray(inp["w_ada"][0])
    m["b_ada0"] = np.ascontiguousarray(inp["b_ada"][0][None, :])
    m["g_mix0"] = np.ascontiguousarray(inp["g_mix"][0][None, :])
    m["w_hin"] = np.ascontiguousarray(inp["w_hgrn_in"][0])
    g = np.asarray(inp["hgrn_gamma"], np.float32).reshape(2, 2, 8, 128)
    m["gam"] = np.ascontiguousarray(g.transpose(3, 0, 1, 2))
    return m


def emit_ffn(p, cm, x1_d, x1_key, xo_d, xo_key, modt, mo, gffn_d, wr_d, br_d, w1_d, b1c_d, w2_d, b2_d, tl, final_g=None):
    NQ = NQ_FFN
    TQ = T // NQ
    hfT = tl["hfT"]
    xt, h, st = tl["xt"], tl["h"], tl["st"]
    gf = h
    p.dma("sp", gf[:], gffn_d.partition_broadcast(128), writes=[gf])
    p.op("dve", lambda e: e.scalar_tensor_tensor(modt[:, mo + 1024:mo + 2048], modt[:, mo + 1024:mo + 2048], 1.0, gf[:], ALU.add, ALU.mult),
         [modt, gf], [modt])
    if DBG_LVL == 16:
        return
    wr = p.sb("wr", [128, 8, NE], F32)
    brb = p.sb("brb", [128, NE], F32)
    p.dma("sp", wr[:], wr_d.rearrange("(kc q) n -> q kc n", q=128), writes=[wr])
    p.dma("sp", brb[:], br_d.partition_broadcast(128), writes=[brb])
    G = p.sb("G_all", [128, T // 128, NE], F32)
    GT = p.sb("GT_q", [NE, T // NQ_FFN], F32)
    b2s = p.sb("b2s", [NE, D], F32)
    b1c = p.sb("b1c", [128, NE, 16], F32)
    p.dma("sp", b2s[:], b2_d, writes=[b2s])
    p.dma("sp", b1c[:], b1c_d, writes=[b1c])
    if DBG_LVL == 17:
        return
    hTf = p.sb("hTf", [128, 8, 128], F32)
    lg = p.sb("lg", [128, NE], F32)
    m8 = p.sb("m8", [128, 8], F32)
    nmx = p.sb("nmx", [128, 1], F32)
    msk = p.sb("msk", [128, NE], F32)
    ex = p.sb("ex", [128, NE], F32)
    sm = p.sb("sm", [128, 1], F32)
    ps_l = cm.psb[2]
    ss, rstd = st
    for t in range(T // 128):
        xx = xt[t % 2]
        p.dma("sp", xx[:], x1_d[t * 128:(t + 1) * 128, :], reads=[x1_key], writes=[xx])
        p.act(h[:], xx[:], AF.Square, [xx], [h, ss], accum_out=ss[:])
        p.act(rstd[:], ss[:], AF.Sqrt, [ss], [rstd], scale=1.0 / D, bias=EPS)
        p.op("dve", lambda e: e.reciprocal(rstd[:], rstd[:]), [rstd], [rstd])
        p.op("dve", lambda e: e.scalar_tensor_tensor(h[:], xx[:], rstd[:], modt[:, mo + 1024:mo + 2048], ALU.mult, ALU.mult),
             [xx, rstd, modt], [h])
        p.tt("dve", h[:], h[:], modt[:, mo:mo + 1024], ALU.add, [h, modt], [h])
        for half in range(2):
            pt = cm.psb[half]
            for j in range(4):
                kc = half * 4 + j
                p.op("pe", lambda e: e.transpose(pt[:, j * 128:(j + 1) * 128], h[:, kc * 128:(kc + 1) * 128], cm.identf[:]),
                     [h, cm.identf], [pt])
            p.copy("act", hfT[:, half * 4:(half + 1) * 4, t * 128:(t + 1) * 128],
                   pt[:].rearrange("q (a b) -> q a b", a=4), [pt], [hfT])
            p.copy("dve", hTf[:, half * 4:(half + 1) * 4, :], pt[:].rearrange("q (a b) -> q a b", a=4), [pt], [hTf])
        if DBG_LVL == 20:
            return
        for kc in range(8):
            p.mm(ps_l[:, 0:NE], hTf[:, kc, :], wr[:, kc, :], kc == 0, kc == 7, [hTf, wr], [ps_l])
        p.tt("dve", lg[:], ps_l[:, 0:NE], brb[:], ALU.add, [ps_l, brb], [lg])
        if DBG_LVL == 19:
            return
        p.op("dve", lambda e: e.max(m8[:], lg[:]), [lg], [m8])
        p.ts("dve", msk[:], lg[:], m8[:, 3:4], None, ALU.is_ge, None, [lg, m8], [msk])
        p.ts("dve", nmx[:], m8[:, 0:1], -1.0, None, ALU.mult, None, [m8], [nmx])
        if DBG_LVL == 18:
            return
        p.act(ex[:], lg[:], AF.Exp, [lg, nmx], [ex], bias=nmx[:])
        p.tt("dve", ex[:], ex[:], msk[:], ALU.mult, [ex, msk], [ex])
        p.op("dve", lambda e: e.tensor_reduce(sm[:], ex[:], AX.X, ALU.add), [ex], [sm])
        p.op("dve", lambda e: e.reciprocal(sm[:], sm[:]), [sm], [sm])
        p.ts("dve", G[:, t, :], ex[:], sm[:], None, ALU.mult, None, [ex, sm], [G])
        if DBG_LVL == 21:
            return
    if DBG_LVL == 22:
        return
    yacc = tl["yacc"]
    actT = tl["actT"]
    w2b = tl["w2b"]
    wst = tl["wst"]
    w2st = tl["w2st"]
    wgb = tl["wgb"]
    wub = tl["wub"]
    tg, tsg, tu = tl["tg"], tl["tsg"], tl["tu"]
    psg, psu, psy = cm.psb[3], cm.psb[4], cm.psb[5]
    cnt = 0
    for qd in range(NQ):
        tq0 = qd * TQ
        for ti in range(TQ // 128):
            tg_ = qd * (TQ // 128) + ti
            p.op("pe", lambda e: e.transpose(ps_l[0:NE, 128:256], G[:, tg_, :], cm.identf[:]), [G, cm.identf], [ps_l])
            p.copy("dve", GT[:, ti * 128:(ti + 1) * 128], ps_l[0:NE, 128:256], [ps_l], [GT])
        for ti in range(TQ // 128):
            tg_ = qd * (TQ // 128) + ti
            for dh in range(2):
                p.mm(psy[:], GT[:, ti * 128:(ti + 1) * 128], b2s[:, dh * 512:(dh + 1) * 512], True, True, [GT, b2s], [psy])
                p.copy("act", yacc[:, ti, dh * 512:(dh + 1) * 512], psy[:], [psy], [yacc])
        if DBG_LVL == 23:
            return
        for ex_ in range(NE):
            if DBG_LVL == 24 and ex_ == 1:
                return
            for j in range(8):
                k = cnt % 2
                cnt += 1
                p.dma("sp", wst[k][:], w1_d[ex_, :, j * 128:(j + 1) * 128].rearrange("(kc q) n -> q kc n", q=128), writes=[wst[k]])
                p.copy("act", wgb[k][:], wst[k][:], [wst[k]], [wgb[k]])
                p.dma("sp", wst[k][:], w1_d[ex_, :, 1024 + j * 128:1024 + (j + 1) * 128].rearrange("(kc q) n -> q kc n", q=128), writes=[wst[k]])
                p.copy("act", wub[k][:], wst[k][:], [wst[k]], [wub[k]])
                p.dma("sp", w2st[k][:], w2_d[ex_, j * 128:(j + 1) * 128, :], writes=[w2st[k]])
                p.copy("act", w2b[:, j, :], w2st[k][:], [w2st[k]], [w2b])
                for tb in range(TQ // 512):
                    tok = slice(tq0 + tb * 512, tq0 + (tb + 1) * 512)
                    for kc in range(8):
                        p.mm(psg[:], wgb[k][:, kc, :], hfT[:, kc, tok], kc == 0, kc == 7, [wgb[k], hfT], [psg])
                    for kc in range(8):
                        p.mm(psu[:], wub[k][:, kc, :], hfT[:, kc, tok], kc == 0, kc == 7, [wub[k], hfT], [psu])
                    p.ts("dve", tg[:], psg[:], b1c[:, ex_, j:j + 1], 7.0, ALU.add, ALU.min, [psg, b1c], [tg])
                    p.act(tsg[:], tg[:], AF.Sigmoid, [tg], [tsg], scale=1.702)
                    p.ts("dve", tu[:], psu[:], b1c[:, ex_, 8 + j:9 + j], 7.0, ALU.add, ALU.min, [psu, b1c], [tu])
                    p.ts("dve", tu[:], tu[:], -7.0, 1.0, ALU.max, ALU.add, [tu], [tu])
                    p.tt("dve", tg[:], tg[:], tsg[:], ALU.mult, [tg, tsg], [tg])
                    p.tt("dve", actT[:, j, tb * 512:(tb + 1) * 512], tg[:], tu[:], ALU.mult, [tg, tu], [actT])
            for ti in range(TQ // 128):
                tg_ = qd * (TQ // 128) + ti
                for dh in range(2):
                    for j in range(8):
                        p.mm(psy[:], actT[:, j, ti * 128:(ti + 1) * 128], w2b[:, j, dh * 512:(dh + 1) * 512], j == 0, j == 7,
                             [actT, w2b], [psy])
                    ya = yacc[:, ti, dh * 512:(dh + 1) * 512]
                    p.op("dve", lambda e: e.scalar_tensor_tensor(ya, psy[:], G[:, tg_, ex_:ex_ + 1], ya, ALU.mult, ALU.add),
                         [psy, G, yacc], [yacc])
        for ti in range(TQ // 128):
            tg_ = qd * (TQ // 128) + ti
            xx = xt[ti % 2]
            p.dma("sp", xx[:], x1_d[tg_ * 128:(tg_ + 1) * 128, :], reads=[x1_key], writes=[xx])
            p.tt("dve", yacc[:, ti, :], yacc[:, ti, :], modt[:, mo + 2048:mo + 3072], ALU.mult, [yacc, modt], [yacc])
            p.tt("dve", xx[:], xx[:], yacc[:, ti, :], ALU.add, [xx, yacc], [xx])
            if final_g is not None:
                p.act(h[:], xx[:], AF.Square, [xx], [h, ss], accum_out=ss[:])
                p.act(rstd[:], ss[:], AF.Sqrt, [ss], [rstd], scale=1.0 / D, bias=EPS)
                p.op("dve", lambda e: e.reciprocal(rstd[:], rstd[:]), [rstd], [rstd])
                p.op("dve", lambda e: e.scalar_tensor_tensor(xx[:], xx[:], rstd[:], final_g[:], ALU.mult, ALU.mult),
                     [xx, rstd, final_g], [xx])
            p.dma("sp", xo_d[tg_ * 128:(tg_ + 1) * 128, :], xx[:], reads=[xx], writes=[xo_key])


def ffn_tiles(p):
    TQ = T // NQ_FFN
    return dict(
        hfT=p.sb("hfT", [128, 8, T], BF16),
        xt=[p.sb("fxt%d" % i, [128, D], F32) for i in range(2)],
        h=p.sb("fh", [128, D], F32),
        st=(p.sb("fss", [128, 1], F32), p.sb("frstd", [128, 1], F32)),
        yacc=p.sb("yacc", [128, TQ // 128, D], F32),
        actT=p.sb("actT", [128, 8, TQ], BF16),
        w2b=p.sb("w2b", [128, 8, D], BF16),
        wst=[p.sb("wst%d" % i, [128, 8, 128], F32) for i in range(2)],
        w2st=[p.sb("w2st%d" % i, [128, D], F32) for i in range(1)] * 2,
        wgb=[p.sb("wgb%d" % i, [128, 8, 128], BF16) for i in range(2)],
        wub=[p.sb("wub%d" % i, [128, 8, 128], BF16) for i in range(2)],
        tg=p.sb("tg", [128, 512], F32), tsg=p.sb("tsg", [128, 512], F32), tu=p.sb("tu", [128, 512], F32))


def build_l2(ctx=None):
    do_ffn = False
    ctx = stage_begin(ctx)
    nc, p, cm = ctx.nc, ctx.p, ctx.cm
    xs = dram_in(nc, "xs", [T, D])
    o_f = dram_in(nc, "o_f", [T, D])
    o_b = dram_in(nc, "o_b", [T, D])
    sg = dram_in(nc, "sg", [T, D], BF16)
    QF = dram_in(nc, "QF", [NH, 128, T], BF16)
    QB = dram_in(nc, "QB", [NH, 128, T], BF16)
    SlocG = dram_in(nc, "SlocG", [8 * 2 * NH * 128, 128])
    DlocG = dram_in(nc, "DlocG", [8 * 128, 16])
    Sctx = dram_in(nc, "Sctx", [2, NH, 128, 128])
    fmask = dram_in(nc, "fmask", [128, 16])
    ccol = dram_in(nc, "ccol", [128, 8])
    w_ada = dram_in(nc, "w_ada0", [D, 6 * D])
    b_ada = dram_in(nc, "b_ada0", [1, 6 * D])
    gout = dram_in(nc, "gout", [1, D])
    w_out = dram_in(nc, "w_hout", [D, D])
    x1_d = nc.dram_tensor("x1_scr", [T, D], F32, kind="Internal").ap() if do_ffn else dram_out(nc, "x1", [T, D])
    x2_d = dram_out(nc, "x2", [T, D]) if do_ffn else None

    modm = p.sb("modm", [128, 4096], F32)
    emit_mods(p, cm, w_ada, b_ada, 2048, 6144, [(ccol, modm)], "b")

    fm = p.sb("fm", [128, 16], F32)
    DA = p.sb("DA", [128, 8, 16], F32)
    p.dma("sp", fm[:], fmask, writes=[fm])
    for i in range(8):
        p.dma("sp", DA[:, i, :], DlocG[i * 128:(i + 1) * 128, :], reads=["DlocG"], writes=[DA])
    Sin = p.sb("Sin", [128, 2, NH, 128], BF16)
    Sf = p.sb("Sfold", [128, 128], F32)
    Sl = [p.sb("Sl%d" % i, [128, 128], F32) for i in range(2)]
    tmp = p.sb("ftmp", [128, 128], F32)
    k = 0
    for d in range(2):
        for hh in range(NH):
            p.dma("sp", Sf[:], Sctx[d, hh], writes=[Sf])
            order = range(8) if d == 0 else range(7, -1, -1)
            for i in order:
                sl = Sl[k % 2]
                k += 1
                r0 = (i * 2 * NH + d * NH + hh) * 128
                p.dma("sp", sl[:], SlocG[r0:r0 + 128, :], reads=["SlocG"], writes=[sl])
                col = d * 8 + hh
                p.op("dve", lambda e: e.scalar_tensor_tensor(tmp[:], Sf[:], DA[:, i, col:col + 1], sl[:], ALU.mult, ALU.add),
                     [Sf, DA, sl], [tmp])
                p.tt("dve", tmp[:], tmp[:], Sf[:], ALU.subtract, [tmp, Sf], [tmp])
                p.op("dve", lambda e: e.scalar_tensor_tensor(Sf[:], tmp[:], fm[:, d * 8 + i:d * 8 + i + 1], Sf[:], ALU.mult, ALU.add),
                     [tmp, fm, Sf], [Sf])
            p.copy("act", Sin[:, d, hh, :], Sf[:], [Sf], [Sin])

    goutb = p.sb("goutb", [128, D], F32)
    p.dma("sp", goutb[:], gout.partition_broadcast(128), writes=[goutb])
    wo = p.sb("wo", [128, 8, D], BF16)
    wos = [p.sb("wos%d" % i, [128, D], F32) for i in range(2)]
    for kc in range(8):
        p.dma("sp", wos[kc % 2][:], w_out[kc * 128:(kc + 1) * 128, :], writes=[wos[kc % 2]])
        p.copy("act", wo[:, kc, :], wos[kc % 2][:], [wos[kc % 2]], [wo])

    xt = [p.sb("xt%d" % i, [128, D], F32) for i in range(2)]
    of_t = [p.sb("oft%d" % i, [128, D], F32) for i in range(2)]
    ob_t = [p.sb("obt%d" % i, [128, D], F32) for i in range(2)]
    sg_t = [p.sb("sgt%d" % i, [128, D], BF16) for i in range(2)]
    qf_t = [p.sb("qft%d" % i, [128, NH, 128], BF16) for i in range(2)]
    qb_t = [p.sb("qbt%d" % i, [128, NH, 128], BF16) for i in range(2)]
    sq = p.sb("sq", [128, D], F32)
    ssq = p.sb("ssq", [128, NH], F32)
    ogT = p.sb("ogT", [128, 8, 128], BF16)
    for t in range(T // 128):
        k = t % 2
        tok = slice(t * 128, (t + 1) * 128)
        p.dma("sp", xt[k][:], xs[tok, :], writes=[xt[k]])
        p.dma("sp", of_t[k][:], o_f[tok, :], writes=[of_t[k]])
        p.dma("sp", ob_t[k][:], o_b[tok, :], writes=[ob_t[k]])
        p.dma("sp", sg_t[k][:], sg[tok, :], writes=[sg_t[k]])
        p.dma("sp", qf_t[k][:], QF[:, :, tok].rearrange("h q t -> q h t"), writes=[qf_t[k]])
        p.dma("sp", qb_t[k][:], QB[:, :, tok].rearrange("h q t -> q h t"), writes=[qb_t[k]])
        o = of_t[k]
        p.tt("dve", o[:], o[:], ob_t[k][:], ALU.add, [o, ob_t[k]], [o])
        for half in range(2):
            ps = cm.psb[half]
            for j in range(4):
                hh = half * 4 + j
                p.mm(ps[:, j * 128:(j + 1) * 128], qf_t[k][:, hh, :], Sin[:, 0, hh, :], True, False, [qf_t[k], Sin], [ps])
                p.mm(ps[:, j * 128:(j + 1) * 128], qb_t[k][:, hh, :], Sin[:, 1, hh, :], False, True, [qb_t[k], Sin], [ps])
            p.tt("dve", o[:, half * 512:(half + 1) * 512], o[:, half * 512:(half + 1) * 512], ps[:], ALU.add, [o, ps], [o])
        p.tt("dve", sq[:], o[:], o[:], ALU.mult, [o], [sq])
        p.op("dve", lambda e: e.tensor_reduce(ssq[:], sq[:].rearrange("q (h v) -> q h v", h=NH), AX.X, ALU.add), [sq], [ssq])
        p.act(ssq[:], ssq[:], AF.Sqrt, [ssq], [ssq], scale=1.0 / 128, bias=EPS)
        p.op("dve", lambda e: e.reciprocal(ssq[:], ssq[:]), [ssq], [ssq])
        p.tt("dve", o[:].rearrange("q (h v) -> q h v", h=NH), o[:].rearrange("q (h v) -> q h v", h=NH),
             ssq[:].rearrange("q (h u) -> q h u", u=1).to_broadcast([128, NH, 128]), ALU.mult, [o, ssq], [o])
        p.tt("dve", o[:], o[:], goutb[:], ALU.mult, [o, goutb], [o])
        p.tt("dve", o[:], o[:], sg_t[k][:], ALU.mult, [o, sg_t[k]], [o])
        for half in range(2):
            pt = cm.psb[2 + half]
            for j in range(4):
                kc = half * 4 + j
                p.op("pe", lambda e: e.transpose(pt[:, j * 128:(j + 1) * 128], o[:, kc * 128:(kc + 1) * 128], cm.identf[:]),
                     [o, cm.identf], [pt])
            p.copy("act", ogT[:, half * 4:(half + 1) * 4, :], pt[:].rearrange("q (a b) -> q a b", a=4), [pt], [ogT])
        for dh in range(2):
            ps = cm.psb[4 + dh]
            for kc in range(8):
                p.mm(ps[:], ogT[:, kc, :], wo[:, kc, dh * 512:(dh + 1) * 512], kc == 0, kc == 7, [ogT, wo], [ps])
            p.tt("dve", sq[:, dh * 512:(dh + 1) * 512], ps[:], modm[:, dh * 512:(dh + 1) * 512], ALU.mult, [ps, modm], [sq])
        p.tt("dve", xt[k][:], xt[k][:], sq[:], ALU.add, [xt[k], sq], [xt[k]])
        p.dma("sp", x1_d[tok, :], xt[k][:], reads=[xt[k]], writes=["x1"])
    if do_ffn:
        tl = ffn_tiles(p)
        emit_ffn(p, cm, x1_d, "x1", x2_d, "x2", modm, 1024, gffn, wr_d, br_d, w1_d, b1c_d, w2_d, b2_d, tl)
    return stage_end(ctx, ["x1"])


def build_ffn(final, ctx=None, layer=0, in_name="x1", out_name="x2"):
    assert ctx is not None, "fused build only"
    nc, p, cm = ctx.nc, ctx.p, ctx.cm
    L = str(layer)
    x1_d = dram_in(nc, in_name, [T, D])
    ccol = dram_in(nc, "ccol", [128, 8])
    w_ada = dram_in(nc, "w_ada" + L, [D, 6 * D])
    b_ada = dram_in(nc, "b_ada" + L, [1, 6 * D])
    gffn_d = dram_in(nc, "g_ffn" + L, [1, D])
    wr_d = dram_in(nc, "w_router" + L, [D, NE])
    br_d = dram_in(nc, "b_router" + L, [1, NE])
    w1_d = dram_in(nc, "w_exp_in" + L, [NE, D, 2 * D])
    b1c_d = dram_in(nc, "b1c" + L, [128, NE, 16])
    w2_d = dram_in(nc, "w_exp_out" + L, [NE, D, D])
    b2_d = dram_in(nc, "b_exp_out" + L, [NE, D])
    gfin_d = dram_in(nc, "g_final", [1, D])
    xo_d = dram_out(nc, out_name, [T, D])
    hfT_scr = nc.dram_tensor("hfT_scr" + L, [8, 128, T], BF16, kind="Internal").ap()
    G_scr = nc.dram_tensor("G_scr" + L, [128, T // 128, NE], F32, kind="Internal").ap()
    gtf_scr = nc.dram_tensor("gtf_scr" + L, [1, D], F32, kind="Internal").ap()

    ctx.standalone = False
    p.begin_phase()
    modf = p.sb("modf", [128, 3072], F32)
    emit_mods(p, cm, w_ada, b_ada, 3072, 6144, [(ccol, modf)], "f")
    p.dma("sp", gtf_scr, modf[0:1, 2048:3072], reads=[modf], writes=["gtf_scr"])
    xt = [p.sb("fxt%d" % i, [128, D], F32) for i in range(2)]
    h = p.sb("fh", [128, D], F32)
    ss, rstd = p.sb("fss", [128, 1], F32), p.sb("frstd", [128, 1], F32)
    p.dma("sp", h[:], gffn_d.partition_broadcast(128), writes=[h])
    p.op("dve", lambda e: e.scalar_tensor_tensor(modf[:, 1024:2048], modf[:, 1024:2048], 1.0, h[:], ALU.add, ALU.mult),
         [modf, h], [modf])
    wr = p.sb("wr", [128, 8, NE], F32)
    brb = p.sb("brb", [128, NE], F32)
    p.dma("sp", wr[:], wr_d.rearrange("(kc q) n -> q kc n", q=128), writes=[wr])
    p.dma("sp", brb[:], br_d.partition_broadcast(128), writes=[brb])
    G = p.sb("G_all", [128, T // 128, NE], F32)
    hTf = p.sb("hTf", [128, 8, 128], F32)
    hTb = [p.sb("hTb%d" % i, [128, 8, 128], BF16) for i in range(2)]
    lg = p.sb("lg", [128, NE], F32)
    m8 = p.sb("m8", [128, 8], F32)
    nmx = p.sb("nmx", [128, 1], F32)
    msk = p.sb("msk", [128, NE], F32)
    ex = p.sb("ex", [128, NE], F32)
    sm = p.sb("sm", [128, 1], F32)
    ps_l = cm.psb[2]
    for t in range(T // 128):
        xx = xt[t % 2]
        hb = hTb[t % 2]
        p.dma("sp", xx[:], x1_d[t * 128:(t + 1) * 128, :], writes=[xx])
        p.act(h[:], xx[:], AF.Square, [xx], [h, ss], accum_out=ss[:])
        p.act(rstd[:], ss[:], AF.Sqrt, [ss], [rstd], scale=1.0 / D, bias=EPS)
        p.op("dve", lambda e: e.reciprocal(rstd[:], rstd[:]), [rstd], [rstd])
        p.op("dve", lambda e: e.scalar_tensor_tensor(h[:], xx[:], rstd[:], modf[:, 1024:2048], ALU.mult, ALU.mult),
             [xx, rstd, modf], [h])
        p.tt("dve", h[:], h[:], modf[:, 0:1024], ALU.add, [h, modf], [h])
        for half in range(2):
            pt = cm.psb[half]
            for j in range(4):
                kc = half * 4 + j
                p.op("pe", lambda e: e.transpose(pt[:, j * 128:(j + 1) * 128], h[:, kc * 128:(kc + 1) * 128], cm.identf[:]),
                     [h, cm.identf], [pt])
            p.copy("dve", hTf[:, half * 4:(half + 1) * 4, :], pt[:].rearrange("q (a b) -> q a b", a=4), [pt], [hTf])
        p.copy("act", hb[:], hTf[:], [hTf], [hb])
        p.dma("sp", hfT_scr[:, :, t * 128:(t + 1) * 128].rearrange("k q t -> q k t"), hb[:], reads=[hb], writes=["hfT_scr"])
        for kc in range(8):
            p.mm(ps_l[:, 0:NE], hTf[:, kc, :], wr[:, kc, :], kc == 0, kc == 7, [hTf, wr], [ps_l])
        p.tt("dve", lg[:], ps_l[:, 0:NE], brb[:], ALU.add, [ps_l, brb], [lg])
        p.op("dve", lambda e: e.max(m8[:], lg[:]), [lg], [m8])
        p.ts("dve", msk[:], lg[:], m8[:, 3:4], None, ALU.is_ge, None, [lg, m8], [msk])
        p.ts("dve", nmx[:], m8[:, 0:1], -1.0, None, ALU.mult, None, [m8], [nmx])
        p.act(ex[:], lg[:], AF.Exp, [lg, nmx], [ex], bias=nmx[:])
        p.tt("dve", ex[:], ex[:], msk[:], ALU.mult, [ex, msk], [ex])
        p.op("dve", lambda e: e.tensor_reduce(sm[:], ex[:], AX.X, ALU.add), [ex], [sm])
        p.op("dve", lambda e: e.reciprocal(sm[:], sm[:]), [sm], [sm])
        p.ts("dve", G[:, t, :], ex[:], sm[:], None, ALU.mult, None, [ex, sm], [G])
    p.dma("sp", G_scr, G[:], reads=[G], writes=["G_scr"])
    p.end_phase()

    p.begin_phase()
    NPASS = 4
    TH = T // NPASS
    NB = TH // 512
    G = p.sb("G_all", [128, T // 128, NE], F32)
    gtf = p.sb("gtf", [128, D], F32)
    b2s = p.sb("b2s", [NE, D], F32)
    b1c = p.sb("b1c", [128, NE, 16], F32)
    p.dma("sp", G[:], G_scr, writes=[G])
    p.dma("sp", gtf[:], gtf_scr.partition_broadcast(128), writes=[gtf])
    p.dma("sp", b2s[:], b2_d, writes=[b2s])
    p.dma("sp", b1c[:], b1c_d, writes=[b1c])
    gfin = None
    if final:
        gfin = p.sb("gfin", [128, D], F32)
        p.dma("sp", gfin[:], gfin_d.partition_broadcast(128), writes=[gfin])
    yacc = p.sb("yacc", [128, TH // 128, D], F32)
    w1b = [p.sb("w1b%d" % i, [128, 8, 2 * D], BF16) for i in range(2)]
    w2b = [p.sb("w2b%d" % i, [128, 8, D], BF16) for i in range(2)]
    stg = [p.sb("stg%d" % i, [128, D], F32) for i in range(3)]
    hbk = [p.sb("hbk%d" % i, [128, 8, 512], BF16) for i in range(2)]
    actT = [p.sb("actT%d" % i, [128, 8, 512], BF16) for i in range(2)]
    tg = [p.sb("tg0", [128, 512], F32)]
    tsg = p.sb("tsg", [128, 512], F32)
    tu = [p.sb("tu0", [128, 512], F32)]
    xxk, hhk = stg[1], stg[0]
    xx, hh_ = stg[1][:, 0:D], stg[0][:, 0:D]
    ss, rstd = p.sb("fss", [128, 1], F32), p.sb("frstd", [128, 1], F32)
    GTt = p.sb("GTt", [NE, 128], F32)
    psg, psu, psy, ps_l = [cm.psb[0], cm.psb[1]], [cm.psb[2], cm.psb[3]], [cm.psb[4], cm.psb[5]], cm.psb[6]

    def pieces(e, k):
        out = []
        for kc in range(8):
            for hf in range(2):
                def d(sb_, kc=kc, hf=hf):
                    p.dma("sp", sb_[:], w1_d[e, kc * 128:(kc + 1) * 128, hf * D:(hf + 1) * D], writes=[sb_])
                def c(sb_, eng, kc=kc, hf=hf):
                    p.copy(eng, w1b[k][:, kc, hf * D:(hf + 1) * D], sb_[:], [sb_], [w1b[k]])
                out.append((d, c))
        for jj in range(8):
            def d(sb_, jj=jj):
                p.dma("sp", sb_[:], w2_d[e, jj * 128:(jj + 1) * 128, :], writes=[sb_])
            def c(sb_, eng, jj=jj):
                p.copy(eng, w2b[k][:, jj, :], sb_[:], [sb_], [w2b[k]])
            out.append((d, c))
        return out

    NPC = 24
    ROUNDS = NPC // (NB * 3)
    JPR = 8 // ROUNDS
    assert ROUNDS * NB * 3 == NPC and JPR * ROUNDS == 8
    nblk = 0
    ncast = 0
    njj = 0
    for half in range(NPASS):
        for ti in range(TH // 128):
            tg_ = half * (TH // 128) + ti
            p.op("pe", lambda e: e.transpose(ps_l[0:NE, 0:128], G[:, tg_, :], cm.identf[:]), [G, cm.identf], [ps_l])
            p.copy("dve", GTt[:], ps_l[0:NE, 0:128], [ps_l], [GTt])
            for dh in range(2):
                py = psy[dh]
                p.mm(py[:], GTt[:], b2s[:, dh * 512:(dh + 1) * 512], True, True, [GTt, b2s], [py])
                p.copy("act", yacc[:, ti, dh * 512:(dh + 1) * 512], py[:], [py], [yacc])
        for i, (d_, c_) in enumerate(pieces(0, 0)):
            sb_ = stg[i % 3]
            d_(sb_)
            c_(sb_, "dve" if i % 2 else "act")
        hb = hbk[nblk % 2]
        p.dma("sp", hb[:], hfT_scr[:, :, half * TH:half * TH + 512].rearrange("k q t -> q k t"), reads=["hfT_scr"], writes=[hb])
        for ex_ in range(NE):
            k = ex_ % 2
            nxt = pieces(ex_ + 1, 1 - k) if ex_ + 1 < NE else []
            pend = None
            for tb in range(NB):
                hb = hbk[nblk % 2]
                nblk += 1
                if tb + 1 < NB or ex_ + 1 < NE:
                    ntb = (tb + 1) % NB
                    hn = hbk[nblk % 2]
                    p.dma("sp", hn[:], hfT_scr[:, :, half * TH + ntb * 512:half * TH + (ntb + 1) * 512].rearrange("k q t -> q k t"),
                          reads=["hfT_scr"], writes=[hn])
                at = actT[tb % 2]
                for rd in range(ROUNDS):
                    mine = nxt[(tb * ROUNDS + rd) * 3:(tb * ROUNDS + rd + 1) * 3]
                    for i, (d_, c_) in enumerate(mine):
                        d_(stg[i])
                    for j in range(rd * JPR, (rd + 1) * JPR):
                        pg, pu = psg[njj % 2], psu[njj % 2]
                        g1, u1 = tg[0], tu[0]
                        njj += 1
                        for kc in range(8):
                            p.mm(pg[:], w1b[k][:, kc, j * 128:(j + 1) * 128], hb[:, kc, :], kc == 0, kc == 7, [w1b[k], hb], [pg])
                        for kc in range(8):
                            p.mm(pu[:], w1b[k][:, kc, D + j * 128:D + (j + 1) * 128], hb[:, kc, :], kc == 0, kc == 7, [w1b[k], hb], [pu])
                        p.ts("dve", g1[:], pg[:], b1c[:, ex_, j:j + 1], 7.0, ALU.add, ALU.min, [pg, b1c], [g1])
                        p.act(tsg[:], g1[:], AF.Sigmoid, [g1], [tsg], scale=1.702)
                        p.act(u1[:], pu[:], AF.Identity, [pu, b1c], [u1], bias=b1c[:, ex_, 8 + j:9 + j])
                        p.ts("dve", u1[:], u1[:], 7.0, -7.0, ALU.min, ALU.max, [u1], [u1])
                        p.tt("dve", g1[:], g1[:], tsg[:], ALU.mult, [g1, tsg], [g1])
                        p.op("dve", lambda e: e.scalar_tensor_tensor(at[:, j, :], u1[:], 1.0, g1[:], ALU.add, ALU.mult),
                             [u1, g1], [at])
                    for i, (d_, c_) in enumerate(mine):
                        c_(stg[i], "act")
                        ncast += 1
                todo = [pend] if pend is not None else []
                pend = (tb, at)
                if tb == NB - 1:
                    todo.append(pend)
                    pend = None
                for (tb2, at2) in todo:
                    for ti in range(4):
                        tl_ = tb2 * 4 + ti
                        tg_ = half * (TH // 128) + tl_
                        for dh in range(2):
                            py = psy[dh]
                            for j in range(8):
                                p.mm(py[:], at2[:, j, ti * 128:(ti + 1) * 128], w2b[k][:, j, dh * 512:(dh + 1) * 512], j == 0, j == 7,
                                     [at2, w2b[k]], [py])
                            ya = yacc[:, tl_, dh * 512:(dh + 1) * 512]
                            p.op("dve", lambda e: e.scalar_tensor_tensor(ya, py[:], G[:, tg_, ex_:ex_ + 1], ya, ALU.mult, ALU.add),
                                 [py, G, yacc], [yacc])
        for ti in range(TH // 128):
            tg_ = half * (TH // 128) + ti
            p.dma("sp", xx, x1_d[tg_ * 128:(tg_ + 1) * 128, :], writes=[xxk])
            p.tt("dve", yacc[:, ti, :], yacc[:, ti, :], gtf[:], ALU.mult, [yacc, gtf], [yacc])
            p.tt("dve", xx, xx, yacc[:, ti, :], ALU.add, [xxk, yacc], [xxk])
            if final:
                p.act(hh_, xx, AF.Square, [xxk], [hhk, ss], accum_out=ss[:])
                p.act(rstd[:], ss[:], AF.Sqrt, [ss], [rstd], scale=1.0 / D, bias=EPS)
                p.op("dve", lambda e: e.reciprocal(rstd[:], rstd[:]), [rstd], [rstd])
                p.op("dve", lambda e: e.scalar_tensor_tensor(xx, xx, rstd[:], gfin[:], ALU.mult, ALU.mult),
                     [xxk, rstd, gfin], [xxk])
            p.dma("sp", xo_d[tg_ * 128:(tg_ + 1) * 128, :], xx, reads=[xxk], writes=["xo"])
    p.end_phase()
    return nc, p


def ffn_inputs(inp, core, layer, x1):
    b = core // 4
    m = {"ident": np.eye(128, dtype=np.float32)}
    m["x1"] = np.ascontiguousarray(x1)
    m["ccol"] = col_layout(inp["c"][b])
    m["w_ada"] = np.ascontiguousarray(inp["w_ada"][layer])
    m["b_ada"] = np.ascontiguousarray(inp["b_ada"][layer][None, :])
    m["g_ffn"] = np.ascontiguousarray(inp["g_ffn"][layer][None, :])
    m["w_router"] = np.ascontiguousarray(inp["w_router"][layer])
    m["b_router"] = np.ascontiguousarray(inp["b_router"][layer][None, :])
    m["w_exp_in"] = np.ascontiguousarray(inp["w_exp_in"][layer])
    b1 = np.asarray(inp["b_exp_in"][layer], np.float32).reshape(NE, 16, 128)
    m["b1c"] = np.ascontiguousarray(b1.transpose(2, 0, 1))
    m["w_exp_out"] = np.ascontiguousarray(inp["w_exp_out"][layer])
    m["b_exp_out"] = np.ascontiguousarray(inp["b_exp_out"][layer])
    m["g_final"] = np.ascontiguousarray(inp["g_final"][None, :])
    return m


def l2_inputs(inp, core, r1):
    b, j = core // 4, core % 4
    m = {"ident": np.eye(128, dtype=np.float32)}
    m["xs"] = np.ascontiguousarray(inp["x"][b, j * T:(j + 1) * T])
    for k in ("o_f", "o_b", "sg", "QF", "QB", "Sctx"):
        m[k] = r1[core][k]
    m["SlocA"] = np.ascontiguousarray(np.stack([r1[b * 4 + i]["Sloc"] for i in range(4)]))
    m["DlocA"] = np.ascontiguousarray(np.stack([r1[b * 4 + i]["Dloc"] for i in range(4)]))
    fm = np.zeros((128, 8), np.float32)
    for i in range(4):
        fm[:, i] = 1.0 if i < j else 0.0
        fm[:, 4 + i] = 1.0 if i > j else 0.0
    m["fmask"] = fm
    m["ccol"] = col_layout(inp["c"][b])
    m["w_ada0"] = np.ascontiguousarray(inp["w_ada"][0])
    m["b_ada0"] = np.ascontiguousarray(inp["b_ada"][0][None, :])
    m["gout"] = np.ascontiguousarray(inp["g_hgrn_out"][0][None, :])
    m["w_hout"] = np.ascontiguousarray(inp["w_hgrn_out"][0])
    return m


HALO = 15 * 64


def build_conv_a(ctx=None):
    ctx = stage_begin(ctx)
    nc, p, cm = ctx.nc, ctx.p, ctx.cm
    x2 = dram_in(nc, "x2", [T, D])
    ccol = dram_in(nc, "ccol", [128, 8])
    w_ada = dram_in(nc, "w_ada1", [D, 6 * D])
    b_ada = dram_in(nc, "b_ada1", [1, 6 * D])
    g_mix = dram_in(nc, "g_mix1", [1, D])
    w_in = dram_in(nc, "w_cv_in", [D, 2 * D])
    bin_c = dram_in(nc, "bin_c", [128, 16])
    u_d = dram_out(nc, "u", [8, 128, T])
    gtm_d = dram_out(nc, "gtm", [1, D])
    modb = p.sb("modb", [128, 3072], F32)
    emit_mods(p, cm, w_ada, b_ada, 0, 3072, [(ccol, modb)], "c")
    p.dma("sp", gtm_d, modb[0:1, 2048:3072], reads=[modb], writes=["gtm"])
    gmix = p.sb("gmix", [128, D], F32)
    p.dma("sp", gmix[:], g_mix.partition_broadcast(128), writes=[gmix])
    p.op("dve", lambda e: e.scalar_tensor_tensor(modb[:, 1024:2048], modb[:, 1024:2048], 1.0, gmix[:], ALU.add, ALU.mult),
         [modb, gmix], [modb])
    hT = p.sb("hT", [128, 8, T], BF16)
    xt = [p.sb("xt%d" % i, [128, D], F32) for i in range(2)]
    h = p.sb("h", [128, D], F32)
    st = (p.sb("ss", [128, 1], F32), p.sb("rstd", [128, 1], F32))
    emit_norm_T(p, cm, x2, T // 128, modb[:, 1024:2048], modb[:, 0:1024], modb, hT, None, xt, h, h, st)
    bc = p.sb("bc", [128, 16], F32)
    p.dma("sp", bc[:], bin_c, writes=[bc])
    wst = [p.sb("wst%d" % i, [128, 8, 128], F32) for i in range(2)]
    wab = [p.sb("wab%d" % i, [128, 8, 128], BF16) for i in range(2)]
    wgb = [p.sb("wgb%d" % i, [128, 8, 128], BF16) for i in range(2)]
    sgt = [p.sb("sgt%d" % i, [128, 512], F32) for i in range(2)]
    ut = [p.sb("ut%d" % i, [128, 512], F32) for i in range(2)]
    psa, psg = cm.psb[2], cm.psb[3]
    n = 0
    for j in range(8):
        k = j % 2
        p.dma("sp", wst[0][:], w_in[:, j * 128:(j + 1) * 128].rearrange("(kc q) n -> q kc n", q=128), writes=[wst[0]])
        p.copy("act", wab[k][:], wst[0][:], [wst[0]], [wab[k]])
        p.dma("sp", wst[1][:], w_in[:, 1024 + j * 128:1024 + (j + 1) * 128].rearrange("(kc q) n -> q kc n", q=128), writes=[wst[1]])
        p.copy("act", wgb[k][:], wst[1][:], [wst[1]], [wgb[k]])
        for tb in range(T // 512):
            tok = slice(tb * 512, (tb + 1) * 512)
            for kc in range(8):
                p.mm(psa[:], wab[k][:, kc, :], hT[:, kc, tok], kc == 0, kc == 7, [wab[k], hT], [psa])
            for kc in range(8):
                p.mm(psg[:], wgb[k][:, kc, :], hT[:, kc, tok], kc == 0, kc == 7, [wgb[k], hT], [psg])
            sg_, u_ = sgt[n % 2], ut[n % 2]
            n += 1
            p.act(sg_[:], psg[:], AF.Sigmoid, [psg, bc], [sg_], bias=bc[:, 8 + j:9 + j])
            p.op("dve", lambda e: e.scalar_tensor_tensor(u_[:], psa[:], bc[:, j:j + 1], sg_[:], ALU.add, ALU.mult),
                 [psa, bc, sg_], [u_])
            p.dma("sp", u_d[j, :, tok], u_[:], reads=[u_], writes=["u_d"])
    if not ctx.standalone:
        hb = nc.dram_tensor("halo_b", [8 * 128, HALO], F32, kind="Internal").ap()
        REG["halo_b"] = hb
        for jj in range(4):
            p.dma("sp", hb[jj * 128:(jj + 1) * 128, :], u_d[4 + jj, :, T - HALO:T], reads=["u_d"], writes=["halo_b"])
            p.dma("sp", hb[(4 + jj) * 128:(5 + jj) * 128, :], u_d[4 + jj, :, 0:HALO], reads=["u_d"], writes=["halo_b"])
    return stage_end(ctx, ["u_d", "gtm"])


def conv_a_inputs(inp, core, x2):
    b = core // 4
    m = {"ident": np.eye(128, dtype=np.float32)}
    m["x2"] = np.ascontiguousarray(x2)
    m["ccol"] = col_layout(inp["c"][b])
    m["w_ada"] = np.ascontiguousarray(inp["w_ada"][1])
    m["b_ada"] = np.ascontiguousarray(inp["b_ada"][1][None, :])
    m["g_mix"] = np.ascontiguousarray(inp["g_mix"][1][None, :])
    m["w_cv_in"] = np.ascontiguousarray(inp["w_cv_in"][0])
    m["bin_c"] = np.ascontiguousarray(np.asarray(inp["b_cv_in"][0], np.float32).reshape(16, 128).T)
    return m


def build_conv_b(ctx=None):
    ctx = stage_begin(ctx)
    nc, p, cm = ctx.nc, ctx.p, ctx.cm
    x2 = dram_in(nc, "x2", [T, D])
    u_d = dram_in(nc, "u", [8, 128, T])
    G2 = dram_in(nc, "halo_g", [8 * 8 * 128, HALO])
    sel_d = dram_in(nc, "sel", [128, 16])
    gtm_d = dram_in(nc, "gtm", [1, D])
    wdw_d = dram_in(nc, "wdw", [128, 8, 31])
    cvec_d = dram_in(nc, "cvec", [128, 3, 8])
    wout_d = dram_in(nc, "w_cv_out", [D, D])
    bout_d = dram_in(nc, "b_cv_out", [1, D])
    x3_d = dram_out(nc, "x3", [T, D])
    wdw = p.sb("wdw", [128, 8, 31], F32)
    cvec = p.sb("cvec", [128, 3, 8], F32)
    gtm = p.sb("gtm", [128, D], F32)
    bout = p.sb("bout", [128, D], F32)
    sel = p.sb("sel", [128, 16], F32)
    p.dma("sp", sel[:], sel_d, writes=[sel])
    p.dma("sp", wdw[:], wdw_d, writes=[wdw])
    p.dma("sp", cvec[:], cvec_d, writes=[cvec])
    p.dma("sp", gtm[:], gtm_d.partition_broadcast(128), writes=[gtm])
    p.dma("sp", bout[:], bout_d.partition_broadcast(128), writes=[bout])
    wo = p.sb("wo", [128, 8, D], BF16)
    wos = p.sb("wos", [128, D], F32)
    for kc in range(8):
        p.dma("sp", wos[:], wout_d[kc * 128:(kc + 1) * 128, :], writes=[wos])
        p.copy("act", wo[:, kc, :], wos[:], [wos], [wo])
    z = p.sb("z_all", [128, 8, T], F32)
    ue = p.sb("ue", [128, T + 2 * HALO], F32)
    for j in range(8):
        zj = z[:, j, :]
        zkey = ("z", j)
        if j < 4:
            p.dma("sp", ue[:, 0:T], u_d[j], writes=[ue])
            p.ts("dve", zj, ue[:, 0:T], wdw[:, j, 15:16], cvec[:, 0, j:j + 1], ALU.mult, ALU.add, [ue, wdw, cvec], [zkey])
            z3 = zj.rearrange("q (r w) -> q r w", w=64)
            u3 = ue[:, 0:T].rearrange("q (r w) -> q r w", w=64)
            for o in range(-15, 16):
                if o == 0:
                    continue
                lo, hi = max(0, -o), min(64, 64 - o)
                p.op("dve", lambda e: e.scalar_tensor_tensor(z3[:, :, lo:hi], u3[:, :, lo + o:hi + o], wdw[:, j, o + 15:o + 16],
                                                             z3[:, :, lo:hi], ALU.mult, ALU.add), [ue, wdw, zkey], [zkey])
        else:
            p.dma("sp", ue[:, HALO:HALO + T], u_d[j], writes=[ue])
            for side, (c0, dst) in enumerate(((0, ue[:, 0:HALO]), (8, ue[:, HALO + T:HALO + T + HALO]))):
                for r in range(8):
                    r0 = r * 1024 + ((0 if side == 0 else 4) + (j - 4)) * 128
                    stg = wos[:, 0:HALO]
                    p.dma("sp", stg, G2[r0:r0 + 128, :], reads=["halo_g"], writes=[wos])
                    if r == 0:
                        p.ts("dve", dst, stg, sel[:, c0:c0 + 1], None, ALU.mult, None, [wos, sel], [ue])
                    else:
                        p.op("dve", lambda e: e.scalar_tensor_tensor(dst, stg, sel[:, c0 + r:c0 + r + 1], dst, ALU.mult, ALU.add),
                             [wos, sel, ue], [ue])
            p.ts("dve", zj, ue[:, HALO:HALO + T], wdw[:, j, 15:16], cvec[:, 0, j:j + 1], ALU.mult, ALU.add, [ue, wdw, cvec], [zkey])
            for o in range(-15, 16):
                if o == 0:
                    continue
                a0 = HALO + o * 64
                p.op("dve", lambda e: e.scalar_tensor_tensor(zj, ue[:, a0:a0 + T], wdw[:, j, o + 15:o + 16], zj, ALU.mult, ALU.add),
                     [ue, wdw, zkey], [zkey])
    zkeys = [("z", j) for j in range(8)]
    sqt = [p.sb("sqt%d" % i, [128, 512], F32) for i in range(2)]
    mean = p.sb("mean", [128, 512], F32)
    msq = p.sb("msq", [128, 512], F32)
    rstd = p.sb("rstdv", [128, 512], F32)
    zn = [p.sb("zn0", [128, 512], F32)] * 2
    sgm = [p.sb("sgm0", [128, 512], F32)] * 2
    zs = p.sb("zs", [128, 8, 512], BF16)
    yt = wos
    ps1, ps2 = cm.psb[0], cm.psb[1]
    psy = [cm.psb[2], cm.psb[3]]
    n = 0
    for tb in range(T // 512):
        tok = slice(tb * 512, (tb + 1) * 512)
        for j in range(8):
            sq = sqt[n % 2]
            n += 1
            p.act(sq[:], z[:, j, tok], AF.Square, [zkeys[j]], [sq])
            p.mm(ps1[:], cm.ones[:], z[:, j, tok], j == 0, j == 7, [cm.ones, zkeys[j]], [ps1])
            p.mm(ps2[:], cm.ones[:], sq[:], j == 0, j == 7, [cm.ones, sq], [ps2])
        p.act(mean[:], ps1[:], AF.Copy, [ps1], [mean], scale=1.0 / D)
        p.tt("dve", msq[:], mean[:], mean[:], ALU.mult, [mean], [msq])
        p.op("dve", lambda e: e.scalar_tensor_tensor(rstd[:], ps2[:], 1.0 / D, msq[:], ALU.mult, ALU.subtract), [ps2, msq], [rstd])
        p.act(rstd[:], rstd[:], AF.Sqrt, [rstd], [rstd], bias=EPS)
        p.op("dve", lambda e: e.reciprocal(rstd[:], rstd[:]), [rstd], [rstd])
        for j in range(8):
            a, sg_ = zn[j % 2], sgm[j % 2]
            p.tt("dve", a[:], z[:, j, tok], mean[:], ALU.subtract, [zkeys[j], mean], [a])
            p.tt("dve", a[:], a[:], rstd[:], ALU.mult, [a, rstd], [a])
            p.ts("dve", a[:], a[:], cvec[:, 1, j:j + 1], cvec[:, 2, j:j + 1], ALU.mult, ALU.add, [a, cvec], [a])
            p.act(sg_[:], a[:], AF.Sigmoid, [a], [sg_])
            p.tt("dve", zs[:, j, :], a[:], sg_[:], ALU.mult, [a, sg_], [zs])
        for ti in range(4):
            t = tb * 4 + ti
            xa = ue[:, 0:D]
            p.dma("sp", xa, x2[t * 128:(t + 1) * 128, :], writes=[ue])
            for dh in range(2):
                ps = psy[dh]
                for j in range(8):
                    p.mm(ps[:], zs[:, j, ti * 128:(ti + 1) * 128], wo[:, j, dh * 512:(dh + 1) * 512], j == 0, j == 7, [zs, wo], [ps])
                p.tt("dve", yt[:, dh * 512:(dh + 1) * 512], ps[:], bout[:, dh * 512:(dh + 1) * 512], ALU.add, [ps, bout], [yt])
            p.tt("dve", yt[:], yt[:], gtm[:], ALU.mult, [yt, gtm], [yt])
            p.tt("dve", xa, xa, yt[:], ALU.add, [ue, yt], [ue])
            p.dma("sp", x3_d[t * 128:(t + 1) * 128, :], xa, reads=[ue], writes=["x3"])
    return stage_end(ctx, ["x3"])


def conv_b_inputs(inp, core, x2, ra):
    b, j = core // 4, core % 4
    m = {"ident": np.eye(128, dtype=np.float32)}
    m["x2"] = np.ascontiguousarray(x2)
    m["u"] = ra[core]["u"]
    zero = np.zeros((4, 128, HALO), np.float32)
    m["u_prev"] = np.ascontiguousarray(ra[core - 1]["u"][4:8, :, T - HALO:T]) if j > 0 else zero
    m["u_next"] = np.ascontiguousarray(ra[core + 1]["u"][4:8, :, 0:HALO]) if j < 3 else zero
    m["gtm"] = ra[core]["gtm"]
    w = np.asarray(inp["w_cv_dw"][0], np.float32)
    m["wdw"] = np.ascontiguousarray(w.reshape(31, 8, 128).transpose(2, 1, 0))
    cv = np.stack([np.asarray(inp[k][0], np.float32).reshape(8, 128).T for k in ("b_cv_dw", "g_cv_ln", "b_cv_ln")], axis=1)
    m["cvec"] = np.ascontiguousarray(cv)
    m["w_cv_out"] = np.ascontiguousarray(inp["w_cv_out"][0])
    m["b_cv_out"] = np.ascontiguousarray(inp["b_cv_out"][0][None, :])
    return m


_DBG = {}


def build_fused():
    REG.clear()
    FUSED[0] = True
    try:
        ctx = Ctx()
        nc, p = ctx.nc, ctx.p
        ctx.cm = Common(p, nc)
        build_l1(ctx)
        SlocG = nc.dram_tensor("SlocG", [8 * 2 * NH * 128, 128], F32, kind="Internal").ap()
        DlocG = nc.dram_tensor("DlocG", [8 * 128, 16], F32, kind="Internal").ap()
        REG["SlocG"], REG["DlocG"] = SlocG, DlocG
        p.allgather(REG["Sloc"].rearrange("d h k v -> (d h k) v"), SlocG, "Sloc_d", "SlocG")
        p.allgather(REG["Dloc"], DlocG, "Dloc_d", "DlocG")
        build_l2(ctx)
        build_ffn(False, ctx, 0, "x1", "x2")
        build_conv_a(ctx)
        halo_g = nc.dram_tensor("halo_g", [8 * 8 * 128, HALO], F32, kind="Internal").ap()
        REG["halo_g"] = halo_g
        p.allgather(REG["halo_b"], halo_g, "halo_b", "halo_g")
        build_conv_b(ctx)
        build_ffn(True, ctx, 1, "x3", FINAL_OUT)
        p.barrier()
    finally:
        FUSED[0] = False
    return nc, p


def fused_inputs(inp, core):
    b, j = core // 4, core % 4
    m = l1_inputs(inp, core)
    fm = np.zeros((128, 16), np.float32)
    sel = np.zeros((128, 16), np.float32)
    for r in range(8):
        if r // 4 == b:
            fm[:, r] = 1.0 if (r % 4) < j else 0.0
            fm[:, 8 + r] = 1.0 if (r % 4) > j else 0.0
    if j > 0:
        sel[:, core - 1] = 1.0
    if j < 3:
        sel[:, 8 + core + 1] = 1.0
    m["fmask"], m["sel"] = fm, sel
    m["gout"] = np.ascontiguousarray(inp["g_hgrn_out"][0][None, :])
    m["w_hout"] = np.ascontiguousarray(inp["w_hgrn_out"][0])
    for layer in range(2):
        L = str(layer)
        m["w_ada" + L] = np.ascontiguousarray(inp["w_ada"][layer])
        m["b_ada" + L] = np.ascontiguousarray(inp["b_ada"][layer][None, :])
        m["g_ffn" + L] = np.ascontiguousarray(inp["g_ffn"][layer][None, :])
        m["w_router" + L] = np.ascontiguousarray(inp["w_router"][layer])
        m["b_router" + L] = np.ascontiguousarray(inp["b_router"][layer][None, :])
        m["w_exp_in" + L] = np.ascontiguousarray(inp["w_exp_in"][layer])
        b1 = np.asarray(inp["b_exp_in"][layer], np.float32).reshape(NE, 16, 128)
        m["b1c" + L] = np.ascontiguousarray(b1.transpose(2, 0, 1))
        m["w_exp_out" + L] = np.ascontiguousarray(inp["w_exp_out"][layer])
        m["b_exp_out" + L] = np.ascontiguousarray(inp["b_exp_out"][layer])
    m["g_final"] = np.ascontiguousarray(inp["g_final"][None, :])
    m["g_mix1"] = np.ascontiguousarray(inp["g_mix"][1][None, :])
    m["w_cv_in"] = np.ascontiguousarray(inp["w_cv_in"][0])
    m["bin_c"] = np.ascontiguousarray(np.asarray(inp["b_cv_in"][0], np.float32).reshape(16, 128).T)
    w = np.asarray(inp["w_cv_dw"][0], np.float32)
    m["wdw"] = np.ascontiguousarray(w.reshape(31, 8, 128).transpose(2, 1, 0))
    cv = np.stack([np.asarray(inp[k][0], np.float32).reshape(8, 128).T for k in ("b_cv_dw", "g_cv_ln", "b_cv_ln")], axis=1)
    m["cvec"] = np.ascontiguousarray(cv)
    m["w_cv_out"] = np.ascontiguousarray(inp["w_cv_out"][0])
    m["b_cv_out"] = np.ascontiguousarray(inp["b_cv_out"][0][None, :])
    return m


def kernel(**inp):
    inp = {k: np.asarray(v) for k, v in inp.items()}
    nc, _ = build_fused()
    res = run_bass_kernel_spmd(nc, [fused_inputs(inp, c) for c in range(8)], core_ids=list(range(8))).results
    out = np.zeros((2, 4 * T, D), np.float32)
    for c in range(8):
        out[c // 4, (c % 4) * T:(c % 4 + 1) * T] = res[c][FINAL_OUT]
    return out
```
